# Optimizing a Trainium2 kernel written in Bass

```python
import jax, jax.numpy as jnp
from jax import lax
import numpy as np

D_MODEL = 2048
BATCH = 32
SEQ = 256
DEPTH = 1
DEC_BATCH = 2
DEC_SEQ = 4096
PAST_LEN = 512

GRID_W = 64
NA_HEADS = 8
NA_HEAD_DIM = 128
NA_KR = 8
NA_KC = 16
MLA_HEADS = 8
Q_LORA = 512
KV_LORA = 256
QK_NOPE = 128
QK_ROPE = 64
V_DIM = 128
ROPE_AXIS = QK_ROPE // 2
ROPE_THETA = 10000.0
NA_WIDTH = NA_HEADS * NA_HEAD_DIM
MLA_WIDTH = MLA_HEADS * V_DIM
MIX_WIDTH = NA_WIDTH + MLA_WIDTH
IN_SPLITS = (NA_WIDTH, NA_WIDTH, NA_WIDTH, Q_LORA, KV_LORA, QK_ROPE)
IN_COLS = sum(IN_SPLITS)
IN_OFFSETS = [int(o) for o in np.cumsum(IN_SPLITS)[:-1]]
N_EXPERTS = 32
TOP_K = 4
D_FF = D_MODEL
SWIGLU_ALPHA = 1.702
SWIGLU_LIMIT = 7.0
MOE_BLOCK = 256
Q_BLOCK = 128
EPS = 1e-6

kernel_name = 'hybrid_na_mla_moe_dit_step'


def rms_norm(x, g):
    xf = x.astype(jnp.float32)
    y = xf * lax.rsqrt(jnp.mean(xf * xf, axis=-1, keepdims=True) + EPS)
    return (y * g.astype(jnp.float32)).astype(x.dtype)


def modulation(c, w_mod, b_mod):
    m = jax.nn.silu(c) @ w_mod + b_mod
    return jnp.split(m[:, None, :], 6, axis=-1)


def rope_tables(t, dtype):
    pos = jnp.arange(t)
    rows = (pos // GRID_W).astype(jnp.float32)
    cols = (pos % GRID_W).astype(jnp.float32)
    inv = ROPE_THETA ** (-(jnp.arange(ROPE_AXIS // 2, dtype=jnp.float32) * 2.0 / ROPE_AXIS))
    ar = rows[:, None] * inv
    ac = cols[:, None] * inv
    ang = jnp.concatenate([ar, ar, ac, ac], axis=-1)
    return jnp.cos(ang).astype(dtype), jnp.sin(ang).astype(dtype)


def axial_rope(x, cos, sin):
    def rot(z):
        z1, z2 = jnp.split(z, 2, axis=-1)
        return jnp.concatenate([-z2, z1], axis=-1)
    xr = jnp.concatenate([rot(x[..., :ROPE_AXIS]), rot(x[..., ROPE_AXIS:])], axis=-1)
    return x * cos + xr * sin


def block_attn(q, k, v):
    b, nq, h, dq = q.shape
    dv = v.shape[-1]
    nblk = nq // Q_BLOCK
    scale = dq ** -0.5
    qb = q.reshape(b, nblk, Q_BLOCK, h, dq).swapaxes(0, 1)

    def one(qi):
        s = jnp.einsum('bqhd,bkhd->bhqk', qi, k, preferred_element_type=jnp.float32) * scale
        p = jax.nn.softmax(s, axis=-1).astype(v.dtype)
        return jnp.einsum('bhqk,bkhd->bqhd', p, v)

    o = lax.map(one, qb)
    return o.swapaxes(0, 1).reshape(b, nq, h, dv)


def neighborhood_attn(q, k, v, k_ctx, v_ctx, rpb):
    b, t, h, d = q.shape
    rows = t // GRID_W
    kr = min(NA_KR, rows)
    kc = NA_KC
    scale = d ** -0.5
    qg = q.reshape(b, rows, GRID_W, h, d)
    kg = k.reshape(b, rows, GRID_W, h, d)
    vg = v.reshape(b, rows, GRID_W, h, d)
    col = jnp.arange(GRID_W)
    col_idx = jnp.clip(col - kc // 2, 0, GRID_W - kc)[:, None] + jnp.arange(kc)[None, :]
    col_off = col_idx - col[:, None]
    bias_col = rpb[:, :, col_off + NA_KC - 1]
    row_ids = jnp.arange(rows)
    row_start = jnp.clip(row_ids - kr // 2, 0, rows - kr)

    def one_row(args):
        qr, r, rs = args
        kb = lax.dynamic_slice_in_dim(kg, rs, kr, axis=1)
        vb = lax.dynamic_slice_in_dim(vg, rs, kr, axis=1)
        kw = kb[:, :, col_idx]
        vw = vb[:, :, col_idx]
        row_off = rs + jnp.arange(kr) - r
        bias = bias_col[:, row_off + NA_KR - 1].transpose(0, 2, 1, 3)
        s_loc = jnp.einsum('bqhd,bkqjhd->bhqkj', qr, kw, preferred_element_type=jnp.float32) * scale + bias[None]
        s_ctx = jnp.einsum('bqhd,bphd->bhqp', qr, k_ctx, preferred_element_type=jnp.float32) * scale
        s = jnp.concatenate([s_loc.reshape(b, h, GRID_W, kr * kc), s_ctx], axis=-1)
        p = jax.nn.softmax(s, axis=-1).astype(v.dtype)
        p_loc = p[..., :kr * kc].reshape(b, h, GRID_W, kr, kc)
        p_ctx = p[..., kr * kc:]
        return jnp.einsum('bhqkj,bkqjhd->bqhd', p_loc, vw) + jnp.einsum('bhqp,bphd->bqhd', p_ctx, v_ctx)

    o = lax.map(one_row, (qg.swapaxes(0, 1), row_ids, row_start))
    return o.swapaxes(0, 1).reshape(b, t, h, d)


def moe_ffn(h, w_router, b_router, w_gate_up, b_gate_up, w_down, b_down):
    n, d = h.shape
    logits = jnp.einsum('nd,de->ne', h, w_router, preferred_element_type=jnp.float32) + b_router.astype(jnp.float32)
    top_logit, top_idx = lax.top_k(logits, TOP_K)
    gates = jax.nn.softmax(top_logit, axis=-1)
    nk = n * TOP_K
    flat_e = top_idx.reshape(nk)
    order = jnp.argsort(flat_e)
    sorted_e = flat_e[order]
    counts = jnp.bincount(flat_e, length=N_EXPERTS)
    padded = (counts + MOE_BLOCK - 1) // MOE_BLOCK * MOE_BLOCK
    pad_end = jnp.cumsum(padded)
    pad_start = pad_end - padded
    grp_start = jnp.cumsum(counts) - counts
    dest = pad_start[sorted_e] + jnp.arange(nk) - grp_start[sorted_e]
    n_blocks = (nk + MOE_BLOCK - 1) // MOE_BLOCK + N_EXPERTS
    cap = n_blocks * MOE_BLOCK
    row_token = jnp.zeros((cap,), jnp.int32).at[dest].set((order // TOP_K).astype(jnp.int32))
    row_gate = jnp.zeros((cap,), jnp.float32).at[dest].set(gates.reshape(nk)[order])
    rows = h[row_token].reshape(n_blocks, MOE_BLOCK, d)
    block_expert = jnp.minimum(jnp.searchsorted(pad_end, jnp.arange(n_blocks) * MOE_BLOCK, side='right'), N_EXPERTS - 1)

    def expert_block(args):
        xb, e = args
        gu = xb @ w_gate_up[e] + b_gate_up[e]
        g = jnp.minimum(gu[..., 0::2], SWIGLU_LIMIT)
        u = jnp.clip(gu[..., 1::2], -SWIGLU_LIMIT, SWIGLU_LIMIT)
        act = g * jax.nn.sigmoid(g * SWIGLU_ALPHA) * (u + 1)
        return act @ w_down[e] + b_down[e]

    out = lax.map(expert_block, (rows, block_expert)).reshape(cap, d)
    out = (out.astype(jnp.float32) * row_gate[:, None]).astype(h.dtype)
    return jax.ops.segment_sum(out, row_token, num_segments=n)


def attention_inputs(h, w_in, g_q_a, w_q_b, g_kv_a):
    b, t, _ = h.shape
    na_q, na_k, na_v, q_a, kv_a, k_rope = jnp.split(h @ w_in, IN_OFFSETS, axis=-1)
    heads = lambda z: z.reshape(b, t, NA_HEADS, NA_HEAD_DIM)
    q = (rms_norm(q_a, g_q_a) @ w_q_b).reshape(b, t, MLA_HEADS, QK_NOPE + QK_ROPE)
    c_kv = rms_norm(kv_a, g_kv_a)
    return heads(na_q), heads(na_k), heads(na_v), q[..., :QK_NOPE], q[..., QK_NOPE:], c_kv, k_rope


def mla_expand(c_kv, w_kv_b):
    b, t, _ = c_kv.shape
    kv = (c_kv @ w_kv_b).reshape(b, t, MLA_HEADS, QK_NOPE + V_DIM)
    return kv[..., :QK_NOPE], kv[..., QK_NOPE:]


def mla_keys(k_nope, k_rope):
    kr = jnp.broadcast_to(k_rope[:, :, None, :], k_nope.shape[:3] + (QK_ROPE,))
    return jnp.concatenate([k_nope, kr], axis=-1)


def ffn_sublayer(x, shift, scale, gate, g_ffn, w_router, b_router, w_gate_up, b_gate_up, w_down, b_down):
    b, t, d = x.shape
    h = rms_norm(x, g_ffn) * (1 + scale) + shift
    y = moe_ffn(h.reshape(b * t, d), w_router, b_router, w_gate_up, b_gate_up, w_down, b_down)
    return x + gate * y.reshape(b, t, d)


def setup_inputs(seed: int = 0) -> dict:
    key = jax.random.key(seed)
    ks = jax.random.split(key, 26)
    f32 = jnp.float32
    nrm = lambda k, shape, s: (jax.random.normal(k, shape, f32) * s).astype(f32)
    D = D_MODEL
    return {
        'x_prompt': nrm(ks[0], (BATCH, SEQ, D), 1.0),
        'x_sample': nrm(ks[1], (DEC_BATCH, DEC_SEQ, D), 1.0),
        'cache_na_k': nrm(ks[2], (DEC_BATCH, DEPTH, PAST_LEN, NA_HEADS, NA_HEAD_DIM), 1.0),
        'cache_na_v': nrm(ks[3], (DEC_BATCH, DEPTH, PAST_LEN, NA_HEADS, NA_HEAD_DIM), 1.0),
        'cache_mla_ckv': nrm(ks[4], (DEC_BATCH, DEPTH, PAST_LEN, KV_LORA), 1.0),
        'cache_mla_krope': nrm(ks[5], (DEC_BATCH, DEPTH, PAST_LEN, QK_ROPE), 1.0),
        'c': nrm(ks[6], (DEC_BATCH, D), 1.0),
        'c_ctx': nrm(ks[7], (D,), 1.0),
        'g_attn': 1.0 + nrm(ks[8], (DEPTH, D), 0.02),
        'g_ffn': 1.0 + nrm(ks[9], (DEPTH, D), 0.02),
        'g_final': 1.0 + nrm(ks[10], (D,), 0.02),
        'w_mod': nrm(ks[11], (DEPTH, D, 6 * D), D ** -0.5),
        'b_mod': nrm(ks[12], (DEPTH, 6 * D), 0.02),
        'w_in': nrm(ks[13], (DEPTH, D, IN_COLS), D ** -0.5),
        'w_out': nrm(ks[14], (DEPTH, MIX_WIDTH, D), MIX_WIDTH ** -0.5),
        'na_rpb': nrm(ks[15], (DEPTH, NA_HEADS, 2 * NA_KR - 1, 2 * NA_KC - 1), 0.1),
        'g_q_a': 1.0 + nrm(ks[16], (DEPTH, Q_LORA), 0.02),
        'w_q_b': nrm(ks[17], (DEPTH, Q_LORA, MLA_HEADS * (QK_NOPE + QK_ROPE)), Q_LORA ** -0.5),
        'g_kv_a': 1.0 + nrm(ks[18], (DEPTH, KV_LORA), 0.02),
        'w_kv_b': nrm(ks[19], (DEPTH, KV_LORA, MLA_HEADS * (QK_NOPE + V_DIM)), KV_LORA ** -0.5),
        'w_router': nrm(ks[20], (DEPTH, D, N_EXPERTS), D ** -0.5),
        'b_router': nrm(ks[21], (DEPTH, N_EXPERTS), 0.01),
        'w_gate_up': nrm(ks[22], (DEPTH, N_EXPERTS, D, 2 * D_FF), D ** -0.5),
        'b_gate_up': nrm(ks[23], (DEPTH, N_EXPERTS, 2 * D_FF), 0.02),
        'w_down': nrm(ks[24], (DEPTH, N_EXPERTS, D_FF, D), D_FF ** -0.5),
        'b_down': nrm(ks[25], (DEPTH, N_EXPERTS, D), 0.02),
    }


def reference(x_prompt, x_sample, cache_na_k, cache_na_v, cache_mla_ckv, cache_mla_krope, c, c_ctx,
              g_attn, g_ffn, g_final, w_mod, b_mod, w_in, w_out, na_rpb, g_q_a, w_q_b, g_kv_a, w_kv_b,
              w_router, b_router, w_gate_up, b_gate_up, w_down, b_down):
    bp, sp, d = x_prompt.shape
    bd, td, _ = x_sample.shape
    cos, sin = rope_tables(td, x_sample.dtype)
    xp = x_prompt
    xs = x_sample
    st_na_k, st_na_v, st_ckv, st_krope = [], [], [], []
    for l in range(DEPTH):
        moe_w = (w_router[l], b_router[l], w_gate_up[l], b_gate_up[l], w_down[l], b_down[l])
        sa, sca, ga, sf, scf, gf = modulation(c_ctx[None, :], w_mod[l], b_mod[l])
        h = rms_norm(xp, g_attn[l]) * (1 + sca) + sa
        na_q, na_k, na_v, q_nope, q_rope, c_kv, k_rope = attention_inputs(h, w_in[l], g_q_a[l], w_q_b[l], g_kv_a[l])
        o_na = block_attn(na_q, na_k, na_v)
        k_nope, v_mla = mla_expand(c_kv, w_kv_b[l])
        o_mla = block_attn(jnp.concatenate([q_nope, q_rope], axis=-1), mla_keys(k_nope, k_rope), v_mla)
        o = jnp.concatenate([o_na.reshape(bp, sp, NA_WIDTH), o_mla.reshape(bp, sp, MLA_WIDTH)], axis=-1) @ w_out[l]
        xp = xp + ga * o
        xp = ffn_sublayer(xp, sf, scf, gf, g_ffn[l], *moe_w)
        st_na_k.append(na_k)
        st_na_v.append(na_v)
        st_ckv.append(c_kv)
        st_krope.append(k_rope)
        sa, sca, ga, sf, scf, gf = modulation(c, w_mod[l], b_mod[l])
        h = rms_norm(xs, g_attn[l]) * (1 + sca) + sa
        na_q, na_k, na_v, q_nope, q_rope, c_kv, k_rope = attention_inputs(h, w_in[l], g_q_a[l], w_q_b[l], g_kv_a[l])
        o_na = neighborhood_attn(na_q, na_k, na_v, cache_na_k[:, l], cache_na_v[:, l], na_rpb[l])
        q_rope = axial_rope(q_rope, cos[:, None, :], sin[:, None, :])
        k_rope = axial_rope(k_rope, cos, sin)
        k_nope, v_mla = mla_expand(c_kv, w_kv_b[l])
        ck_nope, cv_mla = mla_expand(cache_mla_ckv[:, l], w_kv_b[l])
        k_all = jnp.concatenate([mla_keys(k_nope, k_rope), mla_keys(ck_nope, cache_mla_krope[:, l])], axis=1)
        v_all = jnp.concatenate([v_mla, cv_mla], axis=1)
        o_mla = block_attn(jnp.concatenate([q_nope, q_rope], axis=-1), k_all, v_all)
        o = jnp.concatenate([o_na.reshape(bd, td, NA_WIDTH), o_mla.reshape(bd, td, MLA_WIDTH)], axis=-1) @ w_out[l]
        xs = xs + ga * o
        xs = ffn_sublayer(xs, sf, scf, gf, g_ffn[l], *moe_w)
    y_prompt = rms_norm(xp, g_final)
    y_sample = rms_norm(xs, g_final)
    new_na_k = jnp.stack(st_na_k, axis=1)
    new_na_v = jnp.stack(st_na_v, axis=1)
    new_mla_ckv = jnp.stack(st_ckv, axis=1)
    new_mla_krope = jnp.stack(st_krope, axis=1)
    return (y_prompt, y_sample, new_na_k, new_na_v, new_mla_ckv, new_mla_krope)
```

```python
import numpy as np
import concourse.bass as bass
import concourse.mybir as mybir
from contextlib import ExitStack

F32 = mybir.dt.float32
BF16 = mybir.dt.bfloat16
I32 = mybir.dt.int32
U32 = mybir.dt.uint32
ALU = mybir.AluOpType
ACTF = mybir.ActivationFunctionType
AX = mybir.AxisListType

ENGS = ("pe", "act", "dve", "pool", "sp")
SEM_ROTATE = 12000


class Buf:
    __slots__ = ("name", "last_w", "readers", "ld", "st", "excl")

    def __init__(self, name, excl=False):
        self.name = name
        self.excl = excl
        self.last_w = None
        self.readers = []
        self.ld = None
        self.st = None


class DmaSem:
    __slots__ = ("sem", "count")

    def __init__(self, sem):
        self.sem = sem
        self.count = 0


class Op:
    __slots__ = ("eng", "fn", "waits", "is_dma", "dsem", "dval", "signaled", "sig", "idx", "dma_waits", "inc")

    def __init__(self, eng, fn, is_dma):
        self.eng = eng
        self.fn = fn
        self.is_dma = is_dma
        self.waits = {}
        self.dma_waits = {}
        self.dsem = None
        self.dval = 0
        self.signaled = False
        self.sig = None
        self.idx = 0
        self.inc = 16


class Prog:
    def __init__(self, nc):
        self.nc = nc
        self.ops = {e: [] for e in ENGS}
        self.all_ops = []
        self.stack = ExitStack()
        self.sem_pool = []
        self.n_sems = 0
        self.dma_sems = []
        self._n = 0

    def new_sem(self):
        self.n_sems += 1
        return self.stack.enter_context(self.nc.semaphore(f"s{self.n_sems}"))

    def sbuf(self, name, shape, dtype):
        t = self.stack.enter_context(self.nc.sbuf_tensor(name, list(shape), dtype))
        return t

    def psum(self, name, shape, dtype):
        t = self.stack.enter_context(self.nc.psum_tensor(name, list(shape), dtype))
        return t

    def _dep(self, op, prod):
        if prod is None or prod is op:
            return
        if prod.is_dma:
            ds = prod.dsem
            op.dma_waits[id(ds)] = (ds, ds.count)
        else:
            if prod.eng == op.eng and not op.is_dma and op.eng == "pe":
                return
            cur = op.waits.get(prod.eng)
            if cur is None or cur.idx < prod.idx:
                op.waits[prod.eng] = prod
            prod.signaled = True

    def _record(self, op, reads, writes):
        self._n += 1
        op.idx = self._n
        ex = [b for b in reads if b.excl]
        if ex:
            reads = [b for b in reads if not b.excl]
            writes = list(writes) + [b for b in ex if b not in writes]
        for b in reads:
            self._dep(op, b.last_w)
        for b in writes:
            self._dep(op, b.last_w)
            for r in b.readers:
                self._dep(op, r)
        for b in reads:
            b.readers.append(op)
        for b in writes:
            b.last_w = op
            b.readers = []
        self.ops[op.eng].append(op)
        self.all_ops.append(op)
        return op

    def op(self, eng, fn, reads=(), writes=()):
        return self._record(Op(eng, fn, False), reads, writes)

    def dma(self, eng, fn, reads=(), writes=(), key=None, store=False, inc=16):
        op = Op(eng, fn, True)
        if key is None:
            key = writes[0] if not store else reads[0]
        attr = "st" if store else "ld"
        ds = getattr(key, attr)
        if ds is None:
            ds = DmaSem(self.new_sem())
            setattr(key, attr, ds)
            self.dma_sems.append(ds)
        op.dsem = ds
        op.inc = inc
        self._record(op, reads, writes)
        ds.count += inc
        op.dval = ds.count
        return op

    def coll(self, fn, reads=(), writes=()):
        if not hasattr(self, "_csem"):
            self._csem = self.new_sem()
            self._ccnt = 0
            self._cscr = self.sbuf("coll_scr", [128, 8], F32)
        self._ccnt += 1
        n = self._ccnt
        csem = self._csem
        scr = self._cscr

        def run(eng, fn=fn, n=n):
            fn(eng).then_inc(csem)
            eng.wait_ge(csem, n)
            return eng.memset(scr[:], 0.0)
        return self.op("pool", run, reads=reads, writes=writes)

    def barrier(self, bufs=()):
        lastc = {}
        for e in ENGS:
            lastc[e] = None
            for q in reversed(self.ops[e]):
                if not q.is_dma and q.fn is not None:
                    lastc[e] = q
                    break
        tot = [(ds, ds.count) for ds in self.dma_sems if ds.count > 0]
        for e in ENGS:
            op = Op(e, None, False)
            self._n += 1
            op.idx = self._n
            for e2 in ENGS:
                q = lastc[e2]
                if q is not None and (e2 != e or e != "pe"):
                    op.waits[e2] = q
                    q.signaled = True
            for ds, c in tot:
                op.dma_waits[id(ds)] = (ds, c)
            self.ops[e].append(op)
            self.all_ops.append(op)

    def emit(self):
        nc = self.nc
        for e in ENGS:
            sem = None
            cnt = 0
            for op in self.ops[e]:
                if op.is_dma or not op.signaled or op.fn is None:
                    continue
                if sem is None or cnt >= SEM_ROTATE:
                    sem = self.new_sem()
                    cnt = 0
                cnt += 1
                op.sig = (sem, cnt)
        prog = self

        def run(engname, eng):
            for op in prog.ops[engname]:
                for p in op.waits.values():
                    s, v = p.sig
                    eng.wait_ge(s, v)
                for ds, v in op.dma_waits.values():
                    eng.wait_ge(ds.sem, v)
                if op.fn is None:
                    continue
                ins = op.fn(eng)
                if op.is_dma:
                    ins.then_inc(op.dsem.sem, op.inc)
                elif op.signaled:
                    ins.then_inc(op.sig[0], 1)

        with nc.Block() as block:
            @block.tensor
            def _(eng):
                run("pe", eng)

            @block.scalar
            def _(eng):
                run("act", eng)

            @block.vector
            def _(eng):
                run("dve", eng)

            @block.gpsimd
            def _(eng):
                run("pool", eng)

            @block.sync
            def _(eng):
                run("sp", eng)
                for ds in prog.dma_sems:
                    if ds.count > 0:
                        eng.wait_ge(ds.sem, ds.count)
        self.stack.close()


D = 2048
EPS = 1e-6
NEG = -30000.0


class Ctx:
    def __init__(self, nc):
        self.nc = nc
        self.P = Prog(nc)
        self._cnt = 0

    def T(self, name, shape, dt):
        t = self.P.sbuf(name, shape, dt)
        b = Buf(name)
        return t, b

    def PS(self, name, shape, dt):
        t = self.P.psum(name, shape, dt)
        b = Buf(name, excl=True)
        return t, b

    def din(self, name, shape, dt=F32):
        return self.nc.dram_tensor(name, list(shape), dt, kind="ExternalInput").ap()

    def dout(self, name, shape, dt=F32):
        return self.nc.dram_tensor(name, list(shape), dt, kind="ExternalOutput").ap()

    def load(self, q, out_ap, in_ap, wbuf, reads=()):
        return self.P.dma(q, lambda e: e.dma_start(out=out_ap, in_=in_ap), reads=list(reads), writes=[wbuf])

    def store(self, q, out_ap, in_ap, rbuf, writes=()):
        return self.P.dma(q, lambda e: e.dma_start(out=out_ap, in_=in_ap), reads=[rbuf], writes=list(writes), key=rbuf, store=True)

    def mm(self, out, lhsT, rhs, start, stop, reads, writes):
        return self.P.op("pe", lambda e: e.matmul(out, lhsT=lhsT, rhs=rhs, start=start, stop=stop), reads=reads, writes=writes)

    def tr(self, out, in_, ident, reads, writes):
        return self.P.op("pe", lambda e: e.transpose(out=out, in_=in_, identity=ident), reads=reads, writes=writes)

    def act(self, out, in_, func, reads, writes, bias=None, scale=None, accum_out=None):
        kw = {}
        if bias is not None:
            kw["bias"] = bias
        if scale is not None:
            kw["scale"] = scale
        if accum_out is not None:
            kw["accum_out"] = accum_out
        return self.P.op("act", lambda e: e.activation(out=out, in_=in_, func=func, **kw), reads=reads, writes=writes)

    def ts(self, eng, out, in0, s1, s2, op0, op1, reads, writes):
        if op1 is None:
            return self.P.op(eng, lambda e: e.tensor_scalar(out=out, in0=in0, scalar1=s1, scalar2=None, op0=op0), reads=reads, writes=writes)
        return self.P.op(eng, lambda e: e.tensor_scalar(out=out, in0=in0, scalar1=s1, scalar2=s2, op0=op0, op1=op1), reads=reads, writes=writes)

    def tt(self, eng, out, in0, in1, op, reads, writes):
        return self.P.op(eng, lambda e: e.tensor_tensor(out=out, in0=in0, in1=in1, op=op), reads=reads, writes=writes)

    def stt(self, out, in0, scalar, in1, op0, op1, reads, writes):
        return self.P.op("dve", lambda e: e.scalar_tensor_tensor(out=out, in0=in0, scalar=scalar, in1=in1, op0=op0, op1=op1), reads=reads, writes=writes)

    def copy(self, eng, out, in_, reads, writes):
        if eng == "act":
            return self.P.op("act", lambda e: e.copy(out=out, in_=in_), reads=reads, writes=writes)
        return self.P.op(eng, lambda e: e.tensor_copy(out=out, in_=in_), reads=reads, writes=writes)


def rstd_from_ss(C, rstd, ss, n, bss, brstd):
    C.ts("dve", rstd, ss, 1.0 / n, EPS, ALU.mult, ALU.add, [bss], [brstd])
    C.act(rstd, rstd, ACTF.Sqrt, [brstd], [brstd])
    C.P.op("dve", lambda e: e.reciprocal(out=rstd, in_=rstd), reads=[brstd], writes=[brstd])


def build_prompt(nc, NB=4, do_tail=True, stop=99, sub=99):
    C = Ctx(nc)
    P = C.P
    NT = NB * 256
    xp = C.din("xp", [NT, D])
    mrow = C.din("mrow", [6, D])
    g_attn = C.din("g_attn", [1, D])
    g_ffn = C.din("g_ffn", [1, D])
    g_q_a = C.din("g_q_a", [1, 512])
    g_kv_a = C.din("g_kv_a", [1, 256])
    w_in = C.din("w_in", [D, 3904])
    w_q_b = C.din("w_q_b", [512, 1536])
    w_kv_b = C.din("w_kv_b", [256, 2048])
    w_out = C.din("w_out", [D, D])
    w_router = C.din("w_router", [D, 32])
    b_router = C.din("b_router", [1, 32])
    ident = C.din("ident", [128, 128])
    o_nak = C.dout("o_nak", [NT, 1024])
    o_nav = C.dout("o_nav", [NT, 1024])
    o_ckv = C.dout("o_ckv", [NT, 256])
    o_krope = C.dout("o_krope", [NT, 64])
    o_x1 = C.dout("o_x1", [NT, D])
    o_h2 = C.dout("o_h2", [NT, D], BF16)
    o_G = C.dout("o_G", [NT, 32])

    idF, bidF = C.T("idF", [128, 128], F32)
    idB, bidB = C.T("idB", [128, 128], BF16)
    G1, bG1 = C.T("G1", [128, D], F32)
    SA, bSA = C.T("SA", [128, D], F32)
    GA, bGA = C.T("GA", [128, D], F32)
    G2, bG2 = C.T("G2", [128, D], F32)
    SF, bSF = C.T("SF", [128, D], F32)
    gqa, bgqa = C.T("gqa", [128, 512], F32)
    gkva, bgkva = C.T("gkva", [128, 256], F32)
    brt, bbrt = C.T("brt", [128, 32], F32)
    wr, bwr = C.T("wr", [128, 16, 32], F32)
    wqb, bwqb = C.T("wqb", [128, 4, 1536], BF16)
    wkvb, bwkvb = C.T("wkvb", [128, 2, 2048], BF16)
    xt = [C.T(f"xt{t}", [128, D], F32) for t in range(2)]
    tmpf, btmpf = C.T("tmpf", [128, D], F32)
    hb, bhb = C.T("hb", [128, D], BF16)
    hT, bhT = C.T("hT", [128, 16, 256], BF16)
    NWS = 2
    ws = [C.T(f"ws{i}", [128, 16, 256], BF16) for i in range(NWS)]
    QT, bQT = C.T("QT", [128, 8, 256], BF16)
    Kb, bKb = C.T("Kb", [128, 2, 1024], BF16)
    Vb, bVb = C.T("Vb", [128, 2, 1024], BF16)
    KT, bKT = C.T("KT", [128, 8, 256], BF16)
    qaf, bqaf = C.T("qaf", [128, 2, 512], F32)
    qan, bqan = C.T("qan", [128, 2, 512], BF16)
    qanT, bqanT = C.T("qanT", [128, 4, 256], BF16)
    kvaf, bkvaf = C.T("kvaf", [128, 2, 256], F32)
    ckvf, bckvf = C.T("ckvf", [128, 2, 256], F32)
    ckvb, bckvb = C.T("ckvb", [128, 2, 256], BF16)
    ckvT, bckvT = C.T("ckvT", [128, 2, 256], BF16)
    krf, bkrf = C.T("krf", [128, 2, 64], F32)
    krb, bkrb = C.T("krb", [128, 2, 64], BF16)
    krT, bkrT = C.T("krT", [128, 256], BF16)
    qnT, bqnT = C.T("qnT", [128, 8, 256], BF16)
    qrT, bqrT = C.T("qrT", [128, 8, 256], BF16)
    knT, bknT = C.T("knT", [128, 8, 256], BF16)
    vm, bvm = C.T("vm", [128, 2, 1024], BF16)
    ocat, bocat = C.T("ocat", [128, 2, D], BF16)
    ocT, bocT = C.T("ocT", [128, 16, 256], BF16)
    stg = [C.T(f"stg{i}", [128, 256], F32) for i in range(2)]
    Pb = [C.T(f"Pb{i}", [128, 256], BF16) for i in range(2)]
    PT = [C.T(f"PT{i}", [128, 2, 128], BF16) for i in range(2)]
    st_ss, bst_ss = C.T("st_ss", [128, 8], F32)
    st_r, bst_r = C.T("st_r", [128, 8], F32)
    amx = [C.T(f"amx{i}", [128, 4], F32) for i in range(2)]
    h2f, bh2f = C.T("h2f", [128, D], F32)
    h2b, bh2b = C.T("h2b", [128, D], BF16)
    h2T, bh2T = C.T("h2T", [128, 16, 128], F32)
    lg, blg = C.T("lg", [128, 32], F32)
    lg2, blg2 = C.T("lg2", [128, 32], F32)
    top8, btop8 = C.T("top8", [128, 8], F32)

    pT, bpT = C.PS("pT", [128, 2048], BF16)
    pA = [C.PS(f"pA{i}", [128, 512], F32) for i in range(2)]
    pS = [C.PS(f"pS{i}", [128, 512], F32) for i in range(2)]
    pTp_full, bpTp = C.PS("pTp", [128, 8, 128], BF16)
    pTp = pTp_full[:, 0:2, :]
    pO, bpO = C.PS("pO", [128, 512], F32)

    C.load("sp", idF[:], ident, bidF)
    C.copy("dve", idB[:], idF[:], [bidF], [bidB])

    def bload(dst, row_ap, buf):
        C.load("sp", dst[:], row_ap.partition_broadcast(128), buf)

    bload(SA, mrow[0:1, :], bSA)
    bload(G1, mrow[1:2, :], bG1)
    bload(tmpf, g_attn[0:1, :], btmpf)
    C.stt(G1[:], G1[:], 1.0, tmpf[:], ALU.add, ALU.mult, [bG1, btmpf], [bG1])
    bload(GA, mrow[2:3, :], bGA)
    bload(SF, mrow[3:4, :], bSF)
    bload(G2, mrow[4:5, :], bG2)
    bload(h2f, g_ffn[0:1, :], bh2f)
    C.stt(G2[:], G2[:], 1.0, h2f[:], ALU.add, ALU.mult, [bG2, bh2f], [bG2])
    bload(gqa, g_q_a[0:1, :], bgqa)
    bload(gkva, g_kv_a[0:1, :], bgkva)
    bload(brt, b_router[0:1, :], bbrt)
    C.load("sp", wr[:], w_router.rearrange("(k p) n -> p k n", p=128), bwr)
    C.load("pool", wqb[:], w_q_b.rearrange("(k p) n -> p k n", p=128), bwqb)
    C.load("pool", wkvb[:], w_kv_b.rearrange("(k p) n -> p k n", p=128), bwkvb)

    P.op("pool", lambda e: e.memset(qrT[:], 0.0), writes=[bqrT])
    P.op("pool", lambda e: e.memset(krT[:], 0.0), writes=[bkrT])
    if stop <= 0:
        P.emit()
        return nc
    ws_i = [0]

    def next_ws(src_cols_ap, ncols):
        i = ws_i[0] % NWS
        ws_i[0] += 1
        t, b = ws[i]
        C.load("pool", t[:, :, 0:ncols], src_cols_ap.rearrange("(k p) n -> p k n", p=128), b)
        return t, b

    pa_i = [0]

    def next_pA():
        i = pa_i[0] % 2
        pa_i[0] += 1
        return pA[i]

    stg_i = [0]

    def next_stg():
        i = stg_i[0] % 2
        stg_i[0] += 1
        return stg[i]

    SC_NA = 128 ** -0.5
    SC_MLA = 192 ** -0.5

    def rmsnorm_tile(src, bsrc, col):
        n = src.shape[-1] if len(src.shape) == 2 else None
        C.act(hb[:, 0:src.shape[1]], src, ACTF.Square, [bsrc], [bhb, bst_ss], accum_out=st_ss[:, col:col + 1])

    for b in range(NB):
        r0 = b * 256
        for t in range(2):
            x_t, bx = xt[t]
            C.load("sp", x_t[:], xp[r0 + t * 128: r0 + (t + 1) * 128, :], bx)
            C.act(hb[:], x_t[:], ACTF.Square, [bx], [bhb, bst_ss], accum_out=st_ss[:, 0:1])
            rstd_from_ss(C, st_r[:, 0:1], st_ss[:, 0:1], D, bst_ss, bst_r)
            C.stt(tmpf[:], x_t[:], st_r[:, 0:1], G1[:], ALU.mult, ALU.mult, [bx, bst_r, bG1], [btmpf])
            C.tt("dve", hb[:], tmpf[:], SA[:], ALU.add, [btmpf, bSA], [bhb])
            for k in range(16):
                C.tr(pT[:, k * 128:(k + 1) * 128], hb[:, k * 128:(k + 1) * 128], idB[:], [bhb, bidB], [bpT])
            C.copy("act", hT[:, :, t * 128:(t + 1) * 128], pT[:].rearrange("p (k c) -> p k c", k=16), [bpT], [bhT])

        if stop <= 1:
            continue
        for s in range(4):
            wt, bw = next_ws(w_in[:, s * 256:(s + 1) * 256], 256)
            for hh in range(2):
                head = s * 2 + hh
                pa, bpa = next_pA()
                for k in range(16):
                    C.mm(pa[:, 0:256], wt[:, k, hh * 128:(hh + 1) * 128], hT[:, k, :], k == 0, k == 15, [bw, bhT], [bpa])
                C.act(QT[:, head, :], pa[:, 0:256], ACTF.Copy, [bpa], [bQT], scale=SC_NA)
        if stop == 2 and sub <= 0:
            continue
        for which, (obuf, sb_t, sb_b) in enumerate(((o_nak, Kb, bKb), (o_nav, Vb, bVb))):
            for s in range(4):
                c0 = 1024 * (1 + which) + s * 256
                wt, bw = next_ws(w_in[:, c0:c0 + 256], 256)
                for t in range(2):
                    pa, bpa = next_pA()
                    for k in range(16):
                        C.mm(pa[:, 0:256], hT[:, k, t * 128:(t + 1) * 128], wt[:, k, :], k == 0, k == 15, [bw, bhT], [bpa])
                    sg, bsg = next_stg()
                    C.copy("dve", sg[:], pa[:, 0:256], [bpa], [bsg])
                    C.copy("act", sb_t[:, t, s * 256:(s + 1) * 256], pa[:, 0:256], [bpa], [sb_b])
                    C.store("sp", obuf[r0 + t * 128: r0 + (t + 1) * 128, s * 256:(s + 1) * 256], sg[:], bsg)
        if stop == 2 and sub <= 1:
            continue
        for s in range(2):
            c0 = 3072 + s * 256
            wt, bw = next_ws(w_in[:, c0:c0 + 256], 256)
            for t in range(2):
                pa, bpa = next_pA()
                for k in range(16):
                    C.mm(pa[:, 0:256], hT[:, k, t * 128:(t + 1) * 128], wt[:, k, :], k == 0, k == 15, [bw, bhT], [bpa])
                C.copy("dve", qaf[:, t, s * 256:(s + 1) * 256], pa[:, 0:256], [bpa], [bqaf])
        for t in range(2):
            C.act(hb[:, 0:512], qaf[:, t, :], ACTF.Square, [bqaf], [bhb, bst_ss], accum_out=st_ss[:, 1:2])
            rstd_from_ss(C, st_r[:, 1:2], st_ss[:, 1:2], 512, bst_ss, bst_r)
            C.stt(qan[:, t, :], qaf[:, t, :], st_r[:, 1:2], gqa[:], ALU.mult, ALU.mult, [bqaf, bst_r, bgqa], [bqan])
        if stop == 2 and sub <= 2:
            continue
        wt, bw = next_ws(w_in[:, 3584:3840], 256)
        for t in range(2):
            pa, bpa = next_pA()
            for k in range(16):
                C.mm(pa[:, 0:256], hT[:, k, t * 128:(t + 1) * 128], wt[:, k, :], k == 0, k == 15, [bw, bhT], [bpa])
            C.copy("dve", kvaf[:, t, :], pa[:, 0:256], [bpa], [bkvaf])
            C.act(hb[:, 0:256], kvaf[:, t, :], ACTF.Square, [bkvaf], [bhb, bst_ss], accum_out=st_ss[:, 2:3])
            rstd_from_ss(C, st_r[:, 2:3], st_ss[:, 2:3], 256, bst_ss, bst_r)
            C.stt(ckvf[:, t, :], kvaf[:, t, :], st_r[:, 2:3], gkva[:], ALU.mult, ALU.mult, [bkvaf, bst_r, bgkva], [bckvf])
            C.copy("act", ckvb[:, t, :], ckvf[:, t, :], [bckvf], [bckvb])
        C.store("sp", o_ckv[r0:r0 + 256, :].rearrange("(t p) c -> p t c", p=128), ckvf[:], bckvf)
        if stop == 2 and sub <= 3:
            continue
        wt, bw = next_ws(w_in[:, 3840:3904], 64)
        for t in range(2):
            pa, bpa = next_pA()
            for k in range(16):
                C.mm(pa[:, 0:64], hT[:, k, t * 128:(t + 1) * 128], wt[:, k, 0:64], k == 0, k == 15, [bw, bhT], [bpa])
            C.copy("dve", krf[:, t, :], pa[:, 0:64], [bpa], [bkrf])
            C.copy("act", krb[:, t, :], pa[:, 0:64], [bpa], [bkrb])
        C.store("sp", o_krope[r0:r0 + 256, :].rearrange("(t p) c -> p t c", p=128), krf[:], bkrf)

        if stop <= 2:
            continue
        for t in range(2):
            for hd in range(8):
                C.tr(pT[:, hd * 128:(hd + 1) * 128], Kb[:, t, hd * 128:(hd + 1) * 128], idB[:], [bKb, bidB], [bpT])
            C.copy("act", KT[:, :, t * 128:(t + 1) * 128], pT[:, 0:1024].rearrange("p (k c) -> p k c", k=8), [bpT], [bKT])
        for t in range(2):
            for c in range(4):
                C.tr(pT[:, c * 128:(c + 1) * 128], qan[:, t, c * 128:(c + 1) * 128], idB[:], [bqan, bidB], [bpT])
            for c in range(2):
                C.tr(pT[:, (4 + c) * 128:(5 + c) * 128], ckvb[:, t, c * 128:(c + 1) * 128], idB[:], [bckvb, bidB], [bpT])
            C.tr(pT[0:64, 6 * 128:7 * 128], krb[:, t, :], idB[:], [bkrb, bidB], [bpT])
            C.copy("act", qanT[:, :, t * 128:(t + 1) * 128], pT[:, 0:512].rearrange("p (k c) -> p k c", k=4), [bpT], [bqanT])
            C.copy("dve", ckvT[:, :, t * 128:(t + 1) * 128], pT[:, 512:768].rearrange("p (k c) -> p k c", k=2), [bpT], [bckvT])
            C.copy("dve", krT[0:64, t * 128:(t + 1) * 128], pT[0:64, 768:896], [bpT], [bkrT])
        for hd in range(8):
            pa, bpa = next_pA()
            for c in range(4):
                C.mm(pa[:, 0:256], wqb[:, c, hd * 192: hd * 192 + 128], qanT[:, c, :], c == 0, c == 3, [bwqb, bqanT], [bpa])
            C.act(qnT[:, hd, :], pa[:, 0:256], ACTF.Copy, [bpa], [bqnT], scale=SC_MLA)
            pa, bpa = next_pA()
            for c in range(4):
                C.mm(pa[0:64, 0:256], wqb[:, c, hd * 192 + 128: hd * 192 + 192], qanT[:, c, :], c == 0, c == 3, [bwqb, bqanT], [bpa])
            C.act(qrT[0:64, hd, :], pa[0:64, 0:256], ACTF.Copy, [bpa], [bqrT], scale=SC_MLA)
            pa, bpa = next_pA()
            for c in range(2):
                C.mm(pa[:, 0:256], wkvb[:, c, hd * 256: hd * 256 + 128], ckvT[:, c, :], c == 0, c == 1, [bwkvb, bckvT], [bpa])
            C.copy("dve", knT[:, hd, :], pa[:, 0:256], [bpa], [bknT])
        for t in range(2):
            for half in range(2):
                pa, bpa = next_pA()
                for c in range(2):
                    rhs = wkvb[:, c, :].rearrange("p (h x) -> p h x", h=8)[:, half * 4:(half + 1) * 4, 128:256]
                    C.mm(pa[:, 0:512], ckvT[:, c, t * 128:(t + 1) * 128], rhs, c == 0, c == 1, [bwkvb, bckvT], [bpa])
                C.copy("act", vm[:, t, half * 512:(half + 1) * 512], pa[:, 0:512], [bpa], [bvm])

        if stop <= 3:
            continue
        ai = [0]

        def attend(t, score_ops, vtile, bv, vcol, ocol):
            i = ai[0] % 2
            ai[0] += 1
            ps, bps = pS[i]
            pb, bpb = Pb[i]
            ptt, bptt = PT[i]
            am, bam = amx[i]
            n = len(score_ops)
            for j, (lt, rh, rd) in enumerate(score_ops):
                C.mm(ps[:, 0:256], lt, rh, j == 0, j == n - 1, rd, [bps])
            P.op("dve", lambda e: e.tensor_reduce(out=am[:, 0:1], in_=ps[:, 0:256], axis=AX.X, op=ALU.max, negate=True),
                 reads=[bps], writes=[bam])
            C.act(pb[:], ps[:, 0:256], ACTF.Exp, [bps, bam], [bpb, bam], bias=am[:, 0:1], scale=1.0, accum_out=am[:, 1:2])
            P.op("dve", lambda e: e.reciprocal(out=am[:, 2:3], in_=am[:, 1:2]), reads=[bam], writes=[bam])
            for j in range(2):
                C.tr(pTp_full[:, j, :], pb[:, j * 128:(j + 1) * 128], idB[:], [bpb, bidB], [bpTp])
            C.copy("dve", ptt[:], pTp_full[:, 0:2, :], [bpTp], [bptt])
            for j in range(2):
                C.mm(pO[:, 0:128], ptt[:, j, :], vtile[:, j, vcol:vcol + 128], j == 0, j == 1, [bptt, bv], [bpO])
            C.ts("dve", ocat[:, t, ocol:ocol + 128], pO[:, 0:128], am[:, 2:3], None, ALU.mult, None, [bpO, bam], [bocat])

        for hd in range(8):
            for t in range(2):
                attend(t, [(QT[:, hd, t * 128:(t + 1) * 128], KT[:, hd, :], [bQT, bKT])], Vb, bVb, hd * 128, hd * 128)
        for hd in range(8):
            for t in range(2):
                attend(t, [(qnT[:, hd, t * 128:(t + 1) * 128], knT[:, hd, :], [bqnT, bknT]),
                           (qrT[:, hd, t * 128:(t + 1) * 128], krT[:, :], [bqrT, bkrT])], vm, bvm, hd * 128, 1024 + hd * 128)

        if not do_tail:
            for t in range(2):
                C.copy("dve", h2b[:], ocat[:, t, :], [bocat], [bh2b])
                C.store("sp", o_h2[r0 + t * 128: r0 + (t + 1) * 128, :], h2b[:], bh2b)
            continue

        for t in range(2):
            for k in range(16):
                C.tr(pT[:, k * 128:(k + 1) * 128], ocat[:, t, k * 128:(k + 1) * 128], idB[:], [bocat, bidB], [bpT])
            C.copy("act", ocT[:, :, t * 128:(t + 1) * 128], pT[:].rearrange("p (k c) -> p k c", k=16), [bpT], [bocT])
        for s in range(8):
            wt, bw = next_ws(w_out[:, s * 256:(s + 1) * 256], 256)
            for t in range(2):
                x_t, bx = xt[t]
                pa, bpa = next_pA()
                for k in range(16):
                    C.mm(pa[:, 0:256], ocT[:, k, t * 128:(t + 1) * 128], wt[:, k, :], k == 0, k == 15, [bw, bocT], [bpa])
                sg, bsg = next_stg()
                C.tt("dve", sg[:], pa[:, 0:256], GA[:, s * 256:(s + 1) * 256], ALU.mult, [bpa, bGA], [bsg])
                C.tt("dve", x_t[:, s * 256:(s + 1) * 256], x_t[:, s * 256:(s + 1) * 256], sg[:], ALU.add, [bx, bsg], [bx])
        for t in range(2):
            x_t, bx = xt[t]
            C.store("sp", o_x1[r0 + t * 128: r0 + (t + 1) * 128, :], x_t[:], bx)
            C.act(hb[:], x_t[:], ACTF.Square, [bx], [bhb, bst_ss], accum_out=st_ss[:, 3:4])
            rstd_from_ss(C, st_r[:, 3:4], st_ss[:, 3:4], D, bst_ss, bst_r)
            C.stt(tmpf[:], x_t[:], st_r[:, 3:4], G2[:], ALU.mult, ALU.mult, [bx, bst_r, bG2], [btmpf])
            C.tt("dve", h2f[:], tmpf[:], SF[:], ALU.add, [btmpf, bSF], [bh2f])
            C.copy("act", h2b[:], h2f[:], [bh2f], [bh2b])
            C.store("sp", o_h2[r0 + t * 128: r0 + (t + 1) * 128, :], h2b[:], bh2b)
            for g4 in range(4):
                pa, bpa = next_pA()
                for j in range(4):
                    k = g4 * 4 + j
                    C.tr(pa[:, j * 128:(j + 1) * 128], h2f[:, k * 128:(k + 1) * 128], idF[:], [bh2f, bidF], [bpa])
                C.copy("dve", h2T[:, g4 * 4:(g4 + 1) * 4, :], pa[:, 0:512].rearrange("p (k c) -> p k c", k=4), [bpa], [bh2T])
            pa, bpa = next_pA()
            for k in range(16):
                C.mm(pa[:, 0:32], h2T[:, k, :], wr[:, k, :], k == 0, k == 15, [bh2T, bwr], [bpa])
            C.tt("dve", lg[:], pa[:, 0:32], brt[:], ALU.add, [bpa, bbrt], [blg])
            P.op("dve", lambda e: e.max(out=top8[:], in_=lg[:]), reads=[blg], writes=[btop8])
            C.ts("dve", lg2[:], lg[:], top8[:, 3:4], None, ALU.is_ge, None, [blg, btop8], [blg2])
            C.ts("dve", top8[:, 7:8], top8[:, 0:1], -1.0, None, ALU.mult, None, [btop8], [btop8])
            C.act(lg[:], lg[:], ACTF.Exp, [blg, btop8], [blg], bias=top8[:, 7:8], scale=1.0)
            C.tt("dve", lg[:], lg[:], lg2[:], ALU.mult, [blg, blg2], [blg])
            P.op("dve", lambda e: e.tensor_reduce(out=top8[:, 6:7], in_=lg[:], axis=AX.X, op=ALU.add), reads=[blg], writes=[btop8])
            P.op("dve", lambda e: e.reciprocal(out=top8[:, 6:7], in_=top8[:, 6:7]), reads=[btop8], writes=[btop8])
            C.ts("dve", lg2[:], lg[:], top8[:, 6:7], None, ALU.mult, None, [blg, btop8], [blg2])
            C.store("sp", o_G[r0 + t * 128: r0 + (t + 1) * 128, :], lg2[:], blg2)
    P.emit()
    return nc


def build_mod(nc, NCOL=1536):
    C = Ctx(nc)
    P = C.P
    cT = C.din("cT", [128, 16, 3])
    wm = C.din("wm", [D, NCOL])
    bm = C.din("bm", [1, NCOL])
    o_m = C.dout("o_m", [3, NCOL])
    cTf, bcTf = C.T("cTf", [128, 16, 3], F32)
    cTb, bcTb = C.T("cTb", [128, 16, 3], BF16)
    wmb, bwmb = C.T("wmb", [128, 16, NCOL], BF16)
    bmt, bbmt = C.T("bmt", [3, NCOL], F32)
    ot, bot = C.T("ot", [3, NCOL], F32)
    pm = [C.PS(f"pm{i}", [128, 512], F32) for i in range(2)]
    C.load("sp", cTf[:], cT, bcTf)
    C.load("sp", bmt[:], bm[0:1, :].partition_broadcast(3), bbmt)
    C.load("pool", wmb[:], wm.rearrange("(k p) n -> p k n", p=128), bwmb)
    C.act(cTb[:], cTf[:], ACTF.Silu, [bcTf], [bcTb])
    for n in range(NCOL // 512):
        pa, bpa = pm[n % 2]
        for k in range(16):
            C.mm(pa[0:3, :], cTb[:, k, :], wmb[:, k, n * 512:(n + 1) * 512], k == 0, k == 15, [bcTb, bwmb], [bpa])
        C.tt("dve", ot[:, n * 512:(n + 1) * 512], pa[0:3, :], bmt[:, n * 512:(n + 1) * 512], ALU.add, [bpa, bbmt], [bot])
    C.store("sp", o_m, ot[:], bot)
    P.emit()
    return nc


def build_experts(nc, NTOK=16384, NE=4):
    C = Ctx(nc)
    P = C.P
    TB = 512
    NBLK = NTOK // TB
    h2T = C.din("h2T", [D, NTOK], BF16)
    Gl = C.din("Gl", [NTOK, NE])
    GlT = C.din("GlT", [NE, NTOK])
    w_gu = C.din("w_gu", [NE, D, 4096])
    b_gu = C.din("b_gu", [128, NE, 16, 2])
    w_dn = C.din("w_dn", [NE, D, D])
    b_dn = C.din("b_dn", [NE, D])
    o_y = C.dout("o_y", [NTOK, D], BF16)

    hTb = [C.T(f"hTb{i}", [128, 16, TB], BF16) for i in range(2)]
    wgu = [C.T(f"wgu{i}", [128, 16, 256], BF16) for i in range(3)]
    wdn = [C.T(f"wdn{i}", [128, 16, 512], BF16) for i in range(2)]
    actb = [C.T(f"actb{i}", [128, 16, TB], BF16) for i in range(2)]
    yacc, byacc = C.T("yacc", [128, 4, D], F32)
    yout = [C.T(f"yout{i}", [128, D], BF16) for i in range(2)]
    GTb = [C.T(f"GTb{i}", [NE, TB], F32) for i in range(2)]
    bdn4, bbdn4 = C.T("bdn4", [NE, D], F32)
    Gt, bGt = C.T("Gt", [128, NBLK * 4, NE], F32)
    bgu, bbgu = C.T("bgu", [128, NE, 16, 2], F32)
    gg = [C.T(f"gg{i}", [128, TB], F32) for i in range(2)]
    ss_ = [C.T(f"ss{i}", [128, TB], F32) for i in range(2)]
    uu = [C.T(f"uu{i}", [128, TB], F32) for i in range(2)]
    pg = [C.PS(f"pg{i}", [128, 512], F32) for i in range(2)]
    pu = [C.PS(f"pu{i}", [128, 512], F32) for i in range(2)]
    pd = [C.PS(f"pd{i}", [128, 512], F32) for i in range(4)]

    C.load("sp", Gt[:], Gl.rearrange("(t p) e -> p t e", p=128), bGt)
    C.load("sp", bgu[:], b_gu, bbgu)
    C.load("sp", bdn4[:], b_dn, bbdn4)

    cnt = dict(wgu=0, wdn=0, act=0, pg=0, pd=0, ew=0)
    for blk in range(NBLK):
        hT_t, bhT_ = hTb[blk % 2]
        C.load("sp", hT_t[:], h2T[:, blk * TB:(blk + 1) * TB].rearrange("(k p) n -> p k n", p=128), bhT_)
        gT, bgT = GTb[blk % 2]
        C.load("sp", gT[:], GlT[:, blk * TB:(blk + 1) * TB], bgT)
        for dc in range(4):
            for tt in range(4):
                pdt, bpd = pd[cnt["pd"] % 4]
                cnt["pd"] += 1
                C.mm(pdt[:, :], gT[:, tt * 128:(tt + 1) * 128], bdn4[:, dc * 512:(dc + 1) * 512], True, True, [bgT, bbdn4], [bpd])
                C.copy("act", yacc[:, tt, dc * 512:(dc + 1) * 512], pdt[:, :], [bpd], [byacc])
        for e in range(NE):
            a_t, ba = actb[cnt["act"] % 2]
            cnt["act"] += 1
            for ffc in range(16):
                wt, bw = wgu[cnt["wgu"] % 3]
                cnt["wgu"] += 1
                C.load("pool", wt[:], w_gu[e, :, ffc * 256:(ffc + 1) * 256].rearrange("(k p) n -> p k n", p=128), bw)
                i = cnt["pg"] % 2
                cnt["pg"] += 1
                pgt, bpg = pg[i]
                put, bpu = pu[i]
                for k in range(16):
                    C.mm(pgt[:, 0:TB], wt[:, k, 0:256:2], hT_t[:, k, :], k == 0, k == 15, [bw, bhT_], [bpg])
                for k in range(16):
                    C.mm(put[:, 0:TB], wt[:, k, 1:256:2], hT_t[:, k, :], k == 0, k == 15, [bw, bhT_], [bpu])
                j = cnt["ew"] % 2
                cnt["ew"] += 1
                g_t, bg = gg[j]
                s_t, bs = ss_[j]
                u_t, bu = uu[j]
                C.ts("dve", g_t[:], pgt[:, 0:TB], bgu[:, e, ffc, 0:1], 7.0, ALU.add, ALU.min, [bpg, bbgu], [bg])
                C.act(s_t[:], g_t[:], ACTF.Sigmoid, [bg], [bs], scale=1.702)
                C.ts("dve", u_t[:], put[:, 0:TB], bgu[:, e, ffc, 1:2], 7.0, ALU.add, ALU.min, [bpu, bbgu], [bu])
                C.ts("pool", u_t[:], u_t[:], -7.0, 1.0, ALU.max, ALU.add, [bu], [bu])
                C.tt("pool", g_t[:], g_t[:], s_t[:], ALU.mult, [bg, bs], [bg])
                C.tt("dve", a_t[:, ffc, :], g_t[:], u_t[:], ALU.mult, [bg, bu], [ba])
            for dc in range(4):
                wd, bwd = wdn[cnt["wdn"] % 2]
                cnt["wdn"] += 1
                C.load("pool", wd[:], w_dn[e, :, dc * 512:(dc + 1) * 512].rearrange("(k p) n -> p k n", p=128), bwd)
                for tt in range(4):
                    pdt, bpd = pd[cnt["pd"] % 4]
                    cnt["pd"] += 1
                    for ffc in range(16):
                        C.mm(pdt[:, :], a_t[:, ffc, tt * 128:(tt + 1) * 128], wd[:, ffc, :], ffc == 0, ffc == 15, [ba, bwd], [bpd])
                    gsc = Gt[:, blk * 4 + tt, e:e + 1]
                    ysl = yacc[:, tt, dc * 512:(dc + 1) * 512]
                    C.stt(ysl, pdt[:, :], gsc, ysl, ALU.mult, ALU.add, [bpd, bGt, byacc], [byacc])
        for tt in range(4):
            yo, byo = yout[tt % 2]
            C.copy("act", yo[:], yacc[:, tt, :], [byacc], [byo])
            C.store("sp", o_y[blk * TB + tt * 128: blk * TB + (tt + 1) * 128, :], yo[:], byo)
    P.emit()
    return nc


def build_combine(nc, NT=2048, NP=8):
    C = Ctx(nc)
    P = C.P
    x1 = C.din("x1", [NT, D])
    yp = C.din("yp", [NP, NT, D], BF16)
    gf = C.din("gf", [2, D])
    g_final = C.din("g_final", [1, D])
    o_y = C.dout("o_y", [NT, D])
    GF = [C.T(f"GF{i}", [128, D], F32) for i in range(2)]
    gfin, bgfin = C.T("gfin", [128, D], F32)
    xt = [C.T(f"xt{i}", [128, D], F32) for i in range(2)]
    ypt = [C.T(f"ypt{i}", [128, NP, D], BF16) for i in range(2)]
    acc, bacc_ = C.T("acc", [128, D], F32)
    junk, bjunk = C.T("junk", [128, D], BF16)
    ot = [C.T(f"ot{i}", [128, D], F32) for i in range(2)]
    st, bst = C.T("st", [128, 4], F32)
    for i in range(2):
        C.load("sp", GF[i][0][:], gf[i:i + 1, :].partition_broadcast(128), GF[i][1])
    C.load("sp", gfin[:], g_final[0:1, :].partition_broadcast(128), bgfin)
    ntile = NT // 128
    for t in range(ntile):
        x_t, bx = xt[t % 2]
        y_t, by = ypt[t % 2]
        o_t, bo = ot[t % 2]
        GFt, bGF = GF[0] if t < ntile // 2 else GF[1]
        C.load("sp", x_t[:], x1[t * 128:(t + 1) * 128, :], bx)
        C.load("sp", y_t[:], yp[:, t * 128:(t + 1) * 128, :].rearrange("j p d -> p j d"), by)
        C.tt("dve", acc[:], y_t[:, 0, :], y_t[:, 1, :], ALU.add, [by], [bacc_])
        for j in range(2, NP):
            C.tt("dve", acc[:], acc[:], y_t[:, j, :], ALU.add, [by, bacc_], [bacc_])
        C.tt("dve", acc[:], acc[:], GFt[:], ALU.mult, [bacc_, bGF], [bacc_])
        C.tt("dve", acc[:], acc[:], x_t[:], ALU.add, [bacc_, bx], [bacc_])
        C.act(junk[:], acc[:], ACTF.Square, [bacc_], [bjunk, bst], accum_out=st[:, 0:1])
        rstd_from_ss(C, st[:, 1:2], st[:, 0:1], D, bst, bst)
        C.stt(o_t[:], acc[:], st[:, 1:2], gfin[:], ALU.mult, ALU.mult, [bacc_, bst, bgfin], [bo])
        C.store("sp", o_y[t * 128:(t + 1) * 128, :], o_t[:], bo)
    P.emit()
    return nc


class Arena:
    def __init__(self, C, name, nbytes):
        self.t = C.P.sbuf(name, [128, nbytes // 2], BF16)
        self.cap = nbytes // 2
        self.off = 0
        self.peak = 0

    def mark(self):
        return self.off

    def reset(self, m):
        self.off = m

    def alloc(self, name, shape, dt):
        n = 1
        for d in shape[1:]:
            n *= d
        e16 = n * (2 if dt == F32 else 1)
        e16 = (e16 + 15) // 16 * 16
        assert self.off + e16 <= self.cap, (name, self.off, e16, self.cap)
        ap = self.t[:, self.off:self.off + e16]
        self.off += e16
        self.peak = max(self.peak, self.off)
        if dt == F32:
            ap = ap.bitcast(F32)
        ap = ap[:, 0:n]
        if len(shape) == 3:
            ap = ap.rearrange("p (a b) -> p a b", a=shape[1])
        elif len(shape) == 4:
            ap = ap.rearrange("p (a b c) -> p a b c", a=shape[1], b=shape[2])
        if shape[0] < 128:
            ap = ap[0:shape[0]]
        return ap, Buf(name)


def build_sample(nc, stop=99, NALLT=32, NPAIR=8, NHG=4, NMLAH=8):
    C = Ctx(nc)
    P = C.P
    x_own = C.din("x_own", [1024, D])
    x_halo = C.din("x_halo", [1792, D])
    x_all = C.din("x_all", [4096, D])
    ck_na = C.din("ck_na", [512, 1024])
    cv_na = C.din("cv_na", [512, 1024])
    c_ckv = C.din("c_ckv", [512, 256])
    c_krope = C.din("c_krope", [512, 64])
    mrow = C.din("mrow", [6, D])
    g_attn = C.din("g_attn", [1, D])
    g_ffn = C.din("g_ffn", [1, D])
    g_q_a = C.din("g_q_a", [1, 512])
    g_kv_a = C.din("g_kv_a", [1, 256])
    w_in = C.din("w_in", [D, 3904])
    w_in_rs = C.din("w_in_rs", [D, 64])
    w_q_b = C.din("w_q_b", [512, 1536])
    w_q_b_rs = C.din("w_q_b_rs", [512, 512])
    w_kv_b = C.din("w_kv_b", [256, 2048])
    w_out = C.din("w_out", [D, D])
    w_router = C.din("w_router", [D, 32])
    b_router = C.din("b_router", [1, 32])
    ident = C.din("ident", [128, 128])
    jmat = C.din("jmat", [128, 128])
    amat = C.din("amat", [2, 128])
    rmx = C.din("rmx", [8, 2, 896])
    colmask = C.din("colmask", [128, 15, 64])
    rpbpad = C.din("rpbpad", [8, 15, 160])
    cos_tok = C.din("cos_tok", [4096, 64])
    sinS_tok = C.din("sinS_tok", [4096, 64])
    cosT_own = C.din("cosT_own", [64, 1024])
    sinST_own = C.din("sinST_own", [64, 1024])
    o_x1 = C.dout("o_x1", [1024, D])
    o_h2 = C.dout("o_h2", [1024, D], BF16)
    o_G = C.dout("o_G", [1024, 32])
    ocat_d = nc.dram_tensor("ocat_d", [1024, D], BF16).ap()
    bocat_d = Buf("ocat_d")

    SC_NA = 128 ** -0.5
    SC_MLA = 192 ** -0.5

    idF, bidF = C.T("idF", [128, 128], F32)
    idB, bidB = C.T("idB", [128, 128], BF16)
    jB, bjB = C.T("jB", [128, 128], BF16)
    aB, baB = C.T("aB", [2, 128], BF16)
    T0, bT0 = C.T("T0", [128, D], F32)
    T1, bT1 = C.T("T1", [128, D], F32)
    T2, bT2 = C.T("T2", [128, D], F32)
    gqa, bgqa = C.T("gqa", [128, 512], F32)
    gkva, bgkva = C.T("gkva", [128, 256], F32)
    brt, bbrt = C.T("brt", [128, 32], F32)
    wr, bwr = C.T("wr", [128, 16, 32], F32)
    ckvT, bckvT = C.T("ckvT", [128, 2, 4608], BF16)
    krT, bkrT = C.T("krT", [128, 4608], BF16)
    hTo, bhTo = C.T("hTo", [128, 16, 1024], BF16)
    xt, bxt = C.T("xt", [128, D], F32)
    tmpf, btmpf = C.T("tmpf", [128, D], F32)
    hb, bhb = C.T("hb", [128, D], BF16)
    hTt, bhTt = C.T("hTt", [128, 16, 128], BF16)
    st_ss, bst_ss = C.T("st_ss", [128, 8], F32)
    st_r, bst_r = C.T("st_r", [128, 8], F32)
    AR = Arena(C, "arena", 88 * 1024)

    pT, bpT = C.PS("pT", [128, 2048], BF16)
    pA = [C.PS(f"pA{i}", [128, 512], F32) for i in range(2)]
    pS = [C.PS(f"pS{i}", [128, 512], F32) for i in range(3)]
    pO, bpO = C.PS("pO", [128, 512], F32)

    pa_i = [0]

    def next_pA():
        i = pa_i[0] % 2
        pa_i[0] += 1
        return pA[i]

    def bload(dst, row_ap, buf, q="sp"):
        C.load(q, dst, row_ap.partition_broadcast(128), buf)

    C.load("sp", idF[:], ident, bidF)
    C.copy("dve", idB[:], idF[:], [bidF], [bidB])
    C.load("pool", jB[:], jmat, bjB)
    C.load("pool", aB[:], amat, baB)
    G1, bG1, SA, bSA = T0, bT0, T1, bT1
    bload(SA[:], mrow[0:1, :], bSA)
    bload(G1[:], mrow[1:2, :], bG1)
    bload(tmpf[:], g_attn[0:1, :], btmpf)
    C.stt(G1[:], G1[:], 1.0, tmpf[:], ALU.add, ALU.mult, [bG1, btmpf], [bG1])
    bload(gqa[:], g_q_a[0:1, :], bgqa)
    bload(gkva[:], g_kv_a[0:1, :], bgkva)
    bload(brt[:], b_router[0:1, :], bbrt)
    C.load("sp", wr[:], w_router.rearrange("(k p) n -> p k n", p=128), bwr)
    P.op("pool", lambda e: e.memset(krT[:], 0.0), writes=[bkrT])

    def make_h(src_rows):
        C.load("sp", xt[:], src_rows, bxt)
        C.act(hb[:], xt[:], ACTF.Square, [bxt], [bhb, bst_ss], accum_out=st_ss[:, 0:1])
        rstd_from_ss(C, st_r[:, 0:1], st_ss[:, 0:1], D, bst_ss, bst_r)
        C.stt(tmpf[:], xt[:], st_r[:, 0:1], G1[:], ALU.mult, ALU.mult, [bxt, bst_r, bG1], [btmpf])
        C.tt("dve", hb[:], tmpf[:], SA[:], ALU.add, [btmpf, bSA], [bhb])
        for k in range(16):
            C.tr(pT[:, k * 128:(k + 1) * 128], hb[:, k * 128:(k + 1) * 128], idB[:], [bhb, bidB], [bpT])

    m0 = AR.mark()
    w320, bw320 = AR.alloc("w320", [128, 16, 384], BF16)
    kvaf, bkvaf = AR.alloc("kvaf", [128, 256], F32)
    ckvb, bckvb = AR.alloc("ckvb", [128, 256], BF16)
    krb, bkrb = AR.alloc("krb", [128, 64], BF16)
    cst, bcst = AR.alloc("cst", [128, 2, 64], F32)
    kr1, bkr1 = AR.alloc("kr1", [128, 64], F32)
    kr2, bkr2 = AR.alloc("kr2", [128, 64], F32)
    ccf, bccf = AR.alloc("ccf", [128, 320], F32)
    C.load("pool", w320[:, :, 0:320], w_in[:, 3584:3904].rearrange("(k p) n -> p k n", p=128), bw320)
    C.load("pool", w320[:, :, 320:384], w_in_rs.rearrange("(k p) n -> p k n", p=128), bw320)
    for t in range(NALLT):
        make_h(x_all[t * 128:(t + 1) * 128, :])
        C.copy("act", hTt[:], pT[:].rearrange("p (k c) -> p k c", k=16), [bpT], [bhTt])
        C.load("sp", cst[:, 0, :], cos_tok[t * 128:(t + 1) * 128, :], bcst)
        C.load("sp", cst[:, 1, :], sinS_tok[t * 128:(t + 1) * 128, :], bcst)
        pa, bpa = next_pA()
        for k in range(16):
            C.mm(pa[:, 0:384], hTt[:, k, :], w320[:, k, :], k == 0, k == 15, [bhTt, bw320], [bpa])
        C.copy("dve", kvaf[:], pa[:, 0:256], [bpa], [bkvaf])
        C.tt("dve", kr1[:], pa[:, 256:320], cst[:, 0, :], ALU.mult, [bpa, bcst], [bkr1])
        C.tt("dve", kr2[:], pa[:, 320:384], cst[:, 1, :], ALU.mult, [bpa, bcst], [bkr2])
        C.tt("dve", krb[:], kr1[:], kr2[:], ALU.add, [bkr1, bkr2], [bkrb])
        C.act(hb[:, 0:256], kvaf[:], ACTF.Square, [bkvaf], [bhb, bst_ss], accum_out=st_ss[:, 2:3])
        rstd_from_ss(C, st_r[:, 2:3], st_ss[:, 2:3], 256, bst_ss, bst_r)
        C.stt(ckvb[:], kvaf[:], st_r[:, 2:3], gkva[:], ALU.mult, ALU.mult, [bkvaf, bst_r, bgkva], [bckvb])
        for c in range(2):
            C.tr(pT[:, c * 128:(c + 1) * 128], ckvb[:, c * 128:(c + 1) * 128], idB[:], [bckvb, bidB], [bpT])
        C.tr(pT[0:64, 256:384], krb[:], idB[:], [bkrb, bidB], [bpT])
        C.copy("act", ckvT[:, :, t * 128:(t + 1) * 128], pT[:, 0:256].rearrange("p (k c) -> p k c", k=2), [bpT], [bckvT])
        C.copy("dve", krT[0:64, t * 128:(t + 1) * 128], pT[0:64, 256:384], [bpT], [bkrT])
    for t in range(4):
        C.load("sp", ccf[:, 0:256], c_ckv[t * 128:(t + 1) * 128, :], bccf)
        C.load("sp", ccf[:, 256:320], c_krope[t * 128:(t + 1) * 128, :], bccf)
        C.copy("dve", hb[:, 0:320], ccf[:], [bccf], [bhb])
        for c in range(2):
            C.tr(pT[:, c * 128:(c + 1) * 128], hb[:, c * 128:(c + 1) * 128], idB[:], [bhb, bidB], [bpT])
        C.tr(pT[0:64, 256:384], hb[:, 256:320], idB[:], [bhb, bidB], [bpT])
        C.copy("act", ckvT[:, :, 4096 + t * 128:4096 + (t + 1) * 128], pT[:, 0:256].rearrange("p (k c) -> p k c", k=2), [bpT], [bckvT])
        C.copy("dve", krT[0:64, 4096 + t * 128:4096 + (t + 1) * 128], pT[0:64, 256:384], [bpT], [bkrT])
    P.barrier()
    AR.reset(m0)
    if stop <= 1:
        dbg = C.dout("dbg", [128, 3, 4608], BF16)
        C.store("sp", dbg[:, 0:2, :], ckvT[:], bckvT)
        C.store("sp", dbg[:, 2, :], krT[:], bkrT)
        P.emit()
        return nc

    for t in range(8):
        make_h(x_own[t * 128:(t + 1) * 128, :])
        C.copy("act", hTo[:, :, t * 128:(t + 1) * 128], pT[:].rearrange("p (k c) -> p k c", k=16), [bpT], [bhTo])

    m1 = AR.mark()
    Bstat, bBstat = AR.alloc("Bstat", [128, 8, 896], BF16)
    mB = AR.mark()
    Bfull, bBfull = AR.alloc("Bfull", [128, 8, 15, 64], F32)
    cmk, bcmk = AR.alloc("cmk", [128, 15, 64], F32)
    C.load("sp", cmk[:], colmask, bcmk)
    rp_t = rpbpad.tensor
    for hd in range(8):
        for half in range(2):
            src = bass.AP(rp_t, hd * 2400 + 16, [[1, 64], [160, 15], [1, 64]])
            C.load("sp", Bfull[half * 64:(half + 1) * 64, hd, :, :], src, bBfull)
    for hd in range(8):
        C.tt("dve", Bfull[:, hd, :, :], Bfull[:, hd, :, :], cmk[:], ALU.add, [bBfull, bcmk], [bBfull])
        C.copy("act", Bstat[0:64, hd, :].rearrange("p (j c) -> p j c", j=14), Bfull[0:64, hd, 1:15, :], [bBfull], [bBstat])
        C.copy("act", Bstat[64:128, hd, :].rearrange("p (j c) -> p j c", j=14), Bfull[64:128, hd, 0:14, :], [bBfull], [bBstat])
    P.barrier()
    AR.reset(mB)

    mG = AR.mark()
    for g in range(NHG):
        AR.reset(mG)
        wk, bwk = AR.alloc("wk", [128, 16, 256], BF16)
        wv, bwv = AR.alloc("wv", [128, 16, 256], BF16)
        wq, bwq = AR.alloc("wq", [128, 16, 256], BF16)
        KTh, bKTh = AR.alloc("KTh", [128, 2, 1792], BF16)
        Vh, bVh = AR.alloc("Vh", [128, 14, 256], BF16)
        cKT, bcKT = AR.alloc("cKT", [128, 2, 512], BF16)
        cV, bcV = AR.alloc("cV", [128, 4, 256], BF16)
        QTg, bQTg = AR.alloc("QTg", [128, 2, 1024], BF16)
        kbt, bkbt = AR.alloc("kbt", [128, 256], BF16)
        ccn, bccn = AR.alloc("ccn", [128, 2, 256], F32)
        ccb, bccb = AR.alloc("ccb", [128, 256], BF16)
        rmt = [AR.alloc(f"rmt{i}", [2, 896], BF16) for i in range(2)]
        Pb = [AR.alloc(f"Pb{i}", [128, 1408], BF16) for i in range(2)]
        PTt = [AR.alloc(f"PTt{i}", [128, 11, 128], BF16) for i in range(2)]
        otl = [AR.alloc(f"otl{i}", [128, 256], BF16) for i in range(2)]
        amx = [AR.alloc(f"amx{i}", [128, 8], F32) for i in range(2)]
        c0 = g * 256
        C.load("pool", wq[:], w_in[:, c0:c0 + 256].rearrange("(k p) n -> p k n", p=128), bwq)
        C.load("pool", wk[:], w_in[:, 1024 + c0:1024 + c0 + 256].rearrange("(k p) n -> p k n", p=128), bwk)
        C.load("pool", wv[:], w_in[:, 2048 + c0:2048 + c0 + 256].rearrange("(k p) n -> p k n", p=128), bwv)
        for t in range(14):
            make_h(x_halo[t * 128:(t + 1) * 128, :])
            C.copy("act", hTt[:], pT[:].rearrange("p (k c) -> p k c", k=16), [bpT], [bhTt])
            pa, bpa = next_pA()
            for k in range(16):
                C.mm(pa[:, 0:256], hTt[:, k, :], wk[:, k, :], k == 0, k == 15, [bhTt, bwk], [bpa])
            C.copy("act", kbt[:], pa[:, 0:256], [bpa], [bkbt])
            pa, bpa = next_pA()
            for k in range(16):
                C.mm(pa[:, 0:256], hTt[:, k, :], wv[:, k, :], k == 0, k == 15, [bhTt, bwv], [bpa])
            C.copy("dve", Vh[:, t, :], pa[:, 0:256], [bpa], [bVh])
            for hh in range(2):
                C.tr(pT[:, hh * 128:(hh + 1) * 128], kbt[:, hh * 128:(hh + 1) * 128], idB[:], [bkbt, bidB], [bpT])
            C.copy("act", KTh[:, :, t * 128:(t + 1) * 128], pT[:, 0:256].rearrange("p (k c) -> p k c", k=2), [bpT], [bKTh])
        for t in range(4):
            C.load("sp", ccn[:, 0, :], ck_na[t * 128:(t + 1) * 128, c0:c0 + 256], bccn)
            C.load("sp", ccn[:, 1, :], cv_na[t * 128:(t + 1) * 128, c0:c0 + 256], bccn)
            C.copy("dve", ccb[:], ccn[:, 0, :], [bccn], [bccb])
            C.copy("act", cV[:, t, :], ccn[:, 1, :], [bccn], [bcV])
            for hh in range(2):
                C.tr(pT[:, hh * 128:(hh + 1) * 128], ccb[:, hh * 128:(hh + 1) * 128], idB[:], [bccb, bidB], [bpT])
            C.copy("act", cKT[:, :, t * 128:(t + 1) * 128], pT[:, 0:256].rearrange("p (k c) -> p k c", k=2), [bpT], [bcKT])
        for hh in range(2):
            for half in range(2):
                pa, bpa = next_pA()
                for k in range(16):
                    C.mm(pa[:, :], wq[:, k, hh * 128:(hh + 1) * 128], hTo[:, k, half * 512:(half + 1) * 512], k == 0, k == 15, [bwq, bhTo], [bpa])
                C.act(QTg[:, hh, half * 512:(half + 1) * 512], pa[:, :], ACTF.Copy, [bpa], [bQTg], scale=SC_NA)
        ui = 0
        for p in range(NPAIR):
            rm_t, brm = rmt[p % 2]
            C.load("pool", rm_t[:], rmx[p], brm)
            ot_t, bot = otl[p % 2]
            for hh in range(2):
                hd = g * 2 + hh
                pb, bpb = Pb[ui % 2]
                ptt, bptt = PTt[ui % 2]
                am, bam = amx[ui % 2]
                ui += 1
                q_l = QTg[:, hh, p * 128:(p + 1) * 128]
                k0 = p * 128
                segs = [(pS[0], 0, 512), (pS[1], 512, 384)]
                for (ps, bps), o, n in segs:
                    C.mm(ps[:, 0:n], q_l, KTh[:, hh, k0 + o:k0 + o + n], True, False, [bQTg, bKTh], [bps])
                    C.mm(ps[:, 0:n], jB[:], Bstat[:, hd, o:o + n], False, False, [bjB, bBstat], [bps])
                    C.mm(ps[:, 0:n], aB[:], rm_t[:, o:o + n], False, True, [baB, brm], [bps])
                ps2, bps2 = pS[2]
                C.mm(ps2[:, 0:512], q_l, cKT[:, hh, :], True, True, [bQTg, bcKT], [bps2])
                for i, ((ps, bps), n) in enumerate(((pS[0], 512), (pS[1], 384), (pS[2], 512))):
                    P.op("dve", lambda e, ps=ps, n=n, i=i, am=am: e.tensor_reduce(out=am[:, i:i + 1], in_=ps[:, 0:n], axis=AX.X, op=ALU.max),
                         reads=[bps], writes=[bam])
                P.op("dve", lambda e, am=am: e.tensor_reduce(out=am[:, 3:4], in_=am[:, 0:3], axis=AX.X, op=ALU.max, negate=True), reads=[bam], writes=[bam])
                for i, ((ps, bps), o, n) in enumerate(((pS[0], 0, 512), (pS[1], 512, 384), (pS[2], 896, 512))):
                    C.act(pb[:, o:o + n], ps[:, 0:n], ACTF.Exp, [bps, bam], [bpb, bam], bias=am[:, 3:4], scale=1.0, accum_out=am[:, 4 + i:5 + i])
                P.op("dve", lambda e, am=am: e.tensor_reduce(out=am[:, 7:8], in_=am[:, 4:7], axis=AX.X, op=ALU.add), reads=[bam], writes=[bam])
                P.op("dve", lambda e, am=am: e.reciprocal(out=am[:, 7:8], in_=am[:, 7:8]), reads=[bam], writes=[bam])
                for j in range(11):
                    C.tr(pT[:, j * 128:(j + 1) * 128], pb[:, j * 128:(j + 1) * 128], idB[:], [bpb, bidB], [bpT])
                C.copy("act", ptt[:, 0:6, :], pT[:, 0:768].rearrange("p (k c) -> p k c", k=6), [bpT], [bptt])
                C.copy("dve", ptt[:, 6:11, :], pT[:, 768:1408].rearrange("p (k c) -> p k c", k=5), [bpT], [bptt])
                for j in range(7):
                    C.mm(pO[:, 0:128], ptt[:, j, :], Vh[:, p + j, hh * 128:(hh + 1) * 128], j == 0, False, [bptt, bVh], [bpO])
                for j in range(4):
                    C.mm(pO[:, 0:128], ptt[:, 7 + j, :], cV[:, j, hh * 128:(hh + 1) * 128], False, j == 3, [bptt, bcV], [bpO])
                C.ts("dve", ot_t[:, hh * 128:(hh + 1) * 128], pO[:, 0:128], am[:, 7:8], None, ALU.mult, None, [bpO, bam], [bot])
            P.dma("sp", lambda e, ot_t=ot_t, p=p, c0=c0: e.dma_start(out=ocat_d[p * 128:(p + 1) * 128, c0:c0 + 256], in_=ot_t[:]),
                  reads=[bot], writes=[bocat_d], key=bot, store=True)
        P.barrier()
    AR.reset(m1)
    if stop <= 2:
        P.emit()
        return nc

    qanT, bqanT = AR.alloc("qanT", [128, 4, 1024], BF16)
    mQ = AR.mark()
    wqa, bwqa = AR.alloc("wqa", [128, 16, 512], BF16)
    C.load("pool", wqa[:], w_in[:, 3072:3584].rearrange("(k p) n -> p k n", p=128), bwqa)
    qaf, bqaf = AR.alloc("qaf", [128, 512], F32)
    qan, bqan = AR.alloc("qan", [128, 512], BF16)
    for t in range(8):
        pa, bpa = next_pA()
        for k in range(16):
            C.mm(pa[:, :], hTo[:, k, t * 128:(t + 1) * 128], wqa[:, k, :], k == 0, k == 15, [bhTo, bwqa], [bpa])
        C.copy("dve", qaf[:], pa[:, :], [bpa], [bqaf])
        C.act(hb[:, 0:512], qaf[:], ACTF.Square, [bqaf], [bhb, bst_ss], accum_out=st_ss[:, 1:2])
        rstd_from_ss(C, st_r[:, 1:2], st_ss[:, 1:2], 512, bst_ss, bst_r)
        C.stt(qan[:], qaf[:], st_r[:, 1:2], gqa[:], ALU.mult, ALU.mult, [bqaf, bst_r, bgqa], [bqan])
        for c in range(4):
            C.tr(pT[:, c * 128:(c + 1) * 128], qan[:, c * 128:(c + 1) * 128], idB[:], [bqan, bidB], [bpT])
        C.copy("act", qanT[:, :, t * 128:(t + 1) * 128], pT[:, 0:512].rearrange("p (k c) -> p k c", k=4), [bpT], [bqanT])
    P.barrier()
    AR.reset(mQ)
    wqb, bwqb = AR.alloc("wqb", [128, 4, 1536], BF16)
    wqbr, bwqbr = AR.alloc("wqbr", [128, 4, 512], BF16)
    wkvb, bwkvb = AR.alloc("wkvb", [128, 2, 2048], BF16)
    csT, bcsT = AR.alloc("csT", [64, 2, 1024], F32)
    C.load("pool", wqb[:], w_q_b.rearrange("(k p) n -> p k n", p=128), bwqb)
    C.load("pool", wqbr[:], w_q_b_rs.rearrange("(k p) n -> p k n", p=128), bwqbr)
    C.load("pool", wkvb[:], w_kv_b.rearrange("(k p) n -> p k n", p=128), bwkvb)
    C.load("sp", csT[:, 0, :], cosT_own, bcsT)
    C.load("sp", csT[:, 1, :], sinST_own, bcsT)
    qnh, bqnh = AR.alloc("qnh", [128, 1024], BF16)
    qrh, bqrh = AR.alloc("qrh", [128, 1024], BF16)
    knh, bknh = AR.alloc("knh", [128, 4608], BF16)
    vh, bvh = AR.alloc("vh", [128, 36, 128], BF16)
    r1, br1 = AR.alloc("r1", [64, 512], F32)
    r2, br2 = AR.alloc("r2", [64, 512], F32)
    Pm = [AR.alloc(f"Pm{i}", [128, 512], BF16) for i in range(2)]
    PTm = [AR.alloc(f"PTm{i}", [128, 4, 128], BF16) for i in range(2)]
    om = [AR.alloc(f"om{i}", [128, 128], BF16) for i in range(2)]
    mst = [AR.alloc(f"mst{i}", [128, 24], F32) for i in range(2)]
    P.op("pool", lambda e: e.memset(qrh[:], 0.0), writes=[bqrh])
    ci = 0
    for hd in range(NMLAH):
        for half in range(2):
            sl = slice(half * 512, (half + 1) * 512)
            pa, bpa = next_pA()
            for c in range(4):
                C.mm(pa[:, :], wqb[:, c, hd * 192:hd * 192 + 128], qanT[:, c, sl], c == 0, c == 3, [bwqb, bqanT], [bpa])
            C.act(qnh[:, sl], pa[:, :], ACTF.Copy, [bpa], [bqnh], scale=SC_MLA)
            pa, bpa = next_pA()
            for c in range(4):
                C.mm(pa[0:64, :], wqb[:, c, hd * 192 + 128:hd * 192 + 192], qanT[:, c, sl], c == 0, c == 3, [bwqb, bqanT], [bpa])
            C.stt(r1[:], pa[0:64, :], SC_MLA, csT[:, 0, sl], ALU.mult, ALU.mult, [bpa, bcsT], [br1])
            pa, bpa = next_pA()
            for c in range(4):
                C.mm(pa[0:64, :], wqbr[:, c, hd * 64:(hd + 1) * 64], qanT[:, c, sl], c == 0, c == 3, [bwqbr, bqanT], [bpa])
            C.stt(r2[:], pa[0:64, :], SC_MLA, csT[:, 1, sl], ALU.mult, ALU.mult, [bpa, bcsT], [br2])
            C.tt("dve", qrh[0:64, sl], r1[:], r2[:], ALU.add, [br1, br2], [bqrh])
        for ch in range(9):
            pa, bpa = next_pA()
            for c in range(2):
                C.mm(pa[:, :], wkvb[:, c, hd * 256:hd * 256 + 128], ckvT[:, c, ch * 512:(ch + 1) * 512], c == 0, c == 1, [bwkvb, bckvT], [bpa])
            C.copy("act", knh[:, ch * 512:(ch + 1) * 512], pa[:, :], [bpa], [bknh])
        for kt4 in range(9):
            pa, bpa = next_pA()
            for j in range(4):
                kt = kt4 * 4 + j
                for c in range(2):
                    C.mm(pa[:, j * 128:(j + 1) * 128], ckvT[:, c, kt * 128:(kt + 1) * 128], wkvb[:, c, hd * 256 + 128:hd * 256 + 256], c == 0, c == 1, [bckvT, bwkvb], [bpa])
            C.copy("dve", vh[:, kt4 * 4:(kt4 + 1) * 4, :], pa[:, :].rearrange("p (k c) -> p k c", k=4), [bpa], [bvh])
        for qt in range(8):
            ms, bms = mst[qt % 2]
            o_t, bo = om[qt % 2]
            qs = slice(qt * 128, (qt + 1) * 128)
            for ch in range(9):
                ps, bps = pS[ch % 3]
                C.mm(ps[:, :], qnh[:, qs], knh[:, ch * 512:(ch + 1) * 512], True, False, [bqnh, bknh], [bps])
                C.mm(ps[:, :], qrh[:, qs], krT[:, ch * 512:(ch + 1) * 512], False, True, [bqrh, bkrT], [bps])
                P.op("dve", lambda e, ps=ps, ms=ms, ch=ch: e.tensor_reduce(out=ms[:, ch:ch + 1], in_=ps[:, :], axis=AX.X, op=ALU.max), reads=[bps], writes=[bms])
            P.op("dve", lambda e, ms=ms: e.tensor_reduce(out=ms[:, 9:10], in_=ms[:, 0:9], axis=AX.X, op=ALU.max, negate=True), reads=[bms], writes=[bms])
            for ch in range(9):
                ps, bps = pS[ch % 3]
                pm, bpm = Pm[ci % 2]
                ptm, bptm = PTm[ci % 2]
                ci += 1
                C.mm(ps[:, :], qnh[:, qs], knh[:, ch * 512:(ch + 1) * 512], True, False, [bqnh, bknh], [bps])
                C.mm(ps[:, :], qrh[:, qs], krT[:, ch * 512:(ch + 1) * 512], False, True, [bqrh, bkrT], [bps])
                C.act(pm[:], ps[:, :], ACTF.Exp, [bps, bms], [bpm, bms], bias=ms[:, 9:10], scale=1.0, accum_out=ms[:, 10 + ch:11 + ch])
                for j in range(4):
                    C.tr(pT[:, j * 128:(j + 1) * 128], pm[:, j * 128:(j + 1) * 128], idB[:], [bpm, bidB], [bpT])
                C.copy("act" if ch % 2 else "dve", ptm[:], pT[:, 0:512].rearrange("p (k c) -> p k c", k=4), [bpT], [bptm])
                for j in range(4):
                    kt = ch * 4 + j
                    C.mm(pO[:, 0:128], ptm[:, j, :], vh[:, kt, :], kt == 0, kt == 35, [bptm, bvh], [bpO])
            P.op("dve", lambda e, ms=ms: e.tensor_reduce(out=ms[:, 20:21], in_=ms[:, 10:19], axis=AX.X, op=ALU.add), reads=[bms], writes=[bms])
            P.op("dve", lambda e, ms=ms: e.reciprocal(out=ms[:, 20:21], in_=ms[:, 20:21]), reads=[bms], writes=[bms])
            C.ts("dve", o_t[:], pO[:, 0:128], ms[:, 20:21], None, ALU.mult, None, [bpO, bms], [bo])
            P.dma("sp", lambda e, o_t=o_t, qt=qt, hd=hd: e.dma_start(out=ocat_d[qt * 128:(qt + 1) * 128, 1024 + hd * 128:1024 + (hd + 1) * 128], in_=o_t[:]),
                  reads=[bo], writes=[bocat_d], key=bo, store=True)
    P.barrier()
    AR.reset(m1)
    if stop <= 3:
        P.emit()
        return nc

    GA, bGA, G2, bG2, SF, bSF = T0, bT0, T1, bT1, T2, bT2
    bload(GA[:], mrow[2:3, :], bGA)
    bload(SF[:], mrow[3:4, :], bSF)
    bload(G2[:], mrow[4:5, :], bG2)
    bload(tmpf[:], g_ffn[0:1, :], btmpf)
    C.stt(G2[:], G2[:], 1.0, tmpf[:], ALU.add, ALU.mult, [bG2, btmpf], [bG2])
    xt2 = [AR.alloc(f"xt2_{i}", [128, D], F32) for i in range(2)]
    oct_ = [AR.alloc(f"oct{i}", [128, D], BF16) for i in range(2)]
    ocT, bocT = AR.alloc("ocT", [128, 16, 256], BF16)
    ws = [AR.alloc(f"ws{i}", [128, 16, 256], BF16) for i in range(2)]
    stg = [AR.alloc(f"stg{i}", [128, 256], F32) for i in range(2)]
    h2f, bh2f = AR.alloc("h2f", [128, D], F32)
    h2b, bh2b = AR.alloc("h2b", [128, D], BF16)
    h2T, bh2T = AR.alloc("h2T", [128, 16, 128], F32)
    lg, blg = AR.alloc("lg", [128, 32], F32)
    lg2, blg2 = AR.alloc("lg2", [128, 32], F32)
    top8, btop8 = AR.alloc("top8", [128, 8], F32)
    wsi = [0]
    sgi = [0]
    for b in range(4):
        r0 = b * 256
        for t in range(2):
            x_t, bx = xt2[t]
            oc_t, boc = oct_[t]
            C.load("sp", x_t[:], x_own[r0 + t * 128:r0 + (t + 1) * 128, :], bx)
            P.dma("sp", lambda e, oc_t=oc_t, r0=r0, t=t: e.dma_start(out=oc_t[:], in_=ocat_d[r0 + t * 128:r0 + (t + 1) * 128, :]),
                  reads=[bocat_d], writes=[boc])
            for k in range(16):
                C.tr(pT[:, k * 128:(k + 1) * 128], oc_t[:, k * 128:(k + 1) * 128], idB[:], [boc, bidB], [bpT])
            C.copy("act", ocT[:, :, t * 128:(t + 1) * 128], pT[:].rearrange("p (k c) -> p k c", k=16), [bpT], [bocT])
        for s in range(8):
            wt, bw = ws[wsi[0] % 2]
            wsi[0] += 1
            C.load("pool", wt[:], w_out[:, s * 256:(s + 1) * 256].rearrange("(k p) n -> p k n", p=128), bw)
            for t in range(2):
                x_t, bx = xt2[t]
                pa, bpa = next_pA()
                for k in range(16):
                    C.mm(pa[:, 0:256], ocT[:, k, t * 128:(t + 1) * 128], wt[:, k, :], k == 0, k == 15, [bw, bocT], [bpa])
                sg, bsg = stg[sgi[0] % 2]
                sgi[0] += 1
                C.tt("dve", sg[:], pa[:, 0:256], GA[:, s * 256:(s + 1) * 256], ALU.mult, [bpa, bGA], [bsg])
                C.tt("dve", x_t[:, s * 256:(s + 1) * 256], x_t[:, s * 256:(s + 1) * 256], sg[:], ALU.add, [bx, bsg], [bx])
        for t in range(2):
            x_t, bx = xt2[t]
            C.store("sp", o_x1[r0 + t * 128:r0 + (t + 1) * 128, :], x_t[:], bx)
            C.act(hb[:], x_t[:], ACTF.Square, [bx], [bhb, bst_ss], accum_out=st_ss[:, 3:4])
            rstd_from_ss(C, st_r[:, 3:4], st_ss[:, 3:4], D, bst_ss, bst_r)
            C.stt(tmpf[:], x_t[:], st_r[:, 3:4], G2[:], ALU.mult, ALU.mult, [bx, bst_r, bG2], [btmpf])
            C.tt("dve", h2f[:], tmpf[:], SF[:], ALU.add, [btmpf, bSF], [bh2f])
            C.copy("act", h2b[:], h2f[:], [bh2f], [bh2b])
            C.store("sp", o_h2[r0 + t * 128:r0 + (t + 1) * 128, :], h2b[:], bh2b)
            for g4 in range(4):
                pa, bpa = next_pA()
                for j in range(4):
                    k = g4 * 4 + j
                    C.tr(pa[:, j * 128:(j + 1) * 128], h2f[:, k * 128:(k + 1) * 128], idF[:], [bh2f, bidF], [bpa])
                C.copy("dve", h2T[:, g4 * 4:(g4 + 1) * 4, :], pa[:, 0:512].rearrange("p (k c) -> p k c", k=4), [bpa], [bh2T])
            pa, bpa = next_pA()
            for k in range(16):
                C.mm(pa[:, 0:32], h2T[:, k, :], wr[:, k, :], k == 0, k == 15, [bh2T, bwr], [bpa])
            C.tt("dve", lg[:], pa[:, 0:32], brt[:], ALU.add, [bpa, bbrt], [blg])
            P.op("dve", lambda e: e.max(out=top8[:], in_=lg[:]), reads=[blg], writes=[btop8])
            C.ts("dve", lg2[:], lg[:], top8[:, 3:4], None, ALU.is_ge, None, [blg, btop8], [blg2])
            C.ts("dve", top8[:, 7:8], top8[:, 0:1], -1.0, None, ALU.mult, None, [btop8], [btop8])
            C.act(lg[:], lg[:], ACTF.Exp, [blg, btop8], [blg], bias=top8[:, 7:8], scale=1.0)
            C.tt("dve", lg[:], lg[:], lg2[:], ALU.mult, [blg, blg2], [blg])
            P.op("dve", lambda e: e.tensor_reduce(out=top8[:, 6:7], in_=lg[:], axis=AX.X, op=ALU.add), reads=[blg], writes=[btop8])
            P.op("dve", lambda e: e.reciprocal(out=top8[:, 6:7], in_=top8[:, 6:7]), reads=[btop8], writes=[btop8])
            C.ts("dve", lg2[:], lg[:], top8[:, 6:7], None, ALU.mult, None, [blg, btop8], [blg2])
            C.store("sp", o_G[r0 + t * 128:r0 + (t + 1) * 128, :], lg2[:], blg2)
    P.emit()
    return nc


def rope_tables():
    T = 4096
    pos = np.arange(T); rows = (pos // 64).astype(np.float32); cols = (pos % 64).astype(np.float32)
    inv = (10000.0 ** (-(np.arange(16, dtype=np.float32) * 2.0 / 32))).astype(np.float32)
    ar = rows[:, None] * inv; ac = cols[:, None] * inv
    ang = np.concatenate([ar, ar, ac, ac], -1)
    cos, sin = np.cos(ang).astype(np.float32), np.sin(ang).astype(np.float32)
    sign = np.concatenate([-np.ones(16), np.ones(16), -np.ones(16), np.ones(16)]).astype(np.float32)
    return cos, sin * sign

SWAP = np.concatenate([np.arange(16, 32), np.arange(0, 16), np.arange(48, 64), np.arange(32, 48)])

def sample_consts(r0):
    ident = np.eye(128, dtype=np.float32)
    jmat = np.zeros((128, 128), np.float32)
    for p in range(128):
        jmat[p, (p // 64) * 64 + 63 - p % 64] = 1.0
    amat = np.zeros((2, 128), np.float32)
    amat[0, :64] = 1.0; amat[1, 64:] = 1.0
    rmx = np.zeros((8, 2, 14, 64), np.float32)
    for p in range(8):
        for rho in range(2):
            r = r0 + 2 * p + rho
            rs = min(max(r - 4, 0), 56)
            for j in range(14):
                kr = r0 + 2 * p - 6 + j
                ok = (0 <= kr < 64) and (rs <= kr < rs + 8)
                if not ok:
                    rmx[p, rho, j, :] = NEG
    colmask = np.zeros((128, 15, 64), np.float32)
    for pp in range(128):
        c = 63 - pp % 64
        cs = min(max(c - 8, 0), 48)
        m = np.full(64, NEG, np.float32); m[cs:cs + 16] = 0.0
        colmask[pp, :, :] = m
    return dict(ident=ident, jmat=jmat, amat=amat, rmx=rmx.reshape(8, 2, 896), colmask=colmask)

def sample_inputs(z, mrow_b, b, r0):
    cos, sinS = rope_tables()
    xs = z['x_sample'][b]
    halo = np.zeros((28, 64, 2048), np.float32)
    for i in range(28):
        r = r0 - 6 + i
        if 0 <= r < 64:
            halo[i] = xs[r * 64:(r + 1) * 64]
    rpbpad = np.zeros((8, 15, 160), np.float32)
    rpbpad[:, :, 64:95] = z['na_rpb'][0]
    wqb = z['w_q_b'][0]
    wqb_rs = np.concatenate([wqb[:, h * 192 + 128:h * 192 + 192][:, SWAP] for h in range(8)], 1)
    own = slice(r0 * 64, r0 * 64 + 1024)
    d = dict(x_own=np.ascontiguousarray(xs[own]), x_halo=halo.reshape(1792, 2048), x_all=xs,
             ck_na=z['cache_na_k'][b, 0].reshape(512, 1024), cv_na=z['cache_na_v'][b, 0].reshape(512, 1024),
             c_ckv=z['cache_mla_ckv'][b, 0], c_krope=z['cache_mla_krope'][b, 0],
             mrow=np.ascontiguousarray(mrow_b.reshape(6, 2048)),
             g_attn=z['g_attn'], g_ffn=z['g_ffn'], g_q_a=z['g_q_a'], g_kv_a=z['g_kv_a'],
             w_in=z['w_in'][0], w_in_rs=np.ascontiguousarray(z['w_in'][0][:, 3840:3904][:, SWAP]),
             w_q_b=wqb, w_q_b_rs=np.ascontiguousarray(wqb_rs), w_kv_b=z['w_kv_b'][0], w_out=z['w_out'][0],
             w_router=z['w_router'][0], b_router=z['b_router'], rpbpad=rpbpad,
             cos_tok=cos, sinS_tok=sinS, cosT_own=np.ascontiguousarray(cos[own].T), sinST_own=np.ascontiguousarray(sinS[own].T))
    d.update(sample_consts(r0))
    return d


from concourse.bass_utils import run_bass_kernel_spmd
import ml_dtypes as _mld

_BF = _mld.bfloat16
NCORES = 8


def _run(build, in_maps):
    nc = bass.Bass("TRN2", target_bir_lowering=False)
    build(nc)
    res = run_bass_kernel_spmd(nc, in_maps, core_ids=list(range(NCORES)))
    return res.results


def kernel(x_prompt, x_sample, cache_na_k, cache_na_v, cache_mla_ckv, cache_mla_krope, c, c_ctx,
           g_attn, g_ffn, g_final, w_mod, b_mod, w_in, w_out, na_rpb, g_q_a, w_q_b, g_kv_a, w_kv_b,
           w_router, b_router, w_gate_up, b_gate_up, w_down, b_down):
    f32 = np.float32
    z = dict(x_sample=np.asarray(x_sample, f32), cache_na_k=np.asarray(cache_na_k, f32), cache_na_v=np.asarray(cache_na_v, f32),
             cache_mla_ckv=np.asarray(cache_mla_ckv, f32), cache_mla_krope=np.asarray(cache_mla_krope, f32),
             g_attn=np.asarray(g_attn, f32), g_ffn=np.asarray(g_ffn, f32), g_q_a=np.asarray(g_q_a, f32), g_kv_a=np.asarray(g_kv_a, f32),
             w_in=np.asarray(w_in, f32), w_q_b=np.asarray(w_q_b, f32), w_kv_b=np.asarray(w_kv_b, f32), w_out=np.asarray(w_out, f32),
             w_router=np.asarray(w_router, f32), b_router=np.asarray(b_router, f32), na_rpb=np.asarray(na_rpb, f32))
    x_prompt = np.asarray(x_prompt, f32)
    ident = np.eye(128, dtype=f32)

    c3 = np.concatenate([np.asarray(c_ctx, f32)[None], np.asarray(c, f32)], 0)
    cT = np.ascontiguousarray(c3.T.reshape(16, 128, 3).transpose(1, 0, 2))
    wm = np.asarray(w_mod, f32)[0]
    bm = np.asarray(b_mod, f32)
    maps = [dict(cT=cT, wm=np.ascontiguousarray(wm[:, i * 1536:(i + 1) * 1536]), bm=np.ascontiguousarray(bm[:, i * 1536:(i + 1) * 1536]))
            for i in range(NCORES)]
    rA = _run(build_mod, maps)
    m3 = np.concatenate([np.asarray(r["o_m"], f32) for r in rA], 1)

    common = dict(g_attn=z['g_attn'], g_ffn=z['g_ffn'], g_q_a=z['g_q_a'], g_kv_a=z['g_kv_a'], w_in=z['w_in'][0], w_q_b=z['w_q_b'][0],
                  w_kv_b=z['w_kv_b'][0], w_out=z['w_out'][0], w_router=z['w_router'][0], b_router=z['b_router'], ident=ident)
    maps = []
    for i in range(NCORES):
        d = dict(common)
        d.update(xp=np.ascontiguousarray(x_prompt[4 * i:4 * i + 4].reshape(1024, 2048)), mrow=np.ascontiguousarray(m3[0].reshape(6, 2048)))
        maps.append(d)
    rP = _run(build_prompt, maps)
    new_na_k = np.concatenate([np.asarray(r["o_nak"], f32) for r in rP], 0).reshape(32, 1, 256, 8, 128)
    new_na_v = np.concatenate([np.asarray(r["o_nav"], f32) for r in rP], 0).reshape(32, 1, 256, 8, 128)
    new_ckv = np.concatenate([np.asarray(r["o_ckv"], f32) for r in rP], 0).reshape(32, 1, 256, 256)
    new_krope = np.concatenate([np.asarray(r["o_krope"], f32) for r in rP], 0).reshape(32, 1, 256, 64)

    maps = []
    for i in range(NCORES):
        b, r0 = i // 4, 16 * (i % 4)
        maps.append(sample_inputs(z, m3[1 + b], b, r0))
    rS = _run(build_sample, maps)

    h2_all = np.concatenate([np.asarray(r["o_h2"]) for r in rP] + [np.asarray(r["o_h2"]) for r in rS], 0)
    G_all = np.concatenate([np.asarray(r["o_G"], f32) for r in rP] + [np.asarray(r["o_G"], f32) for r in rS], 0)
    h2T = np.ascontiguousarray(h2_all.T)
    wgu = np.asarray(w_gate_up, f32)[0]
    bgu = np.asarray(b_gate_up, f32)[0]
    wdn = np.asarray(w_down, f32)[0]
    bdn = np.asarray(b_down, f32)[0]
    maps = []
    for i in range(NCORES):
        es = slice(4 * i, 4 * i + 4)
        maps.append(dict(h2T=h2T, Gl=np.ascontiguousarray(G_all[:, es]), GlT=np.ascontiguousarray(G_all[:, es].T),
                         w_gu=wgu[es], b_gu=np.ascontiguousarray(bgu[es].reshape(4, 16, 128, 2).transpose(2, 0, 1, 3)),
                         w_dn=wdn[es], b_dn=bdn[es]))
    rC = _run(build_experts, maps)

    x1P = [np.asarray(r["o_x1"], f32) for r in rP]
    x1S = [np.asarray(r["o_x1"], f32) for r in rS]
    yparts = [np.asarray(r["o_y"]) for r in rC]
    maps = []
    for i in range(NCORES):
        b = i // 4
        rows_p = slice(1024 * i, 1024 * (i + 1))
        rows_s = slice(8192 + 1024 * i, 8192 + 1024 * (i + 1))
        yp = np.stack([np.concatenate([yj[rows_p], yj[rows_s]], 0) for yj in yparts], 0)
        gf = np.stack([m3[0, 5 * 2048:6 * 2048], m3[1 + b, 5 * 2048:6 * 2048]], 0)
        maps.append(dict(x1=np.concatenate([x1P[i], x1S[i]], 0), yp=np.ascontiguousarray(yp), gf=np.ascontiguousarray(gf),
                         g_final=np.asarray(g_final, f32)[None]))
    rD = _run(build_combine, maps)
    y_prompt = np.concatenate([np.asarray(r["o_y"], f32)[:1024] for r in rD], 0).reshape(32, 256, 2048)
    y_sample = np.concatenate([np.asarray(r["o_y"], f32)[1024:] for r in rD], 0).reshape(2, 4096, 2048)
    return (y_prompt, y_sample, new_na_k, new_na_v, new_ckv, new_krope)
```

```python
import numpy as np
import concourse.bass as bass
import concourse.mybir as mybir
from contextlib import ExitStack

F32 = mybir.dt.float32
BF16 = mybir.dt.bfloat16
I32 = mybir.dt.int32
U32 = mybir.dt.uint32
ALU = mybir.AluOpType
ACTF = mybir.ActivationFunctionType
AX = mybir.AxisListType

ENGS = ("pe", "act", "dve", "pool", "sp")
SEM_ROTATE = 12000


class Buf:
    __slots__ = ("name", "last_w", "readers", "ld", "st", "excl")

    def __init__(self, name, excl=False):
        self.name = name
        self.excl = excl
        self.last_w = None
        self.readers = []
        self.ld = None
        self.st = None


class DmaSem:
    __slots__ = ("sem", "count", "kind")

    def __init__(self, sem, kind):
        self.sem = sem
        self.count = 0
        self.kind = kind


class Op:
    __slots__ = ("eng", "fn", "waits", "is_dma", "dsem", "dval", "signaled", "sig", "idx", "dma_waits", "inc")

    def __init__(self, eng, fn, is_dma):
        self.eng = eng
        self.fn = fn
        self.is_dma = is_dma
        self.waits = {}
        self.dma_waits = {}
        self.dsem = None
        self.dval = 0
        self.signaled = False
        self.sig = None
        self.idx = 0
        self.inc = 16


class Prog:
    def __init__(self, nc):
        self.nc = nc
        self.ops = {e: [] for e in ENGS}
        self.all_ops = []
        self.stack = ExitStack()
        self.sem_pool = []
        self.n_sems = 0
        self.dma_sems = []
        self.free_dsems = []
        self._n = 0

    def new_sem(self):
        self.n_sems += 1
        return self.stack.enter_context(self.nc.semaphore(f"s{self.n_sems}"))

    def sbuf(self, name, shape, dtype):
        t = self.stack.enter_context(self.nc.sbuf_tensor(name, list(shape), dtype))
        return t

    def psum(self, name, shape, dtype):
        t = self.stack.enter_context(self.nc.psum_tensor(name, list(shape), dtype))
        return t

    def _dep(self, op, prod):
        if prod is None or prod is op:
            return
        if prod.is_dma:
            ds = prod.dsem
            op.dma_waits[id(ds)] = (ds, ds.count)
        else:
            if prod.eng == op.eng and not op.is_dma and op.eng == "pe":
                return
            cur = op.waits.get(prod.eng)
            if cur is None or cur.idx < prod.idx:
                op.waits[prod.eng] = prod
            prod.signaled = True

    def _record(self, op, reads, writes):
        self._n += 1
        op.idx = self._n
        ex = [b for b in reads if b.excl]
        if ex:
            reads = [b for b in reads if not b.excl]
            writes = list(writes) + [b for b in ex if b not in writes]
        for b in reads:
            self._dep(op, b.last_w)
        for b in writes:
            self._dep(op, b.last_w)
            for r in b.readers:
                self._dep(op, r)
        for b in reads:
            b.readers.append(op)
        for b in writes:
            b.last_w = op
            b.readers = []
        self.ops[op.eng].append(op)
        self.all_ops.append(op)
        return op

    def op(self, eng, fn, reads=(), writes=()):
        return self._record(Op(eng, fn, False), reads, writes)

    def dma(self, eng, fn, reads=(), writes=(), key=None, store=False, inc=16):
        op = Op(eng, fn, True)
        if key is None:
            key = writes[0] if not store else reads[0]
        attr = "st" if store else "ld"
        ds = getattr(key, attr)
        if ds is None:
            kind = "sw" if eng == "pool" else "hw"
            fl = [d for d in self.free_dsems if d.kind == kind]
            if fl:
                ds = fl[-1]
                self.free_dsems.remove(ds)
            else:
                ds = DmaSem(self.new_sem(), kind)
                self.dma_sems.append(ds)
            setattr(key, attr, ds)
        op.dsem = ds
        op.inc = inc
        self._record(op, reads, writes)
        ds.count += inc
        op.dval = ds.count
        return op

    def recycle_dma_sems(self):
        self.free_dsems = list(self.dma_sems)

    def coll(self, fn, reads=(), writes=()):
        if not hasattr(self, "_csem"):
            self._csem = self.new_sem()
            self._ccnt = 0
            self._cscr = self.sbuf("coll_scr", [128, 8], F32)
        self._ccnt += 1
        n = self._ccnt
        csem = self._csem
        scr = self._cscr

        def run(eng, fn=fn, n=n):
            fn(eng).then_inc(csem)
            eng.wait_ge(csem, n)
            return eng.memset(scr[:], 0.0)
        return self.op("pool", run, reads=reads, writes=writes)

    def barrier(self, bufs=()):
        lastc = {}
        for e in ENGS:
            lastc[e] = None
            for q in reversed(self.ops[e]):
                if not q.is_dma and q.fn is not None:
                    lastc[e] = q
                    break
        tot = [(ds, ds.count) for ds in self.dma_sems if ds.count > 0]
        for e in ENGS:
            op = Op(e, None, False)
            self._n += 1
            op.idx = self._n
            for e2 in ENGS:
                q = lastc[e2]
                if q is not None and (e2 != e or e != "pe"):
                    op.waits[e2] = q
                    q.signaled = True
            for ds, c in tot:
                op.dma_waits[id(ds)] = (ds, c)
            self.ops[e].append(op)
            self.all_ops.append(op)

    def emit(self):
        nc = self.nc
        for e in ENGS:
            sem = None
            cnt = 0
            for op in self.ops[e]:
                if op.is_dma or not op.signaled or op.fn is None:
                    continue
                if sem is None or cnt >= SEM_ROTATE:
                    sem = self.new_sem()
                    cnt = 0
                cnt += 1
                op.sig = (sem, cnt)
        prog = self

        def run(engname, eng):
            for op in prog.ops[engname]:
                for p in op.waits.values():
                    s, v = p.sig
                    eng.wait_ge(s, v)
                for ds, v in op.dma_waits.values():
                    eng.wait_ge(ds.sem, v)
                if op.fn is None:
                    continue
                ins = op.fn(eng)
                if op.is_dma:
                    ins.then_inc(op.dsem.sem, op.inc)
                elif op.signaled:
                    ins.then_inc(op.sig[0], 1)

        with nc.Block() as block:
            @block.tensor
            def _(eng):
                run("pe", eng)

            @block.scalar
            def _(eng):
                run("act", eng)

            @block.vector
            def _(eng):
                run("dve", eng)

            @block.gpsimd
            def _(eng):
                run("pool", eng)

            @block.sync
            def _(eng):
                run("sp", eng)
                for ds in prog.dma_sems:
                    if ds.count > 0:
                        eng.wait_ge(ds.sem, ds.count)
        self.stack.close()


D = 2048
EPS = 1e-6
NEG = -30000.0


class Ctx:
    def __init__(self, nc, fused=False):
        self.nc = nc
        self.P = Prog(nc)
        self._cnt = 0
        self.fused = fused
        self.dram = {}
        self.arena = None
        self.ps_pool = None
        self.ps_off = 0

    def T(self, name, shape, dt):
        if self.fused:
            return self.arena.alloc(name, shape, dt)
        t = self.P.sbuf(name, shape, dt)
        b = Buf(name)
        return t, b

    def PS(self, name, shape, dt):
        if self.fused:
            n = 1
            for d in shape[1:]:
                n *= d
            nbytes = n * (4 if dt == F32 else 2)
            nb = (nbytes + 2047) // 2048
            assert self.ps_off + nb <= 8, ("psum", name)
            ap = self.ps_pool[:, self.ps_off * 512:(self.ps_off + nb) * 512]
            self.ps_off += nb
            if dt != F32:
                ap = ap.bitcast(dt)
            ap = ap[:, 0:n]
            if len(shape) == 3:
                ap = ap.rearrange("p (a b) -> p a b", a=shape[1])
            return ap, Buf(name, excl=True)
        t = self.P.psum(name, shape, dt)
        b = Buf(name, excl=True)
        return t, b

    def din(self, name, shape, dt=F32):
        if name in self.dram:
            return self.dram[name]
        ap = self.nc.dram_tensor(name, list(shape), dt, kind="ExternalInput").ap()
        if self.fused:
            self.dram[name] = ap
        return ap

    def dout(self, name, shape, dt=F32):
        if name in self.dram:
            return self.dram[name]
        return self.nc.dram_tensor(name, list(shape), dt, kind="ExternalOutput").ap()

    def scratch(self, name, shape, dt):
        return self.nc.dram_tensor(name, list(shape), dt).ap()

    def finish(self):
        if not self.fused:
            self.P.emit()

    def load(self, q, out_ap, in_ap, wbuf, reads=()):
        return self.P.dma(q, lambda e: e.dma_start(out=out_ap, in_=in_ap), reads=list(reads), writes=[wbuf])

    def store(self, q, out_ap, in_ap, rbuf, writes=()):
        return self.P.dma(q, lambda e: e.dma_start(out=out_ap, in_=in_ap), reads=[rbuf], writes=list(writes), key=rbuf, store=True)

    def mm(self, out, lhsT, rhs, start, stop, reads, writes):
        return self.P.op("pe", lambda e: e.matmul(out, lhsT=lhsT, rhs=rhs, start=start, stop=stop), reads=reads, writes=writes)

    def tr(self, out, in_, ident, reads, writes):
        return self.P.op("pe", lambda e: e.transpose(out=out, in_=in_, identity=ident), reads=reads, writes=writes)

    def act(self, out, in_, func, reads, writes, bias=None, scale=None, accum_out=None):
        kw = {}
        if bias is not None:
            kw["bias"] = bias
        if scale is not None:
            kw["scale"] = scale
        if accum_out is not None:
            kw["accum_out"] = accum_out
        return self.P.op("act", lambda e: e.activation(out=out, in_=in_, func=func, **kw), reads=reads, writes=writes)

    def ts(self, eng, out, in0, s1, s2, op0, op1, reads, writes):
        if op1 is None:
            return self.P.op(eng, lambda e: e.tensor_scalar(out=out, in0=in0, scalar1=s1, scalar2=None, op0=op0), reads=reads, writes=writes)
        return self.P.op(eng, lambda e: e.tensor_scalar(out=out, in0=in0, scalar1=s1, scalar2=s2, op0=op0, op1=op1), reads=reads, writes=writes)

    def tt(self, eng, out, in0, in1, op, reads, writes):
        return self.P.op(eng, lambda e: e.tensor_tensor(out=out, in0=in0, in1=in1, op=op), reads=reads, writes=writes)

    def stt(self, out, in0, scalar, in1, op0, op1, reads, writes):
        return self.P.op("dve", lambda e: e.scalar_tensor_tensor(out=out, in0=in0, scalar=scalar, in1=in1, op0=op0, op1=op1), reads=reads, writes=writes)

    def copy(self, eng, out, in_, reads, writes):
        if eng == "act":
            return self.P.op("act", lambda e: e.copy(out=out, in_=in_), reads=reads, writes=writes)
        return self.P.op(eng, lambda e: e.tensor_copy(out=out, in_=in_), reads=reads, writes=writes)


def rstd_from_ss(C, rstd, ss, n, bss, brstd):
    C.ts("dve", rstd, ss, 1.0 / n, EPS, ALU.mult, ALU.add, [bss], [brstd])
    C.act(rstd, rstd, ACTF.Sqrt, [brstd], [brstd])
    C.P.op("dve", lambda e: e.reciprocal(out=rstd, in_=rstd), reads=[brstd], writes=[brstd])


def build_prompt(nc, NB=4, do_tail=True, stop=99, sub=99, C=None):
    C = C or Ctx(nc)
    P = C.P
    NT = NB * 256
    xp = C.din("xp", [NT, D])
    mrow = C.din("mrow", [6, D])
    g_attn = C.din("g_attn", [1, D])
    g_ffn = C.din("g_ffn", [1, D])
    g_q_a = C.din("g_q_a", [1, 512])
    g_kv_a = C.din("g_kv_a", [1, 256])
    w_in = C.din("w_in", [D, 3904])
    w_q_b = C.din("w_q_b", [512, 1536])
    w_kv_b = C.din("w_kv_b", [256, 2048])
    w_out = C.din("w_out", [D, D])
    w_router = C.din("w_router", [D, 32])
    b_router = C.din("b_router", [1, 32])
    ident = C.din("ident", [128, 128])
    o_nak = C.dout("o_nak", [NT, 1024])
    o_nav = C.dout("o_nav", [NT, 1024])
    o_ckv = C.dout("o_ckv", [NT, 256])
    o_krope = C.dout("o_krope", [NT, 64])
    o_x1 = C.dout("o_x1", [NT, D])
    o_h2 = C.dout("o_h2", [NT, D], BF16)
    o_G = C.dout("o_G", [NT, 32])

    idF, bidF = C.T("idF", [128, 128], F32)
    idB, bidB = C.T("idB", [128, 128], BF16)
    G1, bG1 = C.T("G1", [128, D], F32)
    SA, bSA = C.T("SA", [128, D], F32)
    GA, bGA = C.T("GA", [128, D], F32)
    G2, bG2 = C.T("G2", [128, D], F32)
    SF, bSF = C.T("SF", [128, D], F32)
    gqa, bgqa = C.T("gqa", [128, 512], F32)
    gkva, bgkva = C.T("gkva", [128, 256], F32)
    brt, bbrt = C.T("brt", [128, 32], F32)
    wr, bwr = C.T("wr", [128, 16, 32], F32)
    wqb, bwqb = C.T("wqb", [128, 4, 1536], BF16)
    wkvb, bwkvb = C.T("wkvb", [128, 2, 2048], BF16)
    xt = [C.T(f"xt{t}", [128, D], F32) for t in range(2)]
    tmpf, btmpf = C.T("tmpf", [128, D], F32)
    hb, bhb = C.T("hb", [128, D], BF16)
    hT, bhT = C.T("hT", [128, 16, 256], BF16)
    NWS = 2
    ws = [C.T(f"ws{i}", [128, 16, 256], BF16) for i in range(NWS)]
    QT, bQT = C.T("QT", [128, 8, 256], BF16)
    Kb, bKb = C.T("Kb", [128, 2, 1024], BF16)
    Vb, bVb = C.T("Vb", [128, 2, 1024], BF16)
    KT, bKT = C.T("KT", [128, 8, 256], BF16)
    qaf, bqaf = C.T("qaf", [128, 2, 512], F32)
    qan, bqan = C.T("qan", [128, 2, 512], BF16)
    qanT, bqanT = C.T("qanT", [128, 4, 256], BF16)
    kvaf, bkvaf = C.T("kvaf", [128, 2, 256], F32)
    ckvf, bckvf = C.T("ckvf", [128, 2, 256], F32)
    ckvb, bckvb = C.T("ckvb", [128, 2, 256], BF16)
    ckvT, bckvT = C.T("ckvT", [128, 2, 256], BF16)
    krf, bkrf = C.T("krf", [128, 2, 64], F32)
    krb, bkrb = C.T("krb", [128, 2, 64], BF16)
    krT, bkrT = C.T("krT", [128, 256], BF16)
    qnT, bqnT = C.T("qnT", [128, 8, 256], BF16)
    qrT, bqrT = C.T("qrT", [128, 8, 256], BF16)
    knT, bknT = C.T("knT", [128, 8, 256], BF16)
    vm, bvm = C.T("vm", [128, 2, 1024], BF16)
    ocat, bocat = C.T("ocat", [128, 2, D], BF16)
    ocT, bocT = C.T("ocT", [128, 16, 256], BF16)
    stg = [C.T(f"stg{i}", [128, 256], F32) for i in range(2)]
    Pb = [C.T(f"Pb{i}", [128, 256], BF16) for i in range(2)]
    PT = [C.T(f"PT{i}", [128, 2, 128], BF16) for i in range(2)]
    st_ss, bst_ss = C.T("st_ss", [128, 8], F32)
    st_r, bst_r = C.T("st_r", [128, 8], F32)
    amx = [C.T(f"amx{i}", [128, 4], F32) for i in range(2)]
    h2f, bh2f = C.T("h2f", [128, D], F32)
    h2b, bh2b = C.T("h2b", [128, D], BF16)
    h2T, bh2T = C.T("h2T", [128, 16, 128], F32)
    lg, blg = C.T("lg", [128, 32], F32)
    lg2, blg2 = C.T("lg2", [128, 32], F32)
    top8, btop8 = C.T("top8", [128, 8], F32)

    pT, bpT = C.PS("pT", [128, 2048], BF16)
    pA = [C.PS(f"pA{i}", [128, 512], F32) for i in range(2)]
    pS = [C.PS(f"pS{i}", [128, 512], F32) for i in range(2)]
    pTp_full, bpTp = C.PS("pTp", [128, 8, 128], BF16)
    pTp = pTp_full[:, 0:2, :]
    pO, bpO = C.PS("pO", [128, 512], F32)

    C.load("sp", idF[:], ident, bidF)
    C.copy("dve", idB[:], idF[:], [bidF], [bidB])

    def bload(dst, row_ap, buf):
        C.load("sp", dst[:], row_ap.partition_broadcast(128), buf)

    bload(SA, mrow[0:1, :], bSA)
    bload(G1, mrow[1:2, :], bG1)
    bload(tmpf, g_attn[0:1, :], btmpf)
    C.stt(G1[:], G1[:], 1.0, tmpf[:], ALU.add, ALU.mult, [bG1, btmpf], [bG1])
    bload(GA, mrow[2:3, :], bGA)
    bload(SF, mrow[3:4, :], bSF)
    bload(G2, mrow[4:5, :], bG2)
    bload(h2f, g_ffn[0:1, :], bh2f)
    C.stt(G2[:], G2[:], 1.0, h2f[:], ALU.add, ALU.mult, [bG2, bh2f], [bG2])
    bload(gqa, g_q_a[0:1, :], bgqa)
    bload(gkva, g_kv_a[0:1, :], bgkva)
    bload(brt, b_router[0:1, :], bbrt)
    C.load("sp", wr[:], w_router.rearrange("(k p) n -> p k n", p=128), bwr)
    C.load("pool", wqb[:], w_q_b.rearrange("(k p) n -> p k n", p=128), bwqb)
    C.load("pool", wkvb[:], w_kv_b.rearrange("(k p) n -> p k n", p=128), bwkvb)

    P.op("pool", lambda e: e.memset(qrT[:], 0.0), writes=[bqrT])
    P.op("pool", lambda e: e.memset(krT[:], 0.0), writes=[bkrT])
    if stop <= 0:
        C.finish()
        return nc
    ws_i = [0]

    def next_ws(src_cols_ap, ncols):
        i = ws_i[0] % NWS
        ws_i[0] += 1
        t, b = ws[i]
        C.load("pool", t[:, :, 0:ncols], src_cols_ap.rearrange("(k p) n -> p k n", p=128), b)
        return t, b

    pa_i = [0]

    def next_pA():
        i = pa_i[0] % 2
        pa_i[0] += 1
        return pA[i]

    stg_i = [0]

    def next_stg():
        i = stg_i[0] % 2
        stg_i[0] += 1
        return stg[i]

    SC_NA = 128 ** -0.5
    SC_MLA = 192 ** -0.5

    def rmsnorm_tile(src, bsrc, col):
        n = src.shape[-1] if len(src.shape) == 2 else None
        C.act(hb[:, 0:src.shape[1]], src, ACTF.Square, [bsrc], [bhb, bst_ss], accum_out=st_ss[:, col:col + 1])

    for b in range(NB):
        r0 = b * 256
        for t in range(2):
            x_t, bx = xt[t]
            C.load("sp", x_t[:], xp[r0 + t * 128: r0 + (t + 1) * 128, :], bx)
            C.act(hb[:], x_t[:], ACTF.Square, [bx], [bhb, bst_ss], accum_out=st_ss[:, 0:1])
            rstd_from_ss(C, st_r[:, 0:1], st_ss[:, 0:1], D, bst_ss, bst_r)
            C.stt(tmpf[:], x_t[:], st_r[:, 0:1], G1[:], ALU.mult, ALU.mult, [bx, bst_r, bG1], [btmpf])
            C.tt("dve", hb[:], tmpf[:], SA[:], ALU.add, [btmpf, bSA], [bhb])
            for k in range(16):
                C.tr(pT[:, k * 128:(k + 1) * 128], hb[:, k * 128:(k + 1) * 128], idB[:], [bhb, bidB], [bpT])
            C.copy("act", hT[:, :, t * 128:(t + 1) * 128], pT[:].rearrange("p (k c) -> p k c", k=16), [bpT], [bhT])

        if stop <= 1:
            continue
        for s in range(4):
            wt, bw = next_ws(w_in[:, s * 256:(s + 1) * 256], 256)
            for hh in range(2):
                head = s * 2 + hh
                pa, bpa = next_pA()
                for k in range(16):
                    C.mm(pa[:, 0:256], wt[:, k, hh * 128:(hh + 1) * 128], hT[:, k, :], k == 0, k == 15, [bw, bhT], [bpa])
                C.act(QT[:, head, :], pa[:, 0:256], ACTF.Copy, [bpa], [bQT], scale=SC_NA)
        if stop == 2 and sub <= 0:
            continue
        for which, (obuf, sb_t, sb_b) in enumerate(((o_nak, Kb, bKb), (o_nav, Vb, bVb))):
            for s in range(4):
                c0 = 1024 * (1 + which) + s * 256
                wt, bw = next_ws(w_in[:, c0:c0 + 256], 256)
                for t in range(2):
                    pa, bpa = next_pA()
                    for k in range(16):
                        C.mm(pa[:, 0:256], hT[:, k, t * 128:(t + 1) * 128], wt[:, k, :], k == 0, k == 15, [bw, bhT], [bpa])
                    sg, bsg = next_stg()
                    C.copy("dve", sg[:], pa[:, 0:256], [bpa], [bsg])
                    C.copy("act", sb_t[:, t, s * 256:(s + 1) * 256], pa[:, 0:256], [bpa], [sb_b])
                    C.store("sp", obuf[r0 + t * 128: r0 + (t + 1) * 128, s * 256:(s + 1) * 256], sg[:], bsg)
        if stop == 2 and sub <= 1:
            continue
        for s in range(2):
            c0 = 3072 + s * 256
            wt, bw = next_ws(w_in[:, c0:c0 + 256], 256)
            for t in range(2):
                pa, bpa = next_pA()
                for k in range(16):
                    C.mm(pa[:, 0:256], hT[:, k, t * 128:(t + 1) * 128], wt[:, k, :], k == 0, k == 15, [bw, bhT], [bpa])
                C.copy("dve", qaf[:, t, s * 256:(s + 1) * 256], pa[:, 0:256], [bpa], [bqaf])
        for t in range(2):
            C.act(hb[:, 0:512], qaf[:, t, :], ACTF.Square, [bqaf], [bhb, bst_ss], accum_out=st_ss[:, 1:2])
            rstd_from_ss(C, st_r[:, 1:2], st_ss[:, 1:2], 512, bst_ss, bst_r)
            C.stt(qan[:, t, :], qaf[:, t, :], st_r[:, 1:2], gqa[:], ALU.mult, ALU.mult, [bqaf, bst_r, bgqa], [bqan])
        if stop == 2 and sub <= 2:
            continue
        wt, bw = next_ws(w_in[:, 3584:3840], 256)
        for t in range(2):
            pa, bpa = next_pA()
            for k in range(16):
                C.mm(pa[:, 0:256], hT[:, k, t * 128:(t + 1) * 128], wt[:, k, :], k == 0, k == 15, [bw, bhT], [bpa])
            C.copy("dve", kvaf[:, t, :], pa[:, 0:256], [bpa], [bkvaf])
            C.act(hb[:, 0:256], kvaf[:, t, :], ACTF.Square, [bkvaf], [bhb, bst_ss], accum_out=st_ss[:, 2:3])
            rstd_from_ss(C, st_r[:, 2:3], st_ss[:, 2:3], 256, bst_ss, bst_r)
            C.stt(ckvf[:, t, :], kvaf[:, t, :], st_r[:, 2:3], gkva[:], ALU.mult, ALU.mult, [bkvaf, bst_r, bgkva], [bckvf])
            C.copy("act", ckvb[:, t, :], ckvf[:, t, :], [bckvf], [bckvb])
        C.store("sp", o_ckv[r0:r0 + 256, :].rearrange("(t p) c -> p t c", p=128), ckvf[:], bckvf)
        if stop == 2 and sub <= 3:
            continue
        wt, bw = next_ws(w_in[:, 3840:3904], 64)
        for t in range(2):
            pa, bpa = next_pA()
            for k in range(16):
                C.mm(pa[:, 0:64], hT[:, k, t * 128:(t + 1) * 128], wt[:, k, 0:64], k == 0, k == 15, [bw, bhT], [bpa])
            C.copy("dve", krf[:, t, :], pa[:, 0:64], [bpa], [bkrf])
            C.copy("act", krb[:, t, :], pa[:, 0:64], [bpa], [bkrb])
        C.store("sp", o_krope[r0:r0 + 256, :].rearrange("(t p) c -> p t c", p=128), krf[:], bkrf)

        if stop <= 2:
            continue
        for t in range(2):
            for hd in range(8):
                C.tr(pT[:, hd * 128:(hd + 1) * 128], Kb[:, t, hd * 128:(hd + 1) * 128], idB[:], [bKb, bidB], [bpT])
            C.copy("act", KT[:, :, t * 128:(t + 1) * 128], pT[:, 0:1024].rearrange("p (k c) -> p k c", k=8), [bpT], [bKT])
        for t in range(2):
            for c in range(4):
                C.tr(pT[:, c * 128:(c + 1) * 128], qan[:, t, c * 128:(c + 1) * 128], idB[:], [bqan, bidB], [bpT])
            for c in range(2):
                C.tr(pT[:, (4 + c) * 128:(5 + c) * 128], ckvb[:, t, c * 128:(c + 1) * 128], idB[:], [bckvb, bidB], [bpT])
            C.tr(pT[0:64, 6 * 128:7 * 128], krb[:, t, :], idB[:], [bkrb, bidB], [bpT])
            C.copy("act", qanT[:, :, t * 128:(t + 1) * 128], pT[:, 0:512].rearrange("p (k c) -> p k c", k=4), [bpT], [bqanT])
            C.copy("dve", ckvT[:, :, t * 128:(t + 1) * 128], pT[:, 512:768].rearrange("p (k c) -> p k c", k=2), [bpT], [bckvT])
            C.copy("dve", krT[0:64, t * 128:(t + 1) * 128], pT[0:64, 768:896], [bpT], [bkrT])
        for hd in range(8):
            pa, bpa = next_pA()
            for c in range(4):
                C.mm(pa[:, 0:256], wqb[:, c, hd * 192: hd * 192 + 128], qanT[:, c, :], c == 0, c == 3, [bwqb, bqanT], [bpa])
            C.act(qnT[:, hd, :], pa[:, 0:256], ACTF.Copy, [bpa], [bqnT], scale=SC_MLA)
            pa, bpa = next_pA()
            for c in range(4):
                C.mm(pa[0:64, 0:256], wqb[:, c, hd * 192 + 128: hd * 192 + 192], qanT[:, c, :], c == 0, c == 3, [bwqb, bqanT], [bpa])
            C.act(qrT[0:64, hd, :], pa[0:64, 0:256], ACTF.Copy, [bpa], [bqrT], scale=SC_MLA)
            pa, bpa = next_pA()
            for c in range(2):
                C.mm(pa[:, 0:256], wkvb[:, c, hd * 256: hd * 256 + 128], ckvT[:, c, :], c == 0, c == 1, [bwkvb, bckvT], [bpa])
            C.copy("dve", knT[:, hd, :], pa[:, 0:256], [bpa], [bknT])
        for t in range(2):
            for half in range(2):
                pa, bpa = next_pA()
                for c in range(2):
                    rhs = wkvb[:, c, :].rearrange("p (h x) -> p h x", h=8)[:, half * 4:(half + 1) * 4, 128:256]
                    C.mm(pa[:, 0:512], ckvT[:, c, t * 128:(t + 1) * 128], rhs, c == 0, c == 1, [bwkvb, bckvT], [bpa])
                C.copy("act", vm[:, t, half * 512:(half + 1) * 512], pa[:, 0:512], [bpa], [bvm])

        if stop <= 3:
            continue
        ai = [0]

        def attend(t, score_ops, vtile, bv, vcol, ocol):
            i = ai[0] % 2
            ai[0] += 1
            ps, bps = pS[i]
            pb, bpb = Pb[i]
            ptt, bptt = PT[i]
            am, bam = amx[i]
            n = len(score_ops)
            for j, (lt, rh, rd) in enumerate(score_ops):
                C.mm(ps[:, 0:256], lt, rh, j == 0, j == n - 1, rd, [bps])
            P.op("dve", lambda e: e.tensor_reduce(out=am[:, 0:1], in_=ps[:, 0:256], axis=AX.X, op=ALU.max, negate=True),
                 reads=[bps], writes=[bam])
            C.act(pb[:], ps[:, 0:256], ACTF.Exp, [bps, bam], [bpb, bam], bias=am[:, 0:1], scale=1.0, accum_out=am[:, 1:2])
            P.op("dve", lambda e: e.reciprocal(out=am[:, 2:3], in_=am[:, 1:2]), reads=[bam], writes=[bam])
            for j in range(2):
                C.tr(pTp_full[:, j, :], pb[:, j * 128:(j + 1) * 128], idB[:], [bpb, bidB], [bpTp])
            C.copy("dve", ptt[:], pTp_full[:, 0:2, :], [bpTp], [bptt])
            for j in range(2):
                C.mm(pO[:, 0:128], ptt[:, j, :], vtile[:, j, vcol:vcol + 128], j == 0, j == 1, [bptt, bv], [bpO])
            C.ts("dve", ocat[:, t, ocol:ocol + 128], pO[:, 0:128], am[:, 2:3], None, ALU.mult, None, [bpO, bam], [bocat])

        for hd in range(8):
            for t in range(2):
                attend(t, [(QT[:, hd, t * 128:(t + 1) * 128], KT[:, hd, :], [bQT, bKT])], Vb, bVb, hd * 128, hd * 128)
        for hd in range(8):
            for t in range(2):
                attend(t, [(qnT[:, hd, t * 128:(t + 1) * 128], knT[:, hd, :], [bqnT, bknT]),
                           (qrT[:, hd, t * 128:(t + 1) * 128], krT[:, :], [bqrT, bkrT])], vm, bvm, hd * 128, 1024 + hd * 128)

        if not do_tail:
            for t in range(2):
                C.copy("dve", h2b[:], ocat[:, t, :], [bocat], [bh2b])
                C.store("sp", o_h2[r0 + t * 128: r0 + (t + 1) * 128, :], h2b[:], bh2b)
            continue

        for t in range(2):
            for k in range(16):
                C.tr(pT[:, k * 128:(k + 1) * 128], ocat[:, t, k * 128:(k + 1) * 128], idB[:], [bocat, bidB], [bpT])
            C.copy("act", ocT[:, :, t * 128:(t + 1) * 128], pT[:].rearrange("p (k c) -> p k c", k=16), [bpT], [bocT])
        for s in range(8):
            wt, bw = next_ws(w_out[:, s * 256:(s + 1) * 256], 256)
            for t in range(2):
                x_t, bx = xt[t]
                pa, bpa = next_pA()
                for k in range(16):
                    C.mm(pa[:, 0:256], ocT[:, k, t * 128:(t + 1) * 128], wt[:, k, :], k == 0, k == 15, [bw, bocT], [bpa])
                sg, bsg = next_stg()
                C.tt("dve", sg[:], pa[:, 0:256], GA[:, s * 256:(s + 1) * 256], ALU.mult, [bpa, bGA], [bsg])
                C.tt("dve", x_t[:, s * 256:(s + 1) * 256], x_t[:, s * 256:(s + 1) * 256], sg[:], ALU.add, [bx, bsg], [bx])
        for t in range(2):
            x_t, bx = xt[t]
            C.store("sp", o_x1[r0 + t * 128: r0 + (t + 1) * 128, :], x_t[:], bx)
            C.act(hb[:], x_t[:], ACTF.Square, [bx], [bhb, bst_ss], accum_out=st_ss[:, 3:4])
            rstd_from_ss(C, st_r[:, 3:4], st_ss[:, 3:4], D, bst_ss, bst_r)
            C.stt(tmpf[:], x_t[:], st_r[:, 3:4], G2[:], ALU.mult, ALU.mult, [bx, bst_r, bG2], [btmpf])
            C.tt("dve", h2f[:], tmpf[:], SF[:], ALU.add, [btmpf, bSF], [bh2f])
            C.copy("act", h2b[:], h2f[:], [bh2f], [bh2b])
            C.store("sp", o_h2[r0 + t * 128: r0 + (t + 1) * 128, :], h2b[:], bh2b)
            for g4 in range(4):
                pa, bpa = next_pA()
                for j in range(4):
                    k = g4 * 4 + j
                    C.tr(pa[:, j * 128:(j + 1) * 128], h2f[:, k * 128:(k + 1) * 128], idF[:], [bh2f, bidF], [bpa])
                C.copy("dve", h2T[:, g4 * 4:(g4 + 1) * 4, :], pa[:, 0:512].rearrange("p (k c) -> p k c", k=4), [bpa], [bh2T])
            pa, bpa = next_pA()
            for k in range(16):
                C.mm(pa[:, 0:32], h2T[:, k, :], wr[:, k, :], k == 0, k == 15, [bh2T, bwr], [bpa])
            C.tt("dve", lg[:], pa[:, 0:32], brt[:], ALU.add, [bpa, bbrt], [blg])
            P.op("dve", lambda e: e.max(out=top8[:], in_=lg[:]), reads=[blg], writes=[btop8])
            C.ts("dve", lg2[:], lg[:], top8[:, 3:4], None, ALU.is_ge, None, [blg, btop8], [blg2])
            C.ts("dve", top8[:, 7:8], top8[:, 0:1], -1.0, None, ALU.mult, None, [btop8], [btop8])
            C.act(lg[:], lg[:], ACTF.Exp, [blg, btop8], [blg], bias=top8[:, 7:8], scale=1.0)
            C.tt("dve", lg[:], lg[:], lg2[:], ALU.mult, [blg, blg2], [blg])
            P.op("dve", lambda e: e.tensor_reduce(out=top8[:, 6:7], in_=lg[:], axis=AX.X, op=ALU.add), reads=[blg], writes=[btop8])
            P.op("dve", lambda e: e.reciprocal(out=top8[:, 6:7], in_=top8[:, 6:7]), reads=[btop8], writes=[btop8])
            C.ts("dve", lg2[:], lg[:], top8[:, 6:7], None, ALU.mult, None, [blg, btop8], [blg2])
            C.store("sp", o_G[r0 + t * 128: r0 + (t + 1) * 128, :], lg2[:], blg2)
    C.finish()
    return nc


def build_mod(nc, NCOL=1536, C=None):
    C = C or Ctx(nc)
    P = C.P
    NR = 2 if C.fused else 3
    cT = C.din("cT", [128, 16, NR])
    wm = C.din("wm", [D, NCOL])
    bm = C.din("bm", [1, NCOL])
    o_m = C.dout("o_m", [NR, NCOL])
    cTf, bcTf = C.T("cTf", [128, 16, NR], F32)
    cTb, bcTb = C.T("cTb", [128, 16, NR], BF16)
    wmb = [C.T(f"wmb{i}", [128, 16, 512], BF16) for i in range(3)]
    bmt = [C.T(f"bmt{i}", [NR, 512], F32) for i in range(2)]
    ot = [C.T(f"ot{i}", [NR, 512], F32) for i in range(2)]
    pm = [C.PS(f"pm{i}", [128, 512], F32) for i in range(2)]
    C.load("sp", cTf[:], cT, bcTf)
    C.act(cTb[:], cTf[:], ACTF.Silu, [bcTf], [bcTb])
    for n in range(NCOL // 512):
        pa, bpa = pm[n % 2]
        wt, bw = wmb[n % 3]
        bt, bbt = bmt[n % 2]
        o_t, bo = ot[n % 2]
        C.load("pool", wt[:], wm[:, n * 512:(n + 1) * 512].rearrange("(k p) n -> p k n", p=128), bw)
        C.load("sp", bt[:], bm[0:1, n * 512:(n + 1) * 512].partition_broadcast(NR), bbt)
        for k in range(16):
            C.mm(pa[0:NR, :], cTb[:, k, :], wt[:, k, :], k == 0, k == 15, [bcTb, bw], [bpa])
        C.tt("dve", o_t[:], pa[0:NR, :], bt[:], ALU.add, [bpa, bbt], [bo])
        C.store("sp", o_m[:, n * 512:(n + 1) * 512], o_t[:], bo)
    C.finish()
    return nc


def build_experts(nc, NTOK=16384, NE=4, C=None):
    C = C or Ctx(nc)
    P = C.P
    TB = 512
    NBLK = NTOK // TB
    if C.fused:
        h2_d = C.dram["h2_d"]
        Gl = C.dram["G_d"]
        idB, bidB, idF, bidF = C.consts
    else:
        h2T = C.din("h2T", [D, NTOK], BF16)
        Gl = C.din("Gl", [NTOK, NE])
        GlT = C.din("GlT", [NE, NTOK])
    w_gu = C.din("w_gu", [NE, D, 4096])
    b_gu = C.din("b_gu", [128, NE, 16, 2])
    w_dn = C.din("w_dn", [NE, D, D])
    b_dn = C.din("b_dn", [NE, D])
    o_y = C.dout("o_y", [NTOK, D], BF16)

    hTb = [C.T(f"hTb{i}", [128, 16, TB], BF16) for i in range(2)]
    wgu = [C.T(f"wgu{i}", [128, 16, 256], BF16) for i in range(3)]
    wdn = [C.T(f"wdn{i}", [128, 16, 512], BF16) for i in range(2)]
    actb = [C.T(f"actb{i}", [128, 16, TB], BF16) for i in range(2)]
    yacc, byacc = C.T("yacc", [128, 4, D], F32)
    yout = [C.T(f"yout{i}", [128, D], BF16) for i in range(2)]
    GTb = [C.T(f"GTb{i}", [NE, TB], F32) for i in range(2)]
    bdn4, bbdn4 = C.T("bdn4", [NE, D], F32)
    Gt, bGt = C.T("Gt", [128, NBLK * 4, NE], F32)
    bgu, bbgu = C.T("bgu", [128, NE, 16, 2], F32)
    gg = [C.T(f"gg{i}", [128, TB], F32) for i in range(2)]
    ss_ = [C.T(f"ss{i}", [128, TB], F32) for i in range(2)]
    uu = [C.T(f"uu{i}", [128, TB], F32) for i in range(2)]
    pg = [C.PS(f"pg{i}", [128, 512], F32) for i in range(2)]
    pu = [C.PS(f"pu{i}", [128, 512], F32) for i in range(2)]
    NPD = 2 if C.fused else 4
    pd = [C.PS(f"pd{i}", [128, 512], F32) for i in range(NPD)]
    if C.fused:
        pTx, bpTx = C.PS("pTx", [128, 2048], BF16)
        h2t = [C.T(f"h2t{i}", [128, D], BF16) for i in range(2)]

    C.load("sp", Gt[:], Gl.rearrange("(t p) e -> p t e", p=128), bGt)
    C.load("sp", bgu[:], b_gu, bbgu)
    C.load("sp", bdn4[:], b_dn, bbdn4)

    cnt = dict(wgu=0, wdn=0, act=0, pg=0, pd=0, ew=0)
    for blk in range(NBLK):
        hT_t, bhT_ = hTb[blk % 2]
        gT, bgT = GTb[blk % 2]
        if C.fused:
            for tt in range(4):
                h_t, bh_ = h2t[tt % 2]
                C.load("sp", h_t[:], h2_d[blk * TB + tt * 128: blk * TB + (tt + 1) * 128, :], bh_)
                for k in range(16):
                    C.tr(pTx[:, k * 128:(k + 1) * 128], h_t[:, k * 128:(k + 1) * 128], idB[:], [bh_, bidB], [bpTx])
                C.copy("act", hT_t[:, :, tt * 128:(tt + 1) * 128], pTx[:].rearrange("p (k c) -> p k c", k=16), [bpTx], [bhT_])
            pdt, bpd = pd[cnt["pd"] % NPD]
            cnt["pd"] += 1
            for tt in range(4):
                C.tr(pdt[0:NE, tt * 128:(tt + 1) * 128], Gt[:, blk * 4 + tt, :], idF[:], [bGt, bidF], [bpd])
            C.copy("dve", gT[:], pdt[0:NE, :], [bpd], [bgT])
        else:
            C.load("sp", hT_t[:], h2T[:, blk * TB:(blk + 1) * TB].rearrange("(k p) n -> p k n", p=128), bhT_)
            C.load("sp", gT[:], GlT[:, blk * TB:(blk + 1) * TB], bgT)
        for dc in range(4):
            for tt in range(4):
                pdt, bpd = pd[cnt["pd"] % NPD]
                cnt["pd"] += 1
                C.mm(pdt[:, :], gT[:, tt * 128:(tt + 1) * 128], bdn4[:, dc * 512:(dc + 1) * 512], True, True, [bgT, bbdn4], [bpd])
                C.copy("act", yacc[:, tt, dc * 512:(dc + 1) * 512], pdt[:, :], [bpd], [byacc])
        for e in range(NE):
            a_t, ba = actb[cnt["act"] % 2]
            cnt["act"] += 1
            for ffc in range(16):
                wt, bw = wgu[cnt["wgu"] % 3]
                cnt["wgu"] += 1
                C.load("pool", wt[:], w_gu[e, :, ffc * 256:(ffc + 1) * 256].rearrange("(k p) n -> p k n", p=128), bw)
                i = cnt["pg"] % 2
                cnt["pg"] += 1
                pgt, bpg = pg[i]
                put, bpu = pu[i]
                for k in range(16):
                    C.mm(pgt[:, 0:TB], wt[:, k, 0:256:2], hT_t[:, k, :], k == 0, k == 15, [bw, bhT_], [bpg])
                for k in range(16):
                    C.mm(put[:, 0:TB], wt[:, k, 1:256:2], hT_t[:, k, :], k == 0, k == 15, [bw, bhT_], [bpu])
                j = cnt["ew"] % 2
                cnt["ew"] += 1
                g_t, bg = gg[j]
                s_t, bs = ss_[j]
                u_t, bu = uu[j]
                C.ts("dve", g_t[:], pgt[:, 0:TB], bgu[:, e, ffc, 0:1], 7.0, ALU.add, ALU.min, [bpg, bbgu], [bg])
                C.act(s_t[:], g_t[:], ACTF.Sigmoid, [bg], [bs], scale=1.702)
                C.ts("dve", u_t[:], put[:, 0:TB], bgu[:, e, ffc, 1:2], 7.0, ALU.add, ALU.min, [bpu, bbgu], [bu])
                C.ts("pool", u_t[:], u_t[:], -7.0, 1.0, ALU.max, ALU.add, [bu], [bu])
                C.tt("pool", g_t[:], g_t[:], s_t[:], ALU.mult, [bg, bs], [bg])
                C.tt("dve", a_t[:, ffc, :], g_t[:], u_t[:], ALU.mult, [bg, bu], [ba])
            for dc in range(4):
                wd, bwd = wdn[cnt["wdn"] % 2]
                cnt["wdn"] += 1
                C.load("pool", wd[:], w_dn[e, :, dc * 512:(dc + 1) * 512].rearrange("(k p) n -> p k n", p=128), bwd)
                for tt in range(4):
                    pdt, bpd = pd[cnt["pd"] % NPD]
                    cnt["pd"] += 1
                    for ffc in range(16):
                        C.mm(pdt[:, :], a_t[:, ffc, tt * 128:(tt + 1) * 128], wd[:, ffc, :], ffc == 0, ffc == 15, [ba, bwd], [bpd])
                    gsc = Gt[:, blk * 4 + tt, e:e + 1]
                    ysl = yacc[:, tt, dc * 512:(dc + 1) * 512]
                    C.stt(ysl, pdt[:, :], gsc, ysl, ALU.mult, ALU.add, [bpd, bGt, byacc], [byacc])
        for tt in range(4):
            yo, byo = yout[tt % 2]
            C.copy("act", yo[:], yacc[:, tt, :], [byacc], [byo])
            C.store("sp", o_y[blk * TB + tt * 128: blk * TB + (tt + 1) * 128, :], yo[:], byo)
    C.finish()
    return nc


def build_combine(nc, NT=2048, NP=8, C=None):
    C = C or Ctx(nc)
    P = C.P
    x1 = C.din("x1", [NT, D])
    yp = C.din("yp", [NP, NT, D], BF16)
    gf = C.din("gf", [2, D])
    g_final = C.din("g_final", [1, D])
    o_y = C.dout("o_y", [NT, D])
    GF = [C.T(f"GF{i}", [128, D], F32) for i in range(2)]
    gfin, bgfin = C.T("gfin", [128, D], F32)
    xt = [C.T(f"xt{i}", [128, D], F32) for i in range(2)]
    ypt = [C.T(f"ypt{i}", [128, NP, D], BF16) for i in range(2)]
    acc, bacc_ = C.T("acc", [128, D], F32)
    junk, bjunk = C.T("junk", [128, D], BF16)
    ot = [C.T(f"ot{i}", [128, D], F32) for i in range(2)]
    st, bst = C.T("st", [128, 4], F32)
    for i in range(2):
        C.load("sp", GF[i][0][:], gf[i:i + 1, :].partition_broadcast(128), GF[i][1])
    C.load("sp", gfin[:], g_final[0:1, :].partition_broadcast(128), bgfin)
    ntile = NT // 128
    for t in range(ntile):
        x_t, bx = xt[t % 2]
        y_t, by = ypt[t % 2]
        o_t, bo = ot[t % 2]
        GFt, bGF = GF[0] if t < ntile // 2 else GF[1]
        C.load("sp", x_t[:], x1[t * 128:(t + 1) * 128, :], bx)
        C.load("sp", y_t[:], yp[:, t * 128:(t + 1) * 128, :].rearrange("j p d -> p j d"), by)
        if NP == 1:
            C.copy("dve", acc[:], y_t[:, 0, :], [by], [bacc_])
        else:
            C.tt("dve", acc[:], y_t[:, 0, :], y_t[:, 1, :], ALU.add, [by], [bacc_])
        for j in range(2, NP):
            C.tt("dve", acc[:], acc[:], y_t[:, j, :], ALU.add, [by, bacc_], [bacc_])
        C.tt("dve", acc[:], acc[:], GFt[:], ALU.mult, [bacc_, bGF], [bacc_])
        C.tt("dve", acc[:], acc[:], x_t[:], ALU.add, [bacc_, bx], [bacc_])
        C.act(junk[:], acc[:], ACTF.Square, [bacc_], [bjunk, bst], accum_out=st[:, 0:1])
        rstd_from_ss(C, st[:, 1:2], st[:, 0:1], D, bst, bst)
        C.stt(o_t[:], acc[:], st[:, 1:2], gfin[:], ALU.mult, ALU.mult, [bacc_, bst, bgfin], [bo])
        C.store("sp", o_y[t * 128:(t + 1) * 128, :], o_t[:], bo)
    C.finish()
    return nc


class Arena:
    def __init__(self, C, name, nbytes):
        self.t = C.P.sbuf(name, [128, nbytes // 2], BF16)
        self.cap = nbytes // 2
        self.off = 0
        self.peak = 0

    def mark(self):
        return self.off

    def reset(self, m):
        self.off = m

    def alloc(self, name, shape, dt):
        n = 1
        for d in shape[1:]:
            n *= d
        e16 = n * (2 if dt == F32 else 1)
        e16 = (e16 + 15) // 16 * 16
        assert self.off + e16 <= self.cap, (name, self.off, e16, self.cap)
        ap = self.t[:, self.off:self.off + e16]
        self.off += e16
        self.peak = max(self.peak, self.off)
        if dt == F32:
            ap = ap.bitcast(F32)
        ap = ap[:, 0:n]
        if len(shape) == 3:
            ap = ap.rearrange("p (a b) -> p a b", a=shape[1])
        elif len(shape) == 4:
            ap = ap.rearrange("p (a b c) -> p a b c", a=shape[1], b=shape[2])
        if shape[0] < 128:
            ap = ap[0:shape[0]]
        return ap, Buf(name)


def build_sample(nc, stop=99, NALLT=32, NPAIR=8, NHG=4, NMLAH=8, C=None):
    C = C or Ctx(nc)
    P = C.P
    x_own = C.din("x_own", [1024, D])
    x_halo = C.din("x_halo", [1792, D])
    x_all = C.din("x_all", [4096, D])
    ck_na = C.din("ck_na", [512, 1024])
    cv_na = C.din("cv_na", [512, 1024])
    c_ckv = C.din("c_ckv", [512, 256])
    c_krope = C.din("c_krope", [512, 64])
    mrow = C.din("mrow", [6, D])
    g_attn = C.din("g_attn", [1, D])
    g_ffn = C.din("g_ffn", [1, D])
    g_q_a = C.din("g_q_a", [1, 512])
    g_kv_a = C.din("g_kv_a", [1, 256])
    w_in = C.din("w_in", [D, 3904])
    w_in_rs = C.din("w_in_rs", [D, 64])
    w_q_b = C.din("w_q_b", [512, 1536])
    w_q_b_rs = C.din("w_q_b_rs", [512, 512])
    w_kv_b = C.din("w_kv_b", [256, 2048])
    w_out = C.din("w_out", [D, D])
    w_router = C.din("w_router", [D, 32])
    b_router = C.din("b_router", [1, 32])
    ident = C.din("ident", [128, 128])
    jmat = C.din("jmat", [128, 128])
    amat = C.din("amat", [2, 128])
    rmx = C.din("rmx", [8, 2, 896])
    colmask = C.din("colmask", [128, 15, 64])
    rpbpad = C.din("rpbpad", [8, 15, 160])
    cos_tok = C.din("cos_tok", [4096, 64])
    sinS_tok = C.din("sinS_tok", [4096, 64])
    cosT_own = C.din("cosT_own", [64, 1024])
    sinST_own = C.din("sinST_own", [64, 1024])
    o_x1 = C.dout("o_x1", [1024, D])
    o_h2 = C.dout("o_h2", [1024, D], BF16)
    o_G = C.dout("o_G", [1024, 32])
    ocat_d = C.scratch("ocat_d", [1024, D], BF16)
    bocat_d = Buf("ocat_d")

    SC_NA = 128 ** -0.5
    SC_MLA = 192 ** -0.5

    idF, bidF = C.T("idF", [128, 128], F32)
    idB, bidB = C.T("idB", [128, 128], BF16)
    jB, bjB = C.T("jB", [128, 128], BF16)
    aB, baB = C.T("aB", [2, 128], BF16)
    T0, bT0 = C.T("T0", [128, D], F32)
    T1, bT1 = C.T("T1", [128, D], F32)
    T2, bT2 = C.T("T2", [128, D], F32)
    gqa, bgqa = C.T("gqa", [128, 512], F32)
    gkva, bgkva = C.T("gkva", [128, 256], F32)
    brt, bbrt = C.T("brt", [128, 32], F32)
    wr, bwr = C.T("wr", [128, 16, 32], F32)
    ckvT, bckvT = C.T("ckvT", [128, 2, 4608], BF16)
    krT, bkrT = C.T("krT", [128, 4608], BF16)
    hTo, bhTo = C.T("hTo", [128, 16, 1024], BF16)
    xt, bxt = C.T("xt", [128, D], F32)
    tmpf, btmpf = C.T("tmpf", [128, D], F32)
    hb, bhb = C.T("hb", [128, D], BF16)
    hTt, bhTt = C.T("hTt", [128, 16, 128], BF16)
    st_ss, bst_ss = C.T("st_ss", [128, 8], F32)
    st_r, bst_r = C.T("st_r", [128, 8], F32)
    AR = C.arena if C.fused else Arena(C, "arena", 88 * 1024)

    pT, bpT = C.PS("pT", [128, 2048], BF16)
    pA = [C.PS(f"pA{i}", [128, 512], F32) for i in range(2)]
    pS = [C.PS(f"pS{i}", [128, 512], F32) for i in range(3)]
    pO, bpO = C.PS("pO", [128, 512], F32)

    pa_i = [0]

    def next_pA():
        i = pa_i[0] % 2
        pa_i[0] += 1
        return pA[i]

    def bload(dst, row_ap, buf, q="sp"):
        C.load(q, dst, row_ap.partition_broadcast(128), buf)

    C.load("sp", idF[:], ident, bidF)
    C.copy("dve", idB[:], idF[:], [bidF], [bidB])
    C.load("pool", jB[:], jmat, bjB)
    C.load("pool", aB[:], amat, baB)
    G1, bG1, SA, bSA = T0, bT0, T1, bT1
    bload(SA[:], mrow[0:1, :], bSA)
    bload(G1[:], mrow[1:2, :], bG1)
    bload(tmpf[:], g_attn[0:1, :], btmpf)
    C.stt(G1[:], G1[:], 1.0, tmpf[:], ALU.add, ALU.mult, [bG1, btmpf], [bG1])
    bload(gqa[:], g_q_a[0:1, :], bgqa)
    bload(gkva[:], g_kv_a[0:1, :], bgkva)
    bload(brt[:], b_router[0:1, :], bbrt)
    C.load("sp", wr[:], w_router.rearrange("(k p) n -> p k n", p=128), bwr)
    P.op("pool", lambda e: e.memset(krT[:], 0.0), writes=[bkrT])

    def make_h(src_rows):
        C.load("sp", xt[:], src_rows, bxt)
        C.act(hb[:], xt[:], ACTF.Square, [bxt], [bhb, bst_ss], accum_out=st_ss[:, 0:1])
        rstd_from_ss(C, st_r[:, 0:1], st_ss[:, 0:1], D, bst_ss, bst_r)
        C.stt(tmpf[:], xt[:], st_r[:, 0:1], G1[:], ALU.mult, ALU.mult, [bxt, bst_r, bG1], [btmpf])
        C.tt("dve", hb[:], tmpf[:], SA[:], ALU.add, [btmpf, bSA], [bhb])
        for k in range(16):
            C.tr(pT[:, k * 128:(k + 1) * 128], hb[:, k * 128:(k + 1) * 128], idB[:], [bhb, bidB], [bpT])

    m0 = AR.mark()
    w320, bw320 = AR.alloc("w320", [128, 16, 384], BF16)
    kvaf, bkvaf = AR.alloc("kvaf", [128, 256], F32)
    ckvb, bckvb = AR.alloc("ckvb", [128, 256], BF16)
    krb, bkrb = AR.alloc("krb", [128, 64], BF16)
    cst, bcst = AR.alloc("cst", [128, 2, 64], F32)
    kr1, bkr1 = AR.alloc("kr1", [128, 64], F32)
    kr2, bkr2 = AR.alloc("kr2", [128, 64], F32)
    ccf, bccf = AR.alloc("ccf", [128, 320], F32)
    C.load("pool", w320[:, :, 0:320], w_in[:, 3584:3904].rearrange("(k p) n -> p k n", p=128), bw320)
    C.load("pool", w320[:, :, 320:384], w_in_rs.rearrange("(k p) n -> p k n", p=128), bw320)
    for t in range(NALLT):
        make_h(x_all[t * 128:(t + 1) * 128, :])
        C.copy("act", hTt[:], pT[:].rearrange("p (k c) -> p k c", k=16), [bpT], [bhTt])
        C.load("sp", cst[:, 0, :], cos_tok[t * 128:(t + 1) * 128, :], bcst)
        C.load("sp", cst[:, 1, :], sinS_tok[t * 128:(t + 1) * 128, :], bcst)
        pa, bpa = next_pA()
        for k in range(16):
            C.mm(pa[:, 0:384], hTt[:, k, :], w320[:, k, :], k == 0, k == 15, [bhTt, bw320], [bpa])
        C.copy("dve", kvaf[:], pa[:, 0:256], [bpa], [bkvaf])
        C.tt("dve", kr1[:], pa[:, 256:320], cst[:, 0, :], ALU.mult, [bpa, bcst], [bkr1])
        C.tt("dve", kr2[:], pa[:, 320:384], cst[:, 1, :], ALU.mult, [bpa, bcst], [bkr2])
        C.tt("dve", krb[:], kr1[:], kr2[:], ALU.add, [bkr1, bkr2], [bkrb])
        C.act(hb[:, 0:256], kvaf[:], ACTF.Square, [bkvaf], [bhb, bst_ss], accum_out=st_ss[:, 2:3])
        rstd_from_ss(C, st_r[:, 2:3], st_ss[:, 2:3], 256, bst_ss, bst_r)
        C.stt(ckvb[:], kvaf[:], st_r[:, 2:3], gkva[:], ALU.mult, ALU.mult, [bkvaf, bst_r, bgkva], [bckvb])
        for c in range(2):
            C.tr(pT[:, c * 128:(c + 1) * 128], ckvb[:, c * 128:(c + 1) * 128], idB[:], [bckvb, bidB], [bpT])
        C.tr(pT[0:64, 256:384], krb[:], idB[:], [bkrb, bidB], [bpT])
        C.copy("act", ckvT[:, :, t * 128:(t + 1) * 128], pT[:, 0:256].rearrange("p (k c) -> p k c", k=2), [bpT], [bckvT])
        C.copy("dve", krT[0:64, t * 128:(t + 1) * 128], pT[0:64, 256:384], [bpT], [bkrT])
    for t in range(4):
        C.load("sp", ccf[:, 0:256], c_ckv[t * 128:(t + 1) * 128, :], bccf)
        C.load("sp", ccf[:, 256:320], c_krope[t * 128:(t + 1) * 128, :], bccf)
        C.copy("dve", hb[:, 0:320], ccf[:], [bccf], [bhb])
        for c in range(2):
            C.tr(pT[:, c * 128:(c + 1) * 128], hb[:, c * 128:(c + 1) * 128], idB[:], [bhb, bidB], [bpT])
        C.tr(pT[0:64, 256:384], hb[:, 256:320], idB[:], [bhb, bidB], [bpT])
        C.copy("act", ckvT[:, :, 4096 + t * 128:4096 + (t + 1) * 128], pT[:, 0:256].rearrange("p (k c) -> p k c", k=2), [bpT], [bckvT])
        C.copy("dve", krT[0:64, 4096 + t * 128:4096 + (t + 1) * 128], pT[0:64, 256:384], [bpT], [bkrT])
    P.barrier()
    AR.reset(m0)
    if stop <= 1:
        dbg = C.dout("dbg", [128, 3, 4608], BF16)
        C.store("sp", dbg[:, 0:2, :], ckvT[:], bckvT)
        C.store("sp", dbg[:, 2, :], krT[:], bkrT)
        C.finish()
        return nc

    for t in range(8):
        make_h(x_own[t * 128:(t + 1) * 128, :])
        C.copy("act", hTo[:, :, t * 128:(t + 1) * 128], pT[:].rearrange("p (k c) -> p k c", k=16), [bpT], [bhTo])

    m1 = AR.mark()
    Bstat, bBstat = AR.alloc("Bstat", [128, 8, 896], BF16)
    mB = AR.mark()
    Bfull, bBfull = AR.alloc("Bfull", [128, 8, 15, 64], F32)
    cmk, bcmk = AR.alloc("cmk", [128, 15, 64], F32)
    C.load("sp", cmk[:], colmask, bcmk)
    rp_t = rpbpad.tensor
    for hd in range(8):
        for half in range(2):
            src = bass.AP(rp_t, hd * 2400 + 16, [[1, 64], [160, 15], [1, 64]])
            C.load("sp", Bfull[half * 64:(half + 1) * 64, hd, :, :], src, bBfull)
    for hd in range(8):
        C.tt("dve", Bfull[:, hd, :, :], Bfull[:, hd, :, :], cmk[:], ALU.add, [bBfull, bcmk], [bBfull])
        C.copy("act", Bstat[0:64, hd, :].rearrange("p (j c) -> p j c", j=14), Bfull[0:64, hd, 1:15, :], [bBfull], [bBstat])
        C.copy("act", Bstat[64:128, hd, :].rearrange("p (j c) -> p j c", j=14), Bfull[64:128, hd, 0:14, :], [bBfull], [bBstat])
    P.barrier()
    AR.reset(mB)

    mG = AR.mark()
    for g in range(NHG):
        AR.reset(mG)
        wk, bwk = AR.alloc("wk", [128, 16, 256], BF16)
        wv, bwv = AR.alloc("wv", [128, 16, 256], BF16)
        wq, bwq = AR.alloc("wq", [128, 16, 256], BF16)
        KTh, bKTh = AR.alloc("KTh", [128, 2, 1792], BF16)
        Vh, bVh = AR.alloc("Vh", [128, 14, 256], BF16)
        cKT, bcKT = AR.alloc("cKT", [128, 2, 512], BF16)
        cV, bcV = AR.alloc("cV", [128, 4, 256], BF16)
        QTg, bQTg = AR.alloc("QTg", [128, 2, 1024], BF16)
        kbt, bkbt = AR.alloc("kbt", [128, 256], BF16)
        ccn, bccn = AR.alloc("ccn", [128, 2, 256], F32)
        ccb, bccb = AR.alloc("ccb", [128, 256], BF16)
        rmt = [AR.alloc(f"rmt{i}", [2, 896], BF16) for i in range(2)]
        Pb = [AR.alloc(f"Pb{i}", [128, 1408], BF16) for i in range(2)]
        PTt = [AR.alloc(f"PTt{i}", [128, 11, 128], BF16) for i in range(2)]
        otl = [AR.alloc(f"otl{i}", [128, 256], BF16) for i in range(2)]
        amx = [AR.alloc(f"amx{i}", [128, 8], F32) for i in range(2)]
        c0 = g * 256
        C.load("pool", wq[:], w_in[:, c0:c0 + 256].rearrange("(k p) n -> p k n", p=128), bwq)
        C.load("pool", wk[:], w_in[:, 1024 + c0:1024 + c0 + 256].rearrange("(k p) n -> p k n", p=128), bwk)
        C.load("pool", wv[:], w_in[:, 2048 + c0:2048 + c0 + 256].rearrange("(k p) n -> p k n", p=128), bwv)
        for t in range(14):
            make_h(x_halo[t * 128:(t + 1) * 128, :])
            C.copy("act", hTt[:], pT[:].rearrange("p (k c) -> p k c", k=16), [bpT], [bhTt])
            pa, bpa = next_pA()
            for k in range(16):
                C.mm(pa[:, 0:256], hTt[:, k, :], wk[:, k, :], k == 0, k == 15, [bhTt, bwk], [bpa])
            C.copy("act", kbt[:], pa[:, 0:256], [bpa], [bkbt])
            pa, bpa = next_pA()
            for k in range(16):
                C.mm(pa[:, 0:256], hTt[:, k, :], wv[:, k, :], k == 0, k == 15, [bhTt, bwv], [bpa])
            C.copy("dve", Vh[:, t, :], pa[:, 0:256], [bpa], [bVh])
            for hh in range(2):
                C.tr(pT[:, hh * 128:(hh + 1) * 128], kbt[:, hh * 128:(hh + 1) * 128], idB[:], [bkbt, bidB], [bpT])
            C.copy("act", KTh[:, :, t * 128:(t + 1) * 128], pT[:, 0:256].rearrange("p (k c) -> p k c", k=2), [bpT], [bKTh])
        for t in range(4):
            C.load("sp", ccn[:, 0, :], ck_na[t * 128:(t + 1) * 128, c0:c0 + 256], bccn)
            C.load("sp", ccn[:, 1, :], cv_na[t * 128:(t + 1) * 128, c0:c0 + 256], bccn)
            C.copy("dve", ccb[:], ccn[:, 0, :], [bccn], [bccb])
            C.copy("act", cV[:, t, :], ccn[:, 1, :], [bccn], [bcV])
            for hh in range(2):
                C.tr(pT[:, hh * 128:(hh + 1) * 128], ccb[:, hh * 128:(hh + 1) * 128], idB[:], [bccb, bidB], [bpT])
            C.copy("act", cKT[:, :, t * 128:(t + 1) * 128], pT[:, 0:256].rearrange("p (k c) -> p k c", k=2), [bpT], [bcKT])
        for hh in range(2):
            for half in range(2):
                pa, bpa = next_pA()
                for k in range(16):
                    C.mm(pa[:, :], wq[:, k, hh * 128:(hh + 1) * 128], hTo[:, k, half * 512:(half + 1) * 512], k == 0, k == 15, [bwq, bhTo], [bpa])
                C.act(QTg[:, hh, half * 512:(half + 1) * 512], pa[:, :], ACTF.Copy, [bpa], [bQTg], scale=SC_NA)
        ui = 0
        for p in range(NPAIR):
            rm_t, brm = rmt[p % 2]
            C.load("pool", rm_t[:], rmx[p], brm)
            ot_t, bot = otl[p % 2]
            for hh in range(2):
                hd = g * 2 + hh
                pb, bpb = Pb[ui % 2]
                ptt, bptt = PTt[ui % 2]
                am, bam = amx[ui % 2]
                ui += 1
                q_l = QTg[:, hh, p * 128:(p + 1) * 128]
                k0 = p * 128
                segs = [(pS[0], 0, 512), (pS[1], 512, 384)]
                for (ps, bps), o, n in segs:
                    C.mm(ps[:, 0:n], q_l, KTh[:, hh, k0 + o:k0 + o + n], True, False, [bQTg, bKTh], [bps])
                    C.mm(ps[:, 0:n], jB[:], Bstat[:, hd, o:o + n], False, False, [bjB, bBstat], [bps])
                    C.mm(ps[:, 0:n], aB[:], rm_t[:, o:o + n], False, True, [baB, brm], [bps])
                ps2, bps2 = pS[2]
                C.mm(ps2[:, 0:512], q_l, cKT[:, hh, :], True, True, [bQTg, bcKT], [bps2])
                for i, ((ps, bps), n) in enumerate(((pS[0], 512), (pS[1], 384), (pS[2], 512))):
                    P.op("dve", lambda e, ps=ps, n=n, i=i, am=am: e.tensor_reduce(out=am[:, i:i + 1], in_=ps[:, 0:n], axis=AX.X, op=ALU.max),
                         reads=[bps], writes=[bam])
                P.op("dve", lambda e, am=am: e.tensor_reduce(out=am[:, 3:4], in_=am[:, 0:3], axis=AX.X, op=ALU.max, negate=True), reads=[bam], writes=[bam])
                for i, ((ps, bps), o, n) in enumerate(((pS[0], 0, 512), (pS[1], 512, 384), (pS[2], 896, 512))):
                    C.act(pb[:, o:o + n], ps[:, 0:n], ACTF.Exp, [bps, bam], [bpb, bam], bias=am[:, 3:4], scale=1.0, accum_out=am[:, 4 + i:5 + i])
                P.op("dve", lambda e, am=am: e.tensor_reduce(out=am[:, 7:8], in_=am[:, 4:7], axis=AX.X, op=ALU.add), reads=[bam], writes=[bam])
                P.op("dve", lambda e, am=am: e.reciprocal(out=am[:, 7:8], in_=am[:, 7:8]), reads=[bam], writes=[bam])
                for j in range(11):
                    C.tr(pT[:, j * 128:(j + 1) * 128], pb[:, j * 128:(j + 1) * 128], idB[:], [bpb, bidB], [bpT])
                C.copy("act", ptt[:, 0:6, :], pT[:, 0:768].rearrange("p (k c) -> p k c", k=6), [bpT], [bptt])
                C.copy("dve", ptt[:, 6:11, :], pT[:, 768:1408].rearrange("p (k c) -> p k c", k=5), [bpT], [bptt])
                for j in range(7):
                    C.mm(pO[:, 0:128], ptt[:, j, :], Vh[:, p + j, hh * 128:(hh + 1) * 128], j == 0, False, [bptt, bVh], [bpO])
                for j in range(4):
                    C.mm(pO[:, 0:128], ptt[:, 7 + j, :], cV[:, j, hh * 128:(hh + 1) * 128], False, j == 3, [bptt, bcV], [bpO])
                C.ts("dve", ot_t[:, hh * 128:(hh + 1) * 128], pO[:, 0:128], am[:, 7:8], None, ALU.mult, None, [bpO, bam], [bot])
            P.dma("sp", lambda e, ot_t=ot_t, p=p, c0=c0: e.dma_start(out=ocat_d[p * 128:(p + 1) * 128, c0:c0 + 256], in_=ot_t[:]),
                  reads=[bot], writes=[bocat_d], key=bot, store=True)
        P.barrier()
    AR.reset(m1)
    if stop <= 2:
        C.finish()
        return nc

    qanT, bqanT = AR.alloc("qanT", [128, 4, 1024], BF16)
    mQ = AR.mark()
    wqa, bwqa = AR.alloc("wqa", [128, 16, 512], BF16)
    C.load("pool", wqa[:], w_in[:, 3072:3584].rearrange("(k p) n -> p k n", p=128), bwqa)
    qaf, bqaf = AR.alloc("qaf", [128, 512], F32)
    qan, bqan = AR.alloc("qan", [128, 512], BF16)
    for t in range(8):
        pa, bpa = next_pA()
        for k in range(16):
            C.mm(pa[:, :], hTo[:, k, t * 128:(t + 1) * 128], wqa[:, k, :], k == 0, k == 15, [bhTo, bwqa], [bpa])
        C.copy("dve", qaf[:], pa[:, :], [bpa], [bqaf])
        C.act(hb[:, 0:512], qaf[:], ACTF.Square, [bqaf], [bhb, bst_ss], accum_out=st_ss[:, 1:2])
        rstd_from_ss(C, st_r[:, 1:2], st_ss[:, 1:2], 512, bst_ss, bst_r)
        C.stt(qan[:], qaf[:], st_r[:, 1:2], gqa[:], ALU.mult, ALU.mult, [bqaf, bst_r, bgqa], [bqan])
        for c in range(4):
            C.tr(pT[:, c * 128:(c + 1) * 128], qan[:, c * 128:(c + 1) * 128], idB[:], [bqan, bidB], [bpT])
        C.copy("act", qanT[:, :, t * 128:(t + 1) * 128], pT[:, 0:512].rearrange("p (k c) -> p k c", k=4), [bpT], [bqanT])
    P.barrier()
    AR.reset(mQ)
    wqb, bwqb = AR.alloc("wqb", [128, 4, 1536], BF16)
    wqbr, bwqbr = AR.alloc("wqbr", [128, 4, 512], BF16)
    wkvb, bwkvb = AR.alloc("wkvb", [128, 2, 2048], BF16)
    csT, bcsT = AR.alloc("csT", [64, 2, 1024], F32)
    C.load("pool", wqb[:], w_q_b.rearrange("(k p) n -> p k n", p=128), bwqb)
    C.load("pool", wqbr[:], w_q_b_rs.rearrange("(k p) n -> p k n", p=128), bwqbr)
    C.load("pool", wkvb[:], w_kv_b.rearrange("(k p) n -> p k n", p=128), bwkvb)
    C.load("sp", csT[:, 0, :], cosT_own, bcsT)
    C.load("sp", csT[:, 1, :], sinST_own, bcsT)
    qnh, bqnh = AR.alloc("qnh", [128, 1024], BF16)
    qrh, bqrh = AR.alloc("qrh", [128, 1024], BF16)
    knh, bknh = AR.alloc("knh", [128, 4608], BF16)
    vh, bvh = AR.alloc("vh", [128, 36, 128], BF16)
    r1, br1 = AR.alloc("r1", [64, 512], F32)
    r2, br2 = AR.alloc("r2", [64, 512], F32)
    Pm = [AR.alloc(f"Pm{i}", [128, 512], BF16) for i in range(2)]
    PTm = [AR.alloc(f"PTm{i}", [128, 4, 128], BF16) for i in range(2)]
    om = [AR.alloc(f"om{i}", [128, 128], BF16) for i in range(2)]
    mst = [AR.alloc(f"mst{i}", [128, 24], F32) for i in range(2)]
    P.op("pool", lambda e: e.memset(qrh[:], 0.0), writes=[bqrh])
    ci = 0
    for hd in range(NMLAH):
        for half in range(2):
            sl = slice(half * 512, (half + 1) * 512)
            pa, bpa = next_pA()
            for c in range(4):
                C.mm(pa[:, :], wqb[:, c, hd * 192:hd * 192 + 128], qanT[:, c, sl], c == 0, c == 3, [bwqb, bqanT], [bpa])
            C.act(qnh[:, sl], pa[:, :], ACTF.Copy, [bpa], [bqnh], scale=SC_MLA)
            pa, bpa = next_pA()
            for c in range(4):
                C.mm(pa[0:64, :], wqb[:, c, hd * 192 + 128:hd * 192 + 192], qanT[:, c, sl], c == 0, c == 3, [bwqb, bqanT], [bpa])
            C.stt(r1[:], pa[0:64, :], SC_MLA, csT[:, 0, sl], ALU.mult, ALU.mult, [bpa, bcsT], [br1])
            pa, bpa = next_pA()
            for c in range(4):
                C.mm(pa[0:64, :], wqbr[:, c, hd * 64:(hd + 1) * 64], qanT[:, c, sl], c == 0, c == 3, [bwqbr, bqanT], [bpa])
            C.stt(r2[:], pa[0:64, :], SC_MLA, csT[:, 1, sl], ALU.mult, ALU.mult, [bpa, bcsT], [br2])
            C.tt("dve", qrh[0:64, sl], r1[:], r2[:], ALU.add, [br1, br2], [bqrh])
        for ch in range(9):
            pa, bpa = next_pA()
            for c in range(2):
                C.mm(pa[:, :], wkvb[:, c, hd * 256:hd * 256 + 128], ckvT[:, c, ch * 512:(ch + 1) * 512], c == 0, c == 1, [bwkvb, bckvT], [bpa])
            C.copy("act", knh[:, ch * 512:(ch + 1) * 512], pa[:, :], [bpa], [bknh])
        for kt4 in range(9):
            pa, bpa = next_pA()
            for j in range(4):
                kt = kt4 * 4 + j
                for c in range(2):
                    C.mm(pa[:, j * 128:(j + 1) * 128], ckvT[:, c, kt * 128:(kt + 1) * 128], wkvb[:, c, hd * 256 + 128:hd * 256 + 256], c == 0, c == 1, [bckvT, bwkvb], [bpa])
            C.copy("dve", vh[:, kt4 * 4:(kt4 + 1) * 4, :], pa[:, :].rearrange("p (k c) -> p k c", k=4), [bpa], [bvh])
        for qt in range(8):
            ms, bms = mst[qt % 2]
            o_t, bo = om[qt % 2]
            qs = slice(qt * 128, (qt + 1) * 128)
            for ch in range(9):
                ps, bps = pS[ch % 3]
                C.mm(ps[:, :], qnh[:, qs], knh[:, ch * 512:(ch + 1) * 512], True, False, [bqnh, bknh], [bps])
                C.mm(ps[:, :], qrh[:, qs], krT[:, ch * 512:(ch + 1) * 512], False, True, [bqrh, bkrT], [bps])
                P.op("dve", lambda e, ps=ps, ms=ms, ch=ch: e.tensor_reduce(out=ms[:, ch:ch + 1], in_=ps[:, :], axis=AX.X, op=ALU.max), reads=[bps], writes=[bms])
            P.op("dve", lambda e, ms=ms: e.tensor_reduce(out=ms[:, 9:10], in_=ms[:, 0:9], axis=AX.X, op=ALU.max, negate=True), reads=[bms], writes=[bms])
            for ch in range(9):
                ps, bps = pS[ch % 3]
                pm, bpm = Pm[ci % 2]
                ptm, bptm = PTm[ci % 2]
                ci += 1
                C.mm(ps[:, :], qnh[:, qs], knh[:, ch * 512:(ch + 1) * 512], True, False, [bqnh, bknh], [bps])
                C.mm(ps[:, :], qrh[:, qs], krT[:, ch * 512:(ch + 1) * 512], False, True, [bqrh, bkrT], [bps])
                C.act(pm[:], ps[:, :], ACTF.Exp, [bps, bms], [bpm, bms], bias=ms[:, 9:10], scale=1.0, accum_out=ms[:, 10 + ch:11 + ch])
                for j in range(4):
                    C.tr(pT[:, j * 128:(j + 1) * 128], pm[:, j * 128:(j + 1) * 128], idB[:], [bpm, bidB], [bpT])
                C.copy("act" if ch % 2 else "dve", ptm[:], pT[:, 0:512].rearrange("p (k c) -> p k c", k=4), [bpT], [bptm])
                for j in range(4):
                    kt = ch * 4 + j
                    C.mm(pO[:, 0:128], ptm[:, j, :], vh[:, kt, :], kt == 0, kt == 35, [bptm, bvh], [bpO])
            P.op("dve", lambda e, ms=ms: e.tensor_reduce(out=ms[:, 20:21], in_=ms[:, 10:19], axis=AX.X, op=ALU.add), reads=[bms], writes=[bms])
            P.op("dve", lambda e, ms=ms: e.reciprocal(out=ms[:, 20:21], in_=ms[:, 20:21]), reads=[bms], writes=[bms])
            C.ts("dve", o_t[:], pO[:, 0:128], ms[:, 20:21], None, ALU.mult, None, [bpO, bms], [bo])
            P.dma("sp", lambda e, o_t=o_t, qt=qt, hd=hd: e.dma_start(out=ocat_d[qt * 128:(qt + 1) * 128, 1024 + hd * 128:1024 + (hd + 1) * 128], in_=o_t[:]),
                  reads=[bo], writes=[bocat_d], key=bo, store=True)
    P.barrier()
    AR.reset(m1)
    if stop <= 3:
        C.finish()
        return nc

    GA, bGA, G2, bG2, SF, bSF = T0, bT0, T1, bT1, T2, bT2
    bload(GA[:], mrow[2:3, :], bGA)
    bload(SF[:], mrow[3:4, :], bSF)
    bload(G2[:], mrow[4:5, :], bG2)
    bload(tmpf[:], g_ffn[0:1, :], btmpf)
    C.stt(G2[:], G2[:], 1.0, tmpf[:], ALU.add, ALU.mult, [bG2, btmpf], [bG2])
    xt2 = [AR.alloc(f"xt2_{i}", [128, D], F32) for i in range(2)]
    oct_ = [AR.alloc(f"oct{i}", [128, D], BF16) for i in range(2)]
    ocT, bocT = AR.alloc("ocT", [128, 16, 256], BF16)
    ws = [AR.alloc(f"ws{i}", [128, 16, 256], BF16) for i in range(2)]
    stg = [AR.alloc(f"stg{i}", [128, 256], F32) for i in range(2)]
    h2f, bh2f = AR.alloc("h2f", [128, D], F32)
    h2b, bh2b = AR.alloc("h2b", [128, D], BF16)
    h2T, bh2T = AR.alloc("h2T", [128, 16, 128], F32)
    lg, blg = AR.alloc("lg", [128, 32], F32)
    lg2, blg2 = AR.alloc("lg2", [128, 32], F32)
    top8, btop8 = AR.alloc("top8", [128, 8], F32)
    wsi = [0]
    sgi = [0]
    for b in range(4):
        r0 = b * 256
        for t in range(2):
            x_t, bx = xt2[t]
            oc_t, boc = oct_[t]
            C.load("sp", x_t[:], x_own[r0 + t * 128:r0 + (t + 1) * 128, :], bx)
            P.dma("sp", lambda e, oc_t=oc_t, r0=r0, t=t: e.dma_start(out=oc_t[:], in_=ocat_d[r0 + t * 128:r0 + (t + 1) * 128, :]),
                  reads=[bocat_d], writes=[boc])
            for k in range(16):
                C.tr(pT[:, k * 128:(k + 1) * 128], oc_t[:, k * 128:(k + 1) * 128], idB[:], [boc, bidB], [bpT])
            C.copy("act", ocT[:, :, t * 128:(t + 1) * 128], pT[:].rearrange("p (k c) -> p k c", k=16), [bpT], [bocT])
        for s in range(8):
            wt, bw = ws[wsi[0] % 2]
            wsi[0] += 1
            C.load("pool", wt[:], w_out[:, s * 256:(s + 1) * 256].rearrange("(k p) n -> p k n", p=128), bw)
            for t in range(2):
                x_t, bx = xt2[t]
                pa, bpa = next_pA()
                for k in range(16):
                    C.mm(pa[:, 0:256], ocT[:, k, t * 128:(t + 1) * 128], wt[:, k, :], k == 0, k == 15, [bw, bocT], [bpa])
                sg, bsg = stg[sgi[0] % 2]
                sgi[0] += 1
                C.tt("dve", sg[:], pa[:, 0:256], GA[:, s * 256:(s + 1) * 256], ALU.mult, [bpa, bGA], [bsg])
                C.tt("dve", x_t[:, s * 256:(s + 1) * 256], x_t[:, s * 256:(s + 1) * 256], sg[:], ALU.add, [bx, bsg], [bx])
        for t in range(2):
            x_t, bx = xt2[t]
            C.store("sp", o_x1[r0 + t * 128:r0 + (t + 1) * 128, :], x_t[:], bx)
            C.act(hb[:], x_t[:], ACTF.Square, [bx], [bhb, bst_ss], accum_out=st_ss[:, 3:4])
            rstd_from_ss(C, st_r[:, 3:4], st_ss[:, 3:4], D, bst_ss, bst_r)
            C.stt(tmpf[:], x_t[:], st_r[:, 3:4], G2[:], ALU.mult, ALU.mult, [bx, bst_r, bG2], [btmpf])
            C.tt("dve", h2f[:], tmpf[:], SF[:], ALU.add, [btmpf, bSF], [bh2f])
            C.copy("act", h2b[:], h2f[:], [bh2f], [bh2b])
            C.store("sp", o_h2[r0 + t * 128:r0 + (t + 1) * 128, :], h2b[:], bh2b)
            for g4 in range(4):
                pa, bpa = next_pA()
                for j in range(4):
                    k = g4 * 4 + j
                    C.tr(pa[:, j * 128:(j + 1) * 128], h2f[:, k * 128:(k + 1) * 128], idF[:], [bh2f, bidF], [bpa])
                C.copy("dve", h2T[:, g4 * 4:(g4 + 1) * 4, :], pa[:, 0:512].rearrange("p (k c) -> p k c", k=4), [bpa], [bh2T])
            pa, bpa = next_pA()
            for k in range(16):
                C.mm(pa[:, 0:32], h2T[:, k, :], wr[:, k, :], k == 0, k == 15, [bh2T, bwr], [bpa])
            C.tt("dve", lg[:], pa[:, 0:32], brt[:], ALU.add, [bpa, bbrt], [blg])
            P.op("dve", lambda e: e.max(out=top8[:], in_=lg[:]), reads=[blg], writes=[btop8])
            C.ts("dve", lg2[:], lg[:], top8[:, 3:4], None, ALU.is_ge, None, [blg, btop8], [blg2])
            C.ts("dve", top8[:, 7:8], top8[:, 0:1], -1.0, None, ALU.mult, None, [btop8], [btop8])
            C.act(lg[:], lg[:], ACTF.Exp, [blg, btop8], [blg], bias=top8[:, 7:8], scale=1.0)
            C.tt("dve", lg[:], lg[:], lg2[:], ALU.mult, [blg, blg2], [blg])
            P.op("dve", lambda e: e.tensor_reduce(out=top8[:, 6:7], in_=lg[:], axis=AX.X, op=ALU.add), reads=[blg], writes=[btop8])
            P.op("dve", lambda e: e.reciprocal(out=top8[:, 6:7], in_=top8[:, 6:7]), reads=[btop8], writes=[btop8])
            C.ts("dve", lg2[:], lg[:], top8[:, 6:7], None, ALU.mult, None, [blg, btop8], [blg2])
            C.store("sp", o_G[r0 + t * 128:r0 + (t + 1) * 128, :], lg2[:], blg2)
    C.finish()
    return nc


def build_fused(nc):
    C = Ctx(nc, fused=True)
    P = C.P
    C.ps_pool = P.psum("pspool", [128, 4096], F32)[:]
    idF_t = P.sbuf("c_idF", [128, 128], F32)
    idB_t = P.sbuf("c_idB", [128, 128], BF16)
    bidF, bidB = Buf("c_idF"), Buf("c_idB")
    ident = C.din("ident", [128, 128])
    C.load("sp", idF_t[:], ident, bidF)
    C.copy("dve", idB_t[:], idF_t[:], [bidF], [bidB])
    C.consts = (idB_t, bidB, idF_t, bidF)
    C.arena = Arena(C, "arena", 206 * 1024)
    m_d = C.scratch("m_d", [2, 6 * D], F32)
    x1_d = C.scratch("x1_d", [2048, D], F32)
    h2_d = C.scratch("h2_d", [2048, D], BF16)
    G_d = C.scratch("G_d", [2048, 32], F32)
    y_d = C.scratch("y_d", [2048, D], BF16)

    def phase_end():
        P.barrier()
        P.recycle_dma_sems()
        C.arena.reset(0)
        C.ps_off = 0

    C.dram["o_m"] = m_d
    build_mod(nc, NCOL=6 * D, C=C)
    phase_end()
    C.dram["mrow"] = m_d[0:1, :].rearrange("o (s d) -> (o s) d", s=6)
    C.dram["o_x1"], C.dram["o_h2"], C.dram["o_G"] = x1_d[0:1024, :], h2_d[0:1024, :], G_d[0:1024, :]
    build_prompt(nc, NB=4, C=C)
    phase_end()
    C.dram["mrow"] = m_d[1:2, :].rearrange("o (s d) -> (o s) d", s=6)
    C.dram["o_x1"], C.dram["o_h2"], C.dram["o_G"] = x1_d[1024:2048, :], h2_d[1024:2048, :], G_d[1024:2048, :]
    build_sample(nc, C=C)
    phase_end()
    C.dram["h2_d"], C.dram["G_d"], C.dram["o_y"] = h2_d, G_d, y_d
    build_experts(nc, NTOK=2048, NE=32, C=C)
    phase_end()
    del C.dram["o_y"]
    C.dram["x1"] = x1_d
    C.dram["yp"] = y_d.rearrange("(o n) d -> o n d", o=1)
    C.dram["gf"] = m_d[:, 5 * D:6 * D]
    build_combine(nc, NT=2048, NP=1, C=C)
    P.emit()
    return nc


def rope_tables():
    T = 4096
    pos = np.arange(T); rows = (pos // 64).astype(np.float32); cols = (pos % 64).astype(np.float32)
    inv = (10000.0 ** (-(np.arange(16, dtype=np.float32) * 2.0 / 32))).astype(np.float32)
    ar = rows[:, None] * inv; ac = cols[:, None] * inv
    ang = np.concatenate([ar, ar, ac, ac], -1)
    cos, sin = np.cos(ang).astype(np.float32), np.sin(ang).astype(np.float32)
    sign = np.concatenate([-np.ones(16), np.ones(16), -np.ones(16), np.ones(16)]).astype(np.float32)
    return cos, sin * sign

SWAP = np.concatenate([np.arange(16, 32), np.arange(0, 16), np.arange(48, 64), np.arange(32, 48)])

def sample_consts(r0):
    ident = np.eye(128, dtype=np.float32)
    jmat = np.zeros((128, 128), np.float32)
    for p in range(128):
        jmat[p, (p // 64) * 64 + 63 - p % 64] = 1.0
    amat = np.zeros((2, 128), np.float32)
    amat[0, :64] = 1.0; amat[1, 64:] = 1.0
    rmx = np.zeros((8, 2, 14, 64), np.float32)
    for p in range(8):
        for rho in range(2):
            r = r0 + 2 * p + rho
            rs = min(max(r - 4, 0), 56)
            for j in range(14):
                kr = r0 + 2 * p - 6 + j
                ok = (0 <= kr < 64) and (rs <= kr < rs + 8)
                if not ok:
                    rmx[p, rho, j, :] = NEG
    colmask = np.zeros((128, 15, 64), np.float32)
    for pp in range(128):
        c = 63 - pp % 64
        cs = min(max(c - 8, 0), 48)
        m = np.full(64, NEG, np.float32); m[cs:cs + 16] = 0.0
        colmask[pp, :, :] = m
    return dict(ident=ident, jmat=jmat, amat=amat, rmx=rmx.reshape(8, 2, 896), colmask=colmask)

def sample_inputs(z, mrow_b, b, r0):
    cos, sinS = rope_tables()
    xs = z['x_sample'][b]
    halo = np.zeros((28, 64, 2048), np.float32)
    for i in range(28):
        r = r0 - 6 + i
        if 0 <= r < 64:
            halo[i] = xs[r * 64:(r + 1) * 64]
    rpbpad = np.zeros((8, 15, 160), np.float32)
    rpbpad[:, :, 64:95] = z['na_rpb'][0]
    wqb = z['w_q_b'][0]
    wqb_rs = np.concatenate([wqb[:, h * 192 + 128:h * 192 + 192][:, SWAP] for h in range(8)], 1)
    own = slice(r0 * 64, r0 * 64 + 1024)
    d = dict(x_own=np.ascontiguousarray(xs[own]), x_halo=halo.reshape(1792, 2048), x_all=xs,
             ck_na=z['cache_na_k'][b, 0].reshape(512, 1024), cv_na=z['cache_na_v'][b, 0].reshape(512, 1024),
             c_ckv=z['cache_mla_ckv'][b, 0], c_krope=z['cache_mla_krope'][b, 0],
             mrow=np.ascontiguousarray(mrow_b.reshape(6, 2048)),
             g_attn=z['g_attn'], g_ffn=z['g_ffn'], g_q_a=z['g_q_a'], g_kv_a=z['g_kv_a'],
             w_in=z['w_in'][0], w_in_rs=np.ascontiguousarray(z['w_in'][0][:, 3840:3904][:, SWAP]),
             w_q_b=wqb, w_q_b_rs=np.ascontiguousarray(wqb_rs), w_kv_b=z['w_kv_b'][0], w_out=z['w_out'][0],
             w_router=z['w_router'][0], b_router=z['b_router'], rpbpad=rpbpad,
             cos_tok=cos, sinS_tok=sinS, cosT_own=np.ascontiguousarray(cos[own].T), sinST_own=np.ascontiguousarray(sinS[own].T))
    d.update(sample_consts(r0))
    return d


def fused_inputs(z, core):
    f32 = np.float32
    b, r0 = core // 4, 16 * (core % 4)
    c2 = np.stack([np.asarray(z['c_ctx'], f32), np.asarray(z['c'], f32)[b]], 0)
    d = sample_inputs(z, np.zeros(6 * 2048, f32), b, r0)
    del d['mrow']
    d.update(cT=np.ascontiguousarray(c2.T.reshape(16, 128, 2).transpose(1, 0, 2)),
             wm=np.asarray(z['w_mod'], f32)[0], bm=np.asarray(z['b_mod'], f32),
             xp=np.ascontiguousarray(np.asarray(z['x_prompt'], f32)[4 * core:4 * core + 4].reshape(1024, 2048)),
             w_gu=np.asarray(z['w_gate_up'], f32)[0],
             b_gu=np.ascontiguousarray(np.asarray(z['b_gate_up'], f32)[0].reshape(32, 16, 128, 2).transpose(2, 0, 1, 3)),
             w_dn=np.asarray(z['w_down'], f32)[0], b_dn=np.asarray(z['b_down'], f32)[0],
             g_final=np.asarray(z['g_final'], f32)[None])
    return d


from concourse.bass_utils import run_bass_kernel_spmd

NCORES = 8


def kernel(x_prompt, x_sample, cache_na_k, cache_na_v, cache_mla_ckv, cache_mla_krope, c, c_ctx,
           g_attn, g_ffn, g_final, w_mod, b_mod, w_in, w_out, na_rpb, g_q_a, w_q_b, g_kv_a, w_kv_b,
           w_router, b_router, w_gate_up, b_gate_up, w_down, b_down):
    f32 = np.float32
    z = dict(x_prompt=x_prompt, x_sample=x_sample, cache_na_k=cache_na_k, cache_na_v=cache_na_v, cache_mla_ckv=cache_mla_ckv,
             cache_mla_krope=cache_mla_krope, c=c, c_ctx=c_ctx, g_attn=g_attn, g_ffn=g_ffn, g_final=g_final, w_mod=w_mod, b_mod=b_mod,
             w_in=w_in, w_out=w_out, na_rpb=na_rpb, g_q_a=g_q_a, w_q_b=w_q_b, g_kv_a=g_kv_a, w_kv_b=w_kv_b, w_router=w_router,
             b_router=b_router, w_gate_up=w_gate_up, b_gate_up=b_gate_up, w_down=w_down, b_down=b_down)
    z = {k: np.asarray(v, f32) for k, v in z.items()}
    nc = bass.Bass("TRN2", target_bir_lowering=False)
    build_fused(nc)
    in_maps = [fused_inputs(z, i) for i in range(NCORES)]
    res = run_bass_kernel_spmd(nc, in_maps, core_ids=list(range(NCORES))).results
    y = [np.asarray(r["o_y"], f32) for r in res]
    y_prompt = np.concatenate([t[:1024] for t in y], 0).reshape(32, 256, 2048)
    y_sample = np.concatenate([t[1024:] for t in y], 0).reshape(2, 4096, 2048)
    new_na_k = np.concatenate([np.asarray(r["o_nak"], f32) for r in res], 0).reshape(32, 1, 256, 8, 128)
    new_na_v = np.concatenate([np.asarray(r["o_nav"], f32) for r in res], 0).reshape(32, 1, 256, 8, 128)
    new_ckv = np.concatenate([np.asarray(r["o_ckv"], f32) for r in res], 0).reshape(32, 1, 256, 256)
    new_krope = np.concatenate([np.asarray(r["o_krope"], f32) for r in res], 0).reshape(32, 1, 256, 64)
    return (y_prompt, y_sample, new_na_k, new_na_v, new_ckv, new_krope)
```

```python
import numpy as np
import concourse.bass as bass
import concourse.mybir as mybir
from contextlib import ExitStack

F32 = mybir.dt.float32
BF16 = mybir.dt.bfloat16
I32 = mybir.dt.int32
U32 = mybir.dt.uint32
ALU = mybir.AluOpType
ACTF = mybir.ActivationFunctionType
AX = mybir.AxisListType

ENGS = ("pe", "act", "dve", "pool", "sp")
SEM_ROTATE = 12000


class Buf:
    __slots__ = ("name", "last_w", "readers", "ld", "st", "excl")

    def __init__(self, name, excl=False):
        self.name = name
        self.excl = excl
        self.last_w = None
        self.readers = []
        self.ld = None
        self.st = None


class DmaSem:
    __slots__ = ("sem", "count", "kind")

    def __init__(self, sem, kind):
        self.sem = sem
        self.count = 0
        self.kind = kind


class Op:
    __slots__ = ("eng", "fn", "waits", "is_dma", "dsem", "dval", "signaled", "sig", "idx", "dma_waits", "inc")

    def __init__(self, eng, fn, is_dma):
        self.eng = eng
        self.fn = fn
        self.is_dma = is_dma
        self.waits = {}
        self.dma_waits = {}
        self.dsem = None
        self.dval = 0
        self.signaled = False
        self.sig = None
        self.idx = 0
        self.inc = 16


class Prog:
    def __init__(self, nc):
        self.nc = nc
        self.ops = {e: [] for e in ENGS}
        self.all_ops = []
        self.stack = ExitStack()
        self.sem_pool = []
        self.n_sems = 0
        self.dma_sems = []
        self.free_dsems = []
        self._n = 0

    def new_sem(self):
        self.n_sems += 1
        return self.stack.enter_context(self.nc.semaphore(f"s{self.n_sems}"))

    def sbuf(self, name, shape, dtype):
        t = self.stack.enter_context(self.nc.sbuf_tensor(name, list(shape), dtype))
        return t

    def psum(self, name, shape, dtype):
        t = self.stack.enter_context(self.nc.psum_tensor(name, list(shape), dtype))
        return t

    def _dep(self, op, prod):
        if prod is None or prod is op:
            return
        if prod.is_dma:
            ds = prod.dsem
            op.dma_waits[id(ds)] = (ds, ds.count)
        else:
            if prod.eng == op.eng and not op.is_dma and op.eng == "pe":
                return
            cur = op.waits.get(prod.eng)
            if cur is None or cur.idx < prod.idx:
                op.waits[prod.eng] = prod
            prod.signaled = True

    def _record(self, op, reads, writes):
        self._n += 1
        op.idx = self._n
        ex = [b for b in reads if b.excl]
        if ex:
            reads = [b for b in reads if not b.excl]
            writes = list(writes) + [b for b in ex if b not in writes]
        for b in reads:
            self._dep(op, b.last_w)
        for b in writes:
            self._dep(op, b.last_w)
            for r in b.readers:
                self._dep(op, r)
        for b in reads:
            b.readers.append(op)
        for b in writes:
            b.last_w = op
            b.readers = []
        self.ops[op.eng].append(op)
        self.all_ops.append(op)
        return op

    def op(self, eng, fn, reads=(), writes=()):
        return self._record(Op(eng, fn, False), reads, writes)

    def dma(self, eng, fn, reads=(), writes=(), key=None, store=False, inc=16):
        op = Op(eng, fn, True)
        if key is None:
            key = writes[0] if not store else reads[0]
        attr = "st" if store else "ld"
        ds = getattr(key, attr)
        if ds is None:
            kind = "sw" if eng == "pool" else "hw"
            fl = [d for d in self.free_dsems if d.kind == kind]
            if fl:
                ds = fl[-1]
                self.free_dsems.remove(ds)
            else:
                ds = DmaSem(self.new_sem(), kind)
                self.dma_sems.append(ds)
            setattr(key, attr, ds)
        op.dsem = ds
        op.inc = inc
        self._record(op, reads, writes)
        ds.count += inc
        op.dval = ds.count
        return op

    def recycle_dma_sems(self):
        self.free_dsems = list(self.dma_sems)

    def coll(self, fn, reads=(), writes=()):
        if not hasattr(self, "_csem"):
            self._csem = self.new_sem()
            self._ccnt = 0
            self._cscr = self.sbuf("coll_scr", [128, 8], F32)
        self._ccnt += 1
        n = self._ccnt
        csem = self._csem
        scr = self._cscr

        def run(eng, fn=fn, n=n):
            fn(eng).then_inc(csem)
            eng.wait_ge(csem, n)
            return eng.memset(scr[:], 0.0)
        return self.op("pool", run, reads=reads, writes=writes)

    def barrier(self, bufs=()):
        lastc = {}
        for e in ENGS:
            lastc[e] = None
            for q in reversed(self.ops[e]):
                if not q.is_dma and q.fn is not None:
                    lastc[e] = q
                    break
        tot = [(ds, ds.count) for ds in self.dma_sems if ds.count > 0]
        for e in ENGS:
            op = Op(e, None, False)
            self._n += 1
            op.idx = self._n
            for e2 in ENGS:
                q = lastc[e2]
                if q is not None and (e2 != e or e != "pe"):
                    op.waits[e2] = q
                    q.signaled = True
            for ds, c in tot:
                op.dma_waits[id(ds)] = (ds, c)
            self.ops[e].append(op)
            self.all_ops.append(op)

    def emit(self):
        nc = self.nc
        for e in ENGS:
            sem = None
            cnt = 0
            for op in self.ops[e]:
                if op.is_dma or not op.signaled or op.fn is None:
                    continue
                if sem is None or cnt >= SEM_ROTATE:
                    sem = self.new_sem()
                    cnt = 0
                cnt += 1
                op.sig = (sem, cnt)
        prog = self

        def run(engname, eng):
            for op in prog.ops[engname]:
                for p in op.waits.values():
                    s, v = p.sig
                    eng.wait_ge(s, v)
                for ds, v in op.dma_waits.values():
                    eng.wait_ge(ds.sem, v)
                if op.fn is None:
                    continue
                ins = op.fn(eng)
                if op.is_dma:
                    ins.then_inc(op.dsem.sem, op.inc)
                elif op.signaled:
                    ins.then_inc(op.sig[0], 1)

        with nc.Block() as block:
            @block.tensor
            def _(eng):
                run("pe", eng)

            @block.scalar
            def _(eng):
                run("act", eng)

            @block.vector
            def _(eng):
                run("dve", eng)

            @block.gpsimd
            def _(eng):
                run("pool", eng)

            @block.sync
            def _(eng):
                run("sp", eng)
                for ds in prog.dma_sems:
                    if ds.count > 0:
                        eng.wait_ge(ds.sem, ds.count)
        self.stack.close()


D = 2048
EPS = 1e-6
NEG = -30000.0


class Ctx:
    def __init__(self, nc, fused=False):
        self.nc = nc
        self.P = Prog(nc)
        self._cnt = 0
        self.fused = fused
        self.dram = {}
        self.arena = None
        self.ps_pool = None
        self.ps_off = 0

    def T(self, name, shape, dt):
        if self.fused:
            return self.arena.alloc(name, shape, dt)
        t = self.P.sbuf(name, shape, dt)
        b = Buf(name)
        return t, b

    def PS(self, name, shape, dt):
        if self.fused:
            n = 1
            for d in shape[1:]:
                n *= d
            nbytes = n * (4 if dt == F32 else 2)
            nb = (nbytes + 2047) // 2048
            assert self.ps_off + nb <= 8, ("psum", name)
            ap = self.ps_pool[:, self.ps_off * 512:(self.ps_off + nb) * 512]
            self.ps_off += nb
            if dt != F32:
                ap = ap.bitcast(dt)
            ap = ap[:, 0:n]
            if len(shape) == 3:
                ap = ap.rearrange("p (a b) -> p a b", a=shape[1])
            return ap, Buf(name, excl=True)
        t = self.P.psum(name, shape, dt)
        b = Buf(name, excl=True)
        return t, b

    def din(self, name, shape, dt=F32):
        if name in self.dram:
            return self.dram[name]
        ap = self.nc.dram_tensor(name, list(shape), dt, kind="ExternalInput").ap()
        if self.fused:
            self.dram[name] = ap
        return ap

    def dout(self, name, shape, dt=F32):
        if name in self.dram:
            return self.dram[name]
        return self.nc.dram_tensor(name, list(shape), dt, kind="ExternalOutput").ap()

    def scratch(self, name, shape, dt):
        return self.nc.dram_tensor(name, list(shape), dt).ap()

    def finish(self):
        if not self.fused:
            self.P.emit()

    def load(self, q, out_ap, in_ap, wbuf, reads=()):
        return self.P.dma(q, lambda e: e.dma_start(out=out_ap, in_=in_ap), reads=list(reads), writes=[wbuf])

    def store(self, q, out_ap, in_ap, rbuf, writes=()):
        return self.P.dma(q, lambda e: e.dma_start(out=out_ap, in_=in_ap), reads=[rbuf], writes=list(writes), key=rbuf, store=True)

    def mm(self, out, lhsT, rhs, start, stop, reads, writes):
        return self.P.op("pe", lambda e: e.matmul(out, lhsT=lhsT, rhs=rhs, start=start, stop=stop), reads=reads, writes=writes)

    def tr(self, out, in_, ident, reads, writes):
        return self.P.op("pe", lambda e: e.transpose(out=out, in_=in_, identity=ident), reads=reads, writes=writes)

    def act(self, out, in_, func, reads, writes, bias=None, scale=None, accum_out=None):
        kw = {}
        if bias is not None:
            kw["bias"] = bias
        if scale is not None:
            kw["scale"] = scale
        if accum_out is not None:
            kw["accum_out"] = accum_out
        return self.P.op("act", lambda e: e.activation(out=out, in_=in_, func=func, **kw), reads=reads, writes=writes)

    def ts(self, eng, out, in0, s1, s2, op0, op1, reads, writes):
        if op1 is None:
            return self.P.op(eng, lambda e: e.tensor_scalar(out=out, in0=in0, scalar1=s1, scalar2=None, op0=op0), reads=reads, writes=writes)
        return self.P.op(eng, lambda e: e.tensor_scalar(out=out, in0=in0, scalar1=s1, scalar2=s2, op0=op0, op1=op1), reads=reads, writes=writes)

    def tt(self, eng, out, in0, in1, op, reads, writes):
        return self.P.op(eng, lambda e: e.tensor_tensor(out=out, in0=in0, in1=in1, op=op), reads=reads, writes=writes)

    def stt(self, out, in0, scalar, in1, op0, op1, reads, writes):
        return self.P.op("dve", lambda e: e.scalar_tensor_tensor(out=out, in0=in0, scalar=scalar, in1=in1, op0=op0, op1=op1), reads=reads, writes=writes)

    def copy(self, eng, out, in_, reads, writes):
        if eng == "act":
            return self.P.op("act", lambda e: e.copy(out=out, in_=in_), reads=reads, writes=writes)
        return self.P.op(eng, lambda e: e.tensor_copy(out=out, in_=in_), reads=reads, writes=writes)


def rstd_from_ss(C, rstd, ss, n, bss, brstd):
    C.ts("dve", rstd, ss, 1.0 / n, EPS, ALU.mult, ALU.add, [bss], [brstd])
    C.act(rstd, rstd, ACTF.Sqrt, [brstd], [brstd])
    C.P.op("dve", lambda e: e.reciprocal(out=rstd, in_=rstd), reads=[brstd], writes=[brstd])


def build_prompt(nc, NB=4, do_tail=True, stop=99, sub=99, C=None):
    C = C or Ctx(nc)
    P = C.P
    NT = NB * 256
    xp = C.din("xp", [NT, D])
    mrow = C.din("mrow", [6, D])
    g_attn = C.din("g_attn", [1, D])
    g_ffn = C.din("g_ffn", [1, D])
    g_q_a = C.din("g_q_a", [1, 512])
    g_kv_a = C.din("g_kv_a", [1, 256])
    w_in = C.din("w_in", [D, 3904])
    w_q_b = C.din("w_q_b", [512, 1536])
    w_kv_b = C.din("w_kv_b", [256, 2048])
    w_out = C.din("w_out", [D, D])
    w_router = C.din("w_router", [D, 32])
    b_router = C.din("b_router", [1, 32])
    ident = C.din("ident", [128, 128])
    o_nak = C.dout("o_nak", [NT, 1024])
    o_nav = C.dout("o_nav", [NT, 1024])
    o_ckv = C.dout("o_ckv", [NT, 256])
    o_krope = C.dout("o_krope", [NT, 64])
    o_x1 = C.dout("o_x1", [NT, D])
    o_h2 = C.dout("o_h2", [NT, D], BF16)
    o_G = C.dout("o_G", [NT, 32])

    idF, bidF = C.T("idF", [128, 128], F32)
    idB, bidB = C.T("idB", [128, 128], BF16)
    G1, bG1 = C.T("G1", [128, D], F32)
    SA, bSA = C.T("SA", [128, D], F32)
    GA, bGA = C.T("GA", [128, D], F32)
    G2, bG2 = C.T("G2", [128, D], F32)
    SF, bSF = C.T("SF", [128, D], F32)
    gqa, bgqa = C.T("gqa", [128, 512], F32)
    gkva, bgkva = C.T("gkva", [128, 256], F32)
    brt, bbrt = C.T("brt", [128, 32], F32)
    wr, bwr = C.T("wr", [128, 16, 32], F32)
    wqb, bwqb = C.T("wqb", [128, 4, 1536], BF16)
    wkvb, bwkvb = C.T("wkvb", [128, 2, 2048], BF16)
    xt = [C.T(f"xt{t}", [128, D], F32) for t in range(2)]
    tmpf, btmpf = C.T("tmpf", [128, D], F32)
    hb, bhb = C.T("hb", [128, D], BF16)
    hT, bhT = C.T("hT", [128, 16, 256], BF16)
    NWS = 2
    ws = [C.T(f"ws{i}", [128, 16, 256], BF16) for i in range(NWS)]
    QT, bQT = C.T("QT", [128, 8, 256], BF16)
    Kb, bKb = C.T("Kb", [128, 2, 1024], BF16)
    Vb, bVb = C.T("Vb", [128, 2, 1024], BF16)
    KT, bKT = C.T("KT", [128, 8, 256], BF16)
    qaf, bqaf = C.T("qaf", [128, 2, 512], F32)
    qan, bqan = C.T("qan", [128, 2, 512], BF16)
    qanT, bqanT = C.T("qanT", [128, 4, 256], BF16)
    kvaf, bkvaf = C.T("kvaf", [128, 2, 256], F32)
    ckvf, bckvf = C.T("ckvf", [128, 2, 256], F32)
    ckvb, bckvb = C.T("ckvb", [128, 2, 256], BF16)
    ckvT, bckvT = C.T("ckvT", [128, 2, 256], BF16)
    krf, bkrf = C.T("krf", [128, 2, 64], F32)
    krb, bkrb = C.T("krb", [128, 2, 64], BF16)
    krT, bkrT = C.T("krT", [128, 256], BF16)
    qnT, bqnT = C.T("qnT", [128, 8, 256], BF16)
    qrT, bqrT = C.T("qrT", [128, 8, 256], BF16)
    knT, bknT = C.T("knT", [128, 8, 256], BF16)
    vm, bvm = C.T("vm", [128, 2, 1024], BF16)
    ocat, bocat = C.T("ocat", [128, 2, D], BF16)
    ocT, bocT = C.T("ocT", [128, 16, 256], BF16)
    stg = [C.T(f"stg{i}", [128, 256], F32) for i in range(2)]
    Pb = [C.T(f"Pb{i}", [128, 256], BF16) for i in range(2)]
    PT = [C.T(f"PT{i}", [128, 2, 128], BF16) for i in range(2)]
    st_ss, bst_ss = C.T("st_ss", [128, 8], F32)
    st_r, bst_r = C.T("st_r", [128, 8], F32)
    amx = [C.T(f"amx{i}", [128, 4], F32) for i in range(2)]
    h2f, bh2f = C.T("h2f", [128, D], F32)
    h2b, bh2b = C.T("h2b", [128, D], BF16)
    h2T, bh2T = C.T("h2T", [128, 16, 128], F32)
    lg, blg = C.T("lg", [128, 32], F32)
    lg2, blg2 = C.T("lg2", [128, 32], F32)
    top8, btop8 = C.T("top8", [128, 8], F32)

    pT, bpT = C.PS("pT", [128, 2048], BF16)
    pA = [C.PS(f"pA{i}", [128, 512], F32) for i in range(2)]
    pS = [C.PS(f"pS{i}", [128, 512], F32) for i in range(2)]
    pTp_full, bpTp = C.PS("pTp", [128, 8, 128], BF16)
    pTp = pTp_full[:, 0:2, :]
    pO, bpO = C.PS("pO", [128, 512], F32)

    C.load("sp", idF[:], ident, bidF)
    C.copy("dve", idB[:], idF[:], [bidF], [bidB])

    def bload(dst, row_ap, buf):
        C.load("sp", dst[:], row_ap.partition_broadcast(128), buf)

    bload(SA, mrow[0:1, :], bSA)
    bload(G1, mrow[1:2, :], bG1)
    bload(tmpf, g_attn[0:1, :], btmpf)
    C.stt(G1[:], G1[:], 1.0, tmpf[:], ALU.add, ALU.mult, [bG1, btmpf], [bG1])
    bload(GA, mrow[2:3, :], bGA)
    bload(SF, mrow[3:4, :], bSF)
    bload(G2, mrow[4:5, :], bG2)
    bload(h2f, g_ffn[0:1, :], bh2f)
    C.stt(G2[:], G2[:], 1.0, h2f[:], ALU.add, ALU.mult, [bG2, bh2f], [bG2])
    bload(gqa, g_q_a[0:1, :], bgqa)
    bload(gkva, g_kv_a[0:1, :], bgkva)
    bload(brt, b_router[0:1, :], bbrt)
    C.load("sp", wr[:], w_router.rearrange("(k p) n -> p k n", p=128), bwr)
    C.load("pool", wqb[:], w_q_b.rearrange("(k p) n -> p k n", p=128), bwqb)
    C.load("pool", wkvb[:], w_kv_b.rearrange("(k p) n -> p k n", p=128), bwkvb)

    P.op("pool", lambda e: e.memset(qrT[:], 0.0), writes=[bqrT])
    P.op("pool", lambda e: e.memset(krT[:], 0.0), writes=[bkrT])
    if stop <= 0:
        C.finish()
        return nc
    ws_i = [0]

    def next_ws(src_cols_ap, ncols):
        i = ws_i[0] % NWS
        ws_i[0] += 1
        t, b = ws[i]
        C.load("pool", t[:, :, 0:ncols], src_cols_ap.rearrange("(k p) n -> p k n", p=128), b)
        return t, b

    pa_i = [0]

    def next_pA():
        i = pa_i[0] % 2
        pa_i[0] += 1
        return pA[i]

    stg_i = [0]

    def next_stg():
        i = stg_i[0] % 2
        stg_i[0] += 1
        return stg[i]

    SC_NA = 128 ** -0.5
    SC_MLA = 192 ** -0.5

    def rmsnorm_tile(src, bsrc, col):
        n = src.shape[-1] if len(src.shape) == 2 else None
        C.act(hb[:, 0:src.shape[1]], src, ACTF.Square, [bsrc], [bhb, bst_ss], accum_out=st_ss[:, col:col + 1])

    for b in range(NB):
        r0 = b * 256
        for t in range(2):
            x_t, bx = xt[t]
            C.load("sp", x_t[:], xp[r0 + t * 128: r0 + (t + 1) * 128, :], bx)
            C.act(hb[:], x_t[:], ACTF.Square, [bx], [bhb, bst_ss], accum_out=st_ss[:, 0:1])
            rstd_from_ss(C, st_r[:, 0:1], st_ss[:, 0:1], D, bst_ss, bst_r)
            C.stt(tmpf[:], x_t[:], st_r[:, 0:1], G1[:], ALU.mult, ALU.mult, [bx, bst_r, bG1], [btmpf])
            C.tt("dve", hb[:], tmpf[:], SA[:], ALU.add, [btmpf, bSA], [bhb])
            for k in range(16):
                C.tr(pT[:, k * 128:(k + 1) * 128], hb[:, k * 128:(k + 1) * 128], idB[:], [bhb, bidB], [bpT])
            C.copy("act", hT[:, :, t * 128:(t + 1) * 128], pT[:].rearrange("p (k c) -> p k c", k=16), [bpT], [bhT])

        if stop <= 1:
            continue
        for s in range(4):
            wt, bw = next_ws(w_in[:, s * 256:(s + 1) * 256], 256)
            for hh in range(2):
                head = s * 2 + hh
                pa, bpa = next_pA()
                for k in range(16):
                    C.mm(pa[:, 0:256], wt[:, k, hh * 128:(hh + 1) * 128], hT[:, k, :], k == 0, k == 15, [bw, bhT], [bpa])
                C.act(QT[:, head, :], pa[:, 0:256], ACTF.Copy, [bpa], [bQT], scale=SC_NA)
        if stop == 2 and sub <= 0:
            continue
        for which, (obuf, sb_t, sb_b) in enumerate(((o_nak, Kb, bKb), (o_nav, Vb, bVb))):
            for s in range(4):
                c0 = 1024 * (1 + which) + s * 256
                wt, bw = next_ws(w_in[:, c0:c0 + 256], 256)
                for t in range(2):
                    pa, bpa = next_pA()
                    for k in range(16):
                        C.mm(pa[:, 0:256], hT[:, k, t * 128:(t + 1) * 128], wt[:, k, :], k == 0, k == 15, [bw, bhT], [bpa])
                    sg, bsg = next_stg()
                    C.copy("dve", sg[:], pa[:, 0:256], [bpa], [bsg])
                    C.copy("act", sb_t[:, t, s * 256:(s + 1) * 256], pa[:, 0:256], [bpa], [sb_b])
                    C.store("sp", obuf[r0 + t * 128: r0 + (t + 1) * 128, s * 256:(s + 1) * 256], sg[:], bsg)
        if stop == 2 and sub <= 1:
            continue
        for s in range(2):
            c0 = 3072 + s * 256
            wt, bw = next_ws(w_in[:, c0:c0 + 256], 256)
            for t in range(2):
                pa, bpa = next_pA()
                for k in range(16):
                    C.mm(pa[:, 0:256], hT[:, k, t * 128:(t + 1) * 128], wt[:, k, :], k == 0, k == 15, [bw, bhT], [bpa])
                C.copy("dve", qaf[:, t, s * 256:(s + 1) * 256], pa[:, 0:256], [bpa], [bqaf])
        for t in range(2):
            C.act(hb[:, 0:512], qaf[:, t, :], ACTF.Square, [bqaf], [bhb, bst_ss], accum_out=st_ss[:, 1:2])
            rstd_from_ss(C, st_r[:, 1:2], st_ss[:, 1:2], 512, bst_ss, bst_r)
            C.stt(qan[:, t, :], qaf[:, t, :], st_r[:, 1:2], gqa[:], ALU.mult, ALU.mult, [bqaf, bst_r, bgqa], [bqan])
        if stop == 2 and sub <= 2:
            continue
        wt, bw = next_ws(w_in[:, 3584:3840], 256)
        for t in range(2):
            pa, bpa = next_pA()
            for k in range(16):
                C.mm(pa[:, 0:256], hT[:, k, t * 128:(t + 1) * 128], wt[:, k, :], k == 0, k == 15, [bw, bhT], [bpa])
            C.copy("dve", kvaf[:, t, :], pa[:, 0:256], [bpa], [bkvaf])
            C.act(hb[:, 0:256], kvaf[:, t, :], ACTF.Square, [bkvaf], [bhb, bst_ss], accum_out=st_ss[:, 2:3])
            rstd_from_ss(C, st_r[:, 2:3], st_ss[:, 2:3], 256, bst_ss, bst_r)
            C.stt(ckvf[:, t, :], kvaf[:, t, :], st_r[:, 2:3], gkva[:], ALU.mult, ALU.mult, [bkvaf, bst_r, bgkva], [bckvf])
            C.copy("act", ckvb[:, t, :], ckvf[:, t, :], [bckvf], [bckvb])
        C.store("sp", o_ckv[r0:r0 + 256, :].rearrange("(t p) c -> p t c", p=128), ckvf[:], bckvf)
        if stop == 2 and sub <= 3:
            continue
        wt, bw = next_ws(w_in[:, 3840:3904], 64)
        for t in range(2):
            pa, bpa = next_pA()
            for k in range(16):
                C.mm(pa[:, 0:64], hT[:, k, t * 128:(t + 1) * 128], wt[:, k, 0:64], k == 0, k == 15, [bw, bhT], [bpa])
            C.copy("dve", krf[:, t, :], pa[:, 0:64], [bpa], [bkrf])
            C.copy("act", krb[:, t, :], pa[:, 0:64], [bpa], [bkrb])
        C.store("sp", o_krope[r0:r0 + 256, :].rearrange("(t p) c -> p t c", p=128), krf[:], bkrf)

        if stop <= 2:
            continue
        for t in range(2):
            for hd in range(8):
                C.tr(pT[:, hd * 128:(hd + 1) * 128], Kb[:, t, hd * 128:(hd + 1) * 128], idB[:], [bKb, bidB], [bpT])
            C.copy("act", KT[:, :, t * 128:(t + 1) * 128], pT[:, 0:1024].rearrange("p (k c) -> p k c", k=8), [bpT], [bKT])
        for t in range(2):
            for c in range(4):
                C.tr(pT[:, c * 128:(c + 1) * 128], qan[:, t, c * 128:(c + 1) * 128], idB[:], [bqan, bidB], [bpT])
            for c in range(2):
                C.tr(pT[:, (4 + c) * 128:(5 + c) * 128], ckvb[:, t, c * 128:(c + 1) * 128], idB[:], [bckvb, bidB], [bpT])
            C.tr(pT[0:64, 6 * 128:7 * 128], krb[:, t, :], idB[:], [bkrb, bidB], [bpT])
            C.copy("act", qanT[:, :, t * 128:(t + 1) * 128], pT[:, 0:512].rearrange("p (k c) -> p k c", k=4), [bpT], [bqanT])
            C.copy("dve", ckvT[:, :, t * 128:(t + 1) * 128], pT[:, 512:768].rearrange("p (k c) -> p k c", k=2), [bpT], [bckvT])
            C.copy("dve", krT[0:64, t * 128:(t + 1) * 128], pT[0:64, 768:896], [bpT], [bkrT])
        for hd in range(8):
            pa, bpa = next_pA()
            for c in range(4):
                C.mm(pa[:, 0:256], wqb[:, c, hd * 192: hd * 192 + 128], qanT[:, c, :], c == 0, c == 3, [bwqb, bqanT], [bpa])
            C.act(qnT[:, hd, :], pa[:, 0:256], ACTF.Copy, [bpa], [bqnT], scale=SC_MLA)
            pa, bpa = next_pA()
            for c in range(4):
                C.mm(pa[0:64, 0:256], wqb[:, c, hd * 192 + 128: hd * 192 + 192], qanT[:, c, :], c == 0, c == 3, [bwqb, bqanT], [bpa])
            C.act(qrT[0:64, hd, :], pa[0:64, 0:256], ACTF.Copy, [bpa], [bqrT], scale=SC_MLA)
            pa, bpa = next_pA()
            for c in range(2):
                C.mm(pa[:, 0:256], wkvb[:, c, hd * 256: hd * 256 + 128], ckvT[:, c, :], c == 0, c == 1, [bwkvb, bckvT], [bpa])
            C.copy("dve", knT[:, hd, :], pa[:, 0:256], [bpa], [bknT])
        for t in range(2):
            for half in range(2):
                pa, bpa = next_pA()
                for c in range(2):
                    rhs = wkvb[:, c, :].rearrange("p (h x) -> p h x", h=8)[:, half * 4:(half + 1) * 4, 128:256]
                    C.mm(pa[:, 0:512], ckvT[:, c, t * 128:(t + 1) * 128], rhs, c == 0, c == 1, [bwkvb, bckvT], [bpa])
                C.copy("act", vm[:, t, half * 512:(half + 1) * 512], pa[:, 0:512], [bpa], [bvm])

        if stop <= 3:
            continue
        ai = [0]

        def attend(t, score_ops, vtile, bv, vcol, ocol):
            i = ai[0] % 2
            ai[0] += 1
            ps, bps = pS[i]
            pb, bpb = Pb[i]
            ptt, bptt = PT[i]
            am, bam = amx[i]
            n = len(score_ops)
            for j, (lt, rh, rd) in enumerate(score_ops):
                C.mm(ps[:, 0:256], lt, rh, j == 0, j == n - 1, rd, [bps])
            P.op("dve", lambda e: e.tensor_reduce(out=am[:, 0:1], in_=ps[:, 0:256], axis=AX.X, op=ALU.max, negate=True),
                 reads=[bps], writes=[bam])
            C.act(pb[:], ps[:, 0:256], ACTF.Exp, [bps, bam], [bpb, bam], bias=am[:, 0:1], scale=1.0, accum_out=am[:, 1:2])
            P.op("dve", lambda e: e.reciprocal(out=am[:, 2:3], in_=am[:, 1:2]), reads=[bam], writes=[bam])
            for j in range(2):
                C.tr(pTp_full[:, j, :], pb[:, j * 128:(j + 1) * 128], idB[:], [bpb, bidB], [bpTp])
            C.copy("dve", ptt[:], pTp_full[:, 0:2, :], [bpTp], [bptt])
            for j in range(2):
                C.mm(pO[:, 0:128], ptt[:, j, :], vtile[:, j, vcol:vcol + 128], j == 0, j == 1, [bptt, bv], [bpO])
            C.ts("dve", ocat[:, t, ocol:ocol + 128], pO[:, 0:128], am[:, 2:3], None, ALU.mult, None, [bpO, bam], [bocat])

        for hd in range(8):
            for t in range(2):
                attend(t, [(QT[:, hd, t * 128:(t + 1) * 128], KT[:, hd, :], [bQT, bKT])], Vb, bVb, hd * 128, hd * 128)
        for hd in range(8):
            for t in range(2):
                attend(t, [(qnT[:, hd, t * 128:(t + 1) * 128], knT[:, hd, :], [bqnT, bknT]),
                           (qrT[:, hd, t * 128:(t + 1) * 128], krT[:, :], [bqrT, bkrT])], vm, bvm, hd * 128, 1024 + hd * 128)

        if not do_tail:
            for t in range(2):
                C.copy("dve", h2b[:], ocat[:, t, :], [bocat], [bh2b])
                C.store("sp", o_h2[r0 + t * 128: r0 + (t + 1) * 128, :], h2b[:], bh2b)
            continue

        for t in range(2):
            for k in range(16):
                C.tr(pT[:, k * 128:(k + 1) * 128], ocat[:, t, k * 128:(k + 1) * 128], idB[:], [bocat, bidB], [bpT])
            C.copy("act", ocT[:, :, t * 128:(t + 1) * 128], pT[:].rearrange("p (k c) -> p k c", k=16), [bpT], [bocT])
        for s in range(8):
            wt, bw = next_ws(w_out[:, s * 256:(s + 1) * 256], 256)
            for t in range(2):
                x_t, bx = xt[t]
                pa, bpa = next_pA()
                for k in range(16):
                    C.mm(pa[:, 0:256], ocT[:, k, t * 128:(t + 1) * 128], wt[:, k, :], k == 0, k == 15, [bw, bocT], [bpa])
                sg, bsg = next_stg()
                C.tt("dve", sg[:], pa[:, 0:256], GA[:, s * 256:(s + 1) * 256], ALU.mult, [bpa, bGA], [bsg])
                C.tt("dve", x_t[:, s * 256:(s + 1) * 256], x_t[:, s * 256:(s + 1) * 256], sg[:], ALU.add, [bx, bsg], [bx])
        for t in range(2):
            x_t, bx = xt[t]
            C.store("sp", o_x1[r0 + t * 128: r0 + (t + 1) * 128, :], x_t[:], bx)
            C.act(hb[:], x_t[:], ACTF.Square, [bx], [bhb, bst_ss], accum_out=st_ss[:, 3:4])
            rstd_from_ss(C, st_r[:, 3:4], st_ss[:, 3:4], D, bst_ss, bst_r)
            C.stt(tmpf[:], x_t[:], st_r[:, 3:4], G2[:], ALU.mult, ALU.mult, [bx, bst_r, bG2], [btmpf])
            C.tt("dve", h2f[:], tmpf[:], SF[:], ALU.add, [btmpf, bSF], [bh2f])
            C.copy("act", h2b[:], h2f[:], [bh2f], [bh2b])
            C.store("sp", o_h2[r0 + t * 128: r0 + (t + 1) * 128, :], h2b[:], bh2b)
            for g4 in range(4):
                pa, bpa = next_pA()
                for j in range(4):
                    k = g4 * 4 + j
                    C.tr(pa[:, j * 128:(j + 1) * 128], h2f[:, k * 128:(k + 1) * 128], idF[:], [bh2f, bidF], [bpa])
                C.copy("dve", h2T[:, g4 * 4:(g4 + 1) * 4, :], pa[:, 0:512].rearrange("p (k c) -> p k c", k=4), [bpa], [bh2T])
            pa, bpa = next_pA()
            for k in range(16):
                C.mm(pa[:, 0:32], h2T[:, k, :], wr[:, k, :], k == 0, k == 15, [bh2T, bwr], [bpa])
            C.tt("dve", lg[:], pa[:, 0:32], brt[:], ALU.add, [bpa, bbrt], [blg])
            P.op("dve", lambda e: e.max(out=top8[:], in_=lg[:]), reads=[blg], writes=[btop8])
            C.ts("dve", lg2[:], lg[:], top8[:, 3:4], None, ALU.is_ge, None, [blg, btop8], [blg2])
            C.ts("dve", top8[:, 7:8], top8[:, 0:1], -1.0, None, ALU.mult, None, [btop8], [btop8])
            C.act(lg[:], lg[:], ACTF.Exp, [blg, btop8], [blg], bias=top8[:, 7:8], scale=1.0)
            C.tt("dve", lg[:], lg[:], lg2[:], ALU.mult, [blg, blg2], [blg])
            P.op("dve", lambda e: e.tensor_reduce(out=top8[:, 6:7], in_=lg[:], axis=AX.X, op=ALU.add), reads=[blg], writes=[btop8])
            P.op("dve", lambda e: e.reciprocal(out=top8[:, 6:7], in_=top8[:, 6:7]), reads=[btop8], writes=[btop8])
            C.ts("dve", lg2[:], lg[:], top8[:, 6:7], None, ALU.mult, None, [blg, btop8], [blg2])
            C.store("sp", o_G[r0 + t * 128: r0 + (t + 1) * 128, :], lg2[:], blg2)
    C.finish()
    return nc


def build_mod(nc, NCOL=1536, C=None):
    C = C or Ctx(nc)
    P = C.P
    NR = 2 if C.fused else 3
    cT = C.din("cT", [128, 16, NR])
    wm = C.din("wm", [D, NCOL])
    bm = C.din("bm", [1, NCOL])
    o_m = C.dout("o_m", [NR, NCOL])
    cTf, bcTf = C.T("cTf", [128, 16, NR], F32)
    cTb, bcTb = C.T("cTb", [128, 16, NR], BF16)
    wmb = [C.T(f"wmb{i}", [128, 16, 512], BF16) for i in range(3)]
    bmt = [C.T(f"bmt{i}", [NR, 512], F32) for i in range(2)]
    ot = [C.T(f"ot{i}", [NR, 512], F32) for i in range(2)]
    pm = [C.PS(f"pm{i}", [128, 512], F32) for i in range(2)]
    C.load("sp", cTf[:], cT, bcTf)
    C.act(cTb[:], cTf[:], ACTF.Silu, [bcTf], [bcTb])
    for n in range(NCOL // 512):
        pa, bpa = pm[n % 2]
        wt, bw = wmb[n % 3]
        bt, bbt = bmt[n % 2]
        o_t, bo = ot[n % 2]
        C.load("pool", wt[:], wm[:, n * 512:(n + 1) * 512].rearrange("(k p) n -> p k n", p=128), bw)
        C.load("sp", bt[:], bm[0:1, n * 512:(n + 1) * 512].partition_broadcast(NR), bbt)
        for k in range(16):
            C.mm(pa[0:NR, :], cTb[:, k, :], wt[:, k, :], k == 0, k == 15, [bcTb, bw], [bpa])
        C.tt("dve", o_t[:], pa[0:NR, :], bt[:], ALU.add, [bpa, bbt], [bo])
        C.store("sp", o_m[:, n * 512:(n + 1) * 512], o_t[:], bo)
    C.finish()
    return nc


def build_experts(nc, NTOK=16384, NE=4, C=None):
    C = C or Ctx(nc)
    P = C.P
    TB = 512
    NBLK = NTOK // TB
    if C.fused:
        h2_d = C.dram["h2_d"]
        Gl = C.dram["G_d"]
        idB, bidB, idF, bidF = C.consts
    else:
        h2T = C.din("h2T", [D, NTOK], BF16)
        Gl = C.din("Gl", [NTOK, NE])
        GlT = C.din("GlT", [NE, NTOK])
    w_gu = C.din("w_gu", [NE, D, 4096])
    b_gu = C.din("b_gu", [128, NE, 16, 2])
    w_dn = C.din("w_dn", [NE, D, D])
    b_dn = C.din("b_dn", [NE, D])
    o_y = C.dout("o_y", [NTOK, D], BF16)

    hTb = [C.T(f"hTb{i}", [128, 16, TB], BF16) for i in range(1)]
    wgu = [C.T(f"wgu{i}", [128, 16, 256], BF16) for i in range(2)]
    sgu = [C.T(f"sgu{i}", [128, 16, 256], F32) for i in range(2)]
    wdn = [C.T(f"wdn{i}", [128, 16, 512], BF16) for i in range(2)]
    sdn = [C.T(f"sdn{i}", [128, 16, 256], F32) for i in range(1)]
    actb = [C.T(f"actb{i}", [128, 16, TB], BF16) for i in range(1)]
    yacc, byacc = C.T("yacc", [128, 4, D], F32)
    yout = [C.T(f"yout{i}", [128, D], BF16) for i in range(2)]
    GTb = [C.T(f"GTb{i}", [NE, TB], F32) for i in range(2)]
    bdn4, bbdn4 = C.T("bdn4", [NE, D], F32)
    Gt, bGt = C.T("Gt", [128, NBLK * 4, NE], F32)
    bgu, bbgu = C.T("bgu", [128, NE, 16, 2], F32)
    gg = [C.T(f"gg{i}", [128, TB], F32) for i in range(2)]
    ss_ = [C.T(f"ss{i}", [128, TB], F32) for i in range(2)]
    uu = [C.T(f"uu{i}", [128, TB], F32) for i in range(2)]
    pg = [C.PS(f"pg{i}", [128, 512], F32) for i in range(2)]
    pu = [C.PS(f"pu{i}", [128, 512], F32) for i in range(2)]
    NPD = 2 if C.fused else 4
    pd = [C.PS(f"pd{i}", [128, 512], F32) for i in range(NPD)]
    if C.fused:
        pTx, bpTx = C.PS("pTx", [128, 2048], BF16)
        h2t = [C.T(f"h2t{i}", [128, D], BF16) for i in range(2)]

    C.load("sp", Gt[:], Gl.rearrange("(t p) e -> p t e", p=128), bGt)
    C.load("sp", bgu[:], b_gu, bbgu)
    C.load("sp", bdn4[:], b_dn, bbdn4)

    cnt = dict(wgu=0, wdn=0, act=0, pg=0, pd=0, ew=0, sgu=0, sdn=0)
    for blk in range(NBLK):
        hT_t, bhT_ = hTb[0]
        gT, bgT = GTb[blk % 2]
        if C.fused:
            for tt in range(4):
                h_t, bh_ = h2t[tt % 2]
                C.load("sp", h_t[:], h2_d[blk * TB + tt * 128: blk * TB + (tt + 1) * 128, :], bh_)
                for k in range(16):
                    C.tr(pTx[:, k * 128:(k + 1) * 128], h_t[:, k * 128:(k + 1) * 128], idB[:], [bh_, bidB], [bpTx])
                C.copy("act", hT_t[:, :, tt * 128:(tt + 1) * 128], pTx[:].rearrange("p (k c) -> p k c", k=16), [bpTx], [bhT_])
            pdt, bpd = pd[cnt["pd"] % NPD]
            cnt["pd"] += 1
            for tt in range(4):
                C.tr(pdt[0:NE, tt * 128:(tt + 1) * 128], Gt[:, blk * 4 + tt, :], idF[:], [bGt, bidF], [bpd])
            C.copy("dve", gT[:], pdt[0:NE, :], [bpd], [bgT])
        else:
            C.load("sp", hT_t[:], h2T[:, blk * TB:(blk + 1) * TB].rearrange("(k p) n -> p k n", p=128), bhT_)
            C.load("sp", gT[:], GlT[:, blk * TB:(blk + 1) * TB], bgT)
        for dc in range(4):
            for tt in range(4):
                pdt, bpd = pd[cnt["pd"] % NPD]
                cnt["pd"] += 1
                C.mm(pdt[:, :], gT[:, tt * 128:(tt + 1) * 128], bdn4[:, dc * 512:(dc + 1) * 512], True, True, [bgT, bbdn4], [bpd])
                C.copy("act", yacc[:, tt, dc * 512:(dc + 1) * 512], pdt[:, :], [bpd], [byacc])
        for e in range(NE):
            a_t, ba = actb[0]
            for ffc in range(16):
                wt, bw = wgu[cnt["wgu"] % 2]
                cnt["wgu"] += 1
                sg_, bsg_ = sgu[cnt["sgu"] % 2]
                cnt["sgu"] += 1
                C.load("sp", sg_[:], w_gu[e, :, ffc * 256:(ffc + 1) * 256].rearrange("(k p) n -> p k n", p=128), bsg_)
                C.copy("act", wt[:], sg_[:], [bsg_], [bw])
                i = cnt["pg"] % 2
                cnt["pg"] += 1
                pgt, bpg = pg[i]
                put, bpu = pu[i]
                for k in range(16):
                    C.mm(pgt[:, 0:TB], wt[:, k, 0:256:2], hT_t[:, k, :], k == 0, k == 15, [bw, bhT_], [bpg])
                for k in range(16):
                    C.mm(put[:, 0:TB], wt[:, k, 1:256:2], hT_t[:, k, :], k == 0, k == 15, [bw, bhT_], [bpu])
                j = cnt["ew"] % 2
                cnt["ew"] += 1
                g_t, bg = gg[j]
                s_t, bs = ss_[j]
                u_t, bu = uu[j]
                C.ts("dve", g_t[:], pgt[:, 0:TB], bgu[:, e, ffc, 0:1], 7.0, ALU.add, ALU.min, [bpg, bbgu], [bg])
                C.act(s_t[:], g_t[:], ACTF.Sigmoid, [bg], [bs], scale=1.702)
                C.ts("dve", u_t[:], put[:, 0:TB], bgu[:, e, ffc, 1:2], 7.0, ALU.add, ALU.min, [bpu, bbgu], [bu])
                C.ts("pool", u_t[:], u_t[:], -7.0, 1.0, ALU.max, ALU.add, [bu], [bu])
                C.tt("pool", g_t[:], g_t[:], s_t[:], ALU.mult, [bg, bs], [bg])
                C.tt("dve", a_t[:, ffc, :], g_t[:], u_t[:], ALU.mult, [bg, bu], [ba])
            for dc in range(4):
                wd, bwd = wdn[cnt["wdn"] % 2]
                cnt["wdn"] += 1
                for hf in range(2):
                    sd_, bsd_ = sdn[0]
                    c0_ = dc * 512 + hf * 256
                    C.load("sp", sd_[:], w_dn[e, :, c0_:c0_ + 256].rearrange("(k p) n -> p k n", p=128), bsd_)
                    C.copy("act", wd[:, :, hf * 256:(hf + 1) * 256], sd_[:], [bsd_], [bwd])
                for tt in range(4):
                    pdt, bpd = pd[cnt["pd"] % NPD]
                    cnt["pd"] += 1
                    for ffc in range(16):
                        C.mm(pdt[:, :], a_t[:, ffc, tt * 128:(tt + 1) * 128], wd[:, ffc, :], ffc == 0, ffc == 15, [ba, bwd], [bpd])
                    gsc = Gt[:, blk * 4 + tt, e:e + 1]
                    ysl = yacc[:, tt, dc * 512:(dc + 1) * 512]
                    C.stt(ysl, pdt[:, :], gsc, ysl, ALU.mult, ALU.add, [bpd, bGt, byacc], [byacc])
        for tt in range(4):
            yo, byo = yout[tt % 2]
            C.copy("act", yo[:], yacc[:, tt, :], [byacc], [byo])
            C.store("sp", o_y[blk * TB + tt * 128: blk * TB + (tt + 1) * 128, :], yo[:], byo)
    C.finish()
    return nc


def build_combine(nc, NT=2048, NP=8, C=None):
    C = C or Ctx(nc)
    P = C.P
    x1 = C.din("x1", [NT, D])
    yp = C.din("yp", [NP, NT, D], BF16)
    gf = C.din("gf", [2, D])
    g_final = C.din("g_final", [1, D])
    o_y = C.dout("o_y", [NT, D])
    GF = [C.T(f"GF{i}", [128, D], F32) for i in range(2)]
    gfin, bgfin = C.T("gfin", [128, D], F32)
    xt = [C.T(f"xt{i}", [128, D], F32) for i in range(2)]
    ypt = [C.T(f"ypt{i}", [128, NP, D], BF16) for i in range(2)]
    acc, bacc_ = C.T("acc", [128, D], F32)
    junk, bjunk = C.T("junk", [128, D], BF16)
    ot = [C.T(f"ot{i}", [128, D], F32) for i in range(2)]
    st, bst = C.T("st", [128, 4], F32)
    for i in range(2):
        C.load("sp", GF[i][0][:], gf[i:i + 1, :].partition_broadcast(128), GF[i][1])
    C.load("sp", gfin[:], g_final[0:1, :].partition_broadcast(128), bgfin)
    ntile = NT // 128
    for t in range(ntile):
        x_t, bx = xt[t % 2]
        y_t, by = ypt[t % 2]
        o_t, bo = ot[t % 2]
        GFt, bGF = GF[0] if t < ntile // 2 else GF[1]
        C.load("sp", x_t[:], x1[t * 128:(t + 1) * 128, :], bx)
        C.load("sp", y_t[:], yp[:, t * 128:(t + 1) * 128, :].rearrange("j p d -> p j d"), by)
        if NP == 1:
            C.copy("dve", acc[:], y_t[:, 0, :], [by], [bacc_])
        else:
            C.tt("dve", acc[:], y_t[:, 0, :], y_t[:, 1, :], ALU.add, [by], [bacc_])
        for j in range(2, NP):
            C.tt("dve", acc[:], acc[:], y_t[:, j, :], ALU.add, [by, bacc_], [bacc_])
        C.tt("dve", acc[:], acc[:], GFt[:], ALU.mult, [bacc_, bGF], [bacc_])
        C.tt("dve", acc[:], acc[:], x_t[:], ALU.add, [bacc_, bx], [bacc_])
        C.act(junk[:], acc[:], ACTF.Square, [bacc_], [bjunk, bst], accum_out=st[:, 0:1])
        rstd_from_ss(C, st[:, 1:2], st[:, 0:1], D, bst, bst)
        C.stt(o_t[:], acc[:], st[:, 1:2], gfin[:], ALU.mult, ALU.mult, [bacc_, bst, bgfin], [bo])
        C.store("sp", o_y[t * 128:(t + 1) * 128, :], o_t[:], bo)
    C.finish()
    return nc


class Arena:
    def __init__(self, C, name, nbytes):
        self.t = C.P.sbuf(name, [128, nbytes // 2], BF16)
        self.cap = nbytes // 2
        self.off = 0
        self.peak = 0

    def mark(self):
        return self.off

    def reset(self, m):
        self.off = m

    def alloc(self, name, shape, dt):
        n = 1
        for d in shape[1:]:
            n *= d
        e16 = n * (2 if dt == F32 else 1)
        e16 = (e16 + 15) // 16 * 16
        assert self.off + e16 <= self.cap, (name, self.off, e16, self.cap)
        ap = self.t[:, self.off:self.off + e16]
        self.off += e16
        self.peak = max(self.peak, self.off)
        if dt == F32:
            ap = ap.bitcast(F32)
        ap = ap[:, 0:n]
        if len(shape) == 3:
            ap = ap.rearrange("p (a b) -> p a b", a=shape[1])
        elif len(shape) == 4:
            ap = ap.rearrange("p (a b c) -> p a b c", a=shape[1], b=shape[2])
        if shape[0] < 128:
            ap = ap[0:shape[0]]
        return ap, Buf(name)


def build_sample(nc, stop=99, NALLT=32, NPAIR=8, NHG=4, NMLAH=8, C=None):
    C = C or Ctx(nc)
    P = C.P
    x_own = C.din("x_own", [1024, D])
    x_halo = C.din("x_halo", [1792, D])
    x_all = C.din("x_all", [4096, D])
    ck_na = C.din("ck_na", [512, 1024])
    cv_na = C.din("cv_na", [512, 1024])
    c_ckv = C.din("c_ckv", [512, 256])
    c_krope = C.din("c_krope", [512, 64])
    mrow = C.din("mrow", [6, D])
    g_attn = C.din("g_attn", [1, D])
    g_ffn = C.din("g_ffn", [1, D])
    g_q_a = C.din("g_q_a", [1, 512])
    g_kv_a = C.din("g_kv_a", [1, 256])
    w_in = C.din("w_in", [D, 3904])
    w_in_rs = C.din("w_in_rs", [D, 64])
    w_q_b = C.din("w_q_b", [512, 1536])
    w_q_b_rs = C.din("w_q_b_rs", [512, 512])
    w_kv_b = C.din("w_kv_b", [256, 2048])
    w_out = C.din("w_out", [D, D])
    w_router = C.din("w_router", [D, 32])
    b_router = C.din("b_router", [1, 32])
    ident = C.din("ident", [128, 128])
    jmat = C.din("jmat", [128, 128])
    amat = C.din("amat", [2, 128])
    rmx = C.din("rmx", [8, 2, 896])
    colmask = C.din("colmask", [128, 15, 64])
    rpbpad = C.din("rpbpad", [8, 15, 160])
    cos_tok = C.din("cos_tok", [4096, 64])
    sinS_tok = C.din("sinS_tok", [4096, 64])
    cosT_own = C.din("cosT_own", [64, 1024])
    sinST_own = C.din("sinST_own", [64, 1024])
    o_x1 = C.dout("o_x1", [1024, D])
    o_h2 = C.dout("o_h2", [1024, D], BF16)
    o_G = C.dout("o_G", [1024, 32])
    ocat_d = C.scratch("ocat_d", [1024, D], BF16)
    bocat_d = Buf("ocat_d")

    SC_NA = 128 ** -0.5
    SC_MLA = 192 ** -0.5

    idF, bidF = C.T("idF", [128, 128], F32)
    idB, bidB = C.T("idB", [128, 128], BF16)
    jB, bjB = C.T("jB", [128, 128], BF16)
    aB, baB = C.T("aB", [2, 128], BF16)
    T0, bT0 = C.T("T0", [128, D], F32)
    T1, bT1 = C.T("T1", [128, D], F32)
    T2, bT2 = C.T("T2", [128, D], F32)
    gqa, bgqa = C.T("gqa", [128, 512], F32)
    gkva, bgkva = C.T("gkva", [128, 256], F32)
    brt, bbrt = C.T("brt", [128, 32], F32)
    wr, bwr = C.T("wr", [128, 16, 32], F32)
    ckvT, bckvT = C.T("ckvT", [128, 2, 4608], BF16)
    krT, bkrT = C.T("krT", [128, 4608], BF16)
    hTo, bhTo = C.T("hTo", [128, 16, 1024], BF16)
    xt, bxt = C.T("xt", [128, D], F32)
    tmpf, btmpf = C.T("tmpf", [128, D], F32)
    hb, bhb = C.T("hb", [128, D], BF16)
    hTt, bhTt = C.T("hTt", [128, 16, 128], BF16)
    st_ss, bst_ss = C.T("st_ss", [128, 8], F32)
    st_r, bst_r = C.T("st_r", [128, 8], F32)
    AR = C.arena if C.fused else Arena(C, "arena", 88 * 1024)

    pT, bpT = C.PS("pT", [128, 2048], BF16)
    pA = [C.PS(f"pA{i}", [128, 512], F32) for i in range(2)]
    pS = [C.PS(f"pS{i}", [128, 512], F32) for i in range(3)]
    pO, bpO = C.PS("pO", [128, 512], F32)

    pa_i = [0]

    def next_pA():
        i = pa_i[0] % 2
        pa_i[0] += 1
        return pA[i]

    def bload(dst, row_ap, buf, q="sp"):
        C.load(q, dst, row_ap.partition_broadcast(128), buf)

    C.load("sp", idF[:], ident, bidF)
    C.copy("dve", idB[:], idF[:], [bidF], [bidB])
    C.load("pool", jB[:], jmat, bjB)
    C.load("pool", aB[:], amat, baB)
    G1, bG1, SA, bSA = T0, bT0, T1, bT1
    bload(SA[:], mrow[0:1, :], bSA)
    bload(G1[:], mrow[1:2, :], bG1)
    bload(tmpf[:], g_attn[0:1, :], btmpf)
    C.stt(G1[:], G1[:], 1.0, tmpf[:], ALU.add, ALU.mult, [bG1, btmpf], [bG1])
    bload(gqa[:], g_q_a[0:1, :], bgqa)
    bload(gkva[:], g_kv_a[0:1, :], bgkva)
    bload(brt[:], b_router[0:1, :], bbrt)
    C.load("sp", wr[:], w_router.rearrange("(k p) n -> p k n", p=128), bwr)
    P.op("pool", lambda e: e.memset(krT[:], 0.0), writes=[bkrT])

    def make_h(src_rows):
        C.load("sp", xt[:], src_rows, bxt)
        C.act(hb[:], xt[:], ACTF.Square, [bxt], [bhb, bst_ss], accum_out=st_ss[:, 0:1])
        rstd_from_ss(C, st_r[:, 0:1], st_ss[:, 0:1], D, bst_ss, bst_r)
        C.stt(tmpf[:], xt[:], st_r[:, 0:1], G1[:], ALU.mult, ALU.mult, [bxt, bst_r, bG1], [btmpf])
        C.tt("dve", hb[:], tmpf[:], SA[:], ALU.add, [btmpf, bSA], [bhb])
        for k in range(16):
            C.tr(pT[:, k * 128:(k + 1) * 128], hb[:, k * 128:(k + 1) * 128], idB[:], [bhb, bidB], [bpT])

    m0 = AR.mark()
    w320, bw320 = AR.alloc("w320", [128, 16, 384], BF16)
    kvaf, bkvaf = AR.alloc("kvaf", [128, 256], F32)
    ckvb, bckvb = AR.alloc("ckvb", [128, 256], BF16)
    krb, bkrb = AR.alloc("krb", [128, 64], BF16)
    cst, bcst = AR.alloc("cst", [128, 2, 64], F32)
    kr1, bkr1 = AR.alloc("kr1", [128, 64], F32)
    kr2, bkr2 = AR.alloc("kr2", [128, 64], F32)
    ccf, bccf = AR.alloc("ccf", [128, 320], F32)
    C.load("pool", w320[:, :, 0:320], w_in[:, 3584:3904].rearrange("(k p) n -> p k n", p=128), bw320)
    C.load("pool", w320[:, :, 320:384], w_in_rs.rearrange("(k p) n -> p k n", p=128), bw320)
    for t in range(NALLT):
        make_h(x_all[t * 128:(t + 1) * 128, :])
        C.copy("act", hTt[:], pT[:].rearrange("p (k c) -> p k c", k=16), [bpT], [bhTt])
        C.load("sp", cst[:, 0, :], cos_tok[t * 128:(t + 1) * 128, :], bcst)
        C.load("sp", cst[:, 1, :], sinS_tok[t * 128:(t + 1) * 128, :], bcst)
        pa, bpa = next_pA()
        for k in range(16):
            C.mm(pa[:, 0:384], hTt[:, k, :], w320[:, k, :], k == 0, k == 15, [bhTt, bw320], [bpa])
        C.copy("dve", kvaf[:], pa[:, 0:256], [bpa], [bkvaf])
        C.tt("dve", kr1[:], pa[:, 256:320], cst[:, 0, :], ALU.mult, [bpa, bcst], [bkr1])
        C.tt("dve", kr2[:], pa[:, 320:384], cst[:, 1, :], ALU.mult, [bpa, bcst], [bkr2])
        C.tt("dve", krb[:], kr1[:], kr2[:], ALU.add, [bkr1, bkr2], [bkrb])
        C.act(hb[:, 0:256], kvaf[:], ACTF.Square, [bkvaf], [bhb, bst_ss], accum_out=st_ss[:, 2:3])
        rstd_from_ss(C, st_r[:, 2:3], st_ss[:, 2:3], 256, bst_ss, bst_r)
        C.stt(ckvb[:], kvaf[:], st_r[:, 2:3], gkva[:], ALU.mult, ALU.mult, [bkvaf, bst_r, bgkva], [bckvb])
        for c in range(2):
            C.tr(pT[:, c * 128:(c + 1) * 128], ckvb[:, c * 128:(c + 1) * 128], idB[:], [bckvb, bidB], [bpT])
        C.tr(pT[0:64, 256:384], krb[:], idB[:], [bkrb, bidB], [bpT])
        C.copy("act", ckvT[:, :, t * 128:(t + 1) * 128], pT[:, 0:256].rearrange("p (k c) -> p k c", k=2), [bpT], [bckvT])
        C.copy("dve", krT[0:64, t * 128:(t + 1) * 128], pT[0:64, 256:384], [bpT], [bkrT])
    for t in range(4):
        C.load("sp", ccf[:, 0:256], c_ckv[t * 128:(t + 1) * 128, :], bccf)
        C.load("sp", ccf[:, 256:320], c_krope[t * 128:(t + 1) * 128, :], bccf)
        C.copy("dve", hb[:, 0:320], ccf[:], [bccf], [bhb])
        for c in range(2):
            C.tr(pT[:, c * 128:(c + 1) * 128], hb[:, c * 128:(c + 1) * 128], idB[:], [bhb, bidB], [bpT])
        C.tr(pT[0:64, 256:384], hb[:, 256:320], idB[:], [bhb, bidB], [bpT])
        C.copy("act", ckvT[:, :, 4096 + t * 128:4096 + (t + 1) * 128], pT[:, 0:256].rearrange("p (k c) -> p k c", k=2), [bpT], [bckvT])
        C.copy("dve", krT[0:64, 4096 + t * 128:4096 + (t + 1) * 128], pT[0:64, 256:384], [bpT], [bkrT])
    P.barrier()
    AR.reset(m0)
    if stop <= 1:
        dbg = C.dout("dbg", [128, 3, 4608], BF16)
        C.store("sp", dbg[:, 0:2, :], ckvT[:], bckvT)
        C.store("sp", dbg[:, 2, :], krT[:], bkrT)
        C.finish()
        return nc

    for t in range(8):
        make_h(x_own[t * 128:(t + 1) * 128, :])
        C.copy("act", hTo[:, :, t * 128:(t + 1) * 128], pT[:].rearrange("p (k c) -> p k c", k=16), [bpT], [bhTo])

    m1 = AR.mark()
    Bstat, bBstat = AR.alloc("Bstat", [128, 8, 896], BF16)
    mB = AR.mark()
    Bfull, bBfull = AR.alloc("Bfull", [128, 8, 15, 64], F32)
    cmk, bcmk = AR.alloc("cmk", [128, 15, 64], F32)
    C.load("sp", cmk[:], colmask, bcmk)
    rp_t = rpbpad.tensor
    for hd in range(8):
        for half in range(2):
            src = bass.AP(rp_t, hd * 2400 + 16, [[1, 64], [160, 15], [1, 64]])
            C.load("sp", Bfull[half * 64:(half + 1) * 64, hd, :, :], src, bBfull)
    for hd in range(8):
        C.tt("dve", Bfull[:, hd, :, :], Bfull[:, hd, :, :], cmk[:], ALU.add, [bBfull, bcmk], [bBfull])
        C.copy("act", Bstat[0:64, hd, :].rearrange("p (j c) -> p j c", j=14), Bfull[0:64, hd, 1:15, :], [bBfull], [bBstat])
        C.copy("act", Bstat[64:128, hd, :].rearrange("p (j c) -> p j c", j=14), Bfull[64:128, hd, 0:14, :], [bBfull], [bBstat])
    P.barrier()
    AR.reset(mB)

    mG = AR.mark()
    for g in range(NHG):
        AR.reset(mG)
        wk, bwk = AR.alloc("wk", [128, 16, 256], BF16)
        wv, bwv = AR.alloc("wv", [128, 16, 256], BF16)
        wq, bwq = AR.alloc("wq", [128, 16, 256], BF16)
        KTh, bKTh = AR.alloc("KTh", [128, 2, 1792], BF16)
        Vh, bVh = AR.alloc("Vh", [128, 14, 256], BF16)
        cKT, bcKT = AR.alloc("cKT", [128, 2, 512], BF16)
        cV, bcV = AR.alloc("cV", [128, 4, 256], BF16)
        QTg, bQTg = AR.alloc("QTg", [128, 2, 1024], BF16)
        kbt, bkbt = AR.alloc("kbt", [128, 256], BF16)
        ccn, bccn = AR.alloc("ccn", [128, 2, 256], F32)
        ccb, bccb = AR.alloc("ccb", [128, 256], BF16)
        rmt = [AR.alloc(f"rmt{i}", [2, 896], BF16) for i in range(2)]
        Pb = [AR.alloc(f"Pb{i}", [128, 1408], BF16) for i in range(2)]
        PTt = [AR.alloc(f"PTt{i}", [128, 11, 128], BF16) for i in range(2)]
        otl = [AR.alloc(f"otl{i}", [128, 256], BF16) for i in range(2)]
        amx = [AR.alloc(f"amx{i}", [128, 8], F32) for i in range(2)]
        c0 = g * 256
        C.load("pool", wq[:], w_in[:, c0:c0 + 256].rearrange("(k p) n -> p k n", p=128), bwq)
        C.load("pool", wk[:], w_in[:, 1024 + c0:1024 + c0 + 256].rearrange("(k p) n -> p k n", p=128), bwk)
        C.load("pool", wv[:], w_in[:, 2048 + c0:2048 + c0 + 256].rearrange("(k p) n -> p k n", p=128), bwv)
        for t in range(14):
            make_h(x_halo[t * 128:(t + 1) * 128, :])
            C.copy("act", hTt[:], pT[:].rearrange("p (k c) -> p k c", k=16), [bpT], [bhTt])
            pa, bpa = next_pA()
            for k in range(16):
                C.mm(pa[:, 0:256], hTt[:, k, :], wk[:, k, :], k == 0, k == 15, [bhTt, bwk], [bpa])
            C.copy("act", kbt[:], pa[:, 0:256], [bpa], [bkbt])
            pa, bpa = next_pA()
            for k in range(16):
                C.mm(pa[:, 0:256], hTt[:, k, :], wv[:, k, :], k == 0, k == 15, [bhTt, bwv], [bpa])
            C.copy("dve", Vh[:, t, :], pa[:, 0:256], [bpa], [bVh])
            for hh in range(2):
                C.tr(pT[:, hh * 128:(hh + 1) * 128], kbt[:, hh * 128:(hh + 1) * 128], idB[:], [bkbt, bidB], [bpT])
            C.copy("act", KTh[:, :, t * 128:(t + 1) * 128], pT[:, 0:256].rearrange("p (k c) -> p k c", k=2), [bpT], [bKTh])
        for t in range(4):
            C.load("sp", ccn[:, 0, :], ck_na[t * 128:(t + 1) * 128, c0:c0 + 256], bccn)
            C.load("sp", ccn[:, 1, :], cv_na[t * 128:(t + 1) * 128, c0:c0 + 256], bccn)
            C.copy("dve", ccb[:], ccn[:, 0, :], [bccn], [bccb])
            C.copy("act", cV[:, t, :], ccn[:, 1, :], [bccn], [bcV])
            for hh in range(2):
                C.tr(pT[:, hh * 128:(hh + 1) * 128], ccb[:, hh * 128:(hh + 1) * 128], idB[:], [bccb, bidB], [bpT])
            C.copy("act", cKT[:, :, t * 128:(t + 1) * 128], pT[:, 0:256].rearrange("p (k c) -> p k c", k=2), [bpT], [bcKT])
        for hh in range(2):
            for half in range(2):
                pa, bpa = next_pA()
                for k in range(16):
                    C.mm(pa[:, :], wq[:, k, hh * 128:(hh + 1) * 128], hTo[:, k, half * 512:(half + 1) * 512], k == 0, k == 15, [bwq, bhTo], [bpa])
                C.act(QTg[:, hh, half * 512:(half + 1) * 512], pa[:, :], ACTF.Copy, [bpa], [bQTg], scale=SC_NA)
        ui = 0
        for p in range(NPAIR):
            rm_t, brm = rmt[p % 2]
            C.load("pool", rm_t[:], rmx[p], brm)
            ot_t, bot = otl[p % 2]
            for hh in range(2):
                hd = g * 2 + hh
                pb, bpb = Pb[ui % 2]
                ptt, bptt = PTt[ui % 2]
                am, bam = amx[ui % 2]
                ui += 1
                q_l = QTg[:, hh, p * 128:(p + 1) * 128]
                k0 = p * 128
                segs = [(pS[0], 0, 512), (pS[1], 512, 384)]
                for (ps, bps), o, n in segs:
                    C.mm(ps[:, 0:n], q_l, KTh[:, hh, k0 + o:k0 + o + n], True, False, [bQTg, bKTh], [bps])
                    C.mm(ps[:, 0:n], jB[:], Bstat[:, hd, o:o + n], False, False, [bjB, bBstat], [bps])
                    C.mm(ps[:, 0:n], aB[:], rm_t[:, o:o + n], False, True, [baB, brm], [bps])
                ps2, bps2 = pS[2]
                C.mm(ps2[:, 0:512], q_l, cKT[:, hh, :], True, True, [bQTg, bcKT], [bps2])
                for i, ((ps, bps), n) in enumerate(((pS[0], 512), (pS[1], 384), (pS[2], 512))):
                    P.op("dve", lambda e, ps=ps, n=n, i=i, am=am: e.tensor_reduce(out=am[:, i:i + 1], in_=ps[:, 0:n], axis=AX.X, op=ALU.max),
                         reads=[bps], writes=[bam])
                P.op("dve", lambda e, am=am: e.tensor_reduce(out=am[:, 3:4], in_=am[:, 0:3], axis=AX.X, op=ALU.max, negate=True), reads=[bam], writes=[bam])
                for i, ((ps, bps), o, n) in enumerate(((pS[0], 0, 512), (pS[1], 512, 384), (pS[2], 896, 512))):
                    C.act(pb[:, o:o + n], ps[:, 0:n], ACTF.Exp, [bps, bam], [bpb, bam], bias=am[:, 3:4], scale=1.0, accum_out=am[:, 4 + i:5 + i])
                P.op("dve", lambda e, am=am: e.tensor_reduce(out=am[:, 7:8], in_=am[:, 4:7], axis=AX.X, op=ALU.add), reads=[bam], writes=[bam])
                P.op("dve", lambda e, am=am: e.reciprocal(out=am[:, 7:8], in_=am[:, 7:8]), reads=[bam], writes=[bam])
                for j in range(11):
                    C.tr(pT[:, j * 128:(j + 1) * 128], pb[:, j * 128:(j + 1) * 128], idB[:], [bpb, bidB], [bpT])
                C.copy("act", ptt[:, 0:6, :], pT[:, 0:768].rearrange("p (k c) -> p k c", k=6), [bpT], [bptt])
                C.copy("dve", ptt[:, 6:11, :], pT[:, 768:1408].rearrange("p (k c) -> p k c", k=5), [bpT], [bptt])
                for j in range(7):
                    C.mm(pO[:, 0:128], ptt[:, j, :], Vh[:, p + j, hh * 128:(hh + 1) * 128], j == 0, False, [bptt, bVh], [bpO])
                for j in range(4):
                    C.mm(pO[:, 0:128], ptt[:, 7 + j, :], cV[:, j, hh * 128:(hh + 1) * 128], False, j == 3, [bptt, bcV], [bpO])
                C.ts("dve", ot_t[:, hh * 128:(hh + 1) * 128], pO[:, 0:128], am[:, 7:8], None, ALU.mult, None, [bpO, bam], [bot])
            P.dma("sp", lambda e, ot_t=ot_t, p=p, c0=c0: e.dma_start(out=ocat_d[p * 128:(p + 1) * 128, c0:c0 + 256], in_=ot_t[:]),
                  reads=[bot], writes=[bocat_d], key=bot, store=True)
        P.barrier()
    AR.reset(m1)
    if stop <= 2:
        C.finish()
        return nc

    qanT, bqanT = AR.alloc("qanT", [128, 4, 1024], BF16)
    mQ = AR.mark()
    wqa, bwqa = AR.alloc("wqa", [128, 16, 512], BF16)
    C.load("pool", wqa[:], w_in[:, 3072:3584].rearrange("(k p) n -> p k n", p=128), bwqa)
    qaf, bqaf = AR.alloc("qaf", [128, 512], F32)
    qan, bqan = AR.alloc("qan", [128, 512], BF16)
    for t in range(8):
        pa, bpa = next_pA()
        for k in range(16):
            C.mm(pa[:, :], hTo[:, k, t * 128:(t + 1) * 128], wqa[:, k, :], k == 0, k == 15, [bhTo, bwqa], [bpa])
        C.copy("dve", qaf[:], pa[:, :], [bpa], [bqaf])
        C.act(hb[:, 0:512], qaf[:], ACTF.Square, [bqaf], [bhb, bst_ss], accum_out=st_ss[:, 1:2])
        rstd_from_ss(C, st_r[:, 1:2], st_ss[:, 1:2], 512, bst_ss, bst_r)
        C.stt(qan[:], qaf[:], st_r[:, 1:2], gqa[:], ALU.mult, ALU.mult, [bqaf, bst_r, bgqa], [bqan])
        for c in range(4):
            C.tr(pT[:, c * 128:(c + 1) * 128], qan[:, c * 128:(c + 1) * 128], idB[:], [bqan, bidB], [bpT])
        C.copy("act", qanT[:, :, t * 128:(t + 1) * 128], pT[:, 0:512].rearrange("p (k c) -> p k c", k=4), [bpT], [bqanT])
    P.barrier()
    AR.reset(mQ)
    wqb, bwqb = AR.alloc("wqb", [128, 4, 1536], BF16)
    wqbr, bwqbr = AR.alloc("wqbr", [128, 4, 512], BF16)
    wkvb, bwkvb = AR.alloc("wkvb", [128, 2, 2048], BF16)
    csT, bcsT = AR.alloc("csT", [64, 2, 1024], F32)
    C.load("pool", wqb[:], w_q_b.rearrange("(k p) n -> p k n", p=128), bwqb)
    C.load("pool", wqbr[:], w_q_b_rs.rearrange("(k p) n -> p k n", p=128), bwqbr)
    C.load("pool", wkvb[:], w_kv_b.rearrange("(k p) n -> p k n", p=128), bwkvb)
    C.load("sp", csT[:, 0, :], cosT_own, bcsT)
    C.load("sp", csT[:, 1, :], sinST_own, bcsT)
    qnh, bqnh = AR.alloc("qnh", [128, 1024], BF16)
    qrh, bqrh = AR.alloc("qrh", [128, 1024], BF16)
    knh, bknh = AR.alloc("knh", [128, 4608], BF16)
    vh, bvh = AR.alloc("vh", [128, 36, 128], BF16)
    r1, br1 = AR.alloc("r1", [64, 512], F32)
    r2, br2 = AR.alloc("r2", [64, 512], F32)
    Pm = [AR.alloc(f"Pm{i}", [128, 512], BF16) for i in range(2)]
    PTm = [AR.alloc(f"PTm{i}", [128, 4, 128], BF16) for i in range(2)]
    om = [AR.alloc(f"om{i}", [128, 128], BF16) for i in range(2)]
    mst = [AR.alloc(f"mst{i}", [128, 24], F32) for i in range(2)]
    P.op("pool", lambda e: e.memset(qrh[:], 0.0), writes=[bqrh])
    ci = 0
    for hd in range(NMLAH):
        for half in range(2):
            sl = slice(half * 512, (half + 1) * 512)
            pa, bpa = next_pA()
            for c in range(4):
                C.mm(pa[:, :], wqb[:, c, hd * 192:hd * 192 + 128], qanT[:, c, sl], c == 0, c == 3, [bwqb, bqanT], [bpa])
            C.act(qnh[:, sl], pa[:, :], ACTF.Copy, [bpa], [bqnh], scale=SC_MLA)
            pa, bpa = next_pA()
            for c in range(4):
                C.mm(pa[0:64, :], wqb[:, c, hd * 192 + 128:hd * 192 + 192], qanT[:, c, sl], c == 0, c == 3, [bwqb, bqanT], [bpa])
            C.stt(r1[:], pa[0:64, :], SC_MLA, csT[:, 0, sl], ALU.mult, ALU.mult, [bpa, bcsT], [br1])
            pa, bpa = next_pA()
            for c in range(4):
                C.mm(pa[0:64, :], wqbr[:, c, hd * 64:(hd + 1) * 64], qanT[:, c, sl], c == 0, c == 3, [bwqbr, bqanT], [bpa])
            C.stt(r2[:], pa[0:64, :], SC_MLA, csT[:, 1, sl], ALU.mult, ALU.mult, [bpa, bcsT], [br2])
            C.tt("dve", qrh[0:64, sl], r1[:], r2[:], ALU.add, [br1, br2], [bqrh])
        for ch in range(9):
            pa, bpa = next_pA()
            for c in range(2):
                C.mm(pa[:, :], wkvb[:, c, hd * 256:hd * 256 + 128], ckvT[:, c, ch * 512:(ch + 1) * 512], c == 0, c == 1, [bwkvb, bckvT], [bpa])
            C.copy("act", knh[:, ch * 512:(ch + 1) * 512], pa[:, :], [bpa], [bknh])
        for kt4 in range(9):
            pa, bpa = next_pA()
            for j in range(4):
                kt = kt4 * 4 + j
                for c in range(2):
                    C.mm(pa[:, j * 128:(j + 1) * 128], ckvT[:, c, kt * 128:(kt + 1) * 128], wkvb[:, c, hd * 256 + 128:hd * 256 + 256], c == 0, c == 1, [bckvT, bwkvb], [bpa])
            C.copy("dve", vh[:, kt4 * 4:(kt4 + 1) * 4, :], pa[:, :].rearrange("p (k c) -> p k c", k=4), [bpa], [bvh])
        for qt in range(8):
            ms, bms = mst[qt % 2]
            o_t, bo = om[qt % 2]
            qs = slice(qt * 128, (qt + 1) * 128)
            for ch in range(9):
                ps, bps = pS[ch % 3]
                C.mm(ps[:, :], qnh[:, qs], knh[:, ch * 512:(ch + 1) * 512], True, False, [bqnh, bknh], [bps])
                C.mm(ps[:, :], qrh[:, qs], krT[:, ch * 512:(ch + 1) * 512], False, True, [bqrh, bkrT], [bps])
                P.op("dve", lambda e, ps=ps, ms=ms, ch=ch: e.tensor_reduce(out=ms[:, ch:ch + 1], in_=ps[:, :], axis=AX.X, op=ALU.max), reads=[bps], writes=[bms])
            P.op("dve", lambda e, ms=ms: e.tensor_reduce(out=ms[:, 9:10], in_=ms[:, 0:9], axis=AX.X, op=ALU.max, negate=True), reads=[bms], writes=[bms])
            for ch in range(9):
                ps, bps = pS[ch % 3]
                pm, bpm = Pm[ci % 2]
                ptm, bptm = PTm[ci % 2]
                ci += 1
                C.mm(ps[:, :], qnh[:, qs], knh[:, ch * 512:(ch + 1) * 512], True, False, [bqnh, bknh], [bps])
                C.mm(ps[:, :], qrh[:, qs], krT[:, ch * 512:(ch + 1) * 512], False, True, [bqrh, bkrT], [bps])
                C.act(pm[:], ps[:, :], ACTF.Exp, [bps, bms], [bpm, bms], bias=ms[:, 9:10], scale=1.0, accum_out=ms[:, 10 + ch:11 + ch])
                for j in range(4):
                    C.tr(pT[:, j * 128:(j + 1) * 128], pm[:, j * 128:(j + 1) * 128], idB[:], [bpm, bidB], [bpT])
                C.copy("act" if ch % 2 else "dve", ptm[:], pT[:, 0:512].rearrange("p (k c) -> p k c", k=4), [bpT], [bptm])
                for j in range(4):
                    kt = ch * 4 + j
                    C.mm(pO[:, 0:128], ptm[:, j, :], vh[:, kt, :], kt == 0, kt == 35, [bptm, bvh], [bpO])
            P.op("dve", lambda e, ms=ms: e.tensor_reduce(out=ms[:, 20:21], in_=ms[:, 10:19], axis=AX.X, op=ALU.add), reads=[bms], writes=[bms])
            P.op("dve", lambda e, ms=ms: e.reciprocal(out=ms[:, 20:21], in_=ms[:, 20:21]), reads=[bms], writes=[bms])
            C.ts("dve", o_t[:], pO[:, 0:128], ms[:, 20:21], None, ALU.mult, None, [bpO, bms], [bo])
            P.dma("sp", lambda e, o_t=o_t, qt=qt, hd=hd: e.dma_start(out=ocat_d[qt * 128:(qt + 1) * 128, 1024 + hd * 128:1024 + (hd + 1) * 128], in_=o_t[:]),
                  reads=[bo], writes=[bocat_d], key=bo, store=True)
    P.barrier()
    AR.reset(m1)
    if stop <= 3:
        C.finish()
        return nc

    GA, bGA, G2, bG2, SF, bSF = T0, bT0, T1, bT1, T2, bT2
    bload(GA[:], mrow[2:3, :], bGA)
    bload(SF[:], mrow[3:4, :], bSF)
    bload(G2[:], mrow[4:5, :], bG2)
    bload(tmpf[:], g_ffn[0:1, :], btmpf)
    C.stt(G2[:], G2[:], 1.0, tmpf[:], ALU.add, ALU.mult, [bG2, btmpf], [bG2])
    xt2 = [AR.alloc(f"xt2_{i}", [128, D], F32) for i in range(2)]
    oct_ = [AR.alloc(f"oct{i}", [128, D], BF16) for i in range(2)]
    ocT, bocT = AR.alloc("ocT", [128, 16, 256], BF16)
    ws = [AR.alloc(f"ws{i}", [128, 16, 256], BF16) for i in range(2)]
    stg = [AR.alloc(f"stg{i}", [128, 256], F32) for i in range(2)]
    h2f, bh2f = AR.alloc("h2f", [128, D], F32)
    h2b, bh2b = AR.alloc("h2b", [128, D], BF16)
    h2T, bh2T = AR.alloc("h2T", [128, 16, 128], F32)
    lg, blg = AR.alloc("lg", [128, 32], F32)
    lg2, blg2 = AR.alloc("lg2", [128, 32], F32)
    top8, btop8 = AR.alloc("top8", [128, 8], F32)
    wsi = [0]
    sgi = [0]
    for b in range(4):
        r0 = b * 256
        for t in range(2):
            x_t, bx = xt2[t]
            oc_t, boc = oct_[t]
            C.load("sp", x_t[:], x_own[r0 + t * 128:r0 + (t + 1) * 128, :], bx)
            P.dma("sp", lambda e, oc_t=oc_t, r0=r0, t=t: e.dma_start(out=oc_t[:], in_=ocat_d[r0 + t * 128:r0 + (t + 1) * 128, :]),
                  reads=[bocat_d], writes=[boc])
            for k in range(16):
                C.tr(pT[:, k * 128:(k + 1) * 128], oc_t[:, k * 128:(k + 1) * 128], idB[:], [boc, bidB], [bpT])
            C.copy("act", ocT[:, :, t * 128:(t + 1) * 128], pT[:].rearrange("p (k c) -> p k c", k=16), [bpT], [bocT])
        for s in range(8):
            wt, bw = ws[wsi[0] % 2]
            wsi[0] += 1
            C.load("pool", wt[:], w_out[:, s * 256:(s + 1) * 256].rearrange("(k p) n -> p k n", p=128), bw)
            for t in range(2):
                x_t, bx = xt2[t]
                pa, bpa = next_pA()
                for k in range(16):
                    C.mm(pa[:, 0:256], ocT[:, k, t * 128:(t + 1) * 128], wt[:, k, :], k == 0, k == 15, [bw, bocT], [bpa])
                sg, bsg = stg[sgi[0] % 2]
                sgi[0] += 1
                C.tt("dve", sg[:], pa[:, 0:256], GA[:, s * 256:(s + 1) * 256], ALU.mult, [bpa, bGA], [bsg])
                C.tt("dve", x_t[:, s * 256:(s + 1) * 256], x_t[:, s * 256:(s + 1) * 256], sg[:], ALU.add, [bx, bsg], [bx])
        for t in range(2):
            x_t, bx = xt2[t]
            C.store("sp", o_x1[r0 + t * 128:r0 + (t + 1) * 128, :], x_t[:], bx)
            C.act(hb[:], x_t[:], ACTF.Square, [bx], [bhb, bst_ss], accum_out=st_ss[:, 3:4])
            rstd_from_ss(C, st_r[:, 3:4], st_ss[:, 3:4], D, bst_ss, bst_r)
            C.stt(tmpf[:], x_t[:], st_r[:, 3:4], G2[:], ALU.mult, ALU.mult, [bx, bst_r, bG2], [btmpf])
            C.tt("dve", h2f[:], tmpf[:], SF[:], ALU.add, [btmpf, bSF], [bh2f])
            C.copy("act", h2b[:], h2f[:], [bh2f], [bh2b])
            C.store("sp", o_h2[r0 + t * 128:r0 + (t + 1) * 128, :], h2b[:], bh2b)
            for g4 in range(4):
                pa, bpa = next_pA()
                for j in range(4):
                    k = g4 * 4 + j
                    C.tr(pa[:, j * 128:(j + 1) * 128], h2f[:, k * 128:(k + 1) * 128], idF[:], [bh2f, bidF], [bpa])
                C.copy("dve", h2T[:, g4 * 4:(g4 + 1) * 4, :], pa[:, 0:512].rearrange("p (k c) -> p k c", k=4), [bpa], [bh2T])
            pa, bpa = next_pA()
            for k in range(16):
                C.mm(pa[:, 0:32], h2T[:, k, :], wr[:, k, :], k == 0, k == 15, [bh2T, bwr], [bpa])
            C.tt("dve", lg[:], pa[:, 0:32], brt[:], ALU.add, [bpa, bbrt], [blg])
            P.op("dve", lambda e: e.max(out=top8[:], in_=lg[:]), reads=[blg], writes=[btop8])
            C.ts("dve", lg2[:], lg[:], top8[:, 3:4], None, ALU.is_ge, None, [blg, btop8], [blg2])
            C.ts("dve", top8[:, 7:8], top8[:, 0:1], -1.0, None, ALU.mult, None, [btop8], [btop8])
            C.act(lg[:], lg[:], ACTF.Exp, [blg, btop8], [blg], bias=top8[:, 7:8], scale=1.0)
            C.tt("dve", lg[:], lg[:], lg2[:], ALU.mult, [blg, blg2], [blg])
            P.op("dve", lambda e: e.tensor_reduce(out=top8[:, 6:7], in_=lg[:], axis=AX.X, op=ALU.add), reads=[blg], writes=[btop8])
            P.op("dve", lambda e: e.reciprocal(out=top8[:, 6:7], in_=top8[:, 6:7]), reads=[btop8], writes=[btop8])
            C.ts("dve", lg2[:], lg[:], top8[:, 6:7], None, ALU.mult, None, [blg, btop8], [blg2])
            C.store("sp", o_G[r0 + t * 128:r0 + (t + 1) * 128, :], lg2[:], blg2)
    C.finish()
    return nc


def build_fused(nc):
    C = Ctx(nc, fused=True)
    P = C.P
    C.ps_pool = P.psum("pspool", [128, 4096], F32)[:]
    idF_t = P.sbuf("c_idF", [128, 128], F32)
    idB_t = P.sbuf("c_idB", [128, 128], BF16)
    bidF, bidB = Buf("c_idF"), Buf("c_idB")
    ident = C.din("ident", [128, 128])
    C.load("sp", idF_t[:], ident, bidF)
    C.copy("dve", idB_t[:], idF_t[:], [bidF], [bidB])
    C.consts = (idB_t, bidB, idF_t, bidF)
    C.arena = Arena(C, "arena", 206 * 1024)
    m_d = C.scratch("m_d", [2, 6 * D], F32)
    x1_d = C.scratch("x1_d", [2048, D], F32)
    h2_d = C.scratch("h2_d", [2048, D], BF16)
    G_d = C.scratch("G_d", [2048, 32], F32)
    y_d = C.scratch("y_d", [2048, D], BF16)

    def phase_end():
        P.barrier()
        P.recycle_dma_sems()
        C.arena.reset(0)
        C.ps_off = 0

    C.dram["o_m"] = m_d
    build_mod(nc, NCOL=6 * D, C=C)
    phase_end()
    C.dram["mrow"] = m_d[0:1, :].rearrange("o (s d) -> (o s) d", s=6)
    C.dram["o_x1"], C.dram["o_h2"], C.dram["o_G"] = x1_d[0:1024, :], h2_d[0:1024, :], G_d[0:1024, :]
    build_prompt(nc, NB=4, C=C)
    phase_end()
    C.dram["mrow"] = m_d[1:2, :].rearrange("o (s d) -> (o s) d", s=6)
    C.dram["o_x1"], C.dram["o_h2"], C.dram["o_G"] = x1_d[1024:2048, :], h2_d[1024:2048, :], G_d[1024:2048, :]
    build_sample(nc, C=C)
    phase_end()
    C.dram["h2_d"], C.dram["G_d"], C.dram["o_y"] = h2_d, G_d, y_d
    build_experts(nc, NTOK=2048, NE=32, C=C)
    phase_end()
    del C.dram["o_y"]
    C.dram["x1"] = x1_d
    C.dram["yp"] = y_d.rearrange("(o n) d -> o n d", o=1)
    C.dram["gf"] = m_d[:, 5 * D:6 * D]
    build_combine(nc, NT=2048, NP=1, C=C)
    P.emit()
    return nc


def rope_tables():
    T = 4096
    pos = np.arange(T); rows = (pos // 64).astype(np.float32); cols = (pos % 64).astype(np.float32)
    inv = (10000.0 ** (-(np.arange(16, dtype=np.float32) * 2.0 / 32))).astype(np.float32)
    ar = rows[:, None] * inv; ac = cols[:, None] * inv
    ang = np.concatenate([ar, ar, ac, ac], -1)
    cos, sin = np.cos(ang).astype(np.float32), np.sin(ang).astype(np.float32)
    sign = np.concatenate([-np.ones(16), np.ones(16), -np.ones(16), np.ones(16)]).astype(np.float32)
    return cos, sin * sign

SWAP = np.concatenate([np.arange(16, 32), np.arange(0, 16), np.arange(48, 64), np.arange(32, 48)])

def sample_consts(r0):
    ident = np.eye(128, dtype=np.float32)
    jmat = np.zeros((128, 128), np.float32)
    for p in range(128):
        jmat[p, (p // 64) * 64 + 63 - p % 64] = 1.0
    amat = np.zeros((2, 128), np.float32)
    amat[0, :64] = 1.0; amat[1, 64:] = 1.0
    rmx = np.zeros((8, 2, 14, 64), np.float32)
    for p in range(8):
        for rho in range(2):
            r = r0 + 2 * p + rho
            rs = min(max(r - 4, 0), 56)
            for j in range(14):
                kr = r0 + 2 * p - 6 + j
                ok = (0 <= kr < 64) and (rs <= kr < rs + 8)
                if not ok:
                    rmx[p, rho, j, :] = NEG
    colmask = np.zeros((128, 15, 64), np.float32)
    for pp in range(128):
        c = 63 - pp % 64
        cs = min(max(c - 8, 0), 48)
        m = np.full(64, NEG, np.float32); m[cs:cs + 16] = 0.0
        colmask[pp, :, :] = m
    return dict(ident=ident, jmat=jmat, amat=amat, rmx=rmx.reshape(8, 2, 896), colmask=colmask)

def sample_inputs(z, mrow_b, b, r0):
    cos, sinS = rope_tables()
    xs = z['x_sample'][b]
    halo = np.zeros((28, 64, 2048), np.float32)
    for i in range(28):
        r = r0 - 6 + i
        if 0 <= r < 64:
            halo[i] = xs[r * 64:(r + 1) * 64]
    rpbpad = np.zeros((8, 15, 160), np.float32)
    rpbpad[:, :, 64:95] = z['na_rpb'][0]
    wqb = z['w_q_b'][0]
    wqb_rs = np.concatenate([wqb[:, h * 192 + 128:h * 192 + 192][:, SWAP] for h in range(8)], 1)
    own = slice(r0 * 64, r0 * 64 + 1024)
    d = dict(x_own=np.ascontiguousarray(xs[own]), x_halo=halo.reshape(1792, 2048), x_all=xs,
             ck_na=z['cache_na_k'][b, 0].reshape(512, 1024), cv_na=z['cache_na_v'][b, 0].reshape(512, 1024),
             c_ckv=z['cache_mla_ckv'][b, 0], c_krope=z['cache_mla_krope'][b, 0],
             mrow=np.ascontiguousarray(mrow_b.reshape(6, 2048)),
             g_attn=z['g_attn'], g_ffn=z['g_ffn'], g_q_a=z['g_q_a'], g_kv_a=z['g_kv_a'],
             w_in=z['w_in'][0], w_in_rs=np.ascontiguousarray(z['w_in'][0][:, 3840:3904][:, SWAP]),
             w_q_b=wqb, w_q_b_rs=np.ascontiguousarray(wqb_rs), w_kv_b=z['w_kv_b'][0], w_out=z['w_out'][0],
             w_router=z['w_router'][0], b_router=z['b_router'], rpbpad=rpbpad,
             cos_tok=cos, sinS_tok=sinS, cosT_own=np.ascontiguousarray(cos[own].T), sinST_own=np.ascontiguousarray(sinS[own].T))
    d.update(sample_consts(r0))
    return d


def fused_inputs(z, core):
    f32 = np.float32
    b, r0 = core // 4, 16 * (core % 4)
    c2 = np.stack([np.asarray(z['c_ctx'], f32), np.asarray(z['c'], f32)[b]], 0)
    d = sample_inputs(z, np.zeros(6 * 2048, f32), b, r0)
    del d['mrow']
    d.update(cT=np.ascontiguousarray(c2.T.reshape(16, 128, 2).transpose(1, 0, 2)),
             wm=np.asarray(z['w_mod'], f32)[0], bm=np.asarray(z['b_mod'], f32),
             xp=np.ascontiguousarray(np.asarray(z['x_prompt'], f32)[4 * core:4 * core + 4].reshape(1024, 2048)),
             w_gu=np.asarray(z['w_gate_up'], f32)[0],
             b_gu=np.ascontiguousarray(np.asarray(z['b_gate_up'], f32)[0].reshape(32, 16, 128, 2).transpose(2, 0, 1, 3)),
             w_dn=np.asarray(z['w_down'], f32)[0], b_dn=np.asarray(z['b_down'], f32)[0],
             g_final=np.asarray(z['g_final'], f32)[None])
    return d


from concourse.bass_utils import run_bass_kernel_spmd

NCORES = 8


def kernel(x_prompt, x_sample, cache_na_k, cache_na_v, cache_mla_ckv, cache_mla_krope, c, c_ctx,
           g_attn, g_ffn, g_final, w_mod, b_mod, w_in, w_out, na_rpb, g_q_a, w_q_b, g_kv_a, w_kv_b,
           w_router, b_router, w_gate_up, b_gate_up, w_down, b_down):
    f32 = np.float32
    z = dict(x_prompt=x_prompt, x_sample=x_sample, cache_na_k=cache_na_k, cache_na_v=cache_na_v, cache_mla_ckv=cache_mla_ckv,
             cache_mla_krope=cache_mla_krope, c=c, c_ctx=c_ctx, g_attn=g_attn, g_ffn=g_ffn, g_final=g_final, w_mod=w_mod, b_mod=b_mod,
             w_in=w_in, w_out=w_out, na_rpb=na_rpb, g_q_a=g_q_a, w_q_b=w_q_b, g_kv_a=g_kv_a, w_kv_b=w_kv_b, w_router=w_router,
             b_router=b_router, w_gate_up=w_gate_up, b_gate_up=b_gate_up, w_down=w_down, b_down=b_down)
    z = {k: np.asarray(v, f32) for k, v in z.items()}
    nc = bass.Bass("TRN2", target_bir_lowering=False)
    build_fused(nc)
    in_maps = [fused_inputs(z, i) for i in range(NCORES)]
    res = run_bass_kernel_spmd(nc, in_maps, core_ids=list(range(NCORES))).results
    y = [np.asarray(r["o_y"], f32) for r in res]
    y_prompt = np.concatenate([t[:1024] for t in y], 0).reshape(32, 256, 2048)
    y_sample = np.concatenate([t[1024:] for t in y], 0).reshape(2, 4096, 2048)
    new_na_k = np.concatenate([np.asarray(r["o_nak"], f32) for r in res], 0).reshape(32, 1, 256, 8, 128)
    new_na_v = np.concatenate([np.asarray(r["o_nav"], f32) for r in res], 0).reshape(32, 1, 256, 8, 128)
    new_ckv = np.concatenate([np.asarray(r["o_ckv"], f32) for r in res], 0).reshape(32, 1, 256, 256)
    new_krope = np.concatenate([np.asarray(r["o_krope"], f32) for r in res], 0).reshape(32, 1, 256, 64)
    return (y_prompt, y_sample, new_na_k, new_na_v, new_ckv, new_krope)
```

```python
import numpy as np
import concourse.bass as bass
import concourse.mybir as mybir
from contextlib import ExitStack

F32 = mybir.dt.float32
BF16 = mybir.dt.bfloat16
I32 = mybir.dt.int32
U32 = mybir.dt.uint32
ALU = mybir.AluOpType
ACTF = mybir.ActivationFunctionType
AX = mybir.AxisListType

ENGS = ("pe", "act", "dve", "pool", "sp")
SEM_ROTATE = 12000


class Buf:
    __slots__ = ("name", "last_w", "readers", "ld", "st", "excl")

    def __init__(self, name, excl=False):
        self.name = name
        self.excl = excl
        self.last_w = None
        self.readers = []
        self.ld = None
        self.st = None


class DmaSem:
    __slots__ = ("sem", "count", "kind")

    def __init__(self, sem, kind):
        self.sem = sem
        self.count = 0
        self.kind = kind


class Op:
    __slots__ = ("eng", "fn", "waits", "is_dma", "dsem", "dval", "signaled", "sig", "idx", "dma_waits", "inc")

    def __init__(self, eng, fn, is_dma):
        self.eng = eng
        self.fn = fn
        self.is_dma = is_dma
        self.waits = {}
        self.dma_waits = {}
        self.dsem = None
        self.dval = 0
        self.signaled = False
        self.sig = None
        self.idx = 0
        self.inc = 16


class Prog:
    def __init__(self, nc):
        self.nc = nc
        self.ops = {e: [] for e in ENGS}
        self.all_ops = []
        self.stack = ExitStack()
        self.sem_pool = []
        self.n_sems = 0
        self.dma_sems = []
        self.free_dsems = []
        self._n = 0

    def new_sem(self):
        self.n_sems += 1
        return self.stack.enter_context(self.nc.semaphore(f"s{self.n_sems}"))

    def sbuf(self, name, shape, dtype):
        t = self.stack.enter_context(self.nc.sbuf_tensor(name, list(shape), dtype))
        return t

    def psum(self, name, shape, dtype):
        t = self.stack.enter_context(self.nc.psum_tensor(name, list(shape), dtype))
        return t

    def _dep(self, op, prod):
        if prod is None or prod is op:
            return
        if prod.is_dma:
            ds = prod.dsem
            op.dma_waits[id(ds)] = (ds, ds.count)
        else:
            if prod.eng == op.eng and not op.is_dma and op.eng == "pe":
                return
            cur = op.waits.get(prod.eng)
            if cur is None or cur.idx < prod.idx:
                op.waits[prod.eng] = prod
            prod.signaled = True

    def _record(self, op, reads, writes):
        self._n += 1
        op.idx = self._n
        ex = [b for b in reads if b.excl]
        if ex:
            reads = [b for b in reads if not b.excl]
            writes = list(writes) + [b for b in ex if b not in writes]
        for b in reads:
            self._dep(op, b.last_w)
        for b in writes:
            self._dep(op, b.last_w)
            for r in b.readers:
                self._dep(op, r)
        for b in reads:
            b.readers.append(op)
        for b in writes:
            b.last_w = op
            b.readers = []
        self.ops[op.eng].append(op)
        self.all_ops.append(op)
        return op

    def op(self, eng, fn, reads=(), writes=()):
        return self._record(Op(eng, fn, False), reads, writes)

    def dma(self, eng, fn, reads=(), writes=(), key=None, store=False, inc=16):
        op = Op(eng, fn, True)
        if key is None:
            key = writes[0] if not store else reads[0]
        attr = "st" if store else "ld"
        ds = getattr(key, attr)
        if ds is None:
            kind = "sw" if eng == "pool" else "hw"
            fl = [d for d in self.free_dsems if d.kind == kind]
            if fl:
                ds = fl[-1]
                self.free_dsems.remove(ds)
            else:
                ds = DmaSem(self.new_sem(), kind)
                self.dma_sems.append(ds)
            setattr(key, attr, ds)
        op.dsem = ds
        op.inc = inc
        self._record(op, reads, writes)
        ds.count += inc
        op.dval = ds.count
        return op

    def recycle_dma_sems(self):
        self.free_dsems = list(self.dma_sems)

    def coll(self, fn, reads=(), writes=()):
        if not hasattr(self, "_csem"):
            self._csem = self.new_sem()
            self._ccnt = 0
            self._cscr = self.sbuf("coll_scr", [128, 8], F32)
        self._ccnt += 1
        n = self._ccnt
        csem = self._csem
        scr = self._cscr

        def run(eng, fn=fn, n=n):
            fn(eng).then_inc(csem)
            eng.wait_ge(csem, n)
            return eng.memset(scr[:], 0.0)
        return self.op("pool", run, reads=reads, writes=writes)

    def barrier(self, bufs=()):
        lastc = {}
        for e in ENGS:
            lastc[e] = None
            for q in reversed(self.ops[e]):
                if not q.is_dma and q.fn is not None:
                    lastc[e] = q
                    break
        tot = [(ds, ds.count) for ds in self.dma_sems if ds.count > 0]
        for e in ENGS:
            op = Op(e, None, False)
            self._n += 1
            op.idx = self._n
            for e2 in ENGS:
                q = lastc[e2]
                if q is not None and (e2 != e or e != "pe"):
                    op.waits[e2] = q
                    q.signaled = True
            for ds, c in tot:
                op.dma_waits[id(ds)] = (ds, c)
            self.ops[e].append(op)
            self.all_ops.append(op)

    def emit(self):
        nc = self.nc
        for e in ENGS:
            sem = None
            cnt = 0
            for op in self.ops[e]:
                if op.is_dma or not op.signaled or op.fn is None:
                    continue
                if sem is None or cnt >= SEM_ROTATE:
                    sem = self.new_sem()
                    cnt = 0
                cnt += 1
                op.sig = (sem, cnt)
        prog = self

        def run(engname, eng):
            for op in prog.ops[engname]:
                for p in op.waits.values():
                    s, v = p.sig
                    eng.wait_ge(s, v)
                for ds, v in op.dma_waits.values():
                    eng.wait_ge(ds.sem, v)
                if op.fn is None:
                    continue
                ins = op.fn(eng)
                if op.is_dma:
                    ins.then_inc(op.dsem.sem, op.inc)
                elif op.signaled:
                    ins.then_inc(op.sig[0], 1)

        with nc.Block() as block:
            @block.tensor
            def _(eng):
                run("pe", eng)

            @block.scalar
            def _(eng):
                run("act", eng)

            @block.vector
            def _(eng):
                run("dve", eng)

            @block.gpsimd
            def _(eng):
                run("pool", eng)

            @block.sync
            def _(eng):
                run("sp", eng)
                for ds in prog.dma_sems:
                    if ds.count > 0:
                        eng.wait_ge(ds.sem, ds.count)
        self.stack.close()


D = 2048
EPS = 1e-6
NEG = -30000.0


class Ctx:
    def __init__(self, nc, fused=False):
        self.nc = nc
        self.P = Prog(nc)
        self._cnt = 0
        self.fused = fused
        self.dram = {}
        self.arena = None
        self.ps_pool = None
        self.ps_off = 0

    def T(self, name, shape, dt):
        if self.fused:
            return self.arena.alloc(name, shape, dt)
        t = self.P.sbuf(name, shape, dt)
        b = Buf(name)
        return t, b

    def PS(self, name, shape, dt):
        if self.fused:
            n = 1
            for d in shape[1:]:
                n *= d
            nbytes = n * (4 if dt == F32 else 2)
            nb = (nbytes + 2047) // 2048
            assert self.ps_off + nb <= 8, ("psum", name)
            ap = self.ps_pool[:, self.ps_off * 512:(self.ps_off + nb) * 512]
            self.ps_off += nb
            if dt != F32:
                ap = ap.bitcast(dt)
            ap = ap[:, 0:n]
            if len(shape) == 3:
                ap = ap.rearrange("p (a b) -> p a b", a=shape[1])
            return ap, Buf(name, excl=True)
        t = self.P.psum(name, shape, dt)
        b = Buf(name, excl=True)
        return t, b

    def din(self, name, shape, dt=F32):
        if name in self.dram:
            return self.dram[name]
        ap = self.nc.dram_tensor(name, list(shape), dt, kind="ExternalInput").ap()
        if self.fused:
            self.dram[name] = ap
        return ap

    def dout(self, name, shape, dt=F32):
        if name in self.dram:
            return self.dram[name]
        return self.nc.dram_tensor(name, list(shape), dt, kind="ExternalOutput").ap()

    def scratch(self, name, shape, dt):
        return self.nc.dram_tensor(name, list(shape), dt).ap()

    def finish(self):
        if not self.fused:
            self.P.emit()

    def load(self, q, out_ap, in_ap, wbuf, reads=()):
        return self.P.dma(q, lambda e: e.dma_start(out=out_ap, in_=in_ap), reads=list(reads), writes=[wbuf])

    def store(self, q, out_ap, in_ap, rbuf, writes=()):
        return self.P.dma(q, lambda e: e.dma_start(out=out_ap, in_=in_ap), reads=[rbuf], writes=list(writes), key=rbuf, store=True)

    def mm(self, out, lhsT, rhs, start, stop, reads, writes):
        return self.P.op("pe", lambda e: e.matmul(out, lhsT=lhsT, rhs=rhs, start=start, stop=stop), reads=reads, writes=writes)

    def tr(self, out, in_, ident, reads, writes):
        return self.P.op("pe", lambda e: e.transpose(out=out, in_=in_, identity=ident), reads=reads, writes=writes)

    def act(self, out, in_, func, reads, writes, bias=None, scale=None, accum_out=None):
        kw = {}
        if bias is not None:
            kw["bias"] = bias
        if scale is not None:
            kw["scale"] = scale
        if accum_out is not None:
            kw["accum_out"] = accum_out
        return self.P.op("act", lambda e: e.activation(out=out, in_=in_, func=func, **kw), reads=reads, writes=writes)

    def ts(self, eng, out, in0, s1, s2, op0, op1, reads, writes):
        if op1 is None:
            return self.P.op(eng, lambda e: e.tensor_scalar(out=out, in0=in0, scalar1=s1, scalar2=None, op0=op0), reads=reads, writes=writes)
        return self.P.op(eng, lambda e: e.tensor_scalar(out=out, in0=in0, scalar1=s1, scalar2=s2, op0=op0, op1=op1), reads=reads, writes=writes)

    def tt(self, eng, out, in0, in1, op, reads, writes):
        return self.P.op(eng, lambda e: e.tensor_tensor(out=out, in0=in0, in1=in1, op=op), reads=reads, writes=writes)

    def stt(self, out, in0, scalar, in1, op0, op1, reads, writes):
        return self.P.op("dve", lambda e: e.scalar_tensor_tensor(out=out, in0=in0, scalar=scalar, in1=in1, op0=op0, op1=op1), reads=reads, writes=writes)

    def copy(self, eng, out, in_, reads, writes):
        if eng == "act":
            return self.P.op("act", lambda e: e.copy(out=out, in_=in_), reads=reads, writes=writes)
        return self.P.op(eng, lambda e: e.tensor_copy(out=out, in_=in_), reads=reads, writes=writes)


def rstd_from_ss(C, rstd, ss, n, bss, brstd):
    C.ts("dve", rstd, ss, 1.0 / n, EPS, ALU.mult, ALU.add, [bss], [brstd])
    C.act(rstd, rstd, ACTF.Sqrt, [brstd], [brstd])
    C.P.op("dve", lambda e: e.reciprocal(out=rstd, in_=rstd), reads=[brstd], writes=[brstd])


def build_prompt(nc, NB=4, do_tail=True, stop=99, sub=99, C=None):
    C = C or Ctx(nc)
    P = C.P
    NT = NB * 256
    xp = C.din("xp", [NT, D])
    mrow = C.din("mrow", [6, D])
    g_attn = C.din("g_attn", [1, D])
    g_ffn = C.din("g_ffn", [1, D])
    g_q_a = C.din("g_q_a", [1, 512])
    g_kv_a = C.din("g_kv_a", [1, 256])
    w_in = C.din("w_in", [D, 3904])
    w_q_b = C.din("w_q_b", [512, 1536])
    w_kv_b = C.din("w_kv_b", [256, 2048])
    w_out = C.din("w_out", [D, D])
    w_router = C.din("w_router", [D, 32])
    b_router = C.din("b_router", [1, 32])
    ident = C.din("ident", [128, 128])
    o_nak = C.dout("o_nak", [NT, 1024])
    o_nav = C.dout("o_nav", [NT, 1024])
    o_ckv = C.dout("o_ckv", [NT, 256])
    o_krope = C.dout("o_krope", [NT, 64])
    o_x1 = C.dout("o_x1", [NT, D])
    o_h2 = C.dout("o_h2", [NT, D], BF16)
    o_G = C.dout("o_G", [NT, 32])

    idF, bidF = C.T("idF", [128, 128], F32)
    idB, bidB = C.T("idB", [128, 128], BF16)
    G1, bG1 = C.T("G1", [128, D], F32)
    SA, bSA = C.T("SA", [128, D], F32)
    GA, bGA = C.T("GA", [128, D], F32)
    G2, bG2 = C.T("G2", [128, D], F32)
    SF, bSF = C.T("SF", [128, D], F32)
    gqa, bgqa = C.T("gqa", [128, 512], F32)
    gkva, bgkva = C.T("gkva", [128, 256], F32)
    brt, bbrt = C.T("brt", [128, 32], F32)
    wr, bwr = C.T("wr", [128, 16, 32], F32)
    wqb, bwqb = C.T("wqb", [128, 4, 1536], BF16)
    wkvb, bwkvb = C.T("wkvb", [128, 2, 2048], BF16)
    xt = [C.T(f"xt{t}", [128, D], F32) for t in range(2)]
    tmpf, btmpf = C.T("tmpf", [128, D], F32)
    hb, bhb = C.T("hb", [128, D], BF16)
    hT, bhT = C.T("hT", [128, 16, 256], BF16)
    NWS = 2
    ws = [C.T(f"ws{i}", [128, 16, 256], BF16) for i in range(NWS)]
    QT, bQT = C.T("QT", [128, 8, 256], BF16)
    Kb, bKb = C.T("Kb", [128, 2, 1024], BF16)
    Vb, bVb = C.T("Vb", [128, 2, 1024], BF16)
    KT, bKT = C.T("KT", [128, 8, 256], BF16)
    qaf, bqaf = C.T("qaf", [128, 2, 512], F32)
    qan, bqan = C.T("qan", [128, 2, 512], BF16)
    qanT, bqanT = C.T("qanT", [128, 4, 256], BF16)
    kvaf, bkvaf = C.T("kvaf", [128, 2, 256], F32)
    ckvf, bckvf = C.T("ckvf", [128, 2, 256], F32)
    ckvb, bckvb = C.T("ckvb", [128, 2, 256], BF16)
    ckvT, bckvT = C.T("ckvT", [128, 2, 256], BF16)
    krf, bkrf = C.T("krf", [128, 2, 64], F32)
    krb, bkrb = C.T("krb", [128, 2, 64], BF16)
    krT, bkrT = C.T("krT", [128, 256], BF16)
    qnT, bqnT = C.T("qnT", [128, 8, 256], BF16)
    qrT, bqrT = C.T("qrT", [128, 8, 256], BF16)
    knT, bknT = C.T("knT", [128, 8, 256], BF16)
    vm, bvm = C.T("vm", [128, 2, 1024], BF16)
    ocat, bocat = C.T("ocat", [128, 2, D], BF16)
    ocT, bocT = C.T("ocT", [128, 16, 256], BF16)
    stg = [C.T(f"stg{i}", [128, 256], F32) for i in range(2)]
    Pb = [C.T(f"Pb{i}", [128, 256], BF16) for i in range(2)]
    PT = [C.T(f"PT{i}", [128, 2, 128], BF16) for i in range(2)]
    st_ss, bst_ss = C.T("st_ss", [128, 8], F32)
    st_r, bst_r = C.T("st_r", [128, 8], F32)
    amx = [C.T(f"amx{i}", [128, 4], F32) for i in range(2)]
    h2f, bh2f = C.T("h2f", [128, D], F32)
    h2b, bh2b = C.T("h2b", [128, D], BF16)
    h2T, bh2T = C.T("h2T", [128, 16, 128], F32)
    lg, blg = C.T("lg", [128, 32], F32)
    lg2, blg2 = C.T("lg2", [128, 32], F32)
    top8, btop8 = C.T("top8", [128, 8], F32)

    pT, bpT = C.PS("pT", [128, 2048], BF16)
    pA = [C.PS(f"pA{i}", [128, 512], F32) for i in range(2)]
    pS = [C.PS(f"pS{i}", [128, 512], F32) for i in range(2)]
    pTp_full, bpTp = C.PS("pTp", [128, 8, 128], BF16)
    pTp = pTp_full[:, 0:2, :]
    pO, bpO = C.PS("pO", [128, 512], F32)

    C.load("sp", idF[:], ident, bidF)
    C.copy("dve", idB[:], idF[:], [bidF], [bidB])

    def bload(dst, row_ap, buf):
        C.load("sp", dst[:], row_ap.partition_broadcast(128), buf)

    bload(SA, mrow[0:1, :], bSA)
    bload(G1, mrow[1:2, :], bG1)
    bload(tmpf, g_attn[0:1, :], btmpf)
    C.stt(G1[:], G1[:], 1.0, tmpf[:], ALU.add, ALU.mult, [bG1, btmpf], [bG1])
    bload(GA, mrow[2:3, :], bGA)
    bload(SF, mrow[3:4, :], bSF)
    bload(G2, mrow[4:5, :], bG2)
    bload(h2f, g_ffn[0:1, :], bh2f)
    C.stt(G2[:], G2[:], 1.0, h2f[:], ALU.add, ALU.mult, [bG2, bh2f], [bG2])
    bload(gqa, g_q_a[0:1, :], bgqa)
    bload(gkva, g_kv_a[0:1, :], bgkva)
    bload(brt, b_router[0:1, :], bbrt)
    C.load("sp", wr[:], w_router.rearrange("(k p) n -> p k n", p=128), bwr)
    C.load("pool", wqb[:], w_q_b.rearrange("(k p) n -> p k n", p=128), bwqb)
    C.load("pool", wkvb[:], w_kv_b.rearrange("(k p) n -> p k n", p=128), bwkvb)

    P.op("pool", lambda e: e.memset(qrT[:], 0.0), writes=[bqrT])
    P.op("pool", lambda e: e.memset(krT[:], 0.0), writes=[bkrT])
    if stop <= 0:
        C.finish()
        return nc
    ws_i = [0]

    def next_ws(src_cols_ap, ncols):
        i = ws_i[0] % NWS
        ws_i[0] += 1
        t, b = ws[i]
        C.load("pool", t[:, :, 0:ncols], src_cols_ap.rearrange("(k p) n -> p k n", p=128), b)
        return t, b

    pa_i = [0]

    def next_pA():
        i = pa_i[0] % 2
        pa_i[0] += 1
        return pA[i]

    stg_i = [0]

    def next_stg():
        i = stg_i[0] % 2
        stg_i[0] += 1
        return stg[i]

    SC_NA = 128 ** -0.5
    SC_MLA = 192 ** -0.5

    def rmsnorm_tile(src, bsrc, col):
        n = src.shape[-1] if len(src.shape) == 2 else None
        C.act(hb[:, 0:src.shape[1]], src, ACTF.Square, [bsrc], [bhb, bst_ss], accum_out=st_ss[:, col:col + 1])

    for b in range(NB):
        r0 = b * 256
        for t in range(2):
            x_t, bx = xt[t]
            C.load("sp", x_t[:], xp[r0 + t * 128: r0 + (t + 1) * 128, :], bx)
            C.act(hb[:], x_t[:], ACTF.Square, [bx], [bhb, bst_ss], accum_out=st_ss[:, 0:1])
            rstd_from_ss(C, st_r[:, 0:1], st_ss[:, 0:1], D, bst_ss, bst_r)
            C.stt(tmpf[:], x_t[:], st_r[:, 0:1], G1[:], ALU.mult, ALU.mult, [bx, bst_r, bG1], [btmpf])
            C.tt("dve", hb[:], tmpf[:], SA[:], ALU.add, [btmpf, bSA], [bhb])
            for k in range(16):
                C.tr(pT[:, k * 128:(k + 1) * 128], hb[:, k * 128:(k + 1) * 128], idB[:], [bhb, bidB], [bpT])
            C.copy("act", hT[:, :, t * 128:(t + 1) * 128], pT[:].rearrange("p (k c) -> p k c", k=16), [bpT], [bhT])

        if stop <= 1:
            continue
        for s in range(4):
            wt, bw = next_ws(w_in[:, s * 256:(s + 1) * 256], 256)
            for hh in range(2):
                head = s * 2 + hh
                pa, bpa = next_pA()
                for k in range(16):
                    C.mm(pa[:, 0:256], wt[:, k, hh * 128:(hh + 1) * 128], hT[:, k, :], k == 0, k == 15, [bw, bhT], [bpa])
                C.act(QT[:, head, :], pa[:, 0:256], ACTF.Copy, [bpa], [bQT], scale=SC_NA)
        if stop == 2 and sub <= 0:
            continue
        for which, (obuf, sb_t, sb_b) in enumerate(((o_nak, Kb, bKb), (o_nav, Vb, bVb))):
            for s in range(4):
                c0 = 1024 * (1 + which) + s * 256
                wt, bw = next_ws(w_in[:, c0:c0 + 256], 256)
                for t in range(2):
                    pa, bpa = next_pA()
                    for k in range(16):
                        C.mm(pa[:, 0:256], hT[:, k, t * 128:(t + 1) * 128], wt[:, k, :], k == 0, k == 15, [bw, bhT], [bpa])
                    sg, bsg = next_stg()
                    C.copy("dve", sg[:], pa[:, 0:256], [bpa], [bsg])
                    C.copy("act", sb_t[:, t, s * 256:(s + 1) * 256], pa[:, 0:256], [bpa], [sb_b])
                    C.store("sp", obuf[r0 + t * 128: r0 + (t + 1) * 128, s * 256:(s + 1) * 256], sg[:], bsg)
        if stop == 2 and sub <= 1:
            continue
        for s in range(2):
            c0 = 3072 + s * 256
            wt, bw = next_ws(w_in[:, c0:c0 + 256], 256)
            for t in range(2):
                pa, bpa = next_pA()
                for k in range(16):
                    C.mm(pa[:, 0:256], hT[:, k, t * 128:(t + 1) * 128], wt[:, k, :], k == 0, k == 15, [bw, bhT], [bpa])
                C.copy("dve", qaf[:, t, s * 256:(s + 1) * 256], pa[:, 0:256], [bpa], [bqaf])
        for t in range(2):
            C.act(hb[:, 0:512], qaf[:, t, :], ACTF.Square, [bqaf], [bhb, bst_ss], accum_out=st_ss[:, 1:2])
            rstd_from_ss(C, st_r[:, 1:2], st_ss[:, 1:2], 512, bst_ss, bst_r)
            C.stt(qan[:, t, :], qaf[:, t, :], st_r[:, 1:2], gqa[:], ALU.mult, ALU.mult, [bqaf, bst_r, bgqa], [bqan])
        if stop == 2 and sub <= 2:
            continue
        wt, bw = next_ws(w_in[:, 3584:3840], 256)
        for t in range(2):
            pa, bpa = next_pA()
            for k in range(16):
                C.mm(pa[:, 0:256], hT[:, k, t * 128:(t + 1) * 128], wt[:, k, :], k == 0, k == 15, [bw, bhT], [bpa])
            C.copy("dve", kvaf[:, t, :], pa[:, 0:256], [bpa], [bkvaf])
            C.act(hb[:, 0:256], kvaf[:, t, :], ACTF.Square, [bkvaf], [bhb, bst_ss], accum_out=st_ss[:, 2:3])
            rstd_from_ss(C, st_r[:, 2:3], st_ss[:, 2:3], 256, bst_ss, bst_r)
            C.stt(ckvf[:, t, :], kvaf[:, t, :], st_r[:, 2:3], gkva[:], ALU.mult, ALU.mult, [bkvaf, bst_r, bgkva], [bckvf])
            C.copy("act", ckvb[:, t, :], ckvf[:, t, :], [bckvf], [bckvb])
        C.store("sp", o_ckv[r0:r0 + 256, :].rearrange("(t p) c -> p t c", p=128), ckvf[:], bckvf)
        if stop == 2 and sub <= 3:
            continue
        wt, bw = next_ws(w_in[:, 3840:3904], 64)
        for t in range(2):
            pa, bpa = next_pA()
            for k in range(16):
                C.mm(pa[:, 0:64], hT[:, k, t * 128:(t + 1) * 128], wt[:, k, 0:64], k == 0, k == 15, [bw, bhT], [bpa])
            C.copy("dve", krf[:, t, :], pa[:, 0:64], [bpa], [bkrf])
            C.copy("act", krb[:, t, :], pa[:, 0:64], [bpa], [bkrb])
        C.store("sp", o_krope[r0:r0 + 256, :].rearrange("(t p) c -> p t c", p=128), krf[:], bkrf)

        if stop <= 2:
            continue
        for t in range(2):
            for hd in range(8):
                C.tr(pT[:, hd * 128:(hd + 1) * 128], Kb[:, t, hd * 128:(hd + 1) * 128], idB[:], [bKb, bidB], [bpT])
            C.copy("act", KT[:, :, t * 128:(t + 1) * 128], pT[:, 0:1024].rearrange("p (k c) -> p k c", k=8), [bpT], [bKT])
        for t in range(2):
            for c in range(4):
                C.tr(pT[:, c * 128:(c + 1) * 128], qan[:, t, c * 128:(c + 1) * 128], idB[:], [bqan, bidB], [bpT])
            for c in range(2):
                C.tr(pT[:, (4 + c) * 128:(5 + c) * 128], ckvb[:, t, c * 128:(c + 1) * 128], idB[:], [bckvb, bidB], [bpT])
            C.tr(pT[0:64, 6 * 128:7 * 128], krb[:, t, :], idB[:], [bkrb, bidB], [bpT])
            C.copy("act", qanT[:, :, t * 128:(t + 1) * 128], pT[:, 0:512].rearrange("p (k c) -> p k c", k=4), [bpT], [bqanT])
            C.copy("dve", ckvT[:, :, t * 128:(t + 1) * 128], pT[:, 512:768].rearrange("p (k c) -> p k c", k=2), [bpT], [bckvT])
            C.copy("dve", krT[0:64, t * 128:(t + 1) * 128], pT[0:64, 768:896], [bpT], [bkrT])
        for hd in range(8):
            pa, bpa = next_pA()
            for c in range(4):
                C.mm(pa[:, 0:256], wqb[:, c, hd * 192: hd * 192 + 128], qanT[:, c, :], c == 0, c == 3, [bwqb, bqanT], [bpa])
            C.act(qnT[:, hd, :], pa[:, 0:256], ACTF.Copy, [bpa], [bqnT], scale=SC_MLA)
            pa, bpa = next_pA()
            for c in range(4):
                C.mm(pa[0:64, 0:256], wqb[:, c, hd * 192 + 128: hd * 192 + 192], qanT[:, c, :], c == 0, c == 3, [bwqb, bqanT], [bpa])
            C.act(qrT[0:64, hd, :], pa[0:64, 0:256], ACTF.Copy, [bpa], [bqrT], scale=SC_MLA)
            pa, bpa = next_pA()
            for c in range(2):
                C.mm(pa[:, 0:256], wkvb[:, c, hd * 256: hd * 256 + 128], ckvT[:, c, :], c == 0, c == 1, [bwkvb, bckvT], [bpa])
            C.copy("dve", knT[:, hd, :], pa[:, 0:256], [bpa], [bknT])
        for t in range(2):
            for half in range(2):
                pa, bpa = next_pA()
                for c in range(2):
                    rhs = wkvb[:, c, :].rearrange("p (h x) -> p h x", h=8)[:, half * 4:(half + 1) * 4, 128:256]
                    C.mm(pa[:, 0:512], ckvT[:, c, t * 128:(t + 1) * 128], rhs, c == 0, c == 1, [bwkvb, bckvT], [bpa])
                C.copy("act", vm[:, t, half * 512:(half + 1) * 512], pa[:, 0:512], [bpa], [bvm])

        if stop <= 3:
            continue
        ai = [0]

        def attend(t, score_ops, vtile, bv, vcol, ocol):
            i = ai[0] % 2
            ai[0] += 1
            ps, bps = pS[i]
            pb, bpb = Pb[i]
            ptt, bptt = PT[i]
            am, bam = amx[i]
            n = len(score_ops)
            for j, (lt, rh, rd) in enumerate(score_ops):
                C.mm(ps[:, 0:256], lt, rh, j == 0, j == n - 1, rd, [bps])
            P.op("dve", lambda e: e.tensor_reduce(out=am[:, 0:1], in_=ps[:, 0:256], axis=AX.X, op=ALU.max, negate=True),
                 reads=[bps], writes=[bam])
            C.act(pb[:], ps[:, 0:256], ACTF.Exp, [bps, bam], [bpb, bam], bias=am[:, 0:1], scale=1.0, accum_out=am[:, 1:2])
            P.op("dve", lambda e: e.reciprocal(out=am[:, 2:3], in_=am[:, 1:2]), reads=[bam], writes=[bam])
            for j in range(2):
                C.tr(pTp_full[:, j, :], pb[:, j * 128:(j + 1) * 128], idB[:], [bpb, bidB], [bpTp])
            C.copy("dve", ptt[:], pTp_full[:, 0:2, :], [bpTp], [bptt])
            for j in range(2):
                C.mm(pO[:, 0:128], ptt[:, j, :], vtile[:, j, vcol:vcol + 128], j == 0, j == 1, [bptt, bv], [bpO])
            C.ts("dve", ocat[:, t, ocol:ocol + 128], pO[:, 0:128], am[:, 2:3], None, ALU.mult, None, [bpO, bam], [bocat])

        for hd in range(8):
            for t in range(2):
                attend(t, [(QT[:, hd, t * 128:(t + 1) * 128], KT[:, hd, :], [bQT, bKT])], Vb, bVb, hd * 128, hd * 128)
        for hd in range(8):
            for t in range(2):
                attend(t, [(qnT[:, hd, t * 128:(t + 1) * 128], knT[:, hd, :], [bqnT, bknT]),
                           (qrT[:, hd, t * 128:(t + 1) * 128], krT[:, :], [bqrT, bkrT])], vm, bvm, hd * 128, 1024 + hd * 128)

        if not do_tail:
            for t in range(2):
                C.copy("dve", h2b[:], ocat[:, t, :], [bocat], [bh2b])
                C.store("sp", o_h2[r0 + t * 128: r0 + (t + 1) * 128, :], h2b[:], bh2b)
            continue

        for t in range(2):
            for k in range(16):
                C.tr(pT[:, k * 128:(k + 1) * 128], ocat[:, t, k * 128:(k + 1) * 128], idB[:], [bocat, bidB], [bpT])
            C.copy("act", ocT[:, :, t * 128:(t + 1) * 128], pT[:].rearrange("p (k c) -> p k c", k=16), [bpT], [bocT])
        for s in range(8):
            wt, bw = next_ws(w_out[:, s * 256:(s + 1) * 256], 256)
            for t in range(2):
                x_t, bx = xt[t]
                pa, bpa = next_pA()
                for k in range(16):
                    C.mm(pa[:, 0:256], ocT[:, k, t * 128:(t + 1) * 128], wt[:, k, :], k == 0, k == 15, [bw, bocT], [bpa])
                sg, bsg = next_stg()
                C.tt("dve", sg[:], pa[:, 0:256], GA[:, s * 256:(s + 1) * 256], ALU.mult, [bpa, bGA], [bsg])
                C.tt("dve", x_t[:, s * 256:(s + 1) * 256], x_t[:, s * 256:(s + 1) * 256], sg[:], ALU.add, [bx, bsg], [bx])
        for t in range(2):
            x_t, bx = xt[t]
            C.store("sp", o_x1[r0 + t * 128: r0 + (t + 1) * 128, :], x_t[:], bx)
            C.act(hb[:], x_t[:], ACTF.Square, [bx], [bhb, bst_ss], accum_out=st_ss[:, 3:4])
            rstd_from_ss(C, st_r[:, 3:4], st_ss[:, 3:4], D, bst_ss, bst_r)
            C.stt(tmpf[:], x_t[:], st_r[:, 3:4], G2[:], ALU.mult, ALU.mult, [bx, bst_r, bG2], [btmpf])
            C.tt("dve", h2f[:], tmpf[:], SF[:], ALU.add, [btmpf, bSF], [bh2f])
            C.copy("act", h2b[:], h2f[:], [bh2f], [bh2b])
            C.store("sp", o_h2[r0 + t * 128: r0 + (t + 1) * 128, :], h2b[:], bh2b)
            for g4 in range(4):
                pa, bpa = next_pA()
                for j in range(4):
                    k = g4 * 4 + j
                    C.tr(pa[:, j * 128:(j + 1) * 128], h2f[:, k * 128:(k + 1) * 128], idF[:], [bh2f, bidF], [bpa])
                C.copy("dve", h2T[:, g4 * 4:(g4 + 1) * 4, :], pa[:, 0:512].rearrange("p (k c) -> p k c", k=4), [bpa], [bh2T])
            pa, bpa = next_pA()
            for k in range(16):
                C.mm(pa[:, 0:32], h2T[:, k, :], wr[:, k, :], k == 0, k == 15, [bh2T, bwr], [bpa])
            C.tt("dve", lg[:], pa[:, 0:32], brt[:], ALU.add, [bpa, bbrt], [blg])
            P.op("dve", lambda e: e.max(out=top8[:], in_=lg[:]), reads=[blg], writes=[btop8])
            C.ts("dve", lg2[:], lg[:], top8[:, 3:4], None, ALU.is_ge, None, [blg, btop8], [blg2])
            C.ts("dve", top8[:, 7:8], top8[:, 0:1], -1.0, None, ALU.mult, None, [btop8], [btop8])
            C.act(lg[:], lg[:], ACTF.Exp, [blg, btop8], [blg], bias=top8[:, 7:8], scale=1.0)
            C.tt("dve", lg[:], lg[:], lg2[:], ALU.mult, [blg, blg2], [blg])
            P.op("dve", lambda e: e.tensor_reduce(out=top8[:, 6:7], in_=lg[:], axis=AX.X, op=ALU.add), reads=[blg], writes=[btop8])
            P.op("dve", lambda e: e.reciprocal(out=top8[:, 6:7], in_=top8[:, 6:7]), reads=[btop8], writes=[btop8])
            C.ts("dve", lg2[:], lg[:], top8[:, 6:7], None, ALU.mult, None, [blg, btop8], [blg2])
            C.store("sp", o_G[r0 + t * 128: r0 + (t + 1) * 128, :], lg2[:], blg2)
    C.finish()
    return nc


def build_mod(nc, NCOL=1536, C=None):
    C = C or Ctx(nc)
    P = C.P
    NR = 2 if C.fused else 3
    cT = C.din("cT", [128, 16, NR])
    wm = C.din("wm", [D, NCOL])
    bm = C.din("bm", [1, NCOL])
    o_m = C.dout("o_m", [NR, NCOL])
    cTf, bcTf = C.T("cTf", [128, 16, NR], F32)
    cTb, bcTb = C.T("cTb", [128, 16, NR], BF16)
    wmb = [C.T(f"wmb{i}", [128, 16, 512], BF16) for i in range(3)]
    bmt = [C.T(f"bmt{i}", [NR, 512], F32) for i in range(2)]
    ot = [C.T(f"ot{i}", [NR, 512], F32) for i in range(2)]
    pm = [C.PS(f"pm{i}", [128, 512], F32) for i in range(2)]
    C.load("sp", cTf[:], cT, bcTf)
    C.act(cTb[:], cTf[:], ACTF.Silu, [bcTf], [bcTb])
    for n in range(NCOL // 512):
        pa, bpa = pm[n % 2]
        wt, bw = wmb[n % 3]
        bt, bbt = bmt[n % 2]
        o_t, bo = ot[n % 2]
        C.load("pool", wt[:], wm[:, n * 512:(n + 1) * 512].rearrange("(k p) n -> p k n", p=128), bw)
        C.load("sp", bt[:], bm[0:1, n * 512:(n + 1) * 512].partition_broadcast(NR), bbt)
        for k in range(16):
            C.mm(pa[0:NR, :], cTb[:, k, :], wt[:, k, :], k == 0, k == 15, [bcTb, bw], [bpa])
        C.tt("dve", o_t[:], pa[0:NR, :], bt[:], ALU.add, [bpa, bbt], [bo])
        C.store("sp", o_m[:, n * 512:(n + 1) * 512], o_t[:], bo)
    C.finish()
    return nc


def build_experts(nc, NTOK=16384, NE=4, C=None):
    C = C or Ctx(nc)
    P = C.P
    TB = 512
    NBLK = NTOK // TB
    if C.fused:
        h2_d = C.dram["h2_d"]
        Gl = C.dram["G_d"]
        idB, bidB, idF, bidF = C.consts
    else:
        h2T = C.din("h2T", [D, NTOK], BF16)
        Gl = C.din("Gl", [NTOK, NE])
        GlT = C.din("GlT", [NE, NTOK])
    w_gu = C.din("w_gu", [NE, D, 4096])
    b_gu = C.din("b_gu", [128, NE, 16, 2])
    w_dn = C.din("w_dn", [NE, D, D])
    b_dn = C.din("b_dn", [NE, D])
    o_y = C.dout("o_y", [NTOK, D], BF16)

    hTb = [C.T(f"hTb{i}", [128, 16, TB], BF16) for i in range(1)]
    wgu = [C.T(f"wgu{i}", [128, 16, 256], BF16) for i in range(2)]
    sgu = [C.T(f"sgu{i}", [128, 16, 256], F32) for i in range(2)]
    wdn = [C.T(f"wdn{i}", [128, 16, 512], BF16) for i in range(2)]
    sdn = [C.T(f"sdn{i}", [128, 16, 256], F32) for i in range(2)]
    actb = [C.T(f"actb{i}", [128, 16, TB], BF16) for i in range(1)]
    yacc, byacc = C.T("yacc", [128, 4, D], F32)
    yout = [C.T(f"yout{i}", [128, D], BF16) for i in range(1)]
    GTb = [C.T(f"GTb{i}", [NE, TB], F32) for i in range(1)]
    bdn4, bbdn4 = C.T("bdn4", [NE, D], F32)
    Gt, bGt = C.T("Gt", [128, NBLK * 4, NE], F32)
    bgu, bbgu = C.T("bgu", [128, NE, 16, 2], F32)
    gg = [C.T(f"gg{i}", [128, TB], F32) for i in range(1)]
    ss_ = [C.T(f"ss{i}", [128, TB], F32) for i in range(1)]
    uu = [C.T(f"uu{i}", [128, TB], F32) for i in range(1)]
    pg = [C.PS(f"pg{i}", [128, 512], F32) for i in range(2)]
    pu = [C.PS(f"pu{i}", [128, 512], F32) for i in range(2)]
    NPD = 2 if C.fused else 4
    pd = [C.PS(f"pd{i}", [128, 512], F32) for i in range(NPD)]
    if C.fused:
        pTx, bpTx = C.PS("pTx", [128, 2048], BF16)
        h2t = [C.T(f"h2t{i}", [128, D], BF16) for i in range(1)]

    C.load("sp", Gt[:], Gl.rearrange("(t p) e -> p t e", p=128), bGt)
    C.load("sp", bgu[:], b_gu, bbgu)
    C.load("sp", bdn4[:], b_dn, bbdn4)

    cnt = dict(wgu=0, wdn=0, act=0, pg=0, pd=0, ew=0, sgu=0, sdn=0)
    for blk in range(NBLK):
        hT_t, bhT_ = hTb[0]
        gT, bgT = GTb[0]
        if C.fused:
            for tt in range(4):
                h_t, bh_ = h2t[0]
                C.load("sp", h_t[:], h2_d[blk * TB + tt * 128: blk * TB + (tt + 1) * 128, :], bh_)
                for k in range(16):
                    C.tr(pTx[:, k * 128:(k + 1) * 128], h_t[:, k * 128:(k + 1) * 128], idB[:], [bh_, bidB], [bpTx])
                C.copy("act", hT_t[:, :, tt * 128:(tt + 1) * 128], pTx[:].rearrange("p (k c) -> p k c", k=16), [bpTx], [bhT_])
            pdt, bpd = pd[cnt["pd"] % NPD]
            cnt["pd"] += 1
            for tt in range(4):
                C.tr(pdt[0:NE, tt * 128:(tt + 1) * 128], Gt[:, blk * 4 + tt, :], idF[:], [bGt, bidF], [bpd])
            C.copy("dve", gT[:], pdt[0:NE, :], [bpd], [bgT])
        else:
            C.load("sp", hT_t[:], h2T[:, blk * TB:(blk + 1) * TB].rearrange("(k p) n -> p k n", p=128), bhT_)
            C.load("sp", gT[:], GlT[:, blk * TB:(blk + 1) * TB], bgT)
        for dc in range(4):
            for tt in range(4):
                pdt, bpd = pd[cnt["pd"] % NPD]
                cnt["pd"] += 1
                C.mm(pdt[:, :], gT[:, tt * 128:(tt + 1) * 128], bdn4[:, dc * 512:(dc + 1) * 512], True, True, [bgT, bbdn4], [bpd])
                C.copy("act", yacc[:, tt, dc * 512:(dc + 1) * 512], pdt[:, :], [bpd], [byacc])
        for e in range(NE):
            a_t, ba = actb[0]
            for ffc in range(16):
                wt, bw = wgu[cnt["wgu"] % 2]
                cnt["wgu"] += 1
                sg_, bsg_ = sgu[cnt["sgu"] % 2]
                cnt["sgu"] += 1
                C.load("sp", sg_[:], w_gu[e, :, ffc * 256:(ffc + 1) * 256].rearrange("(k p) n -> p k n", p=128), bsg_)
                C.copy("act", wt[:], sg_[:], [bsg_], [bw])
                i = cnt["pg"] % 2
                cnt["pg"] += 1
                pgt, bpg = pg[i]
                put, bpu = pu[i]
                for k in range(16):
                    C.mm(pgt[:, 0:TB], wt[:, k, 0:256:2], hT_t[:, k, :], k == 0, k == 15, [bw, bhT_], [bpg])
                for k in range(16):
                    C.mm(put[:, 0:TB], wt[:, k, 1:256:2], hT_t[:, k, :], k == 0, k == 15, [bw, bhT_], [bpu])
                j = 0
                cnt["ew"] += 1
                g_t, bg = gg[j]
                s_t, bs = ss_[j]
                u_t, bu = uu[j]
                C.ts("dve", g_t[:], pgt[:, 0:TB], bgu[:, e, ffc, 0:1], 7.0, ALU.add, ALU.min, [bpg, bbgu], [bg])
                C.act(s_t[:], g_t[:], ACTF.Sigmoid, [bg], [bs], scale=1.702)
                C.ts("dve", u_t[:], put[:, 0:TB], bgu[:, e, ffc, 1:2], 7.0, ALU.add, ALU.min, [bpu, bbgu], [bu])
                C.ts("pool", u_t[:], u_t[:], -7.0, 1.0, ALU.max, ALU.add, [bu], [bu])
                C.tt("pool", g_t[:], g_t[:], s_t[:], ALU.mult, [bg, bs], [bg])
                C.tt("dve", a_t[:, ffc, :], g_t[:], u_t[:], ALU.mult, [bg, bu], [ba])
            for dc in range(4):
                wd, bwd = wdn[cnt["wdn"] % 2]
                cnt["wdn"] += 1
                for hf in range(2):
                    sd_, bsd_ = sdn[cnt["sdn"] % 2]
                    cnt["sdn"] += 1
                    c0_ = dc * 512 + hf * 256
                    C.load("sp", sd_[:], w_dn[e, :, c0_:c0_ + 256].rearrange("(k p) n -> p k n", p=128), bsd_)
                    C.copy("act", wd[:, :, hf * 256:(hf + 1) * 256], sd_[:], [bsd_], [bwd])
                for tt in range(4):
                    pdt, bpd = pd[cnt["pd"] % NPD]
                    cnt["pd"] += 1
                    for ffc in range(16):
                        C.mm(pdt[:, :], a_t[:, ffc, tt * 128:(tt + 1) * 128], wd[:, ffc, :], ffc == 0, ffc == 15, [ba, bwd], [bpd])
                    gsc = Gt[:, blk * 4 + tt, e:e + 1]
                    ysl = yacc[:, tt, dc * 512:(dc + 1) * 512]
                    C.stt(ysl, pdt[:, :], gsc, ysl, ALU.mult, ALU.add, [bpd, bGt, byacc], [byacc])
        for tt in range(4):
            yo, byo = yout[0]
            C.copy("act", yo[:], yacc[:, tt, :], [byacc], [byo])
            C.store("sp", o_y[blk * TB + tt * 128: blk * TB + (tt + 1) * 128, :], yo[:], byo)
    C.finish()
    return nc


def build_combine(nc, NT=2048, NP=8, C=None):
    C = C or Ctx(nc)
    P = C.P
    x1 = C.din("x1", [NT, D])
    yp = C.din("yp", [NP, NT, D], BF16)
    gf = C.din("gf", [2, D])
    g_final = C.din("g_final", [1, D])
    o_y = C.dout("o_y", [NT, D])
    GF = [C.T(f"GF{i}", [128, D], F32) for i in range(2)]
    gfin, bgfin = C.T("gfin", [128, D], F32)
    xt = [C.T(f"xt{i}", [128, D], F32) for i in range(2)]
    ypt = [C.T(f"ypt{i}", [128, NP, D], BF16) for i in range(2)]
    acc, bacc_ = C.T("acc", [128, D], F32)
    junk, bjunk = C.T("junk", [128, D], BF16)
    ot = [C.T(f"ot{i}", [128, D], F32) for i in range(2)]
    st, bst = C.T("st", [128, 4], F32)
    for i in range(2):
        C.load("sp", GF[i][0][:], gf[i:i + 1, :].partition_broadcast(128), GF[i][1])
    C.load("sp", gfin[:], g_final[0:1, :].partition_broadcast(128), bgfin)
    ntile = NT // 128
    for t in range(ntile):
        x_t, bx = xt[t % 2]
        y_t, by = ypt[t % 2]
        o_t, bo = ot[t % 2]
        GFt, bGF = GF[0] if t < ntile // 2 else GF[1]
        C.load("sp", x_t[:], x1[t * 128:(t + 1) * 128, :], bx)
        C.load("sp", y_t[:], yp[:, t * 128:(t + 1) * 128, :].rearrange("j p d -> p j d"), by)
        if NP == 1:
            C.copy("dve", acc[:], y_t[:, 0, :], [by], [bacc_])
        else:
            C.tt("dve", acc[:], y_t[:, 0, :], y_t[:, 1, :], ALU.add, [by], [bacc_])
        for j in range(2, NP):
            C.tt("dve", acc[:], acc[:], y_t[:, j, :], ALU.add, [by, bacc_], [bacc_])
        C.tt("dve", acc[:], acc[:], GFt[:], ALU.mult, [bacc_, bGF], [bacc_])
        C.tt("dve", acc[:], acc[:], x_t[:], ALU.add, [bacc_, bx], [bacc_])
        C.act(junk[:], acc[:], ACTF.Square, [bacc_], [bjunk, bst], accum_out=st[:, 0:1])
        rstd_from_ss(C, st[:, 1:2], st[:, 0:1], D, bst, bst)
        C.stt(o_t[:], acc[:], st[:, 1:2], gfin[:], ALU.mult, ALU.mult, [bacc_, bst, bgfin], [bo])
        C.store("sp", o_y[t * 128:(t + 1) * 128, :], o_t[:], bo)
    C.finish()
    return nc


class Arena:
    def __init__(self, C, name, nbytes):
        self.t = C.P.sbuf(name, [128, nbytes // 2], BF16)
        self.cap = nbytes // 2
        self.off = 0
        self.peak = 0

    def mark(self):
        return self.off

    def reset(self, m):
        self.off = m

    def alloc(self, name, shape, dt):
        n = 1
        for d in shape[1:]:
            n *= d
        e16 = n * (2 if dt == F32 else 1)
        e16 = (e16 + 15) // 16 * 16
        assert self.off + e16 <= self.cap, (name, self.off, e16, self.cap)
        ap = self.t[:, self.off:self.off + e16]
        self.off += e16
        self.peak = max(self.peak, self.off)
        if dt == F32:
            ap = ap.bitcast(F32)
        ap = ap[:, 0:n]
        if len(shape) == 3:
            ap = ap.rearrange("p (a b) -> p a b", a=shape[1])
        elif len(shape) == 4:
            ap = ap.rearrange("p (a b c) -> p a b c", a=shape[1], b=shape[2])
        if shape[0] < 128:
            ap = ap[0:shape[0]]
        return ap, Buf(name)


def build_sample(nc, stop=99, NALLT=32, NPAIR=8, NHG=4, NMLAH=8, C=None):
    C = C or Ctx(nc)
    P = C.P
    x_own = C.din("x_own", [1024, D])
    x_halo = C.din("x_halo", [1792, D])
    x_all = C.din("x_all", [4096, D])
    ck_na = C.din("ck_na", [512, 1024])
    cv_na = C.din("cv_na", [512, 1024])
    c_ckv = C.din("c_ckv", [512, 256])
    c_krope = C.din("c_krope", [512, 64])
    mrow = C.din("mrow", [6, D])
    g_attn = C.din("g_attn", [1, D])
    g_ffn = C.din("g_ffn", [1, D])
    g_q_a = C.din("g_q_a", [1, 512])
    g_kv_a = C.din("g_kv_a", [1, 256])
    w_in = C.din("w_in", [D, 3904])
    w_in_rs = C.din("w_in_rs", [D, 64])
    w_q_b = C.din("w_q_b", [512, 1536])
    w_q_b_rs = C.din("w_q_b_rs", [512, 512])
    w_kv_b = C.din("w_kv_b", [256, 2048])
    w_out = C.din("w_out", [D, D])
    w_router = C.din("w_router", [D, 32])
    b_router = C.din("b_router", [1, 32])
    ident = C.din("ident", [128, 128])
    jmat = C.din("jmat", [128, 128])
    amat = C.din("amat", [2, 128])
    rmx = C.din("rmx", [8, 2, 896])
    colmask = C.din("colmask", [128, 15, 64])
    rpbpad = C.din("rpbpad", [8, 15, 160])
    cos_tok = C.din("cos_tok", [4096, 64])
    sinS_tok = C.din("sinS_tok", [4096, 64])
    cosT_own = C.din("cosT_own", [64, 1024])
    sinST_own = C.din("sinST_own", [64, 1024])
    o_x1 = C.dout("o_x1", [1024, D])
    o_h2 = C.dout("o_h2", [1024, D], BF16)
    o_G = C.dout("o_G", [1024, 32])
    ocat_d = C.scratch("ocat_d", [1024, D], BF16)
    bocat_d = Buf("ocat_d")

    SC_NA = 128 ** -0.5
    SC_MLA = 192 ** -0.5

    idF, bidF = C.T("idF", [128, 128], F32)
    idB, bidB = C.T("idB", [128, 128], BF16)
    jB, bjB = C.T("jB", [128, 128], BF16)
    aB, baB = C.T("aB", [2, 128], BF16)
    T0, bT0 = C.T("T0", [128, D], F32)
    T1, bT1 = C.T("T1", [128, D], F32)
    T2, bT2 = C.T("T2", [128, D], F32)
    gqa, bgqa = C.T("gqa", [128, 512], F32)
    gkva, bgkva = C.T("gkva", [128, 256], F32)
    brt, bbrt = C.T("brt", [128, 32], F32)
    wr, bwr = C.T("wr", [128, 16, 32], F32)
    ckvT, bckvT = C.T("ckvT", [128, 2, 4608], BF16)
    krT, bkrT = C.T("krT", [128, 4608], BF16)
    hTo, bhTo = C.T("hTo", [128, 16, 1024], BF16)
    xt, bxt = C.T("xt", [128, D], F32)
    tmpf, btmpf = C.T("tmpf", [128, D], F32)
    hb, bhb = C.T("hb", [128, D], BF16)
    hTt, bhTt = C.T("hTt", [128, 16, 128], BF16)
    st_ss, bst_ss = C.T("st_ss", [128, 8], F32)
    st_r, bst_r = C.T("st_r", [128, 8], F32)
    AR = C.arena if C.fused else Arena(C, "arena", 88 * 1024)

    pT, bpT = C.PS("pT", [128, 2048], BF16)
    pA = [C.PS(f"pA{i}", [128, 512], F32) for i in range(2)]
    pS = [C.PS(f"pS{i}", [128, 512], F32) for i in range(3)]
    pO, bpO = C.PS("pO", [128, 512], F32)

    pa_i = [0]

    def next_pA():
        i = pa_i[0] % 2
        pa_i[0] += 1
        return pA[i]

    def bload(dst, row_ap, buf, q="sp"):
        C.load(q, dst, row_ap.partition_broadcast(128), buf)

    C.load("sp", idF[:], ident, bidF)
    C.copy("dve", idB[:], idF[:], [bidF], [bidB])
    C.load("pool", jB[:], jmat, bjB)
    C.load("pool", aB[:], amat, baB)
    G1, bG1, SA, bSA = T0, bT0, T1, bT1
    bload(SA[:], mrow[0:1, :], bSA)
    bload(G1[:], mrow[1:2, :], bG1)
    bload(tmpf[:], g_attn[0:1, :], btmpf)
    C.stt(G1[:], G1[:], 1.0, tmpf[:], ALU.add, ALU.mult, [bG1, btmpf], [bG1])
    bload(gqa[:], g_q_a[0:1, :], bgqa)
    bload(gkva[:], g_kv_a[0:1, :], bgkva)
    bload(brt[:], b_router[0:1, :], bbrt)
    C.load("sp", wr[:], w_router.rearrange("(k p) n -> p k n", p=128), bwr)
    P.op("pool", lambda e: e.memset(krT[:], 0.0), writes=[bkrT])

    def make_h(src_rows):
        C.load("sp", xt[:], src_rows, bxt)
        C.act(hb[:], xt[:], ACTF.Square, [bxt], [bhb, bst_ss], accum_out=st_ss[:, 0:1])
        rstd_from_ss(C, st_r[:, 0:1], st_ss[:, 0:1], D, bst_ss, bst_r)
        C.stt(tmpf[:], xt[:], st_r[:, 0:1], G1[:], ALU.mult, ALU.mult, [bxt, bst_r, bG1], [btmpf])
        C.tt("dve", hb[:], tmpf[:], SA[:], ALU.add, [btmpf, bSA], [bhb])
        for k in range(16):
            C.tr(pT[:, k * 128:(k + 1) * 128], hb[:, k * 128:(k + 1) * 128], idB[:], [bhb, bidB], [bpT])

    m0 = AR.mark()
    w320, bw320 = AR.alloc("w320", [128, 16, 384], BF16)
    kvaf, bkvaf = AR.alloc("kvaf", [128, 256], F32)
    ckvb, bckvb = AR.alloc("ckvb", [128, 256], BF16)
    krb, bkrb = AR.alloc("krb", [128, 64], BF16)
    cst, bcst = AR.alloc("cst", [128, 2, 64], F32)
    kr1, bkr1 = AR.alloc("kr1", [128, 64], F32)
    kr2, bkr2 = AR.alloc("kr2", [128, 64], F32)
    ccf, bccf = AR.alloc("ccf", [128, 320], F32)
    C.load("pool", w320[:, :, 0:320], w_in[:, 3584:3904].rearrange("(k p) n -> p k n", p=128), bw320)
    C.load("pool", w320[:, :, 320:384], w_in_rs.rearrange("(k p) n -> p k n", p=128), bw320)
    for t in range(NALLT):
        make_h(x_all[t * 128:(t + 1) * 128, :])
        C.copy("act", hTt[:], pT[:].rearrange("p (k c) -> p k c", k=16), [bpT], [bhTt])
        C.load("sp", cst[:, 0, :], cos_tok[t * 128:(t + 1) * 128, :], bcst)
        C.load("sp", cst[:, 1, :], sinS_tok[t * 128:(t + 1) * 128, :], bcst)
        pa, bpa = next_pA()
        for k in range(16):
            C.mm(pa[:, 0:384], hTt[:, k, :], w320[:, k, :], k == 0, k == 15, [bhTt, bw320], [bpa])
        C.copy("dve", kvaf[:], pa[:, 0:256], [bpa], [bkvaf])
        C.tt("dve", kr1[:], pa[:, 256:320], cst[:, 0, :], ALU.mult, [bpa, bcst], [bkr1])
        C.tt("dve", kr2[:], pa[:, 320:384], cst[:, 1, :], ALU.mult, [bpa, bcst], [bkr2])
        C.tt("dve", krb[:], kr1[:], kr2[:], ALU.add, [bkr1, bkr2], [bkrb])
        C.act(hb[:, 0:256], kvaf[:], ACTF.Square, [bkvaf], [bhb, bst_ss], accum_out=st_ss[:, 2:3])
        rstd_from_ss(C, st_r[:, 2:3], st_ss[:, 2:3], 256, bst_ss, bst_r)
        C.stt(ckvb[:], kvaf[:], st_r[:, 2:3], gkva[:], ALU.mult, ALU.mult, [bkvaf, bst_r, bgkva], [bckvb])
        for c in range(2):
            C.tr(pT[:, c * 128:(c + 1) * 128], ckvb[:, c * 128:(c + 1) * 128], idB[:], [bckvb, bidB], [bpT])
        C.tr(pT[0:64, 256:384], krb[:], idB[:], [bkrb, bidB], [bpT])
        C.copy("act", ckvT[:, :, t * 128:(t + 1) * 128], pT[:, 0:256].rearrange("p (k c) -> p k c", k=2), [bpT], [bckvT])
        C.copy("dve", krT[0:64, t * 128:(t + 1) * 128], pT[0:64, 256:384], [bpT], [bkrT])
    for t in range(4):
        C.load("sp", ccf[:, 0:256], c_ckv[t * 128:(t + 1) * 128, :], bccf)
        C.load("sp", ccf[:, 256:320], c_krope[t * 128:(t + 1) * 128, :], bccf)
        C.copy("dve", hb[:, 0:320], ccf[:], [bccf], [bhb])
        for c in range(2):
            C.tr(pT[:, c * 128:(c + 1) * 128], hb[:, c * 128:(c + 1) * 128], idB[:], [bhb, bidB], [bpT])
        C.tr(pT[0:64, 256:384], hb[:, 256:320], idB[:], [bhb, bidB], [bpT])
        C.copy("act", ckvT[:, :, 4096 + t * 128:4096 + (t + 1) * 128], pT[:, 0:256].rearrange("p (k c) -> p k c", k=2), [bpT], [bckvT])
        C.copy("dve", krT[0:64, 4096 + t * 128:4096 + (t + 1) * 128], pT[0:64, 256:384], [bpT], [bkrT])
    P.barrier()
    AR.reset(m0)
    if stop <= 1:
        dbg = C.dout("dbg", [128, 3, 4608], BF16)
        C.store("sp", dbg[:, 0:2, :], ckvT[:], bckvT)
        C.store("sp", dbg[:, 2, :], krT[:], bkrT)
        C.finish()
        return nc

    for t in range(8):
        make_h(x_own[t * 128:(t + 1) * 128, :])
        C.copy("act", hTo[:, :, t * 128:(t + 1) * 128], pT[:].rearrange("p (k c) -> p k c", k=16), [bpT], [bhTo])

    m1 = AR.mark()
    Bstat, bBstat = AR.alloc("Bstat", [128, 8, 896], BF16)
    mB = AR.mark()
    Bfull, bBfull = AR.alloc("Bfull", [128, 8, 15, 64], F32)
    cmk, bcmk = AR.alloc("cmk", [128, 15, 64], F32)
    C.load("sp", cmk[:], colmask, bcmk)
    rp_t = rpbpad.tensor
    for hd in range(8):
        for half in range(2):
            src = bass.AP(rp_t, hd * 2400 + 16, [[1, 64], [160, 15], [1, 64]])
            C.load("sp", Bfull[half * 64:(half + 1) * 64, hd, :, :], src, bBfull)
    for hd in range(8):
        C.tt("dve", Bfull[:, hd, :, :], Bfull[:, hd, :, :], cmk[:], ALU.add, [bBfull, bcmk], [bBfull])
        C.copy("act", Bstat[0:64, hd, :].rearrange("p (j c) -> p j c", j=14), Bfull[0:64, hd, 1:15, :], [bBfull], [bBstat])
        C.copy("act", Bstat[64:128, hd, :].rearrange("p (j c) -> p j c", j=14), Bfull[64:128, hd, 0:14, :], [bBfull], [bBstat])
    P.barrier()
    AR.reset(mB)

    mG = AR.mark()
    for g in range(NHG):
        AR.reset(mG)
        wk, bwk = AR.alloc("wk", [128, 16, 256], BF16)
        wv, bwv = AR.alloc("wv", [128, 16, 256], BF16)
        wq, bwq = AR.alloc("wq", [128, 16, 256], BF16)
        KTh, bKTh = AR.alloc("KTh", [128, 2, 1792], BF16)
        Vh, bVh = AR.alloc("Vh", [128, 14, 256], BF16)
        cKT, bcKT = AR.alloc("cKT", [128, 2, 512], BF16)
        cV, bcV = AR.alloc("cV", [128, 4, 256], BF16)
        QTg, bQTg = AR.alloc("QTg", [128, 2, 1024], BF16)
        kbt, bkbt = AR.alloc("kbt", [128, 256], BF16)
        ccn, bccn = AR.alloc("ccn", [128, 2, 256], F32)
        ccb, bccb = AR.alloc("ccb", [128, 256], BF16)
        rmt = [AR.alloc(f"rmt{i}", [2, 896], BF16) for i in range(2)]
        Pb = [AR.alloc(f"Pb{i}", [128, 1408], BF16) for i in range(2)]
        PTt = [AR.alloc(f"PTt{i}", [128, 11, 128], BF16) for i in range(2)]
        otl = [AR.alloc(f"otl{i}", [128, 256], BF16) for i in range(2)]
        amx = [AR.alloc(f"amx{i}", [128, 8], F32) for i in range(2)]
        c0 = g * 256
        C.load("pool", wq[:], w_in[:, c0:c0 + 256].rearrange("(k p) n -> p k n", p=128), bwq)
        C.load("pool", wk[:], w_in[:, 1024 + c0:1024 + c0 + 256].rearrange("(k p) n -> p k n", p=128), bwk)
        C.load("pool", wv[:], w_in[:, 2048 + c0:2048 + c0 + 256].rearrange("(k p) n -> p k n", p=128), bwv)
        for t in range(14):
            make_h(x_halo[t * 128:(t + 1) * 128, :])
            C.copy("act", hTt[:], pT[:].rearrange("p (k c) -> p k c", k=16), [bpT], [bhTt])
            pa, bpa = next_pA()
            for k in range(16):
                C.mm(pa[:, 0:256], hTt[:, k, :], wk[:, k, :], k == 0, k == 15, [bhTt, bwk], [bpa])
            C.copy("act", kbt[:], pa[:, 0:256], [bpa], [bkbt])
            pa, bpa = next_pA()
            for k in range(16):
                C.mm(pa[:, 0:256], hTt[:, k, :], wv[:, k, :], k == 0, k == 15, [bhTt, bwv], [bpa])
            C.copy("dve", Vh[:, t, :], pa[:, 0:256], [bpa], [bVh])
            for hh in range(2):
                C.tr(pT[:, hh * 128:(hh + 1) * 128], kbt[:, hh * 128:(hh + 1) * 128], idB[:], [bkbt, bidB], [bpT])
            C.copy("act", KTh[:, :, t * 128:(t + 1) * 128], pT[:, 0:256].rearrange("p (k c) -> p k c", k=2), [bpT], [bKTh])
        for t in range(4):
            C.load("sp", ccn[:, 0, :], ck_na[t * 128:(t + 1) * 128, c0:c0 + 256], bccn)
            C.load("sp", ccn[:, 1, :], cv_na[t * 128:(t + 1) * 128, c0:c0 + 256], bccn)
            C.copy("dve", ccb[:], ccn[:, 0, :], [bccn], [bccb])
            C.copy("act", cV[:, t, :], ccn[:, 1, :], [bccn], [bcV])
            for hh in range(2):
                C.tr(pT[:, hh * 128:(hh + 1) * 128], ccb[:, hh * 128:(hh + 1) * 128], idB[:], [bccb, bidB], [bpT])
            C.copy("act", cKT[:, :, t * 128:(t + 1) * 128], pT[:, 0:256].rearrange("p (k c) -> p k c", k=2), [bpT], [bcKT])
        for hh in range(2):
            for half in range(2):
                pa, bpa = next_pA()
                for k in range(16):
                    C.mm(pa[:, :], wq[:, k, hh * 128:(hh + 1) * 128], hTo[:, k, half * 512:(half + 1) * 512], k == 0, k == 15, [bwq, bhTo], [bpa])
                C.act(QTg[:, hh, half * 512:(half + 1) * 512], pa[:, :], ACTF.Copy, [bpa], [bQTg], scale=SC_NA)
        ui = 0
        for p in range(NPAIR):
            rm_t, brm = rmt[p % 2]
            C.load("pool", rm_t[:], rmx[p], brm)
            ot_t, bot = otl[p % 2]
            for hh in range(2):
                hd = g * 2 + hh
                pb, bpb = Pb[ui % 2]
                ptt, bptt = PTt[ui % 2]
                am, bam = amx[ui % 2]
                ui += 1
                q_l = QTg[:, hh, p * 128:(p + 1) * 128]
                k0 = p * 128
                segs = [(pS[0], 0, 512), (pS[1], 512, 384)]
                for (ps, bps), o, n in segs:
                    C.mm(ps[:, 0:n], q_l, KTh[:, hh, k0 + o:k0 + o + n], True, False, [bQTg, bKTh], [bps])
                    C.mm(ps[:, 0:n], jB[:], Bstat[:, hd, o:o + n], False, False, [bjB, bBstat], [bps])
                    C.mm(ps[:, 0:n], aB[:], rm_t[:, o:o + n], False, True, [baB, brm], [bps])
                ps2, bps2 = pS[2]
                C.mm(ps2[:, 0:512], q_l, cKT[:, hh, :], True, True, [bQTg, bcKT], [bps2])
                for i, ((ps, bps), n) in enumerate(((pS[0], 512), (pS[1], 384), (pS[2], 512))):
                    P.op("dve", lambda e, ps=ps, n=n, i=i, am=am: e.tensor_reduce(out=am[:, i:i + 1], in_=ps[:, 0:n], axis=AX.X, op=ALU.max),
                         reads=[bps], writes=[bam])
                P.op("dve", lambda e, am=am: e.tensor_reduce(out=am[:, 3:4], in_=am[:, 0:3], axis=AX.X, op=ALU.max, negate=True), reads=[bam], writes=[bam])
                for i, ((ps, bps), o, n) in enumerate(((pS[0], 0, 512), (pS[1], 512, 384), (pS[2], 896, 512))):
                    C.act(pb[:, o:o + n], ps[:, 0:n], ACTF.Exp, [bps, bam], [bpb, bam], bias=am[:, 3:4], scale=1.0, accum_out=am[:, 4 + i:5 + i])
                P.op("dve", lambda e, am=am: e.tensor_reduce(out=am[:, 7:8], in_=am[:, 4:7], axis=AX.X, op=ALU.add), reads=[bam], writes=[bam])
                P.op("dve", lambda e, am=am: e.reciprocal(out=am[:, 7:8], in_=am[:, 7:8]), reads=[bam], writes=[bam])
                for j in range(11):
                    C.tr(pT[:, j * 128:(j + 1) * 128], pb[:, j * 128:(j + 1) * 128], idB[:], [bpb, bidB], [bpT])
                C.copy("act", ptt[:, 0:6, :], pT[:, 0:768].rearrange("p (k c) -> p k c", k=6), [bpT], [bptt])
                C.copy("dve", ptt[:, 6:11, :], pT[:, 768:1408].rearrange("p (k c) -> p k c", k=5), [bpT], [bptt])
                for j in range(7):
                    C.mm(pO[:, 0:128], ptt[:, j, :], Vh[:, p + j, hh * 128:(hh + 1) * 128], j == 0, False, [bptt, bVh], [bpO])
                for j in range(4):
                    C.mm(pO[:, 0:128], ptt[:, 7 + j, :], cV[:, j, hh * 128:(hh + 1) * 128], False, j == 3, [bptt, bcV], [bpO])
                C.ts("dve", ot_t[:, hh * 128:(hh + 1) * 128], pO[:, 0:128], am[:, 7:8], None, ALU.mult, None, [bpO, bam], [bot])
            P.dma("sp", lambda e, ot_t=ot_t, p=p, c0=c0: e.dma_start(out=ocat_d[p * 128:(p + 1) * 128, c0:c0 + 256], in_=ot_t[:]),
                  reads=[bot], writes=[bocat_d], key=bot, store=True)
        P.barrier()
    AR.reset(m1)
    if stop <= 2:
        C.finish()
        return nc

    qanT, bqanT = AR.alloc("qanT", [128, 4, 1024], BF16)
    mQ = AR.mark()
    wqa, bwqa = AR.alloc("wqa", [128, 16, 512], BF16)
    C.load("pool", wqa[:], w_in[:, 3072:3584].rearrange("(k p) n -> p k n", p=128), bwqa)
    qaf, bqaf = AR.alloc("qaf", [128, 512], F32)
    qan, bqan = AR.alloc("qan", [128, 512], BF16)
    for t in range(8):
        pa, bpa = next_pA()
        for k in range(16):
            C.mm(pa[:, :], hTo[:, k, t * 128:(t + 1) * 128], wqa[:, k, :], k == 0, k == 15, [bhTo, bwqa], [bpa])
        C.copy("dve", qaf[:], pa[:, :], [bpa], [bqaf])
        C.act(hb[:, 0:512], qaf[:], ACTF.Square, [bqaf], [bhb, bst_ss], accum_out=st_ss[:, 1:2])
        rstd_from_ss(C, st_r[:, 1:2], st_ss[:, 1:2], 512, bst_ss, bst_r)
        C.stt(qan[:], qaf[:], st_r[:, 1:2], gqa[:], ALU.mult, ALU.mult, [bqaf, bst_r, bgqa], [bqan])
        for c in range(4):
            C.tr(pT[:, c * 128:(c + 1) * 128], qan[:, c * 128:(c + 1) * 128], idB[:], [bqan, bidB], [bpT])
        C.copy("act", qanT[:, :, t * 128:(t + 1) * 128], pT[:, 0:512].rearrange("p (k c) -> p k c", k=4), [bpT], [bqanT])
    P.barrier()
    AR.reset(mQ)
    wqb, bwqb = AR.alloc("wqb", [128, 4, 1536], BF16)
    wqbr, bwqbr = AR.alloc("wqbr", [128, 4, 512], BF16)
    wkvb, bwkvb = AR.alloc("wkvb", [128, 2, 2048], BF16)
    csT, bcsT = AR.alloc("csT", [64, 2, 1024], F32)
    C.load("pool", wqb[:], w_q_b.rearrange("(k p) n -> p k n", p=128), bwqb)
    C.load("pool", wqbr[:], w_q_b_rs.rearrange("(k p) n -> p k n", p=128), bwqbr)
    C.load("pool", wkvb[:], w_kv_b.rearrange("(k p) n -> p k n", p=128), bwkvb)
    C.load("sp", csT[:, 0, :], cosT_own, bcsT)
    C.load("sp", csT[:, 1, :], sinST_own, bcsT)
    qnh, bqnh = AR.alloc("qnh", [128, 1024], BF16)
    qrh, bqrh = AR.alloc("qrh", [128, 1024], BF16)
    knh, bknh = AR.alloc("knh", [128, 4608], BF16)
    vh, bvh = AR.alloc("vh", [128, 36, 128], BF16)
    r1, br1 = AR.alloc("r1", [64, 512], F32)
    r2, br2 = AR.alloc("r2", [64, 512], F32)
    Pm = [AR.alloc(f"Pm{i}", [128, 512], BF16) for i in range(2)]
    PTm = [AR.alloc(f"PTm{i}", [128, 4, 128], BF16) for i in range(2)]
    om = [AR.alloc(f"om{i}", [128, 128], BF16) for i in range(2)]
    mst = [AR.alloc(f"mst{i}", [128, 24], F32) for i in range(2)]
    P.op("pool", lambda e: e.memset(qrh[:], 0.0), writes=[bqrh])
    ci = 0
    for hd in range(NMLAH):
        for half in range(2):
            sl = slice(half * 512, (half + 1) * 512)
            pa, bpa = next_pA()
            for c in range(4):
                C.mm(pa[:, :], wqb[:, c, hd * 192:hd * 192 + 128], qanT[:, c, sl], c == 0, c == 3, [bwqb, bqanT], [bpa])
            C.act(qnh[:, sl], pa[:, :], ACTF.Copy, [bpa], [bqnh], scale=SC_MLA)
            pa, bpa = next_pA()
            for c in range(4):
                C.mm(pa[0:64, :], wqb[:, c, hd * 192 + 128:hd * 192 + 192], qanT[:, c, sl], c == 0, c == 3, [bwqb, bqanT], [bpa])
            C.stt(r1[:], pa[0:64, :], SC_MLA, csT[:, 0, sl], ALU.mult, ALU.mult, [bpa, bcsT], [br1])
            pa, bpa = next_pA()
            for c in range(4):
                C.mm(pa[0:64, :], wqbr[:, c, hd * 64:(hd + 1) * 64], qanT[:, c, sl], c == 0, c == 3, [bwqbr, bqanT], [bpa])
            C.stt(r2[:], pa[0:64, :], SC_MLA, csT[:, 1, sl], ALU.mult, ALU.mult, [bpa, bcsT], [br2])
            C.tt("dve", qrh[0:64, sl], r1[:], r2[:], ALU.add, [br1, br2], [bqrh])
        for ch in range(9):
            pa, bpa = next_pA()
            for c in range(2):
                C.mm(pa[:, :], wkvb[:, c, hd * 256:hd * 256 + 128], ckvT[:, c, ch * 512:(ch + 1) * 512], c == 0, c == 1, [bwkvb, bckvT], [bpa])
            C.copy("act", knh[:, ch * 512:(ch + 1) * 512], pa[:, :], [bpa], [bknh])
        for kt4 in range(9):
            pa, bpa = next_pA()
            for j in range(4):
                kt = kt4 * 4 + j
                for c in range(2):
                    C.mm(pa[:, j * 128:(j + 1) * 128], ckvT[:, c, kt * 128:(kt + 1) * 128], wkvb[:, c, hd * 256 + 128:hd * 256 + 256], c == 0, c == 1, [bckvT, bwkvb], [bpa])
            C.copy("dve", vh[:, kt4 * 4:(kt4 + 1) * 4, :], pa[:, :].rearrange("p (k c) -> p k c", k=4), [bpa], [bvh])
        for qt in range(8):
            ms, bms = mst[qt % 2]
            o_t, bo = om[qt % 2]
            qs = slice(qt * 128, (qt + 1) * 128)
            for ch in range(9):
                ps, bps = pS[ch % 3]
                C.mm(ps[:, :], qnh[:, qs], knh[:, ch * 512:(ch + 1) * 512], True, False, [bqnh, bknh], [bps])
                C.mm(ps[:, :], qrh[:, qs], krT[:, ch * 512:(ch + 1) * 512], False, True, [bqrh, bkrT], [bps])
                P.op("dve", lambda e, ps=ps, ms=ms, ch=ch: e.tensor_reduce(out=ms[:, ch:ch + 1], in_=ps[:, :], axis=AX.X, op=ALU.max), reads=[bps], writes=[bms])
            P.op("dve", lambda e, ms=ms: e.tensor_reduce(out=ms[:, 9:10], in_=ms[:, 0:9], axis=AX.X, op=ALU.max, negate=True), reads=[bms], writes=[bms])
            for ch in range(9):
                ps, bps = pS[ch % 3]
                pm, bpm = Pm[ci % 2]
                ptm, bptm = PTm[ci % 2]
                ci += 1
                C.mm(ps[:, :], qnh[:, qs], knh[:, ch * 512:(ch + 1) * 512], True, False, [bqnh, bknh], [bps])
                C.mm(ps[:, :], qrh[:, qs], krT[:, ch * 512:(ch + 1) * 512], False, True, [bqrh, bkrT], [bps])
                C.act(pm[:], ps[:, :], ACTF.Exp, [bps, bms], [bpm, bms], bias=ms[:, 9:10], scale=1.0, accum_out=ms[:, 10 + ch:11 + ch])
                for j in range(4):
                    C.tr(pT[:, j * 128:(j + 1) * 128], pm[:, j * 128:(j + 1) * 128], idB[:], [bpm, bidB], [bpT])
                C.copy("act" if ch % 2 else "dve", ptm[:], pT[:, 0:512].rearrange("p (k c) -> p k c", k=4), [bpT], [bptm])
                for j in range(4):
                    kt = ch * 4 + j
                    C.mm(pO[:, 0:128], ptm[:, j, :], vh[:, kt, :], kt == 0, kt == 35, [bptm, bvh], [bpO])
            P.op("dve", lambda e, ms=ms: e.tensor_reduce(out=ms[:, 20:21], in_=ms[:, 10:19], axis=AX.X, op=ALU.add), reads=[bms], writes=[bms])
            P.op("dve", lambda e, ms=ms: e.reciprocal(out=ms[:, 20:21], in_=ms[:, 20:21]), reads=[bms], writes=[bms])
            C.ts("dve", o_t[:], pO[:, 0:128], ms[:, 20:21], None, ALU.mult, None, [bpO, bms], [bo])
            P.dma("sp", lambda e, o_t=o_t, qt=qt, hd=hd: e.dma_start(out=ocat_d[qt * 128:(qt + 1) * 128, 1024 + hd * 128:1024 + (hd + 1) * 128], in_=o_t[:]),
                  reads=[bo], writes=[bocat_d], key=bo, store=True)
    P.barrier()
    AR.reset(m1)
    if stop <= 3:
        C.finish()
        return nc

    GA, bGA, G2, bG2, SF, bSF = T0, bT0, T1, bT1, T2, bT2
    bload(GA[:], mrow[2:3, :], bGA)
    bload(SF[:], mrow[3:4, :], bSF)
    bload(G2[:], mrow[4:5, :], bG2)
    bload(tmpf[:], g_ffn[0:1, :], btmpf)
    C.stt(G2[:], G2[:], 1.0, tmpf[:], ALU.add, ALU.mult, [bG2, btmpf], [bG2])
    xt2 = [AR.alloc(f"xt2_{i}", [128, D], F32) for i in range(2)]
    oct_ = [AR.alloc(f"oct{i}", [128, D], BF16) for i in range(2)]
    ocT, bocT = AR.alloc("ocT", [128, 16, 256], BF16)
    ws = [AR.alloc(f"ws{i}", [128, 16, 256], BF16) for i in range(2)]
    stg = [AR.alloc(f"stg{i}", [128, 256], F32) for i in range(2)]
    h2f, bh2f = AR.alloc("h2f", [128, D], F32)
    h2b, bh2b = AR.alloc("h2b", [128, D], BF16)
    h2T, bh2T = AR.alloc("h2T", [128, 16, 128], F32)
    lg, blg = AR.alloc("lg", [128, 32], F32)
    lg2, blg2 = AR.alloc("lg2", [128, 32], F32)
    top8, btop8 = AR.alloc("top8", [128, 8], F32)
    wsi = [0]
    sgi = [0]
    for b in range(4):
        r0 = b * 256
        for t in range(2):
            x_t, bx = xt2[t]
            oc_t, boc = oct_[t]
            C.load("sp", x_t[:], x_own[r0 + t * 128:r0 + (t + 1) * 128, :], bx)
            P.dma("sp", lambda e, oc_t=oc_t, r0=r0, t=t: e.dma_start(out=oc_t[:], in_=ocat_d[r0 + t * 128:r0 + (t + 1) * 128, :]),
                  reads=[bocat_d], writes=[boc])
            for k in range(16):
                C.tr(pT[:, k * 128:(k + 1) * 128], oc_t[:, k * 128:(k + 1) * 128], idB[:], [boc, bidB], [bpT])
            C.copy("act", ocT[:, :, t * 128:(t + 1) * 128], pT[:].rearrange("p (k c) -> p k c", k=16), [bpT], [bocT])
        for s in range(8):
            wt, bw = ws[wsi[0] % 2]
            wsi[0] += 1
            C.load("pool", wt[:], w_out[:, s * 256:(s + 1) * 256].rearrange("(k p) n -> p k n", p=128), bw)
            for t in range(2):
                x_t, bx = xt2[t]
                pa, bpa = next_pA()
                for k in range(16):
                    C.mm(pa[:, 0:256], ocT[:, k, t * 128:(t + 1) * 128], wt[:, k, :], k == 0, k == 15, [bw, bocT], [bpa])
                sg, bsg = stg[sgi[0] % 2]
                sgi[0] += 1
                C.tt("dve", sg[:], pa[:, 0:256], GA[:, s * 256:(s + 1) * 256], ALU.mult, [bpa, bGA], [bsg])
                C.tt("dve", x_t[:, s * 256:(s + 1) * 256], x_t[:, s * 256:(s + 1) * 256], sg[:], ALU.add, [bx, bsg], [bx])
        for t in range(2):
            x_t, bx = xt2[t]
            C.store("sp", o_x1[r0 + t * 128:r0 + (t + 1) * 128, :], x_t[:], bx)
            C.act(hb[:], x_t[:], ACTF.Square, [bx], [bhb, bst_ss], accum_out=st_ss[:, 3:4])
            rstd_from_ss(C, st_r[:, 3:4], st_ss[:, 3:4], D, bst_ss, bst_r)
            C.stt(tmpf[:], x_t[:], st_r[:, 3:4], G2[:], ALU.mult, ALU.mult, [bx, bst_r, bG2], [btmpf])
            C.tt("dve", h2f[:], tmpf[:], SF[:], ALU.add, [btmpf, bSF], [bh2f])
            C.copy("act", h2b[:], h2f[:], [bh2f], [bh2b])
            C.store("sp", o_h2[r0 + t * 128:r0 + (t + 1) * 128, :], h2b[:], bh2b)
            for g4 in range(4):
                pa, bpa = next_pA()
                for j in range(4):
                    k = g4 * 4 + j
                    C.tr(pa[:, j * 128:(j + 1) * 128], h2f[:, k * 128:(k + 1) * 128], idF[:], [bh2f, bidF], [bpa])
                C.copy("dve", h2T[:, g4 * 4:(g4 + 1) * 4, :], pa[:, 0:512].rearrange("p (k c) -> p k c", k=4), [bpa], [bh2T])
            pa, bpa = next_pA()
            for k in range(16):
                C.mm(pa[:, 0:32], h2T[:, k, :], wr[:, k, :], k == 0, k == 15, [bh2T, bwr], [bpa])
            C.tt("dve", lg[:], pa[:, 0:32], brt[:], ALU.add, [bpa, bbrt], [blg])
            P.op("dve", lambda e: e.max(out=top8[:], in_=lg[:]), reads=[blg], writes=[btop8])
            C.ts("dve", lg2[:], lg[:], top8[:, 3:4], None, ALU.is_ge, None, [blg, btop8], [blg2])
            C.ts("dve", top8[:, 7:8], top8[:, 0:1], -1.0, None, ALU.mult, None, [btop8], [btop8])
            C.act(lg[:], lg[:], ACTF.Exp, [blg, btop8], [blg], bias=top8[:, 7:8], scale=1.0)
            C.tt("dve", lg[:], lg[:], lg2[:], ALU.mult, [blg, blg2], [blg])
            P.op("dve", lambda e: e.tensor_reduce(out=top8[:, 6:7], in_=lg[:], axis=AX.X, op=ALU.add), reads=[blg], writes=[btop8])
            P.op("dve", lambda e: e.reciprocal(out=top8[:, 6:7], in_=top8[:, 6:7]), reads=[btop8], writes=[btop8])
            C.ts("dve", lg2[:], lg[:], top8[:, 6:7], None, ALU.mult, None, [blg, btop8], [blg2])
            C.store("sp", o_G[r0 + t * 128:r0 + (t + 1) * 128, :], lg2[:], blg2)
    C.finish()
    return nc


def build_fused(nc):
    C = Ctx(nc, fused=True)
    P = C.P
    C.ps_pool = P.psum("pspool", [128, 4096], F32)[:]
    idF_t = P.sbuf("c_idF", [128, 128], F32)
    idB_t = P.sbuf("c_idB", [128, 128], BF16)
    bidF, bidB = Buf("c_idF"), Buf("c_idB")
    ident = C.din("ident", [128, 128])
    C.load("sp", idF_t[:], ident, bidF)
    C.copy("dve", idB_t[:], idF_t[:], [bidF], [bidB])
    C.consts = (idB_t, bidB, idF_t, bidF)
    C.arena = Arena(C, "arena", 206 * 1024)
    m_d = C.scratch("m_d", [2, 6 * D], F32)
    x1_d = C.scratch("x1_d", [2048, D], F32)
    h2_d = C.scratch("h2_d", [2048, D], BF16)
    G_d = C.scratch("G_d", [2048, 32], F32)
    y_d = C.scratch("y_d", [2048, D], BF16)

    def phase_end():
        P.barrier()
        P.recycle_dma_sems()
        C.arena.reset(0)
        C.ps_off = 0

    C.dram["o_m"] = m_d
    build_mod(nc, NCOL=6 * D, C=C)
    phase_end()
    C.dram["mrow"] = m_d[0:1, :].rearrange("o (s d) -> (o s) d", s=6)
    C.dram["o_x1"], C.dram["o_h2"], C.dram["o_G"] = x1_d[0:1024, :], h2_d[0:1024, :], G_d[0:1024, :]
    build_prompt(nc, NB=4, C=C)
    phase_end()
    C.dram["mrow"] = m_d[1:2, :].rearrange("o (s d) -> (o s) d", s=6)
    C.dram["o_x1"], C.dram["o_h2"], C.dram["o_G"] = x1_d[1024:2048, :], h2_d[1024:2048, :], G_d[1024:2048, :]
    build_sample(nc, C=C)
    phase_end()
    C.dram["h2_d"], C.dram["G_d"], C.dram["o_y"] = h2_d, G_d, y_d
    build_experts(nc, NTOK=2048, NE=32, C=C)
    phase_end()
    del C.dram["o_y"]
    C.dram["x1"] = x1_d
    C.dram["yp"] = y_d.rearrange("(o n) d -> o n d", o=1)
    C.dram["gf"] = m_d[:, 5 * D:6 * D]
    build_combine(nc, NT=2048, NP=1, C=C)
    P.emit()
    return nc


def rope_tables():
    T = 4096
    pos = np.arange(T); rows = (pos // 64).astype(np.float32); cols = (pos % 64).astype(np.float32)
    inv = (10000.0 ** (-(np.arange(16, dtype=np.float32) * 2.0 / 32))).astype(np.float32)
    ar = rows[:, None] * inv; ac = cols[:, None] * inv
    ang = np.concatenate([ar, ar, ac, ac], -1)
    cos, sin = np.cos(ang).astype(np.float32), np.sin(ang).astype(np.float32)
    sign = np.concatenate([-np.ones(16), np.ones(16), -np.ones(16), np.ones(16)]).astype(np.float32)
    return cos, sin * sign

SWAP = np.concatenate([np.arange(16, 32), np.arange(0, 16), np.arange(48, 64), np.arange(32, 48)])

def sample_consts(r0):
    ident = np.eye(128, dtype=np.float32)
    jmat = np.zeros((128, 128), np.float32)
    for p in range(128):
        jmat[p, (p // 64) * 64 + 63 - p % 64] = 1.0
    amat = np.zeros((2, 128), np.float32)
    amat[0, :64] = 1.0; amat[1, 64:] = 1.0
    rmx = np.zeros((8, 2, 14, 64), np.float32)
    for p in range(8):
        for rho in range(2):
            r = r0 + 2 * p + rho
            rs = min(max(r - 4, 0), 56)
            for j in range(14):
                kr = r0 + 2 * p - 6 + j
                ok = (0 <= kr < 64) and (rs <= kr < rs + 8)
                if not ok:
                    rmx[p, rho, j, :] = NEG
    colmask = np.zeros((128, 15, 64), np.float32)
    for pp in range(128):
        c = 63 - pp % 64
        cs = min(max(c - 8, 0), 48)
        m = np.full(64, NEG, np.float32); m[cs:cs + 16] = 0.0
        colmask[pp, :, :] = m
    return dict(ident=ident, jmat=jmat, amat=amat, rmx=rmx.reshape(8, 2, 896), colmask=colmask)

def sample_inputs(z, mrow_b, b, r0):
    cos, sinS = rope_tables()
    xs = z['x_sample'][b]
    halo = np.zeros((28, 64, 2048), np.float32)
    for i in range(28):
        r = r0 - 6 + i
        if 0 <= r < 64:
            halo[i] = xs[r * 64:(r + 1) * 64]
    rpbpad = np.zeros((8, 15, 160), np.float32)
    rpbpad[:, :, 64:95] = z['na_rpb'][0]
    wqb = z['w_q_b'][0]
    wqb_rs = np.concatenate([wqb[:, h * 192 + 128:h * 192 + 192][:, SWAP] for h in range(8)], 1)
    own = slice(r0 * 64, r0 * 64 + 1024)
    d = dict(x_own=np.ascontiguousarray(xs[own]), x_halo=halo.reshape(1792, 2048), x_all=xs,
             ck_na=z['cache_na_k'][b, 0].reshape(512, 1024), cv_na=z['cache_na_v'][b, 0].reshape(512, 1024),
             c_ckv=z['cache_mla_ckv'][b, 0], c_krope=z['cache_mla_krope'][b, 0],
             mrow=np.ascontiguousarray(mrow_b.reshape(6, 2048)),
             g_attn=z['g_attn'], g_ffn=z['g_ffn'], g_q_a=z['g_q_a'], g_kv_a=z['g_kv_a'],
             w_in=z['w_in'][0], w_in_rs=np.ascontiguousarray(z['w_in'][0][:, 3840:3904][:, SWAP]),
             w_q_b=wqb, w_q_b_rs=np.ascontiguousarray(wqb_rs), w_kv_b=z['w_kv_b'][0], w_out=z['w_out'][0],
             w_router=z['w_router'][0], b_router=z['b_router'], rpbpad=rpbpad,
             cos_tok=cos, sinS_tok=sinS, cosT_own=np.ascontiguousarray(cos[own].T), sinST_own=np.ascontiguousarray(sinS[own].T))
    d.update(sample_consts(r0))
    return d


def fused_inputs(z, core):
    f32 = np.float32
    b, r0 = core // 4, 16 * (core % 4)
    c2 = np.stack([np.asarray(z['c_ctx'], f32), np.asarray(z['c'], f32)[b]], 0)
    d = sample_inputs(z, np.zeros(6 * 2048, f32), b, r0)
    del d['mrow']
    d.update(cT=np.ascontiguousarray(c2.T.reshape(16, 128, 2).transpose(1, 0, 2)),
             wm=np.asarray(z['w_mod'], f32)[0], bm=np.asarray(z['b_mod'], f32),
             xp=np.ascontiguousarray(np.asarray(z['x_prompt'], f32)[4 * core:4 * core + 4].reshape(1024, 2048)),
             w_gu=np.asarray(z['w_gate_up'], f32)[0],
             b_gu=np.ascontiguousarray(np.asarray(z['b_gate_up'], f32)[0].reshape(32, 16, 128, 2).transpose(2, 0, 1, 3)),
             w_dn=np.asarray(z['w_down'], f32)[0], b_dn=np.asarray(z['b_down'], f32)[0],
             g_final=np.asarray(z['g_final'], f32)[None])
    return d


from concourse.bass_utils import run_bass_kernel_spmd

NCORES = 8


def kernel(x_prompt, x_sample, cache_na_k, cache_na_v, cache_mla_ckv, cache_mla_krope, c, c_ctx,
           g_attn, g_ffn, g_final, w_mod, b_mod, w_in, w_out, na_rpb, g_q_a, w_q_b, g_kv_a, w_kv_b,
           w_router, b_router, w_gate_up, b_gate_up, w_down, b_down):
    f32 = np.float32
    z = dict(x_prompt=x_prompt, x_sample=x_sample, cache_na_k=cache_na_k, cache_na_v=cache_na_v, cache_mla_ckv=cache_mla_ckv,
             cache_mla_krope=cache_mla_krope, c=c, c_ctx=c_ctx, g_attn=g_attn, g_ffn=g_ffn, g_final=g_final, w_mod=w_mod, b_mod=b_mod,
             w_in=w_in, w_out=w_out, na_rpb=na_rpb, g_q_a=g_q_a, w_q_b=w_q_b, g_kv_a=g_kv_a, w_kv_b=w_kv_b, w_router=w_router,
             b_router=b_router, w_gate_up=w_gate_up, b_gate_up=b_gate_up, w_down=w_down, b_down=b_down)
    z = {k: np.asarray(v, f32) for k, v in z.items()}
    nc = bass.Bass("TRN2", target_bir_lowering=False)
    build_fused(nc)
    in_maps = [fused_inputs(z, i) for i in range(NCORES)]
    res = run_bass_kernel_spmd(nc, in_maps, core_ids=list(range(NCORES))).results
    y = [np.asarray(r["o_y"], f32) for r in res]
    y_prompt = np.concatenate([t[:1024] for t in y], 0).reshape(32, 256, 2048)
    y_sample = np.concatenate([t[1024:] for t in y], 0).reshape(2, 4096, 2048)
    new_na_k = np.concatenate([np.asarray(r["o_nak"], f32) for r in res], 0).reshape(32, 1, 256, 8, 128)
    new_na_v = np.concatenate([np.asarray(r["o_nav"], f32) for r in res], 0).reshape(32, 1, 256, 8, 128)
    new_ckv = np.concatenate([np.asarray(r["o_ckv"], f32) for r in res], 0).reshape(32, 1, 256, 256)
    new_krope = np.concatenate([np.asarray(r["o_krope"], f32) for r in res], 0).reshape(32, 1, 256, 64)
    return (y_prompt, y_sample, new_na_k, new_na_v, new_ckv, new_krope)
```

```python
import numpy as np
import concourse.bass as bass
import concourse.mybir as mybir
from contextlib import ExitStack

F32 = mybir.dt.float32
BF16 = mybir.dt.bfloat16
I32 = mybir.dt.int32
U32 = mybir.dt.uint32
ALU = mybir.AluOpType
ACTF = mybir.ActivationFunctionType
AX = mybir.AxisListType

ENGS = ("pe", "act", "dve", "pool", "sp")
SEM_ROTATE = 12000


class Buf:
    __slots__ = ("name", "last_w", "readers", "ld", "st", "excl")

    def __init__(self, name, excl=False):
        self.name = name
        self.excl = excl
        self.last_w = None
        self.readers = []
        self.ld = None
        self.st = None


class DmaSem:
    __slots__ = ("sem", "count", "kind")

    def __init__(self, sem, kind):
        self.sem = sem
        self.count = 0
        self.kind = kind


class Op:
    __slots__ = ("eng", "fn", "waits", "is_dma", "dsem", "dval", "signaled", "sig", "idx", "dma_waits", "inc")

    def __init__(self, eng, fn, is_dma):
        self.eng = eng
        self.fn = fn
        self.is_dma = is_dma
        self.waits = {}
        self.dma_waits = {}
        self.dsem = None
        self.dval = 0
        self.signaled = False
        self.sig = None
        self.idx = 0
        self.inc = 16


class Prog:
    def __init__(self, nc):
        self.nc = nc
        self.ops = {e: [] for e in ENGS}
        self.all_ops = []
        self.stack = ExitStack()
        self.sem_pool = []
        self.n_sems = 0
        self.dma_sems = []
        self.free_dsems = []
        self._n = 0

    def new_sem(self):
        self.n_sems += 1
        return self.stack.enter_context(self.nc.semaphore(f"s{self.n_sems}"))

    def sbuf(self, name, shape, dtype):
        t = self.stack.enter_context(self.nc.sbuf_tensor(name, list(shape), dtype))
        return t

    def psum(self, name, shape, dtype):
        t = self.stack.enter_context(self.nc.psum_tensor(name, list(shape), dtype))
        return t

    def _dep(self, op, prod):
        if prod is None or prod is op:
            return
        if prod.is_dma:
            ds = prod.dsem
            op.dma_waits[id(ds)] = (ds, ds.count)
        else:
            if prod.eng == op.eng and not op.is_dma and op.eng == "pe":
                return
            cur = op.waits.get(prod.eng)
            if cur is None or cur.idx < prod.idx:
                op.waits[prod.eng] = prod
            prod.signaled = True

    def _record(self, op, reads, writes):
        self._n += 1
        op.idx = self._n
        ex = [b for b in reads if b.excl]
        if ex:
            reads = [b for b in reads if not b.excl]
            writes = list(writes) + [b for b in ex if b not in writes]
        for b in reads:
            self._dep(op, b.last_w)
        for b in writes:
            self._dep(op, b.last_w)
            for r in b.readers:
                self._dep(op, r)
        for b in reads:
            b.readers.append(op)
        for b in writes:
            b.last_w = op
            b.readers = []
        self.ops[op.eng].append(op)
        self.all_ops.append(op)
        return op

    def op(self, eng, fn, reads=(), writes=()):
        return self._record(Op(eng, fn, False), reads, writes)

    def dma(self, eng, fn, reads=(), writes=(), key=None, store=False, inc=16):
        op = Op(eng, fn, True)
        if key is None:
            key = writes[0] if not store else reads[0]
        attr = "st" if store else "ld"
        ds = getattr(key, attr)
        if ds is None:
            kind = "sw" if eng == "pool" else "hw"
            fl = [d for d in self.free_dsems if d.kind == kind]
            if fl:
                ds = fl[-1]
                self.free_dsems.remove(ds)
            else:
                ds = DmaSem(self.new_sem(), kind)
                self.dma_sems.append(ds)
            setattr(key, attr, ds)
        op.dsem = ds
        op.inc = inc
        self._record(op, reads, writes)
        ds.count += inc
        op.dval = ds.count
        return op

    def recycle_dma_sems(self):
        self.free_dsems = list(self.dma_sems)

    def coll(self, fn, reads=(), writes=()):
        if not hasattr(self, "_csem"):
            self._csem = self.new_sem()
            self._ccnt = 0
            self._cscr = self.sbuf("coll_scr", [128, 8], F32)
        self._ccnt += 1
        n = self._ccnt
        csem = self._csem
        scr = self._cscr

        def run(eng, fn=fn, n=n):
            fn(eng).then_inc(csem)
            eng.wait_ge(csem, n)
            return eng.memset(scr[:], 0.0)
        return self.op("pool", run, reads=reads, writes=writes)

    def barrier(self, bufs=()):
        lastc = {}
        for e in ENGS:
            lastc[e] = None
            for q in reversed(self.ops[e]):
                if not q.is_dma and q.fn is not None:
                    lastc[e] = q
                    break
        tot = [(ds, ds.count) for ds in self.dma_sems if ds.count > 0]
        for e in ENGS:
            op = Op(e, None, False)
            self._n += 1
            op.idx = self._n
            for e2 in ENGS:
                q = lastc[e2]
                if q is not None and (e2 != e or e != "pe"):
                    op.waits[e2] = q
                    q.signaled = True
            for ds, c in tot:
                op.dma_waits[id(ds)] = (ds, c)
            self.ops[e].append(op)
            self.all_ops.append(op)

    def emit(self):
        nc = self.nc
        for e in ENGS:
            sem = None
            cnt = 0
            for op in self.ops[e]:
                if op.is_dma or not op.signaled or op.fn is None:
                    continue
                if sem is None or cnt >= SEM_ROTATE:
                    sem = self.new_sem()
                    cnt = 0
                cnt += 1
                op.sig = (sem, cnt)
        prog = self

        def run(engname, eng):
            for op in prog.ops[engname]:
                for p in op.waits.values():
                    s, v = p.sig
                    eng.wait_ge(s, v)
                for ds, v in op.dma_waits.values():
                    eng.wait_ge(ds.sem, v)
                if op.fn is None:
                    continue
                ins = op.fn(eng)
                if op.is_dma:
                    ins.then_inc(op.dsem.sem, op.inc)
                elif op.signaled:
                    ins.then_inc(op.sig[0], 1)

        with nc.Block() as block:
            @block.tensor
            def _(eng):
                run("pe", eng)

            @block.scalar
            def _(eng):
                run("act", eng)

            @block.vector
            def _(eng):
                run("dve", eng)

            @block.gpsimd
            def _(eng):
                run("pool", eng)

            @block.sync
            def _(eng):
                run("sp", eng)
                for ds in prog.dma_sems:
                    if ds.count > 0:
                        eng.wait_ge(ds.sem, ds.count)
        self.stack.close()


D = 2048
EPS = 1e-6
NEG = -30000.0


class Ctx:
    def __init__(self, nc, fused=False):
        self.nc = nc
        self.P = Prog(nc)
        self._cnt = 0
        self.fused = fused
        self.dram = {}
        self.arena = None
        self.ps_pool = None
        self.ps_off = 0

    def T(self, name, shape, dt):
        if self.fused:
            return self.arena.alloc(name, shape, dt)
        t = self.P.sbuf(name, shape, dt)
        b = Buf(name)
        return t, b

    def PS(self, name, shape, dt):
        if self.fused:
            n = 1
            for d in shape[1:]:
                n *= d
            nbytes = n * (4 if dt == F32 else 2)
            nb = (nbytes + 2047) // 2048
            assert self.ps_off + nb <= 8, ("psum", name)
            ap = self.ps_pool[:, self.ps_off * 512:(self.ps_off + nb) * 512]
            self.ps_off += nb
            if dt != F32:
                ap = ap.bitcast(dt)
            ap = ap[:, 0:n]
            if len(shape) == 3:
                ap = ap.rearrange("p (a b) -> p a b", a=shape[1])
            return ap, Buf(name, excl=True)
        t = self.P.psum(name, shape, dt)
        b = Buf(name, excl=True)
        return t, b

    def din(self, name, shape, dt=F32):
        if name in self.dram:
            return self.dram[name]
        ap = self.nc.dram_tensor(name, list(shape), dt, kind="ExternalInput").ap()
        if self.fused:
            self.dram[name] = ap
        return ap

    def dout(self, name, shape, dt=F32):
        if name in self.dram:
            return self.dram[name]
        return self.nc.dram_tensor(name, list(shape), dt, kind="ExternalOutput").ap()

    def scratch(self, name, shape, dt):
        return self.nc.dram_tensor(name, list(shape), dt).ap()

    def finish(self):
        if not self.fused:
            self.P.emit()

    def load(self, q, out_ap, in_ap, wbuf, reads=()):
        return self.P.dma(q, lambda e: e.dma_start(out=out_ap, in_=in_ap), reads=list(reads), writes=[wbuf])

    def store(self, q, out_ap, in_ap, rbuf, writes=()):
        return self.P.dma(q, lambda e: e.dma_start(out=out_ap, in_=in_ap), reads=[rbuf], writes=list(writes), key=rbuf, store=True)

    def mm(self, out, lhsT, rhs, start, stop, reads, writes):
        return self.P.op("pe", lambda e: e.matmul(out, lhsT=lhsT, rhs=rhs, start=start, stop=stop), reads=reads, writes=writes)

    def tr(self, out, in_, ident, reads, writes):
        return self.P.op("pe", lambda e: e.transpose(out=out, in_=in_, identity=ident), reads=reads, writes=writes)

    def act(self, out, in_, func, reads, writes, bias=None, scale=None, accum_out=None):
        kw = {}
        if bias is not None:
            kw["bias"] = bias
        if scale is not None:
            kw["scale"] = scale
        if accum_out is not None:
            kw["accum_out"] = accum_out
        return self.P.op("act", lambda e: e.activation(out=out, in_=in_, func=func, **kw), reads=reads, writes=writes)

    def ts(self, eng, out, in0, s1, s2, op0, op1, reads, writes):
        if op1 is None:
            return self.P.op(eng, lambda e: e.tensor_scalar(out=out, in0=in0, scalar1=s1, scalar2=None, op0=op0), reads=reads, writes=writes)
        return self.P.op(eng, lambda e: e.tensor_scalar(out=out, in0=in0, scalar1=s1, scalar2=s2, op0=op0, op1=op1), reads=reads, writes=writes)

    def tt(self, eng, out, in0, in1, op, reads, writes):
        return self.P.op(eng, lambda e: e.tensor_tensor(out=out, in0=in0, in1=in1, op=op), reads=reads, writes=writes)

    def stt(self, out, in0, scalar, in1, op0, op1, reads, writes):
        return self.P.op("dve", lambda e: e.scalar_tensor_tensor(out=out, in0=in0, scalar=scalar, in1=in1, op0=op0, op1=op1), reads=reads, writes=writes)

    def copy(self, eng, out, in_, reads, writes):
        if eng == "act":
            return self.P.op("act", lambda e: e.copy(out=out, in_=in_), reads=reads, writes=writes)
        return self.P.op(eng, lambda e: e.tensor_copy(out=out, in_=in_), reads=reads, writes=writes)


def rstd_from_ss(C, rstd, ss, n, bss, brstd):
    C.ts("dve", rstd, ss, 1.0 / n, EPS, ALU.mult, ALU.add, [bss], [brstd])
    C.act(rstd, rstd, ACTF.Sqrt, [brstd], [brstd])
    C.P.op("dve", lambda e: e.reciprocal(out=rstd, in_=rstd), reads=[brstd], writes=[brstd])


def build_prompt(nc, NB=4, do_tail=True, stop=99, sub=99, C=None):
    C = C or Ctx(nc)
    P = C.P
    NT = NB * 256
    xp = C.din("xp", [NT, D])
    mrow = C.din("mrow", [6, D])
    g_attn = C.din("g_attn", [1, D])
    g_ffn = C.din("g_ffn", [1, D])
    g_q_a = C.din("g_q_a", [1, 512])
    g_kv_a = C.din("g_kv_a", [1, 256])
    w_in = C.din("w_in", [D, 3904])
    w_q_b = C.din("w_q_b", [512, 1536])
    w_kv_b = C.din("w_kv_b", [256, 2048])
    w_out = C.din("w_out", [D, D])
    w_router = C.din("w_router", [D, 32])
    b_router = C.din("b_router", [1, 32])
    ident = C.din("ident", [128, 128])
    o_nak = C.dout("o_nak", [NT, 1024])
    o_nav = C.dout("o_nav", [NT, 1024])
    o_ckv = C.dout("o_ckv", [NT, 256])
    o_krope = C.dout("o_krope", [NT, 64])
    o_x1 = C.dout("o_x1", [NT, D])
    o_h2 = C.dout("o_h2", [NT, D], BF16)
    o_G = C.dout("o_G", [NT, 32])

    idF, bidF = C.T("idF", [128, 128], F32)
    idB, bidB = C.T("idB", [128, 128], BF16)
    G1, bG1 = C.T("G1", [128, D], F32)
    SA, bSA = C.T("SA", [128, D], F32)
    GA, bGA = C.T("GA", [128, D], F32)
    G2, bG2 = C.T("G2", [128, D], F32)
    SF, bSF = C.T("SF", [128, D], F32)
    gqa, bgqa = C.T("gqa", [128, 512], F32)
    gkva, bgkva = C.T("gkva", [128, 256], F32)
    brt, bbrt = C.T("brt", [128, 32], F32)
    wr, bwr = C.T("wr", [128, 16, 32], F32)
    wqb, bwqb = C.T("wqb", [128, 4, 1536], BF16)
    wkvb, bwkvb = C.T("wkvb", [128, 2, 2048], BF16)
    xt = [C.T(f"xt{t}", [128, D], F32) for t in range(2)]
    tmpf, btmpf = C.T("tmpf", [128, D], F32)
    hb, bhb = C.T("hb", [128, D], BF16)
    hT, bhT = C.T("hT", [128, 16, 256], BF16)
    NWS = 2
    ws = [C.T(f"ws{i}", [128, 16, 256], BF16) for i in range(NWS)]
    QT, bQT = C.T("QT", [128, 8, 256], BF16)
    Kb, bKb = C.T("Kb", [128, 2, 1024], BF16)
    Vb, bVb = C.T("Vb", [128, 2, 1024], BF16)
    KT, bKT = C.T("KT", [128, 8, 256], BF16)
    qaf, bqaf = C.T("qaf", [128, 2, 512], F32)
    qan, bqan = C.T("qan", [128, 2, 512], BF16)
    qanT, bqanT = C.T("qanT", [128, 4, 256], BF16)
    kvaf, bkvaf = C.T("kvaf", [128, 2, 256], F32)
    ckvf, bckvf = C.T("ckvf", [128, 2, 256], F32)
    ckvb, bckvb = C.T("ckvb", [128, 2, 256], BF16)
    ckvT, bckvT = C.T("ckvT", [128, 2, 256], BF16)
    krf, bkrf = C.T("krf", [128, 2, 64], F32)
    krb, bkrb = C.T("krb", [128, 2, 64], BF16)
    krT, bkrT = C.T("krT", [128, 256], BF16)
    qnT, bqnT = C.T("qnT", [128, 8, 256], BF16)
    qrT, bqrT = C.T("qrT", [128, 8, 256], BF16)
    knT, bknT = C.T("knT", [128, 8, 256], BF16)
    vm, bvm = C.T("vm", [128, 2, 1024], BF16)
    ocat, bocat = C.T("ocat", [128, 2, D], BF16)
    ocT, bocT = C.T("ocT", [128, 16, 256], BF16)
    stg = [C.T(f"stg{i}", [128, 256], F32) for i in range(2)]
    Pb = [C.T(f"Pb{i}", [128, 256], BF16) for i in range(2)]
    PT = [C.T(f"PT{i}", [128, 2, 128], BF16) for i in range(2)]
    st_ss, bst_ss = C.T("st_ss", [128, 8], F32)
    st_r, bst_r = C.T("st_r", [128, 8], F32)
    amx = [C.T(f"amx{i}", [128, 4], F32) for i in range(2)]
    h2f, bh2f = C.T("h2f", [128, D], F32)
    h2b, bh2b = C.T("h2b", [128, D], BF16)
    h2T, bh2T = C.T("h2T", [128, 16, 128], F32)
    lg, blg = C.T("lg", [128, 32], F32)
    lg2, blg2 = C.T("lg2", [128, 32], F32)
    top8, btop8 = C.T("top8", [128, 8], F32)

    pT, bpT = C.PS("pT", [128, 2048], BF16)
    pA = [C.PS(f"pA{i}", [128, 512], F32) for i in range(2)]
    pS = [C.PS(f"pS{i}", [128, 512], F32) for i in range(2)]
    pTp_full, bpTp = C.PS("pTp", [128, 8, 128], BF16)
    pTp = pTp_full[:, 0:2, :]
    pO, bpO = C.PS("pO", [128, 512], F32)

    C.load("sp", idF[:], ident, bidF)
    C.copy("dve", idB[:], idF[:], [bidF], [bidB])

    def bload(dst, row_ap, buf):
        C.load("sp", dst[:], row_ap.partition_broadcast(128), buf)

    bload(SA, mrow[0:1, :], bSA)
    bload(G1, mrow[1:2, :], bG1)
    bload(tmpf, g_attn[0:1, :], btmpf)
    C.stt(G1[:], G1[:], 1.0, tmpf[:], ALU.add, ALU.mult, [bG1, btmpf], [bG1])
    bload(GA, mrow[2:3, :], bGA)
    bload(SF, mrow[3:4, :], bSF)
    bload(G2, mrow[4:5, :], bG2)
    bload(h2f, g_ffn[0:1, :], bh2f)
    C.stt(G2[:], G2[:], 1.0, h2f[:], ALU.add, ALU.mult, [bG2, bh2f], [bG2])
    bload(gqa, g_q_a[0:1, :], bgqa)
    bload(gkva, g_kv_a[0:1, :], bgkva)
    bload(brt, b_router[0:1, :], bbrt)
    C.load("sp", wr[:], w_router.rearrange("(k p) n -> p k n", p=128), bwr)
    C.load("pool", wqb[:], w_q_b.rearrange("(k p) n -> p k n", p=128), bwqb)
    C.load("pool", wkvb[:], w_kv_b.rearrange("(k p) n -> p k n", p=128), bwkvb)

    P.op("pool", lambda e: e.memset(qrT[:], 0.0), writes=[bqrT])
    P.op("pool", lambda e: e.memset(krT[:], 0.0), writes=[bkrT])
    if stop <= 0:
        C.finish()
        return nc
    ws_i = [0]

    def next_ws(src_cols_ap, ncols):
        i = ws_i[0] % NWS
        ws_i[0] += 1
        t, b = ws[i]
        C.load("pool", t[:, :, 0:ncols], src_cols_ap.rearrange("(k p) n -> p k n", p=128), b)
        return t, b

    pa_i = [0]

    def next_pA():
        i = pa_i[0] % 2
        pa_i[0] += 1
        return pA[i]

    stg_i = [0]

    def next_stg():
        i = stg_i[0] % 2
        stg_i[0] += 1
        return stg[i]

    SC_NA = 128 ** -0.5
    SC_MLA = 192 ** -0.5

    def rmsnorm_tile(src, bsrc, col):
        n = src.shape[-1] if len(src.shape) == 2 else None
        C.act(hb[:, 0:src.shape[1]], src, ACTF.Square, [bsrc], [bhb, bst_ss], accum_out=st_ss[:, col:col + 1])

    for b in range(NB):
        r0 = b * 256
        for t in range(2):
            x_t, bx = xt[t]
            C.load("sp", x_t[:], xp[r0 + t * 128: r0 + (t + 1) * 128, :], bx)
            C.act(hb[:], x_t[:], ACTF.Square, [bx], [bhb, bst_ss], accum_out=st_ss[:, 0:1])
            rstd_from_ss(C, st_r[:, 0:1], st_ss[:, 0:1], D, bst_ss, bst_r)
            C.stt(tmpf[:], x_t[:], st_r[:, 0:1], G1[:], ALU.mult, ALU.mult, [bx, bst_r, bG1], [btmpf])
            C.tt("dve", hb[:], tmpf[:], SA[:], ALU.add, [btmpf, bSA], [bhb])
            for k in range(16):
                C.tr(pT[:, k * 128:(k + 1) * 128], hb[:, k * 128:(k + 1) * 128], idB[:], [bhb, bidB], [bpT])
            C.copy("act", hT[:, :, t * 128:(t + 1) * 128], pT[:].rearrange("p (k c) -> p k c", k=16), [bpT], [bhT])

        if stop <= 1:
            continue
        for s in range(4):
            wt, bw = next_ws(w_in[:, s * 256:(s + 1) * 256], 256)
            for hh in range(2):
                head = s * 2 + hh
                pa, bpa = next_pA()
                for k in range(16):
                    C.mm(pa[:, 0:256], wt[:, k, hh * 128:(hh + 1) * 128], hT[:, k, :], k == 0, k == 15, [bw, bhT], [bpa])
                C.act(QT[:, head, :], pa[:, 0:256], ACTF.Copy, [bpa], [bQT], scale=SC_NA)
        if stop == 2 and sub <= 0:
            continue
        for which, (obuf, sb_t, sb_b) in enumerate(((o_nak, Kb, bKb), (o_nav, Vb, bVb))):
            for s in range(4):
                c0 = 1024 * (1 + which) + s * 256
                wt, bw = next_ws(w_in[:, c0:c0 + 256], 256)
                for t in range(2):
                    pa, bpa = next_pA()
                    for k in range(16):
                        C.mm(pa[:, 0:256], hT[:, k, t * 128:(t + 1) * 128], wt[:, k, :], k == 0, k == 15, [bw, bhT], [bpa])
                    sg, bsg = next_stg()
                    C.copy("dve", sg[:], pa[:, 0:256], [bpa], [bsg])
                    C.copy("act", sb_t[:, t, s * 256:(s + 1) * 256], pa[:, 0:256], [bpa], [sb_b])
                    C.store("sp", obuf[r0 + t * 128: r0 + (t + 1) * 128, s * 256:(s + 1) * 256], sg[:], bsg)
        if stop == 2 and sub <= 1:
            continue
        for s in range(2):
            c0 = 3072 + s * 256
            wt, bw = next_ws(w_in[:, c0:c0 + 256], 256)
            for t in range(2):
                pa, bpa = next_pA()
                for k in range(16):
                    C.mm(pa[:, 0:256], hT[:, k, t * 128:(t + 1) * 128], wt[:, k, :], k == 0, k == 15, [bw, bhT], [bpa])
                C.copy("dve", qaf[:, t, s * 256:(s + 1) * 256], pa[:, 0:256], [bpa], [bqaf])
        for t in range(2):
            C.act(hb[:, 0:512], qaf[:, t, :], ACTF.Square, [bqaf], [bhb, bst_ss], accum_out=st_ss[:, 1:2])
            rstd_from_ss(C, st_r[:, 1:2], st_ss[:, 1:2], 512, bst_ss, bst_r)
            C.stt(qan[:, t, :], qaf[:, t, :], st_r[:, 1:2], gqa[:], ALU.mult, ALU.mult, [bqaf, bst_r, bgqa], [bqan])
        if stop == 2 and sub <= 2:
            continue
        wt, bw = next_ws(w_in[:, 3584:3840], 256)
        for t in range(2):
            pa, bpa = next_pA()
            for k in range(16):
                C.mm(pa[:, 0:256], hT[:, k, t * 128:(t + 1) * 128], wt[:, k, :], k == 0, k == 15, [bw, bhT], [bpa])
            C.copy("dve", kvaf[:, t, :], pa[:, 0:256], [bpa], [bkvaf])
            C.act(hb[:, 0:256], kvaf[:, t, :], ACTF.Square, [bkvaf], [bhb, bst_ss], accum_out=st_ss[:, 2:3])
            rstd_from_ss(C, st_r[:, 2:3], st_ss[:, 2:3], 256, bst_ss, bst_r)
            C.stt(ckvf[:, t, :], kvaf[:, t, :], st_r[:, 2:3], gkva[:], ALU.mult, ALU.mult, [bkvaf, bst_r, bgkva], [bckvf])
            C.copy("act", ckvb[:, t, :], ckvf[:, t, :], [bckvf], [bckvb])
        C.store("sp", o_ckv[r0:r0 + 256, :].rearrange("(t p) c -> p t c", p=128), ckvf[:], bckvf)
        if stop == 2 and sub <= 3:
            continue
        wt, bw = next_ws(w_in[:, 3840:3904], 64)
        for t in range(2):
            pa, bpa = next_pA()
            for k in range(16):
                C.mm(pa[:, 0:64], hT[:, k, t * 128:(t + 1) * 128], wt[:, k, 0:64], k == 0, k == 15, [bw, bhT], [bpa])
            C.copy("dve", krf[:, t, :], pa[:, 0:64], [bpa], [bkrf])
            C.copy("act", krb[:, t, :], pa[:, 0:64], [bpa], [bkrb])
        C.store("sp", o_krope[r0:r0 + 256, :].rearrange("(t p) c -> p t c", p=128), krf[:], bkrf)

        if stop <= 2:
            continue
        for t in range(2):
            for hd in range(8):
                C.tr(pT[:, hd * 128:(hd + 1) * 128], Kb[:, t, hd * 128:(hd + 1) * 128], idB[:], [bKb, bidB], [bpT])
            C.copy("act", KT[:, :, t * 128:(t + 1) * 128], pT[:, 0:1024].rearrange("p (k c) -> p k c", k=8), [bpT], [bKT])
        for t in range(2):
            for c in range(4):
                C.tr(pT[:, c * 128:(c + 1) * 128], qan[:, t, c * 128:(c + 1) * 128], idB[:], [bqan, bidB], [bpT])
            for c in range(2):
                C.tr(pT[:, (4 + c) * 128:(5 + c) * 128], ckvb[:, t, c * 128:(c + 1) * 128], idB[:], [bckvb, bidB], [bpT])
            C.tr(pT[0:64, 6 * 128:7 * 128], krb[:, t, :], idB[:], [bkrb, bidB], [bpT])
            C.copy("act", qanT[:, :, t * 128:(t + 1) * 128], pT[:, 0:512].rearrange("p (k c) -> p k c", k=4), [bpT], [bqanT])
            C.copy("dve", ckvT[:, :, t * 128:(t + 1) * 128], pT[:, 512:768].rearrange("p (k c) -> p k c", k=2), [bpT], [bckvT])
            C.copy("dve", krT[0:64, t * 128:(t + 1) * 128], pT[0:64, 768:896], [bpT], [bkrT])
        for hd in range(8):
            pa, bpa = next_pA()
            for c in range(4):
                C.mm(pa[:, 0:256], wqb[:, c, hd * 192: hd * 192 + 128], qanT[:, c, :], c == 0, c == 3, [bwqb, bqanT], [bpa])
            C.act(qnT[:, hd, :], pa[:, 0:256], ACTF.Copy, [bpa], [bqnT], scale=SC_MLA)
            pa, bpa = next_pA()
            for c in range(4):
                C.mm(pa[0:64, 0:256], wqb[:, c, hd * 192 + 128: hd * 192 + 192], qanT[:, c, :], c == 0, c == 3, [bwqb, bqanT], [bpa])
            C.act(qrT[0:64, hd, :], pa[0:64, 0:256], ACTF.Copy, [bpa], [bqrT], scale=SC_MLA)
            pa, bpa = next_pA()
            for c in range(2):
                C.mm(pa[:, 0:256], wkvb[:, c, hd * 256: hd * 256 + 128], ckvT[:, c, :], c == 0, c == 1, [bwkvb, bckvT], [bpa])
            C.copy("dve", knT[:, hd, :], pa[:, 0:256], [bpa], [bknT])
        for t in range(2):
            for half in range(2):
                pa, bpa = next_pA()
                for c in range(2):
                    rhs = wkvb[:, c, :].rearrange("p (h x) -> p h x", h=8)[:, half * 4:(half + 1) * 4, 128:256]
                    C.mm(pa[:, 0:512], ckvT[:, c, t * 128:(t + 1) * 128], rhs, c == 0, c == 1, [bwkvb, bckvT], [bpa])
                C.copy("act", vm[:, t, half * 512:(half + 1) * 512], pa[:, 0:512], [bpa], [bvm])

        if stop <= 3:
            continue
        ai = [0]

        def attend(t, score_ops, vtile, bv, vcol, ocol):
            i = ai[0] % 2
            ai[0] += 1
            ps, bps = pS[i]
            pb, bpb = Pb[i]
            ptt, bptt = PT[i]
            am, bam = amx[i]
            n = len(score_ops)
            for j, (lt, rh, rd) in enumerate(score_ops):
                C.mm(ps[:, 0:256], lt, rh, j == 0, j == n - 1, rd, [bps])
            P.op("dve", lambda e: e.tensor_reduce(out=am[:, 0:1], in_=ps[:, 0:256], axis=AX.X, op=ALU.max, negate=True),
                 reads=[bps], writes=[bam])
            C.act(pb[:], ps[:, 0:256], ACTF.Exp, [bps, bam], [bpb, bam], bias=am[:, 0:1], scale=1.0, accum_out=am[:, 1:2])
            P.op("dve", lambda e: e.reciprocal(out=am[:, 2:3], in_=am[:, 1:2]), reads=[bam], writes=[bam])
            for j in range(2):
                C.tr(pTp_full[:, j, :], pb[:, j * 128:(j + 1) * 128], idB[:], [bpb, bidB], [bpTp])
            C.copy("dve", ptt[:], pTp_full[:, 0:2, :], [bpTp], [bptt])
            for j in range(2):
                C.mm(pO[:, 0:128], ptt[:, j, :], vtile[:, j, vcol:vcol + 128], j == 0, j == 1, [bptt, bv], [bpO])
            C.ts("dve", ocat[:, t, ocol:ocol + 128], pO[:, 0:128], am[:, 2:3], None, ALU.mult, None, [bpO, bam], [bocat])

        for hd in range(8):
            for t in range(2):
                attend(t, [(QT[:, hd, t * 128:(t + 1) * 128], KT[:, hd, :], [bQT, bKT])], Vb, bVb, hd * 128, hd * 128)
        for hd in range(8):
            for t in range(2):
                attend(t, [(qnT[:, hd, t * 128:(t + 1) * 128], knT[:, hd, :], [bqnT, bknT]),
                           (qrT[:, hd, t * 128:(t + 1) * 128], krT[:, :], [bqrT, bkrT])], vm, bvm, hd * 128, 1024 + hd * 128)

        if not do_tail:
            for t in range(2):
                C.copy("dve", h2b[:], ocat[:, t, :], [bocat], [bh2b])
                C.store("sp", o_h2[r0 + t * 128: r0 + (t + 1) * 128, :], h2b[:], bh2b)
            continue

        for t in range(2):
            for k in range(16):
                C.tr(pT[:, k * 128:(k + 1) * 128], ocat[:, t, k * 128:(k + 1) * 128], idB[:], [bocat, bidB], [bpT])
            C.copy("act", ocT[:, :, t * 128:(t + 1) * 128], pT[:].rearrange("p (k c) -> p k c", k=16), [bpT], [bocT])
        for s in range(8):
            wt, bw = next_ws(w_out[:, s * 256:(s + 1) * 256], 256)
            for t in range(2):
                x_t, bx = xt[t]
                pa, bpa = next_pA()
                for k in range(16):
                    C.mm(pa[:, 0:256], ocT[:, k, t * 128:(t + 1) * 128], wt[:, k, :], k == 0, k == 15, [bw, bocT], [bpa])
                sg, bsg = next_stg()
                C.tt("dve", sg[:], pa[:, 0:256], GA[:, s * 256:(s + 1) * 256], ALU.mult, [bpa, bGA], [bsg])
                C.tt("dve", x_t[:, s * 256:(s + 1) * 256], x_t[:, s * 256:(s + 1) * 256], sg[:], ALU.add, [bx, bsg], [bx])
        for t in range(2):
            x_t, bx = xt[t]
            C.store("sp", o_x1[r0 + t * 128: r0 + (t + 1) * 128, :], x_t[:], bx)
            C.act(hb[:], x_t[:], ACTF.Square, [bx], [bhb, bst_ss], accum_out=st_ss[:, 3:4])
            rstd_from_ss(C, st_r[:, 3:4], st_ss[:, 3:4], D, bst_ss, bst_r)
            C.stt(tmpf[:], x_t[:], st_r[:, 3:4], G2[:], ALU.mult, ALU.mult, [bx, bst_r, bG2], [btmpf])
            C.tt("dve", h2f[:], tmpf[:], SF[:], ALU.add, [btmpf, bSF], [bh2f])
            C.copy("act", h2b[:], h2f[:], [bh2f], [bh2b])
            C.store("sp", o_h2[r0 + t * 128: r0 + (t + 1) * 128, :], h2b[:], bh2b)
            for g4 in range(4):
                pa, bpa = next_pA()
                for j in range(4):
                    k = g4 * 4 + j
                    C.tr(pa[:, j * 128:(j + 1) * 128], h2f[:, k * 128:(k + 1) * 128], idF[:], [bh2f, bidF], [bpa])
                C.copy("dve", h2T[:, g4 * 4:(g4 + 1) * 4, :], pa[:, 0:512].rearrange("p (k c) -> p k c", k=4), [bpa], [bh2T])
            pa, bpa = next_pA()
            for k in range(16):
                C.mm(pa[:, 0:32], h2T[:, k, :], wr[:, k, :], k == 0, k == 15, [bh2T, bwr], [bpa])
            C.tt("dve", lg[:], pa[:, 0:32], brt[:], ALU.add, [bpa, bbrt], [blg])
            P.op("dve", lambda e: e.max(out=top8[:], in_=lg[:]), reads=[blg], writes=[btop8])
            C.ts("dve", lg2[:], lg[:], top8[:, 3:4], None, ALU.is_ge, None, [blg, btop8], [blg2])
            C.ts("dve", top8[:, 7:8], top8[:, 0:1], -1.0, None, ALU.mult, None, [btop8], [btop8])
            C.act(lg[:], lg[:], ACTF.Exp, [blg, btop8], [blg], bias=top8[:, 7:8], scale=1.0)
            C.tt("dve", lg[:], lg[:], lg2[:], ALU.mult, [blg, blg2], [blg])
            P.op("dve", lambda e: e.tensor_reduce(out=top8[:, 6:7], in_=lg[:], axis=AX.X, op=ALU.add), reads=[blg], writes=[btop8])
            P.op("dve", lambda e: e.reciprocal(out=top8[:, 6:7], in_=top8[:, 6:7]), reads=[btop8], writes=[btop8])
            C.ts("dve", lg2[:], lg[:], top8[:, 6:7], None, ALU.mult, None, [blg, btop8], [blg2])
            C.store("sp", o_G[r0 + t * 128: r0 + (t + 1) * 128, :], lg2[:], blg2)
    C.finish()
    return nc


def build_mod(nc, NCOL=1536, C=None):
    C = C or Ctx(nc)
    P = C.P
    NR = 2 if C.fused else 3
    cT = C.din("cT", [128, 16, NR])
    wm = C.din("wm", [D, NCOL])
    bm = C.din("bm", [1, NCOL])
    o_m = C.dout("o_m", [NR, NCOL])
    cTf, bcTf = C.T("cTf", [128, 16, NR], F32)
    cTb, bcTb = C.T("cTb", [128, 16, NR], BF16)
    wmb = [C.T(f"wmb{i}", [128, 16, 512], BF16) for i in range(3)]
    bmt = [C.T(f"bmt{i}", [NR, 512], F32) for i in range(2)]
    ot = [C.T(f"ot{i}", [NR, 512], F32) for i in range(2)]
    pm = [C.PS(f"pm{i}", [128, 512], F32) for i in range(2)]
    C.load("sp", cTf[:], cT, bcTf)
    C.act(cTb[:], cTf[:], ACTF.Silu, [bcTf], [bcTb])
    for n in range(NCOL // 512):
        pa, bpa = pm[n % 2]
        wt, bw = wmb[n % 3]
        bt, bbt = bmt[n % 2]
        o_t, bo = ot[n % 2]
        C.load("pool", wt[:], wm[:, n * 512:(n + 1) * 512].rearrange("(k p) n -> p k n", p=128), bw)
        C.load("sp", bt[:], bm[0:1, n * 512:(n + 1) * 512].partition_broadcast(NR), bbt)
        for k in range(16):
            C.mm(pa[0:NR, :], cTb[:, k, :], wt[:, k, :], k == 0, k == 15, [bcTb, bw], [bpa])
        C.tt("dve", o_t[:], pa[0:NR, :], bt[:], ALU.add, [bpa, bbt], [bo])
        C.store("sp", o_m[:, n * 512:(n + 1) * 512], o_t[:], bo)
    C.finish()
    return nc


def build_experts(nc, NTOK=16384, NE=4, C=None):
    C = C or Ctx(nc)
    P = C.P
    TB = 512
    NBLK = NTOK // TB
    if C.fused:
        h2_d = C.dram["h2_d"]
        Gl = C.dram["G_d"]
        idB, bidB, idF, bidF = C.consts
    else:
        h2T = C.din("h2T", [D, NTOK], BF16)
        Gl = C.din("Gl", [NTOK, NE])
        GlT = C.din("GlT", [NE, NTOK])
    w_gu = C.din("w_gu", [NE, D, 4096])
    b_gu = C.din("b_gu", [128, NE, 16, 2])
    w_dn = C.din("w_dn", [NE, D, D])
    b_dn = C.din("b_dn", [NE, D])
    o_y = C.dout("o_y", [NTOK, D], BF16)

    hTb = [C.T(f"hTb{i}", [128, 16, TB], BF16) for i in range(1)]
    wgu = [C.T(f"wgu{i}", [128, 16, 256], BF16) for i in range(2)]
    sgu = [C.T(f"sgu{i}", [128, 16, 512], F32) for i in range(2)]
    wdn = [C.T(f"wdn{i}", [128, 16, 512], BF16) for i in range(2)]
    actb = [C.T(f"actb{i}", [128, 16, TB], BF16) for i in range(1)]
    yacc, byacc = C.T("yacc", [128, 4, D], F32)
    yout = [C.T(f"yout{i}", [128, D], BF16) for i in range(1)]
    GTb = [C.T(f"GTb{i}", [NE, TB], F32) for i in range(1)]
    bdn4, bbdn4 = C.T("bdn4", [NE, D], F32)
    Gt, bGt = C.T("Gt", [128, NBLK * 4, NE], F32)
    bgu, bbgu = C.T("bgu", [128, NE, 16, 2], F32)
    gg = [C.T(f"gg{i}", [128, TB], F32) for i in range(1)]
    ss_ = [C.T(f"ss{i}", [128, TB], F32) for i in range(1)]
    uu = [C.T(f"uu{i}", [128, TB], F32) for i in range(1)]
    pg = [C.PS(f"pg{i}", [128, 512], F32) for i in range(2)]
    pu = [C.PS(f"pu{i}", [128, 512], F32) for i in range(2)]
    NPD = 2 if C.fused else 4
    pd = [C.PS(f"pd{i}", [128, 512], F32) for i in range(NPD)]
    if C.fused:
        pTx, bpTx = C.PS("pTx", [128, 2048], BF16)
        h2t = [C.T(f"h2t{i}", [128, D], BF16) for i in range(1)]

    C.load("sp", Gt[:], Gl.rearrange("(t p) e -> p t e", p=128), bGt)
    C.load("sp", bgu[:], b_gu, bbgu)
    C.load("sp", bdn4[:], b_dn, bbdn4)

    cnt = dict(wgu=0, wdn=0, act=0, pg=0, pd=0, ew=0, sgu=0, sdn=0)
    for blk in range(NBLK):
        hT_t, bhT_ = hTb[0]
        gT, bgT = GTb[0]
        if C.fused:
            for tt in range(4):
                h_t, bh_ = h2t[0]
                C.load("sp", h_t[:], h2_d[blk * TB + tt * 128: blk * TB + (tt + 1) * 128, :], bh_)
                for k in range(16):
                    C.tr(pTx[:, k * 128:(k + 1) * 128], h_t[:, k * 128:(k + 1) * 128], idB[:], [bh_, bidB], [bpTx])
                C.copy("act", hT_t[:, :, tt * 128:(tt + 1) * 128], pTx[:].rearrange("p (k c) -> p k c", k=16), [bpTx], [bhT_])
            pdt, bpd = pd[cnt["pd"] % NPD]
            cnt["pd"] += 1
            for tt in range(4):
                C.tr(pdt[0:NE, tt * 128:(tt + 1) * 128], Gt[:, blk * 4 + tt, :], idF[:], [bGt, bidF], [bpd])
            C.copy("dve", gT[:], pdt[0:NE, :], [bpd], [bgT])
        else:
            C.load("sp", hT_t[:], h2T[:, blk * TB:(blk + 1) * TB].rearrange("(k p) n -> p k n", p=128), bhT_)
            C.load("sp", gT[:], GlT[:, blk * TB:(blk + 1) * TB], bgT)
        for dc in range(4):
            for tt in range(4):
                pdt, bpd = pd[cnt["pd"] % NPD]
                cnt["pd"] += 1
                C.mm(pdt[:, :], gT[:, tt * 128:(tt + 1) * 128], bdn4[:, dc * 512:(dc + 1) * 512], True, True, [bgT, bbdn4], [bpd])
                C.copy("act", yacc[:, tt, dc * 512:(dc + 1) * 512], pdt[:, :], [bpd], [byacc])
        for e in range(NE):
            a_t, ba = actb[0]
            for ffc in range(16):
                wt, bw = wgu[cnt["wgu"] % 2]
                cnt["wgu"] += 1
                if ffc % 2 == 0:
                    sg_, bsg_ = sgu[cnt["sgu"] % 2]
                    cnt["sgu"] += 1
                    C.load("sp", sg_[:], w_gu[e, :, ffc * 256:(ffc + 2) * 256].rearrange("(k p) n -> p k n", p=128), bsg_)
                C.copy("act", wt[:], sg_[:, :, (ffc % 2) * 256:(ffc % 2 + 1) * 256], [bsg_], [bw])
                i = cnt["pg"] % 2
                cnt["pg"] += 1
                pgt, bpg = pg[i]
                put, bpu = pu[i]
                for k in range(16):
                    C.mm(pgt[:, 0:TB], wt[:, k, 0:256:2], hT_t[:, k, :], k == 0, k == 15, [bw, bhT_], [bpg])
                for k in range(16):
                    C.mm(put[:, 0:TB], wt[:, k, 1:256:2], hT_t[:, k, :], k == 0, k == 15, [bw, bhT_], [bpu])
                j = 0
                cnt["ew"] += 1
                g_t, bg = gg[j]
                s_t, bs = ss_[j]
                u_t, bu = uu[j]
                C.ts("dve", g_t[:], pgt[:, 0:TB], bgu[:, e, ffc, 0:1], 7.0, ALU.add, ALU.min, [bpg, bbgu], [bg])
                C.act(s_t[:], g_t[:], ACTF.Sigmoid, [bg], [bs], scale=1.702)
                C.ts("dve", u_t[:], put[:, 0:TB], bgu[:, e, ffc, 1:2], 7.0, ALU.add, ALU.min, [bpu, bbgu], [bu])
                C.ts("dve", u_t[:], u_t[:], -7.0, 1.0, ALU.max, ALU.add, [bu], [bu])
                C.tt("dve", g_t[:], g_t[:], s_t[:], ALU.mult, [bg, bs], [bg])
                C.tt("dve", a_t[:, ffc, :], g_t[:], u_t[:], ALU.mult, [bg, bu], [ba])
            for dc in range(4):
                wd, bwd = wdn[cnt["wdn"] % 2]
                cnt["wdn"] += 1
                C.load("pool", wd[:], w_dn[e, :, dc * 512:(dc + 1) * 512].rearrange("(k p) n -> p k n", p=128), bwd)
                for tt in range(4):
                    pdt, bpd = pd[cnt["pd"] % NPD]
                    cnt["pd"] += 1
                    for ffc in range(16):
                        C.mm(pdt[:, :], a_t[:, ffc, tt * 128:(tt + 1) * 128], wd[:, ffc, :], ffc == 0, ffc == 15, [ba, bwd], [bpd])
                    gsc = Gt[:, blk * 4 + tt, e:e + 1]
                    ysl = yacc[:, tt, dc * 512:(dc + 1) * 512]
                    C.stt(ysl, pdt[:, :], gsc, ysl, ALU.mult, ALU.add, [bpd, bGt, byacc], [byacc])
        for tt in range(4):
            yo, byo = yout[0]
            C.copy("act", yo[:], yacc[:, tt, :], [byacc], [byo])
            C.store("sp", o_y[blk * TB + tt * 128: blk * TB + (tt + 1) * 128, :], yo[:], byo)
    C.finish()
    return nc


def build_combine(nc, NT=2048, NP=8, C=None):
    C = C or Ctx(nc)
    P = C.P
    x1 = C.din("x1", [NT, D])
    yp = C.din("yp", [NP, NT, D], BF16)
    gf = C.din("gf", [2, D])
    g_final = C.din("g_final", [1, D])
    o_y = C.dout("o_y", [NT, D])
    GF = [C.T(f"GF{i}", [128, D], F32) for i in range(2)]
    gfin, bgfin = C.T("gfin", [128, D], F32)
    xt = [C.T(f"xt{i}", [128, D], F32) for i in range(2)]
    ypt = [C.T(f"ypt{i}", [128, NP, D], BF16) for i in range(2)]
    acc, bacc_ = C.T("acc", [128, D], F32)
    junk, bjunk = C.T("junk", [128, D], BF16)
    ot = [C.T(f"ot{i}", [128, D], F32) for i in range(2)]
    st, bst = C.T("st", [128, 4], F32)
    for i in range(2):
        C.load("sp", GF[i][0][:], gf[i:i + 1, :].partition_broadcast(128), GF[i][1])
    C.load("sp", gfin[:], g_final[0:1, :].partition_broadcast(128), bgfin)
    ntile = NT // 128
    for t in range(ntile):
        x_t, bx = xt[t % 2]
        y_t, by = ypt[t % 2]
        o_t, bo = ot[t % 2]
        GFt, bGF = GF[0] if t < ntile // 2 else GF[1]
        C.load("sp", x_t[:], x1[t * 128:(t + 1) * 128, :], bx)
        C.load("sp", y_t[:], yp[:, t * 128:(t + 1) * 128, :].rearrange("j p d -> p j d"), by)
        if NP == 1:
            C.copy("dve", acc[:], y_t[:, 0, :], [by], [bacc_])
        else:
            C.tt("dve", acc[:], y_t[:, 0, :], y_t[:, 1, :], ALU.add, [by], [bacc_])
        for j in range(2, NP):
            C.tt("dve", acc[:], acc[:], y_t[:, j, :], ALU.add, [by, bacc_], [bacc_])
        C.tt("dve", acc[:], acc[:], GFt[:], ALU.mult, [bacc_, bGF], [bacc_])
        C.tt("dve", acc[:], acc[:], x_t[:], ALU.add, [bacc_, bx], [bacc_])
        C.act(junk[:], acc[:], ACTF.Square, [bacc_], [bjunk, bst], accum_out=st[:, 0:1])
        rstd_from_ss(C, st[:, 1:2], st[:, 0:1], D, bst, bst)
        C.stt(o_t[:], acc[:], st[:, 1:2], gfin[:], ALU.mult, ALU.mult, [bacc_, bst, bgfin], [bo])
        C.store("sp", o_y[t * 128:(t + 1) * 128, :], o_t[:], bo)
    C.finish()
    return nc


class Arena:
    def __init__(self, C, name, nbytes):
        self.t = C.P.sbuf(name, [128, nbytes // 2], BF16)
        self.cap = nbytes // 2
        self.off = 0
        self.peak = 0

    def mark(self):
        return self.off

    def reset(self, m):
        self.off = m

    def alloc(self, name, shape, dt):
        n = 1
        for d in shape[1:]:
            n *= d
        e16 = n * (2 if dt == F32 else 1)
        e16 = (e16 + 15) // 16 * 16
        assert self.off + e16 <= self.cap, (name, self.off, e16, self.cap)
        ap = self.t[:, self.off:self.off + e16]
        self.off += e16
        self.peak = max(self.peak, self.off)
        if dt == F32:
            ap = ap.bitcast(F32)
        ap = ap[:, 0:n]
        if len(shape) == 3:
            ap = ap.rearrange("p (a b) -> p a b", a=shape[1])
        elif len(shape) == 4:
            ap = ap.rearrange("p (a b c) -> p a b c", a=shape[1], b=shape[2])
        if shape[0] < 128:
            ap = ap[0:shape[0]]
        return ap, Buf(name)


def build_sample(nc, stop=99, NALLT=32, NPAIR=8, NHG=4, NMLAH=8, C=None):
    C = C or Ctx(nc)
    P = C.P
    x_own = C.din("x_own", [1024, D])
    x_halo = C.din("x_halo", [1792, D])
    x_all = C.din("x_all", [4096, D])
    ck_na = C.din("ck_na", [512, 1024])
    cv_na = C.din("cv_na", [512, 1024])
    c_ckv = C.din("c_ckv", [512, 256])
    c_krope = C.din("c_krope", [512, 64])
    mrow = C.din("mrow", [6, D])
    g_attn = C.din("g_attn", [1, D])
    g_ffn = C.din("g_ffn", [1, D])
    g_q_a = C.din("g_q_a", [1, 512])
    g_kv_a = C.din("g_kv_a", [1, 256])
    w_in = C.din("w_in", [D, 3904])
    w_in_rs = C.din("w_in_rs", [D, 64])
    w_q_b = C.din("w_q_b", [512, 1536])
    w_q_b_rs = C.din("w_q_b_rs", [512, 512])
    w_kv_b = C.din("w_kv_b", [256, 2048])
    w_out = C.din("w_out", [D, D])
    w_router = C.din("w_router", [D, 32])
    b_router = C.din("b_router", [1, 32])
    ident = C.din("ident", [128, 128])
    jmat = C.din("jmat", [128, 128])
    amat = C.din("amat", [2, 128])
    rmx = C.din("rmx", [8, 2, 896])
    colmask = C.din("colmask", [128, 15, 64])
    rpbpad = C.din("rpbpad", [8, 15, 160])
    cos_tok = C.din("cos_tok", [4096, 64])
    sinS_tok = C.din("sinS_tok", [4096, 64])
    cosT_own = C.din("cosT_own", [64, 1024])
    sinST_own = C.din("sinST_own", [64, 1024])
    o_x1 = C.dout("o_x1", [1024, D])
    o_h2 = C.dout("o_h2", [1024, D], BF16)
    o_G = C.dout("o_G", [1024, 32])
    ocat_d = C.scratch("ocat_d", [1024, D], BF16)
    bocat_d = Buf("ocat_d")

    SC_NA = 128 ** -0.5
    SC_MLA = 192 ** -0.5

    idF, bidF = C.T("idF", [128, 128], F32)
    idB, bidB = C.T("idB", [128, 128], BF16)
    jB, bjB = C.T("jB", [128, 128], BF16)
    aB, baB = C.T("aB", [2, 128], BF16)
    T0, bT0 = C.T("T0", [128, D], F32)
    T1, bT1 = C.T("T1", [128, D], F32)
    T2, bT2 = C.T("T2", [128, D], F32)
    gqa, bgqa = C.T("gqa", [128, 512], F32)
    gkva, bgkva = C.T("gkva", [128, 256], F32)
    brt, bbrt = C.T("brt", [128, 32], F32)
    wr, bwr = C.T("wr", [128, 16, 32], F32)
    ckvT, bckvT = C.T("ckvT", [128, 2, 4608], BF16)
    krT, bkrT = C.T("krT", [128, 4608], BF16)
    hTo, bhTo = C.T("hTo", [128, 16, 1024], BF16)
    xt, bxt = C.T("xt", [128, D], F32)
    tmpf, btmpf = C.T("tmpf", [128, D], F32)
    hb, bhb = C.T("hb", [128, D], BF16)
    hTt, bhTt = C.T("hTt", [128, 16, 128], BF16)
    st_ss, bst_ss = C.T("st_ss", [128, 8], F32)
    st_r, bst_r = C.T("st_r", [128, 8], F32)
    AR = C.arena if C.fused else Arena(C, "arena", 88 * 1024)

    pT, bpT = C.PS("pT", [128, 2048], BF16)
    pA = [C.PS(f"pA{i}", [128, 512], F32) for i in range(2)]
    pS = [C.PS(f"pS{i}", [128, 512], F32) for i in range(3)]
    pO, bpO = C.PS("pO", [128, 512], F32)

    pa_i = [0]

    def next_pA():
        i = pa_i[0] % 2
        pa_i[0] += 1
        return pA[i]

    def bload(dst, row_ap, buf, q="sp"):
        C.load(q, dst, row_ap.partition_broadcast(128), buf)

    C.load("sp", idF[:], ident, bidF)
    C.copy("dve", idB[:], idF[:], [bidF], [bidB])
    C.load("pool", jB[:], jmat, bjB)
    C.load("pool", aB[:], amat, baB)
    G1, bG1, SA, bSA = T0, bT0, T1, bT1
    bload(SA[:], mrow[0:1, :], bSA)
    bload(G1[:], mrow[1:2, :], bG1)
    bload(tmpf[:], g_attn[0:1, :], btmpf)
    C.stt(G1[:], G1[:], 1.0, tmpf[:], ALU.add, ALU.mult, [bG1, btmpf], [bG1])
    bload(gqa[:], g_q_a[0:1, :], bgqa)
    bload(gkva[:], g_kv_a[0:1, :], bgkva)
    bload(brt[:], b_router[0:1, :], bbrt)
    C.load("sp", wr[:], w_router.rearrange("(k p) n -> p k n", p=128), bwr)
    P.op("pool", lambda e: e.memset(krT[:], 0.0), writes=[bkrT])

    def make_h(src_rows):
        C.load("sp", xt[:], src_rows, bxt)
        C.act(hb[:], xt[:], ACTF.Square, [bxt], [bhb, bst_ss], accum_out=st_ss[:, 0:1])
        rstd_from_ss(C, st_r[:, 0:1], st_ss[:, 0:1], D, bst_ss, bst_r)
        C.stt(tmpf[:], xt[:], st_r[:, 0:1], G1[:], ALU.mult, ALU.mult, [bxt, bst_r, bG1], [btmpf])
        C.tt("dve", hb[:], tmpf[:], SA[:], ALU.add, [btmpf, bSA], [bhb])
        for k in range(16):
            C.tr(pT[:, k * 128:(k + 1) * 128], hb[:, k * 128:(k + 1) * 128], idB[:], [bhb, bidB], [bpT])

    m0 = AR.mark()
    w320, bw320 = AR.alloc("w320", [128, 16, 384], BF16)
    kvaf, bkvaf = AR.alloc("kvaf", [128, 256], F32)
    ckvb, bckvb = AR.alloc("ckvb", [128, 256], BF16)
    krb, bkrb = AR.alloc("krb", [128, 64], BF16)
    cst, bcst = AR.alloc("cst", [128, 2, 64], F32)
    kr1, bkr1 = AR.alloc("kr1", [128, 64], F32)
    kr2, bkr2 = AR.alloc("kr2", [128, 64], F32)
    ccf, bccf = AR.alloc("ccf", [128, 320], F32)
    C.load("pool", w320[:, :, 0:320], w_in[:, 3584:3904].rearrange("(k p) n -> p k n", p=128), bw320)
    C.load("pool", w320[:, :, 320:384], w_in_rs.rearrange("(k p) n -> p k n", p=128), bw320)
    for t in range(NALLT):
        make_h(x_all[t * 128:(t + 1) * 128, :])
        C.copy("act", hTt[:], pT[:].rearrange("p (k c) -> p k c", k=16), [bpT], [bhTt])
        C.load("sp", cst[:, 0, :], cos_tok[t * 128:(t + 1) * 128, :], bcst)
        C.load("sp", cst[:, 1, :], sinS_tok[t * 128:(t + 1) * 128, :], bcst)
        pa, bpa = next_pA()
        for k in range(16):
            C.mm(pa[:, 0:384], hTt[:, k, :], w320[:, k, :], k == 0, k == 15, [bhTt, bw320], [bpa])
        C.copy("dve", kvaf[:], pa[:, 0:256], [bpa], [bkvaf])
        C.tt("dve", kr1[:], pa[:, 256:320], cst[:, 0, :], ALU.mult, [bpa, bcst], [bkr1])
        C.tt("dve", kr2[:], pa[:, 320:384], cst[:, 1, :], ALU.mult, [bpa, bcst], [bkr2])
        C.tt("dve", krb[:], kr1[:], kr2[:], ALU.add, [bkr1, bkr2], [bkrb])
        C.act(hb[:, 0:256], kvaf[:], ACTF.Square, [bkvaf], [bhb, bst_ss], accum_out=st_ss[:, 2:3])
        rstd_from_ss(C, st_r[:, 2:3], st_ss[:, 2:3], 256, bst_ss, bst_r)
        C.stt(ckvb[:], kvaf[:], st_r[:, 2:3], gkva[:], ALU.mult, ALU.mult, [bkvaf, bst_r, bgkva], [bckvb])
        for c in range(2):
            C.tr(pT[:, c * 128:(c + 1) * 128], ckvb[:, c * 128:(c + 1) * 128], idB[:], [bckvb, bidB], [bpT])
        C.tr(pT[0:64, 256:384], krb[:], idB[:], [bkrb, bidB], [bpT])
        C.copy("act", ckvT[:, :, t * 128:(t + 1) * 128], pT[:, 0:256].rearrange("p (k c) -> p k c", k=2), [bpT], [bckvT])
        C.copy("dve", krT[0:64, t * 128:(t + 1) * 128], pT[0:64, 256:384], [bpT], [bkrT])
    for t in range(4):
        C.load("sp", ccf[:, 0:256], c_ckv[t * 128:(t + 1) * 128, :], bccf)
        C.load("sp", ccf[:, 256:320], c_krope[t * 128:(t + 1) * 128, :], bccf)
        C.copy("dve", hb[:, 0:320], ccf[:], [bccf], [bhb])
        for c in range(2):
            C.tr(pT[:, c * 128:(c + 1) * 128], hb[:, c * 128:(c + 1) * 128], idB[:], [bhb, bidB], [bpT])
        C.tr(pT[0:64, 256:384], hb[:, 256:320], idB[:], [bhb, bidB], [bpT])
        C.copy("act", ckvT[:, :, 4096 + t * 128:4096 + (t + 1) * 128], pT[:, 0:256].rearrange("p (k c) -> p k c", k=2), [bpT], [bckvT])
        C.copy("dve", krT[0:64, 4096 + t * 128:4096 + (t + 1) * 128], pT[0:64, 256:384], [bpT], [bkrT])
    P.barrier()
    AR.reset(m0)
    if stop <= 1:
        dbg = C.dout("dbg", [128, 3, 4608], BF16)
        C.store("sp", dbg[:, 0:2, :], ckvT[:], bckvT)
        C.store("sp", dbg[:, 2, :], krT[:], bkrT)
        C.finish()
        return nc

    for t in range(8):
        make_h(x_own[t * 128:(t + 1) * 128, :])
        C.copy("act", hTo[:, :, t * 128:(t + 1) * 128], pT[:].rearrange("p (k c) -> p k c", k=16), [bpT], [bhTo])

    m1 = AR.mark()
    Bstat, bBstat = AR.alloc("Bstat", [128, 8, 896], BF16)
    mB = AR.mark()
    Bfull, bBfull = AR.alloc("Bfull", [128, 8, 15, 64], F32)
    cmk, bcmk = AR.alloc("cmk", [128, 15, 64], F32)
    C.load("sp", cmk[:], colmask, bcmk)
    rp_t = rpbpad.tensor
    for hd in range(8):
        for half in range(2):
            src = bass.AP(rp_t, hd * 2400 + 16, [[1, 64], [160, 15], [1, 64]])
            C.load("sp", Bfull[half * 64:(half + 1) * 64, hd, :, :], src, bBfull)
    for hd in range(8):
        C.tt("dve", Bfull[:, hd, :, :], Bfull[:, hd, :, :], cmk[:], ALU.add, [bBfull, bcmk], [bBfull])
        C.copy("act", Bstat[0:64, hd, :].rearrange("p (j c) -> p j c", j=14), Bfull[0:64, hd, 1:15, :], [bBfull], [bBstat])
        C.copy("act", Bstat[64:128, hd, :].rearrange("p (j c) -> p j c", j=14), Bfull[64:128, hd, 0:14, :], [bBfull], [bBstat])
    P.barrier()
    AR.reset(mB)

    mG = AR.mark()
    for g in range(NHG):
        AR.reset(mG)
        wk, bwk = AR.alloc("wk", [128, 16, 256], BF16)
        wv, bwv = AR.alloc("wv", [128, 16, 256], BF16)
        wq, bwq = AR.alloc("wq", [128, 16, 256], BF16)
        KTh, bKTh = AR.alloc("KTh", [128, 2, 1792], BF16)
        Vh, bVh = AR.alloc("Vh", [128, 14, 256], BF16)
        cKT, bcKT = AR.alloc("cKT", [128, 2, 512], BF16)
        cV, bcV = AR.alloc("cV", [128, 4, 256], BF16)
        QTg, bQTg = AR.alloc("QTg", [128, 2, 1024], BF16)
        kbt, bkbt = AR.alloc("kbt", [128, 256], BF16)
        ccn, bccn = AR.alloc("ccn", [128, 2, 256], F32)
        ccb, bccb = AR.alloc("ccb", [128, 256], BF16)
        rmt = [AR.alloc(f"rmt{i}", [2, 896], BF16) for i in range(2)]
        Pb = [AR.alloc(f"Pb{i}", [128, 1408], BF16) for i in range(2)]
        PTt = [AR.alloc(f"PTt{i}", [128, 11, 128], BF16) for i in range(2)]
        otl = [AR.alloc(f"otl{i}", [128, 256], BF16) for i in range(2)]
        amx = [AR.alloc(f"amx{i}", [128, 8], F32) for i in range(2)]
        c0 = g * 256
        C.load("pool", wq[:], w_in[:, c0:c0 + 256].rearrange("(k p) n -> p k n", p=128), bwq)
        C.load("pool", wk[:], w_in[:, 1024 + c0:1024 + c0 + 256].rearrange("(k p) n -> p k n", p=128), bwk)
        C.load("pool", wv[:], w_in[:, 2048 + c0:2048 + c0 + 256].rearrange("(k p) n -> p k n", p=128), bwv)
        for t in range(14):
            make_h(x_halo[t * 128:(t + 1) * 128, :])
            C.copy("act", hTt[:], pT[:].rearrange("p (k c) -> p k c", k=16), [bpT], [bhTt])
            pa, bpa = next_pA()
            for k in range(16):
                C.mm(pa[:, 0:256], hTt[:, k, :], wk[:, k, :], k == 0, k == 15, [bhTt, bwk], [bpa])
            C.copy("act", kbt[:], pa[:, 0:256], [bpa], [bkbt])
            pa, bpa = next_pA()
            for k in range(16):
                C.mm(pa[:, 0:256], hTt[:, k, :], wv[:, k, :], k == 0, k == 15, [bhTt, bwv], [bpa])
            C.copy("dve", Vh[:, t, :], pa[:, 0:256], [bpa], [bVh])
            for hh in range(2):
                C.tr(pT[:, hh * 128:(hh + 1) * 128], kbt[:, hh * 128:(hh + 1) * 128], idB[:], [bkbt, bidB], [bpT])
            C.copy("act", KTh[:, :, t * 128:(t + 1) * 128], pT[:, 0:256].rearrange("p (k c) -> p k c", k=2), [bpT], [bKTh])
        for t in range(4):
            C.load("sp", ccn[:, 0, :], ck_na[t * 128:(t + 1) * 128, c0:c0 + 256], bccn)
            C.load("sp", ccn[:, 1, :], cv_na[t * 128:(t + 1) * 128, c0:c0 + 256], bccn)
            C.copy("dve", ccb[:], ccn[:, 0, :], [bccn], [bccb])
            C.copy("act", cV[:, t, :], ccn[:, 1, :], [bccn], [bcV])
            for hh in range(2):
                C.tr(pT[:, hh * 128:(hh + 1) * 128], ccb[:, hh * 128:(hh + 1) * 128], idB[:], [bccb, bidB], [bpT])
            C.copy("act", cKT[:, :, t * 128:(t + 1) * 128], pT[:, 0:256].rearrange("p (k c) -> p k c", k=2), [bpT], [bcKT])
        for hh in range(2):
            for half in range(2):
                pa, bpa = next_pA()
                for k in range(16):
                    C.mm(pa[:, :], wq[:, k, hh * 128:(hh + 1) * 128], hTo[:, k, half * 512:(half + 1) * 512], k == 0, k == 15, [bwq, bhTo], [bpa])
                C.act(QTg[:, hh, half * 512:(half + 1) * 512], pa[:, :], ACTF.Copy, [bpa], [bQTg], scale=SC_NA)
        ui = 0
        for p in range(NPAIR):
            rm_t, brm = rmt[p % 2]
            C.load("pool", rm_t[:], rmx[p], brm)
            ot_t, bot = otl[p % 2]
            for hh in range(2):
                hd = g * 2 + hh
                pb, bpb = Pb[ui % 2]
                ptt, bptt = PTt[ui % 2]
                am, bam = amx[ui % 2]
                ui += 1
                q_l = QTg[:, hh, p * 128:(p + 1) * 128]
                k0 = p * 128
                segs = [(pS[0], 0, 512), (pS[1], 512, 384)]
                for (ps, bps), o, n in segs:
                    C.mm(ps[:, 0:n], q_l, KTh[:, hh, k0 + o:k0 + o + n], True, False, [bQTg, bKTh], [bps])
                    C.mm(ps[:, 0:n], jB[:], Bstat[:, hd, o:o + n], False, False, [bjB, bBstat], [bps])
                    C.mm(ps[:, 0:n], aB[:], rm_t[:, o:o + n], False, True, [baB, brm], [bps])
                ps2, bps2 = pS[2]
                C.mm(ps2[:, 0:512], q_l, cKT[:, hh, :], True, True, [bQTg, bcKT], [bps2])
                for i, ((ps, bps), n) in enumerate(((pS[0], 512), (pS[1], 384), (pS[2], 512))):
                    P.op("dve", lambda e, ps=ps, n=n, i=i, am=am: e.tensor_reduce(out=am[:, i:i + 1], in_=ps[:, 0:n], axis=AX.X, op=ALU.max),
                         reads=[bps], writes=[bam])
                P.op("dve", lambda e, am=am: e.tensor_reduce(out=am[:, 3:4], in_=am[:, 0:3], axis=AX.X, op=ALU.max, negate=True), reads=[bam], writes=[bam])
                for i, ((ps, bps), o, n) in enumerate(((pS[0], 0, 512), (pS[1], 512, 384), (pS[2], 896, 512))):
                    C.act(pb[:, o:o + n], ps[:, 0:n], ACTF.Exp, [bps, bam], [bpb, bam], bias=am[:, 3:4], scale=1.0, accum_out=am[:, 4 + i:5 + i])
                P.op("dve", lambda e, am=am: e.tensor_reduce(out=am[:, 7:8], in_=am[:, 4:7], axis=AX.X, op=ALU.add), reads=[bam], writes=[bam])
                P.op("dve", lambda e, am=am: e.reciprocal(out=am[:, 7:8], in_=am[:, 7:8]), reads=[bam], writes=[bam])
                for j in range(11):
                    C.tr(pT[:, j * 128:(j + 1) * 128], pb[:, j * 128:(j + 1) * 128], idB[:], [bpb, bidB], [bpT])
                C.copy("act", ptt[:, 0:6, :], pT[:, 0:768].rearrange("p (k c) -> p k c", k=6), [bpT], [bptt])
                C.copy("dve", ptt[:, 6:11, :], pT[:, 768:1408].rearrange("p (k c) -> p k c", k=5), [bpT], [bptt])
                for j in range(7):
                    C.mm(pO[:, 0:128], ptt[:, j, :], Vh[:, p + j, hh * 128:(hh + 1) * 128], j == 0, False, [bptt, bVh], [bpO])
                for j in range(4):
                    C.mm(pO[:, 0:128], ptt[:, 7 + j, :], cV[:, j, hh * 128:(hh + 1) * 128], False, j == 3, [bptt, bcV], [bpO])
                C.ts("dve", ot_t[:, hh * 128:(hh + 1) * 128], pO[:, 0:128], am[:, 7:8], None, ALU.mult, None, [bpO, bam], [bot])
            P.dma("sp", lambda e, ot_t=ot_t, p=p, c0=c0: e.dma_start(out=ocat_d[p * 128:(p + 1) * 128, c0:c0 + 256], in_=ot_t[:]),
                  reads=[bot], writes=[bocat_d], key=bot, store=True)
        P.barrier()
    AR.reset(m1)
    if stop <= 2:
        C.finish()
        return nc

    qanT, bqanT = AR.alloc("qanT", [128, 4, 1024], BF16)
    mQ = AR.mark()
    wqa, bwqa = AR.alloc("wqa", [128, 16, 512], BF16)
    C.load("pool", wqa[:], w_in[:, 3072:3584].rearrange("(k p) n -> p k n", p=128), bwqa)
    qaf, bqaf = AR.alloc("qaf", [128, 512], F32)
    qan, bqan = AR.alloc("qan", [128, 512], BF16)
    for t in range(8):
        pa, bpa = next_pA()
        for k in range(16):
            C.mm(pa[:, :], hTo[:, k, t * 128:(t + 1) * 128], wqa[:, k, :], k == 0, k == 15, [bhTo, bwqa], [bpa])
        C.copy("dve", qaf[:], pa[:, :], [bpa], [bqaf])
        C.act(hb[:, 0:512], qaf[:], ACTF.Square, [bqaf], [bhb, bst_ss], accum_out=st_ss[:, 1:2])
        rstd_from_ss(C, st_r[:, 1:2], st_ss[:, 1:2], 512, bst_ss, bst_r)
        C.stt(qan[:], qaf[:], st_r[:, 1:2], gqa[:], ALU.mult, ALU.mult, [bqaf, bst_r, bgqa], [bqan])
        for c in range(4):
            C.tr(pT[:, c * 128:(c + 1) * 128], qan[:, c * 128:(c + 1) * 128], idB[:], [bqan, bidB], [bpT])
        C.copy("act", qanT[:, :, t * 128:(t + 1) * 128], pT[:, 0:512].rearrange("p (k c) -> p k c", k=4), [bpT], [bqanT])
    P.barrier()
    AR.reset(mQ)
    wqb, bwqb = AR.alloc("wqb", [128, 4, 1536], BF16)
    wqbr, bwqbr = AR.alloc("wqbr", [128, 4, 512], BF16)
    wkvb, bwkvb = AR.alloc("wkvb", [128, 2, 2048], BF16)
    csT, bcsT = AR.alloc("csT", [64, 2, 1024], F32)
    C.load("pool", wqb[:], w_q_b.rearrange("(k p) n -> p k n", p=128), bwqb)
    C.load("pool", wqbr[:], w_q_b_rs.rearrange("(k p) n -> p k n", p=128), bwqbr)
    C.load("pool", wkvb[:], w_kv_b.rearrange("(k p) n -> p k n", p=128), bwkvb)
    C.load("sp", csT[:, 0, :], cosT_own, bcsT)
    C.load("sp", csT[:, 1, :], sinST_own, bcsT)
    qnh, bqnh = AR.alloc("qnh", [128, 1024], BF16)
    qrh, bqrh = AR.alloc("qrh", [128, 1024], BF16)
    knh, bknh = AR.alloc("knh", [128, 4608], BF16)
    vh, bvh = AR.alloc("vh", [128, 36, 128], BF16)
    r1, br1 = AR.alloc("r1", [64, 512], F32)
    r2, br2 = AR.alloc("r2", [64, 512], F32)
    Pm = [AR.alloc(f"Pm{i}", [128, 512], BF16) for i in range(2)]
    PTm = [AR.alloc(f"PTm{i}", [128, 4, 128], BF16) for i in range(2)]
    om = [AR.alloc(f"om{i}", [128, 128], BF16) for i in range(2)]
    mst = [AR.alloc(f"mst{i}", [128, 24], F32) for i in range(2)]
    P.op("pool", lambda e: e.memset(qrh[:], 0.0), writes=[bqrh])
    ci = 0
    for hd in range(NMLAH):
        for half in range(2):
            sl = slice(half * 512, (half + 1) * 512)
            pa, bpa = next_pA()
            for c in range(4):
                C.mm(pa[:, :], wqb[:, c, hd * 192:hd * 192 + 128], qanT[:, c, sl], c == 0, c == 3, [bwqb, bqanT], [bpa])
            C.act(qnh[:, sl], pa[:, :], ACTF.Copy, [bpa], [bqnh], scale=SC_MLA)
            pa, bpa = next_pA()
            for c in range(4):
                C.mm(pa[0:64, :], wqb[:, c, hd * 192 + 128:hd * 192 + 192], qanT[:, c, sl], c == 0, c == 3, [bwqb, bqanT], [bpa])
            C.stt(r1[:], pa[0:64, :], SC_MLA, csT[:, 0, sl], ALU.mult, ALU.mult, [bpa, bcsT], [br1])
            pa, bpa = next_pA()
            for c in range(4):
                C.mm(pa[0:64, :], wqbr[:, c, hd * 64:(hd + 1) * 64], qanT[:, c, sl], c == 0, c == 3, [bwqbr, bqanT], [bpa])
            C.stt(r2[:], pa[0:64, :], SC_MLA, csT[:, 1, sl], ALU.mult, ALU.mult, [bpa, bcsT], [br2])
            C.tt("dve", qrh[0:64, sl], r1[:], r2[:], ALU.add, [br1, br2], [bqrh])
        for ch in range(9):
            pa, bpa = next_pA()
            for c in range(2):
                C.mm(pa[:, :], wkvb[:, c, hd * 256:hd * 256 + 128], ckvT[:, c, ch * 512:(ch + 1) * 512], c == 0, c == 1, [bwkvb, bckvT], [bpa])
            C.copy("act", knh[:, ch * 512:(ch + 1) * 512], pa[:, :], [bpa], [bknh])
        for kt4 in range(9):
            pa, bpa = next_pA()
            for j in range(4):
                kt = kt4 * 4 + j
                for c in range(2):
                    C.mm(pa[:, j * 128:(j + 1) * 128], ckvT[:, c, kt * 128:(kt + 1) * 128], wkvb[:, c, hd * 256 + 128:hd * 256 + 256], c == 0, c == 1, [bckvT, bwkvb], [bpa])
            C.copy("dve", vh[:, kt4 * 4:(kt4 + 1) * 4, :], pa[:, :].rearrange("p (k c) -> p k c", k=4), [bpa], [bvh])
        for qt in range(8):
            ms, bms = mst[qt % 2]
            o_t, bo = om[qt % 2]
            qs = slice(qt * 128, (qt + 1) * 128)
            for ch in range(9):
                ps, bps = pS[ch % 3]
                C.mm(ps[:, :], qnh[:, qs], knh[:, ch * 512:(ch + 1) * 512], True, False, [bqnh, bknh], [bps])
                C.mm(ps[:, :], qrh[:, qs], krT[:, ch * 512:(ch + 1) * 512], False, True, [bqrh, bkrT], [bps])
                P.op("dve", lambda e, ps=ps, ms=ms, ch=ch: e.tensor_reduce(out=ms[:, ch:ch + 1], in_=ps[:, :], axis=AX.X, op=ALU.max), reads=[bps], writes=[bms])
            P.op("dve", lambda e, ms=ms: e.tensor_reduce(out=ms[:, 9:10], in_=ms[:, 0:9], axis=AX.X, op=ALU.max, negate=True), reads=[bms], writes=[bms])
            for ch in range(9):
                ps, bps = pS[ch % 3]
                pm, bpm = Pm[ci % 2]
                ptm, bptm = PTm[ci % 2]
                ci += 1
                C.mm(ps[:, :], qnh[:, qs], knh[:, ch * 512:(ch + 1) * 512], True, False, [bqnh, bknh], [bps])
                C.mm(ps[:, :], qrh[:, qs], krT[:, ch * 512:(ch + 1) * 512], False, True, [bqrh, bkrT], [bps])
                C.act(pm[:], ps[:, :], ACTF.Exp, [bps, bms], [bpm, bms], bias=ms[:, 9:10], scale=1.0, accum_out=ms[:, 10 + ch:11 + ch])
                for j in range(4):
                    C.tr(pT[:, j * 128:(j + 1) * 128], pm[:, j * 128:(j + 1) * 128], idB[:], [bpm, bidB], [bpT])
                C.copy("act" if ch % 2 else "dve", ptm[:], pT[:, 0:512].rearrange("p (k c) -> p k c", k=4), [bpT], [bptm])
                for j in range(4):
                    kt = ch * 4 + j
                    C.mm(pO[:, 0:128], ptm[:, j, :], vh[:, kt, :], kt == 0, kt == 35, [bptm, bvh], [bpO])
            P.op("dve", lambda e, ms=ms: e.tensor_reduce(out=ms[:, 20:21], in_=ms[:, 10:19], axis=AX.X, op=ALU.add), reads=[bms], writes=[bms])
            P.op("dve", lambda e, ms=ms: e.reciprocal(out=ms[:, 20:21], in_=ms[:, 20:21]), reads=[bms], writes=[bms])
            C.ts("dve", o_t[:], pO[:, 0:128], ms[:, 20:21], None, ALU.mult, None, [bpO, bms], [bo])
            P.dma("sp", lambda e, o_t=o_t, qt=qt, hd=hd: e.dma_start(out=ocat_d[qt * 128:(qt + 1) * 128, 1024 + hd * 128:1024 + (hd + 1) * 128], in_=o_t[:]),
                  reads=[bo], writes=[bocat_d], key=bo, store=True)
    P.barrier()
    AR.reset(m1)
    if stop <= 3:
        C.finish()
        return nc

    GA, bGA, G2, bG2, SF, bSF = T0, bT0, T1, bT1, T2, bT2
    bload(GA[:], mrow[2:3, :], bGA)
    bload(SF[:], mrow[3:4, :], bSF)
    bload(G2[:], mrow[4:5, :], bG2)
    bload(tmpf[:], g_ffn[0:1, :], btmpf)
    C.stt(G2[:], G2[:], 1.0, tmpf[:], ALU.add, ALU.mult, [bG2, btmpf], [bG2])
    xt2 = [AR.alloc(f"xt2_{i}", [128, D], F32) for i in range(2)]
    oct_ = [AR.alloc(f"oct{i}", [128, D], BF16) for i in range(2)]
    ocT, bocT = AR.alloc("ocT", [128, 16, 256], BF16)
    ws = [AR.alloc(f"ws{i}", [128, 16, 256], BF16) for i in range(2)]
    stg = [AR.alloc(f"stg{i}", [128, 256], F32) for i in range(2)]
    h2f, bh2f = AR.alloc("h2f", [128, D], F32)
    h2b, bh2b = AR.alloc("h2b", [128, D], BF16)
    h2T, bh2T = AR.alloc("h2T", [128, 16, 128], F32)
    lg, blg = AR.alloc("lg", [128, 32], F32)
    lg2, blg2 = AR.alloc("lg2", [128, 32], F32)
    top8, btop8 = AR.alloc("top8", [128, 8], F32)
    wsi = [0]
    sgi = [0]
    for b in range(4):
        r0 = b * 256
        for t in range(2):
            x_t, bx = xt2[t]
            oc_t, boc = oct_[t]
            C.load("sp", x_t[:], x_own[r0 + t * 128:r0 + (t + 1) * 128, :], bx)
            P.dma("sp", lambda e, oc_t=oc_t, r0=r0, t=t: e.dma_start(out=oc_t[:], in_=ocat_d[r0 + t * 128:r0 + (t + 1) * 128, :]),
                  reads=[bocat_d], writes=[boc])
            for k in range(16):
                C.tr(pT[:, k * 128:(k + 1) * 128], oc_t[:, k * 128:(k + 1) * 128], idB[:], [boc, bidB], [bpT])
            C.copy("act", ocT[:, :, t * 128:(t + 1) * 128], pT[:].rearrange("p (k c) -> p k c", k=16), [bpT], [bocT])
        for s in range(8):
            wt, bw = ws[wsi[0] % 2]
            wsi[0] += 1
            C.load("pool", wt[:], w_out[:, s * 256:(s + 1) * 256].rearrange("(k p) n -> p k n", p=128), bw)
            for t in range(2):
                x_t, bx = xt2[t]
                pa, bpa = next_pA()
                for k in range(16):
                    C.mm(pa[:, 0:256], ocT[:, k, t * 128:(t + 1) * 128], wt[:, k, :], k == 0, k == 15, [bw, bocT], [bpa])
                sg, bsg = stg[sgi[0] % 2]
                sgi[0] += 1
                C.tt("dve", sg[:], pa[:, 0:256], GA[:, s * 256:(s + 1) * 256], ALU.mult, [bpa, bGA], [bsg])
                C.tt("dve", x_t[:, s * 256:(s + 1) * 256], x_t[:, s * 256:(s + 1) * 256], sg[:], ALU.add, [bx, bsg], [bx])
        for t in range(2):
            x_t, bx = xt2[t]
            C.store("sp", o_x1[r0 + t * 128:r0 + (t + 1) * 128, :], x_t[:], bx)
            C.act(hb[:], x_t[:], ACTF.Square, [bx], [bhb, bst_ss], accum_out=st_ss[:, 3:4])
            rstd_from_ss(C, st_r[:, 3:4], st_ss[:, 3:4], D, bst_ss, bst_r)
            C.stt(tmpf[:], x_t[:], st_r[:, 3:4], G2[:], ALU.mult, ALU.mult, [bx, bst_r, bG2], [btmpf])
            C.tt("dve", h2f[:], tmpf[:], SF[:], ALU.add, [btmpf, bSF], [bh2f])
            C.copy("act", h2b[:], h2f[:], [bh2f], [bh2b])
            C.store("sp", o_h2[r0 + t * 128:r0 + (t + 1) * 128, :], h2b[:], bh2b)
            for g4 in range(4):
                pa, bpa = next_pA()
                for j in range(4):
                    k = g4 * 4 + j
                    C.tr(pa[:, j * 128:(j + 1) * 128], h2f[:, k * 128:(k + 1) * 128], idF[:], [bh2f, bidF], [bpa])
                C.copy("dve", h2T[:, g4 * 4:(g4 + 1) * 4, :], pa[:, 0:512].rearrange("p (k c) -> p k c", k=4), [bpa], [bh2T])
            pa, bpa = next_pA()
            for k in range(16):
                C.mm(pa[:, 0:32], h2T[:, k, :], wr[:, k, :], k == 0, k == 15, [bh2T, bwr], [bpa])
            C.tt("dve", lg[:], pa[:, 0:32], brt[:], ALU.add, [bpa, bbrt], [blg])
            P.op("dve", lambda e: e.max(out=top8[:], in_=lg[:]), reads=[blg], writes=[btop8])
            C.ts("dve", lg2[:], lg[:], top8[:, 3:4], None, ALU.is_ge, None, [blg, btop8], [blg2])
            C.ts("dve", top8[:, 7:8], top8[:, 0:1], -1.0, None, ALU.mult, None, [btop8], [btop8])
            C.act(lg[:], lg[:], ACTF.Exp, [blg, btop8], [blg], bias=top8[:, 7:8], scale=1.0)
            C.tt("dve", lg[:], lg[:], lg2[:], ALU.mult, [blg, blg2], [blg])
            P.op("dve", lambda e: e.tensor_reduce(out=top8[:, 6:7], in_=lg[:], axis=AX.X, op=ALU.add), reads=[blg], writes=[btop8])
            P.op("dve", lambda e: e.reciprocal(out=top8[:, 6:7], in_=top8[:, 6:7]), reads=[btop8], writes=[btop8])
            C.ts("dve", lg2[:], lg[:], top8[:, 6:7], None, ALU.mult, None, [blg, btop8], [blg2])
            C.store("sp", o_G[r0 + t * 128:r0 + (t + 1) * 128, :], lg2[:], blg2)
    C.finish()
    return nc


def build_fused(nc):
    C = Ctx(nc, fused=True)
    P = C.P
    C.ps_pool = P.psum("pspool", [128, 4096], F32)[:]
    idF_t = P.sbuf("c_idF", [128, 128], F32)
    idB_t = P.sbuf("c_idB", [128, 128], BF16)
    bidF, bidB = Buf("c_idF"), Buf("c_idB")
    ident = C.din("ident", [128, 128])
    C.load("sp", idF_t[:], ident, bidF)
    C.copy("dve", idB_t[:], idF_t[:], [bidF], [bidB])
    C.consts = (idB_t, bidB, idF_t, bidF)
    C.arena = Arena(C, "arena", 206 * 1024)
    m_d = C.scratch("m_d", [2, 6 * D], F32)
    x1_d = C.scratch("x1_d", [2048, D], F32)
    h2_d = C.scratch("h2_d", [2048, D], BF16)
    G_d = C.scratch("G_d", [2048, 32], F32)
    y_d = C.scratch("y_d", [2048, D], BF16)

    def phase_end():
        P.barrier()
        P.recycle_dma_sems()
        C.arena.reset(0)
        C.ps_off = 0

    C.dram["o_m"] = m_d
    build_mod(nc, NCOL=6 * D, C=C)
    phase_end()
    C.dram["mrow"] = m_d[0:1, :].rearrange("o (s d) -> (o s) d", s=6)
    C.dram["o_x1"], C.dram["o_h2"], C.dram["o_G"] = x1_d[0:1024, :], h2_d[0:1024, :], G_d[0:1024, :]
    build_prompt(nc, NB=4, C=C)
    phase_end()
    C.dram["mrow"] = m_d[1:2, :].rearrange("o (s d) -> (o s) d", s=6)
    C.dram["o_x1"], C.dram["o_h2"], C.dram["o_G"] = x1_d[1024:2048, :], h2_d[1024:2048, :], G_d[1024:2048, :]
    build_sample(nc, C=C)
    phase_end()
    C.dram["h2_d"], C.dram["G_d"], C.dram["o_y"] = h2_d, G_d, y_d
    build_experts(nc, NTOK=2048, NE=32, C=C)
    phase_end()
    del C.dram["o_y"]
    C.dram["x1"] = x1_d
    C.dram["yp"] = y_d.rearrange("(o n) d -> o n d", o=1)
    C.dram["gf"] = m_d[:, 5 * D:6 * D]
    build_combine(nc, NT=2048, NP=1, C=C)
    P.emit()
    return nc


def rope_tables():
    T = 4096
    pos = np.arange(T); rows = (pos // 64).astype(np.float32); cols = (pos % 64).astype(np.float32)
    inv = (10000.0 ** (-(np.arange(16, dtype=np.float32) * 2.0 / 32))).astype(np.float32)
    ar = rows[:, None] * inv; ac = cols[:, None] * inv
    ang = np.concatenate([ar, ar, ac, ac], -1)
    cos, sin = np.cos(ang).astype(np.float32), np.sin(ang).astype(np.float32)
    sign = np.concatenate([-np.ones(16), np.ones(16), -np.ones(16), np.ones(16)]).astype(np.float32)
    return cos, sin * sign

SWAP = np.concatenate([np.arange(16, 32), np.arange(0, 16), np.arange(48, 64), np.arange(32, 48)])

def sample_consts(r0):
    ident = np.eye(128, dtype=np.float32)
    jmat = np.zeros((128, 128), np.float32)
    for p in range(128):
        jmat[p, (p // 64) * 64 + 63 - p % 64] = 1.0
    amat = np.zeros((2, 128), np.float32)
    amat[0, :64] = 1.0; amat[1, 64:] = 1.0
    rmx = np.zeros((8, 2, 14, 64), np.float32)
    for p in range(8):
        for rho in range(2):
            r = r0 + 2 * p + rho
            rs = min(max(r - 4, 0), 56)
            for j in range(14):
                kr = r0 + 2 * p - 6 + j
                ok = (0 <= kr < 64) and (rs <= kr < rs + 8)
                if not ok:
                    rmx[p, rho, j, :] = NEG
    colmask = np.zeros((128, 15, 64), np.float32)
    for pp in range(128):
        c = 63 - pp % 64
        cs = min(max(c - 8, 0), 48)
        m = np.full(64, NEG, np.float32); m[cs:cs + 16] = 0.0
        colmask[pp, :, :] = m
    return dict(ident=ident, jmat=jmat, amat=amat, rmx=rmx.reshape(8, 2, 896), colmask=colmask)

def sample_inputs(z, mrow_b, b, r0):
    cos, sinS = rope_tables()
    xs = z['x_sample'][b]
    halo = np.zeros((28, 64, 2048), np.float32)
    for i in range(28):
        r = r0 - 6 + i
        if 0 <= r < 64:
            halo[i] = xs[r * 64:(r + 1) * 64]
    rpbpad = np.zeros((8, 15, 160), np.float32)
    rpbpad[:, :, 64:95] = z['na_rpb'][0]
    wqb = z['w_q_b'][0]
    wqb_rs = np.concatenate([wqb[:, h * 192 + 128:h * 192 + 192][:, SWAP] for h in range(8)], 1)
    own = slice(r0 * 64, r0 * 64 + 1024)
    d = dict(x_own=np.ascontiguousarray(xs[own]), x_halo=halo.reshape(1792, 2048), x_all=xs,
             ck_na=z['cache_na_k'][b, 0].reshape(512, 1024), cv_na=z['cache_na_v'][b, 0].reshape(512, 1024),
             c_ckv=z['cache_mla_ckv'][b, 0], c_krope=z['cache_mla_krope'][b, 0],
             mrow=np.ascontiguousarray(mrow_b.reshape(6, 2048)),
             g_attn=z['g_attn'], g_ffn=z['g_ffn'], g_q_a=z['g_q_a'], g_kv_a=z['g_kv_a'],
             w_in=z['w_in'][0], w_in_rs=np.ascontiguousarray(z['w_in'][0][:, 3840:3904][:, SWAP]),
             w_q_b=wqb, w_q_b_rs=np.ascontiguousarray(wqb_rs), w_kv_b=z['w_kv_b'][0], w_out=z['w_out'][0],
             w_router=z['w_router'][0], b_router=z['b_router'], rpbpad=rpbpad,
             cos_tok=cos, sinS_tok=sinS, cosT_own=np.ascontiguousarray(cos[own].T), sinST_own=np.ascontiguousarray(sinS[own].T))
    d.update(sample_consts(r0))
    return d


def fused_inputs(z, core):
    f32 = np.float32
    b, r0 = core // 4, 16 * (core % 4)
    c2 = np.stack([np.asarray(z['c_ctx'], f32), np.asarray(z['c'], f32)[b]], 0)
    d = sample_inputs(z, np.zeros(6 * 2048, f32), b, r0)
    del d['mrow']
    d.update(cT=np.ascontiguousarray(c2.T.reshape(16, 128, 2).transpose(1, 0, 2)),
             wm=np.asarray(z['w_mod'], f32)[0], bm=np.asarray(z['b_mod'], f32),
             xp=np.ascontiguousarray(np.asarray(z['x_prompt'], f32)[4 * core:4 * core + 4].reshape(1024, 2048)),
             w_gu=np.asarray(z['w_gate_up'], f32)[0],
             b_gu=np.ascontiguousarray(np.asarray(z['b_gate_up'], f32)[0].reshape(32, 16, 128, 2).transpose(2, 0, 1, 3)),
             w_dn=np.asarray(z['w_down'], f32)[0], b_dn=np.asarray(z['b_down'], f32)[0],
             g_final=np.asarray(z['g_final'], f32)[None])
    return d


from concourse.bass_utils import run_bass_kernel_spmd

NCORES = 8


def kernel(x_prompt, x_sample, cache_na_k, cache_na_v, cache_mla_ckv, cache_mla_krope, c, c_ctx,
           g_attn, g_ffn, g_final, w_mod, b_mod, w_in, w_out, na_rpb, g_q_a, w_q_b, g_kv_a, w_kv_b,
           w_router, b_router, w_gate_up, b_gate_up, w_down, b_down):
    f32 = np.float32
    z = dict(x_prompt=x_prompt, x_sample=x_sample, cache_na_k=cache_na_k, cache_na_v=cache_na_v, cache_mla_ckv=cache_mla_ckv,
             cache_mla_krope=cache_mla_krope, c=c, c_ctx=c_ctx, g_attn=g_attn, g_ffn=g_ffn, g_final=g_final, w_mod=w_mod, b_mod=b_mod,
             w_in=w_in, w_out=w_out, na_rpb=na_rpb, g_q_a=g_q_a, w_q_b=w_q_b, g_kv_a=g_kv_a, w_kv_b=w_kv_b, w_router=w_router,
             b_router=b_router, w_gate_up=w_gate_up, b_gate_up=b_gate_up, w_down=w_down, b_down=b_down)
    z = {k: np.asarray(v, f32) for k, v in z.items()}
    nc = bass.Bass("TRN2", target_bir_lowering=False)
    build_fused(nc)
    in_maps = [fused_inputs(z, i) for i in range(NCORES)]
    res = run_bass_kernel_spmd(nc, in_maps, core_ids=list(range(NCORES))).results
    y = [np.asarray(r["o_y"], f32) for r in res]
    y_prompt = np.concatenate([t[:1024] for t in y], 0).reshape(32, 256, 2048)
    y_sample = np.concatenate([t[1024:] for t in y], 0).reshape(2, 4096, 2048)
    new_na_k = np.concatenate([np.asarray(r["o_nak"], f32) for r in res], 0).reshape(32, 1, 256, 8, 128)
    new_na_v = np.concatenate([np.asarray(r["o_nav"], f32) for r in res], 0).reshape(32, 1, 256, 8, 128)
    new_ckv = np.concatenate([np.asarray(r["o_ckv"], f32) for r in res], 0).reshape(32, 1, 256, 256)
    new_krope = np.concatenate([np.asarray(r["o_krope"], f32) for r in res], 0).reshape(32, 1, 256, 64)
    return (y_prompt, y_sample, new_na_k, new_na_v, new_ckv, new_krope)
```
